# Optimizing a Trainium2 kernel written in Bass

```python
import math
import jax, jax.numpy as jnp
from jax import lax
import numpy as np

D_MODEL = 2048
BATCH = 4
SEQ = 4096
DEPTH = 1

DA_HEADS = 8
DA_HEAD_DIM = 64
DA_V_DIM = 2 * DA_HEAD_DIM
ROT_DIM = DA_HEAD_DIM // 4
ROPE_THETA = 500000.0
Q_BLOCK = 128
SUBLN_EPS = 1e-5
RW_HEADS = 16
RW_HEAD_DIM = 64
RW_WIDTH = RW_HEADS * RW_HEAD_DIM
DECAY_LORA = 96
AAA_LORA = 96
GATE_LORA = 256
GN_EPS = 64e-5
N_GROUPS = 4
EXPERTS_PER_GROUP = 8
N_EXPERTS = N_GROUPS * EXPERTS_PER_GROUP
TOP_K = 2
D_EXPERT = 1024
ROW_BLOCK = 128
NORM_EPS = 1e-6

DA_QK_WIDTH = DA_HEADS * 2 * DA_HEAD_DIM
DA_V_WIDTH = DA_HEADS * DA_V_DIM
RW_SHIFT_WIDTH = 3 * RW_WIDTH + DECAY_LORA + AAA_LORA + GATE_LORA
IN_SPLITS = (DA_QK_WIDTH, DA_QK_WIDTH, DA_V_WIDTH, RW_SHIFT_WIDTH, D_MODEL, D_MODEL)
IN_WIDTH = sum(IN_SPLITS)
IN_SPLIT_POINTS = [int(v) for v in np.cumsum(IN_SPLITS)[:-1]]
RW_SPLITS = (RW_WIDTH, RW_WIDTH, RW_WIDTH, DECAY_LORA, AAA_LORA, GATE_LORA)
RW_SPLIT_POINTS = [int(v) for v in np.cumsum(RW_SPLITS)[:-1]]

kernel_name = "hybrid_diffattn_rwkv7_hiermoe"


def rms_norm(x, g, eps):
    xf = x.astype(jnp.float32)
    y = xf * lax.rsqrt(jnp.mean(xf * xf, axis=-1, keepdims=True) + eps)
    return (y * g.astype(jnp.float32)).astype(x.dtype)


def rope_tables(seq):
    inv_freq = ROPE_THETA ** (-jnp.arange(0, ROT_DIM, 2, dtype=jnp.float32) / ROT_DIM)
    ang = jnp.arange(seq, dtype=jnp.float32)[:, None] * inv_freq[None, :]
    return jnp.cos(ang), jnp.sin(ang)


def partial_rope(x, cos, sin):
    half = ROT_DIM // 2
    c = cos[None, :, None, None, :].astype(x.dtype)
    s = sin[None, :, None, None, :].astype(x.dtype)
    x1, x2, xp = x[..., :half], x[..., half:ROT_DIM], x[..., ROT_DIM:]
    return jnp.concatenate([x1 * c - x2 * s, x2 * c + x1 * s, xp], axis=-1)


def diff_attention(q, k, v, q_norm_g, k_norm_g, lam_q1, lam_k1, lam_q2, lam_k2,
                   subln_g, lam_init, cos, sin):
    B, S = q.shape[0], q.shape[1]
    nb = S // Q_BLOCK
    q = partial_rope(rms_norm(q, q_norm_g, NORM_EPS), cos, sin) * (DA_HEAD_DIM ** -0.5)
    k = partial_rope(rms_norm(k, k_norm_g, NORM_EPS), cos, sin)
    lam = (jnp.exp(jnp.sum(lam_q1.astype(jnp.float32) * lam_k1.astype(jnp.float32)))
           - jnp.exp(jnp.sum(lam_q2.astype(jnp.float32) * lam_k2.astype(jnp.float32)))
           + lam_init)
    qb = q.reshape(B, nb, Q_BLOCK, DA_HEADS, 2, DA_HEAD_DIM).transpose(1, 0, 3, 4, 2, 5)
    kt = k.transpose(0, 2, 3, 1, 4)
    vt = v.transpose(0, 2, 1, 3)
    kpos = jnp.arange(S)

    def block(args):
        q_blk, i = args
        s = jnp.einsum('bhcqd,bhckd->bhcqk', q_blk, kt).astype(jnp.float32)
        qpos = i * Q_BLOCK + jnp.arange(Q_BLOCK)
        s = jnp.where(kpos[None, :] <= qpos[:, None], s, -jnp.inf)
        p = jax.nn.softmax(s, axis=-1)
        attn = p[:, :, 0] - lam * p[:, :, 1]
        return jnp.einsum('bhqk,bhkd->bhqd', attn.astype(vt.dtype), vt)

    o = lax.map(block, (qb, jnp.arange(nb)))
    o = o.transpose(1, 0, 3, 2, 4).reshape(B, S, DA_HEADS, DA_V_DIM)
    o = rms_norm(o, subln_g, SUBLN_EPS) * (1.0 - lam_init)
    return o.reshape(B, S, DA_V_WIDTH)


def token_shift(z, mu):
    prev = jnp.pad(z, ((0, 0), (1, 0), (0, 0)))[:, :-1]
    return z + (prev - z) * mu


def rwkv7_step(state, inp):
    r_t, w_t, k_t, v_t, a_t, b_t = inp
    sa = jnp.einsum('bhij,bhj->bhi', state, a_t)
    state = (state * w_t[:, :, None, :] + sa[..., None] * b_t[:, :, None, :]
             + v_t[..., None] * k_t[:, :, None, :])
    y = jnp.einsum('bhij,bhj->bhi', state, r_t)
    return state, y


def rwkv7_time_mix(z, w0, w_up, a0, a_up, g_up, k_k, k_a, r_k, lnx_g, lnx_b):
    B, S = z.shape[0], z.shape[1]
    f32 = jnp.float32
    r, k, v, dw, da, dg = jnp.split(z, RW_SPLIT_POINTS, axis=-1)
    w = -jax.nn.softplus(-(w0 + jnp.tanh(dw) @ w_up)) - 0.5
    decay = jnp.exp(-jnp.exp(w.astype(f32)))
    a = jax.nn.sigmoid(a0 + da @ a_up)
    g = jax.nn.sigmoid(dg) @ g_up
    hs = (B, S, RW_HEADS, RW_HEAD_DIM)
    r, k, v, a, decay = (t.reshape(hs).astype(f32) for t in (r, k, v, a, decay))
    kk = k * k_k.reshape(RW_HEADS, RW_HEAD_DIM).astype(f32)
    kk = kk / jnp.maximum(jnp.sqrt(jnp.sum(kk * kk, axis=-1, keepdims=True)), 1e-12)
    k = k * (1.0 + (a - 1.0) * k_a.reshape(RW_HEADS, RW_HEAD_DIM).astype(f32))
    xs = tuple(t.transpose(1, 0, 2, 3) for t in (r, decay, k, v, -kk, kk * a))
    state0 = jnp.zeros((B, RW_HEADS, RW_HEAD_DIM, RW_HEAD_DIM), f32)
    _, ys = lax.scan(rwkv7_step, state0, xs)
    y = ys.transpose(1, 0, 2, 3)
    mu = jnp.mean(y, axis=-1, keepdims=True)
    var = jnp.mean(jnp.square(y - mu), axis=-1, keepdims=True)
    yn = ((y - mu) * lax.rsqrt(var + GN_EPS)).reshape(B, S, RW_WIDTH)
    yn = yn * lnx_g.astype(f32) + lnx_b.astype(f32)
    bonus = jnp.sum(r * k * r_k.astype(f32), axis=-1, keepdims=True) * v
    out = (yn + bonus.reshape(B, S, RW_WIDTH)) * g.astype(f32)
    return out.astype(z.dtype)


def hier_moe(h, router_g, router_g_b, router_e, router_e_b, w_gate_e, w_up_e, w_down_e):
    T, D = h.shape
    f32 = jnp.float32
    p_group = jax.nn.softmax((h @ router_g).astype(f32) + router_g_b.astype(f32), axis=-1)
    p_g_top, g_idx = lax.top_k(p_group, 1)
    e_logits = ((h @ router_e).astype(f32) + router_e_b.astype(f32)).reshape(T, N_GROUPS, EXPERTS_PER_GROUP)
    e_sel = jnp.take_along_axis(e_logits, g_idx[:, :, None], axis=1)[:, 0]
    p_e_top, e_local = lax.top_k(jax.nn.softmax(e_sel, axis=-1), TOP_K)
    gates = p_g_top * p_e_top / jnp.sum(p_e_top, axis=-1, keepdims=True)
    expert = g_idx * EXPERTS_PER_GROUP + e_local

    A = T * TOP_K
    flat_e = expert.reshape(A)
    flat_g = gates.reshape(A)
    flat_t = jnp.repeat(jnp.arange(T, dtype=jnp.int32), TOP_K)
    order = jnp.argsort(flat_e)
    se = flat_e[order]
    counts = jnp.zeros((N_EXPERTS,), jnp.int32).at[flat_e].add(1)
    padded = (counts + ROW_BLOCK - 1) // ROW_BLOCK * ROW_BLOCK
    start = jnp.cumsum(counts) - counts
    pend = jnp.cumsum(padded)
    pstart = pend - padded
    dest = pstart[se] + jnp.arange(A, dtype=jnp.int32) - start[se]
    n_rows = A + N_EXPERTS * ROW_BLOCK
    n_blocks = n_rows // ROW_BLOCK
    row_tok = jnp.zeros((n_rows,), jnp.int32).at[dest].set(flat_t[order])
    row_gate = jnp.zeros((n_rows,), f32).at[dest].set(flat_g[order])
    block_e = jnp.minimum(jnp.searchsorted(pend, jnp.arange(n_blocks, dtype=jnp.int32) * ROW_BLOCK,
                                           side='right'), N_EXPERTS - 1).astype(jnp.int32)

    def expert_block(args):
        tok, e = args
        xb = h[tok]
        return (jax.nn.silu(xb @ w_gate_e[e]) * (xb @ w_up_e[e])) @ w_down_e[e]

    out = lax.map(expert_block, (row_tok.reshape(n_blocks, ROW_BLOCK), block_e))
    contrib = (out.reshape(n_rows, D).astype(f32) * row_gate[:, None]).astype(h.dtype)
    return jnp.zeros_like(h).at[row_tok].add(contrib)


def setup_inputs(seed: int = 0) -> dict:
    key = jax.random.key(seed)
    ks = iter(jax.random.split(key, 48))
    L, D = DEPTH, D_MODEL
    f32 = jnp.float32

    def nrm(shape, scale):
        return jax.random.normal(next(ks), shape, f32) * scale

    def gain(shape):
        return 1.0 + nrm(shape, 0.02)

    def unif(shape, lo, hi):
        return jax.random.uniform(next(ks), shape, f32, lo, hi)

    return {
        "x": nrm((BATCH, SEQ, D), 1.0),
        "norm1_g": gain((L, D)),
        "w_in": nrm((L, D, IN_WIDTH), D ** -0.5),
        "q_norm_g": gain((L, DA_HEAD_DIM)),
        "k_norm_g": gain((L, DA_HEAD_DIM)),
        "lam_q1": nrm((L, DA_HEAD_DIM), 0.1),
        "lam_k1": nrm((L, DA_HEAD_DIM), 0.1),
        "lam_q2": nrm((L, DA_HEAD_DIM), 0.1),
        "lam_k2": nrm((L, DA_HEAD_DIM), 0.1),
        "subln_g": gain((L, DA_V_DIM)),
        "shift_mu": unif((L, RW_SHIFT_WIDTH), 0.0, 1.0),
        "w0": unif((L, RW_WIDTH), -6.0, 1.0),
        "w_up": nrm((L, DECAY_LORA, RW_WIDTH), 0.5 * DECAY_LORA ** -0.5),
        "a0": nrm((L, RW_WIDTH), 0.5),
        "a_up": nrm((L, AAA_LORA, RW_WIDTH), AAA_LORA ** -0.5),
        "g_up": nrm((L, GATE_LORA, RW_WIDTH), GATE_LORA ** -0.5),
        "k_k": 0.85 + nrm((L, RW_WIDTH), 0.05),
        "k_a": 1.0 + nrm((L, RW_WIDTH), 0.05),
        "r_k": nrm((L, RW_HEADS, RW_HEAD_DIM), 0.1),
        "lnx_g": gain((L, RW_WIDTH)),
        "lnx_b": nrm((L, RW_WIDTH), 0.02),
        "proj_a": nrm((L, DA_V_WIDTH, D), DA_V_WIDTH ** -0.5),
        "proj_b": nrm((L, RW_WIDTH, D), RW_WIDTH ** -0.5),
        "w_out": nrm((L, D, D), D ** -0.5),
        "norm2_g": gain((L, D)),
        "router_g": nrm((L, D, N_GROUPS), D ** -0.5),
        "router_g_b": nrm((L, N_GROUPS), 0.01),
        "router_e": nrm((L, D, N_EXPERTS), D ** -0.5),
        "router_e_b": nrm((L, N_EXPERTS), 0.01),
        "w_gate_e": nrm((L, N_EXPERTS, D, D_EXPERT), D ** -0.5),
        "w_up_e": nrm((L, N_EXPERTS, D, D_EXPERT), D ** -0.5),
        "w_down_e": nrm((L, N_EXPERTS, D_EXPERT, D), D_EXPERT ** -0.5),
    }


def reference(x, norm1_g, w_in, q_norm_g, k_norm_g, lam_q1, lam_k1, lam_q2, lam_k2, subln_g,
              shift_mu, w0, w_up, a0, a_up, g_up, k_k, k_a, r_k, lnx_g, lnx_b,
              proj_a, proj_b, w_out, norm2_g, router_g, router_g_b, router_e, router_e_b,
              w_gate_e, w_up_e, w_down_e):
    B, S, D = x.shape
    cos, sin = rope_tables(S)
    for l in range(DEPTH):
        lam_init = 0.8 - 0.6 * math.exp(-0.3 * l)
        h = rms_norm(x, norm1_g[l], NORM_EPS)
        p = h @ w_in[l]
        qa, ka, va, rw, ga, gb = jnp.split(p, IN_SPLIT_POINTS, axis=-1)
        ya = diff_attention(qa.reshape(B, S, DA_HEADS, 2, DA_HEAD_DIM),
                            ka.reshape(B, S, DA_HEADS, 2, DA_HEAD_DIM),
                            va.reshape(B, S, DA_HEADS, DA_V_DIM),
                            q_norm_g[l], k_norm_g[l], lam_q1[l], lam_k1[l], lam_q2[l], lam_k2[l],
                            subln_g[l], lam_init, cos, sin)
        yb = rwkv7_time_mix(token_shift(rw, shift_mu[l]), w0[l], w_up[l], a0[l], a_up[l], g_up[l],
                            k_k[l], k_a[l], r_k[l], lnx_g[l], lnx_b[l])
        merged = jax.nn.sigmoid(ga) * (ya @ proj_a[l]) + jax.nn.sigmoid(gb) * (yb @ proj_b[l])
        x = x + merged @ w_out[l]
        h2 = rms_norm(x, norm2_g[l], NORM_EPS).reshape(B * S, D)
        x = x + hier_moe(h2, router_g[l], router_g_b[l], router_e[l], router_e_b[l],
                         w_gate_e[l], w_up_e[l], w_down_e[l]).reshape(B, S, D)
    return x
```

```python
import numpy as np
from contextlib import ExitStack
import concourse.bass as bass
import concourse.mybir as mybir
from concourse.bass_utils import run_bass_kernel_spmd

F32 = mybir.dt.float32
F32R = mybir.dt.float32r
BF16 = mybir.dt.bfloat16
I32 = mybir.dt.int32
U32 = mybir.dt.uint32
AF = mybir.ActivationFunctionType
ALU = mybir.AluOpType
AX = mybir.AxisListType

D = 2048
NTOK = 2048
WTOK = 4096
INW = 10688
RWW = 3520
NCORES = 8
import os
RSTOP = int(os.environ.get('RSTOP', '9'))


class Prog:
    ENG = ("pe", "act", "dve", "pool", "sp")

    def __init__(self, nc, es):
        self.nc = nc
        self.es = es
        self.q = {e: [] for e in self.ENG}
        self.sem = {e: es.enter_context(nc.semaphore("c_" + e)) for e in ("pe", "act", "dve", "pool")}
        self.cnt = {e: 0 for e in self.ENG}
        self.NR = 24
        self.ring = {e: [es.enter_context(nc.semaphore("r_%s%d" % (e, i))) for i in range(self.NR)]
                     for e in ("sp", "pool")}
        self.ring_n = {e: 0 for e in ("sp", "pool")}
        self.ring_val = {e: [0] * self.NR for e in ("sp", "pool")}
        self.seen = {e: {} for e in self.ENG}
        self.lastw = {}
        self.reads = {}
        self.cur = es
        self.stack = []

    def sb(self, name, shape, dt):
        return self.cur.enter_context(self.nc.sbuf_tensor(name, list(shape), dt))

    def ps(self, name, shape, dt=F32):
        return self.cur.enter_context(self.nc.psum_tensor(name, list(shape), dt))

    def push(self):
        self.stack.append(self.cur)
        self.cur = ExitStack()

    def pop(self):
        self.barrier()
        self.cur.close()
        self.cur = self.stack.pop()

    def barrier(self):
        for eng in self.ENG:
            for e2 in ("pe", "act", "dve", "pool"):
                if self.cnt[e2]:
                    self._wait(eng, (self.sem[e2], self.cnt[e2], "bar"))
            for qn in self.ring:
                for i in range(self.NR):
                    if self.ring_val[qn][i]:
                        self._wait(eng, (self.ring[qn][i], self.ring_val[qn][i], "dma"))

    def dram(self, name, shape, dt, kind="Internal"):
        return self.nc.dram_tensor(name, list(shape), dt, kind=kind)

    def _wait(self, eng, ev):
        sem, val, src = ev
        if eng == "pe" and src == "pe":
            return
        key = id(sem)
        if self.seen[eng].get(key, 0) >= val:
            return
        self.seen[eng][key] = val
        self.q[eng].append(lambda e, sem=sem, val=val: e.wait_ge(sem, val))

    def _deps(self, eng, reads, writes):
        for k in reads:
            ev = self.lastw.get(k)
            if ev is not None:
                self._wait(eng, ev)
        for k in writes:
            ev = self.lastw.get(k)
            if ev is not None:
                self._wait(eng, ev)
            for ev in self.reads.get(k, ()):
                self._wait(eng, ev)

    def _commit(self, ev, reads, writes):
        for k in writes:
            self.lastw[k] = ev
            self.reads[k] = []
        for k in reads:
            self.reads.setdefault(k, []).append(ev)
            if len(self.reads[k]) > 64:
                self.reads[k] = self.reads[k][-64:]

    def op(self, eng, fn, reads=(), writes=()):
        self._deps(eng, reads, writes)
        self.cnt[eng] += 1
        n = self.cnt[eng]
        sem = self.sem[eng]
        self.q[eng].append(lambda e, fn=fn, sem=sem: fn(e).then_inc(sem, 1))
        self._commit((sem, n, eng), reads, writes)

    def dma(self, eng, out, in_, reads=(), writes=(), **kw):
        self._deps(eng, reads, writes)
        i = self.ring_n[eng] % self.NR
        self.ring_n[eng] += 1
        sem = self.ring[eng][i]
        prev = self.ring_val[eng][i]
        if prev:
            self._wait(eng, (sem, prev, "dma"))
        val = prev + 16
        self.ring_val[eng][i] = val
        self.q[eng].append(lambda e, out=out, in_=in_, sem=sem, kw=kw: e.dma_start(out=out, in_=in_, **kw).then_inc(sem, 16))
        ev = (sem, val, "dma")
        self._commit(ev, reads, writes)
        return ev

    def _idma(self, mk, reads, writes):
        eng = "pool"
        self._deps(eng, reads, writes)
        i = self.ring_n[eng] % self.NR
        self.ring_n[eng] += 1
        sem = self.ring[eng][i]
        prev = self.ring_val[eng][i]
        if prev:
            self._wait(eng, (sem, prev, "dma"))
        val = prev + 16
        self.ring_val[eng][i] = val
        self.q[eng].append(lambda e, mk=mk, sem=sem: mk(e).then_inc(sem, 16))
        self._commit((sem, val, "dma"), reads, writes)

    def idma_scatter(self, dram, idx, src, reads=(), writes=()):
        nrow = dram.shape[0]
        self._idma(lambda e: e.indirect_dma_start(out=dram[:, :], out_offset=bass.IndirectOffsetOnAxis(ap=idx, axis=0), in_=src,
                                                  in_offset=None), reads, writes)

    def idma_gather(self, dst, dram, idx, reads=(), writes=()):
        nrow = dram.shape[0]
        self._idma(lambda e: e.indirect_dma_start(out=dst, out_offset=None, in_=dram[:, :],
                                                  in_offset=bass.IndirectOffsetOnAxis(ap=idx, axis=0)), reads, writes)

    def wait_all(self, eng, keys):
        for k in keys:
            ev = self.lastw.get(k)
            if ev is not None:
                self._wait(eng, ev)

    def emit(self):
        nc = self.nc
        with nc.Block() as blk:
            @blk.tensor
            def _(e):
                for f in self.q["pe"]:
                    f(e)

            @blk.scalar
            def _(e):
                for f in self.q["act"]:
                    f(e)

            @blk.vector
            def _(e):
                for f in self.q["dve"]:
                    f(e)

            @blk.gpsimd
            def _(e):
                for f in self.q["pool"]:
                    f(e)

            @blk.sync
            def _(e):
                for f in self.q["sp"]:
                    f(e)


def AP(t, off, dims):
    return bass.AP(t, off, [list(d) for d in dims])


def host_consts():
    c = {}
    c["ident_h"] = np.eye(128, dtype=np.float32)
    c["tri_h"] = np.triu(np.ones((128, 128), np.float32))
    c.update(rwkv_consts())
    return c


def build(stages=("A",), debug=False):
    nc = bass.Bass("TRN2", target_bir_lowering=False)
    es = ExitStack()
    P = Prog(nc, es)
    dr = {}

    def din(name, shape, dt=F32):
        dr[name] = nc.dram_tensor(name, list(shape), dt, kind="ExternalInput")
        return dr[name]

    xw = din("xw", [WTOK, D])
    cs = din("cs", [WTOK, 16])
    norm1_g = din("norm1_g", [1, D])
    w_in = [din("win_%d" % i, [D, cw]) for i, (c0, cw, kind, bi) in enumerate(col_blocks())]
    q_norm_g = din("q_norm_g", [1, 64])
    k_norm_g = din("k_norm_g", [1, 64])
    ident_d = din("ident_h", [128, 128])
    qT_s = P.dram("qT_s", [8, 128, NTOK], BF16)
    kT_s = P.dram("kT_s", [8, 128, WTOK], BF16)
    v_s = P.dram("v_s", [WTOK, 1024], BF16)
    prw = P.dram("prw", [WTOK + 1, RWW], F32)
    sg_s = P.dram("sg_s", [NTOK, 4096], BF16)
    outs = {}
    ya_s = P.dram("ya_s", [NTOK, 1024], F32)
    ybT_s = P.dram("ybT_s", [1024, NTOK], BF16)
    if debug:
        dbg = debug if isinstance(debug, (list, tuple, set)) else ("qT", "kT", "v", "prw", "sg")
        shp = {"qT": ([8, 128, NTOK], BF16), "kT": ([8, 128, WTOK], BF16), "v": ([WTOK, 1024], BF16),
               "prw": ([WTOK + 1, RWW], F32), "sg": ([NTOK, 4096], BF16), "ya": ([NTOK, 1024], F32), "ybT": ([1024, NTOK], BF16),
               "x1": ([NTOK, D], F32), "slot": ([16, 128, 2], I32), "gate": ([16, 128, 2], F32)}
        for k in dbg:
            outs["dbg_" + k] = nc.dram_tensor("dbg_" + k, shp[k][0], shp[k][1], kind="ExternalOutput")
        qT_s = outs.get("dbg_qT", qT_s); kT_s = outs.get("dbg_kT", kT_s); v_s = outs.get("dbg_v", v_s)
        prw = outs.get("dbg_prw", prw); sg_s = outs.get("dbg_sg", sg_s); ya_s = outs.get("dbg_ya", ya_s)
        ybT_s = outs.get("dbg_ybT", ybT_s)
    if not outs and "M" not in stages:
        outs["dummy_out"] = nc.dram_tensor("dummy_out", [1, 64], F32, kind="ExternalOutput")
    validT = din("validT", [128, 32])
    tri_d = din("tri_h", [128, 128])
    lamv = [din(n, [1, 64]) for n in ("lam_q1", "lam_k1", "lam_q2", "lam_k2")]
    subln_g = din("subln_g", [1, 128])

    ident = P.sb("ident", [128, 128], F32)
    identb = P.sb("identb", [128, 128], BF16)
    g1b = P.sb("g1b", [128, D], F32)
    qgb = P.sb("qgb", [128, 512], F32)
    kgb = P.sb("kgb", [128, 512], F32)
    zrow = P.sb("zrow", [1, RWW], F32)
    epsb = P.sb("epsb", [128, 4], F32)
    EPSB[0] = epsb
    P.op("dve", lambda e: e.memset(epsb[:, 0:1], 1e-6), writes=["epsb"])
    P.op("dve", lambda e: e.memset(epsb[:, 1:2], 1e-5), writes=["epsb"])
    P.op("dve", lambda e: e.memset(epsb[:, 2:3], 64e-5), writes=["epsb"])
    P.op("dve", lambda e: e.memset(epsb[:, 3:4], 1.0), writes=["epsb"])
    P.dma("sp", ident[:], ident_d.ap(), writes=["ident"])
    P.op("dve", lambda e: e.tensor_copy(identb[:], ident[:]), reads=["ident"], writes=["identb"])
    P.dma("sp", g1b[:], AP(norm1_g, 0, [[0, 128], [1, D]]), writes=["g1b"])
    for j in range(8):
        P.dma("sp", qgb[:, j * 64:(j + 1) * 64], AP(q_norm_g, 0, [[0, 128], [1, 64]]), writes=["qgb"])
        P.dma("sp", kgb[:, j * 64:(j + 1) * 64], AP(k_norm_g, 0, [[0, 128], [1, 64]]), writes=["kgb"])
    P.op("dve", lambda e: e.tensor_scalar(qgb[:], qgb[:], 0.125, None, ALU.mult), reads=["qgb"], writes=["qgb"])
    P.op("dve", lambda e: e.memset(zrow[:], 0.0), writes=["zrow"])
    P.dma("sp", prw.ap()[0:1, :], zrow[:], reads=["zrow"], writes=["prw"])

    if "A" in stages:
        P.push()
        stage_A(P, nc, xw, cs, w_in, ident, identb, g1b, qgb, kgb, qT_s, kT_s, v_s, prw, sg_s)
        P.pop()
    if "B" in stages:
        P.push()
        stage_B(P, nc, qT_s, kT_s, v_s, validT, tri_d, lamv, subln_g, ya_s)
        P.pop()

    if "R" in stages:
        P.push()
        stage_R(P, nc, din, prw, ident, identb, ybT_s)
        P.pop()

    if "O" in stages:
        mgT_s = P.dram("mgT_s", [16, 128, 16, 128], BF16)
        x1_s = outs.get("dbg_x1") or P.dram("x1_s", [NTOK, D], F32)
        xs_d = P.dram("xs_d", [NSLOT + 1, D], BF16)
        slot_s = outs.get("dbg_slot") or P.dram("slot_s", [16, 128, 2], I32)
        gate_s = outs.get("dbg_gate") or P.dram("gate_s", [16, 128, 2], F32)
        P.push()
        stage_O1(P, nc, din, identb, ya_s, ybT_s, sg_s, mgT_s)
        P.pop()
        P.push()
        stage_O2(P, nc, din, ident, xw, mgT_s, x1_s, xs_d, slot_s, gate_s)
        P.pop()
    if "M" in stages:
        y_d = P.dram("y_d", [NSLOT + 1, D], F32)
        out_d = nc.dram_tensor("out", [NTOK, D], F32, kind="ExternalOutput")
        outs["out"] = out_d
        P.push()
        stage_M(P, nc, din, identb, xs_d, y_d, x1_s, slot_s, gate_s, out_d)
        P.pop()

    P.wait_all("sp", list(P.lastw.keys()))
    P.emit()
    es.close()
    LASTP[0] = P
    return nc, list(outs.keys())


def col_blocks():
    blks = []
    for i in range(2):
        blks.append((i * 512, 512, "q", i))
    for i in range(2):
        blks.append((1024 + i * 512, 512, "k", i))
    for i in range(2):
        blks.append((2048 + i * 512, 512, "v", i))
    for i in range(7):
        w = 512 if i < 6 else RWW - 6 * 512
        blks.append((3072 + i * 512, w, "rw", i))
    for i in range(8):
        blks.append((6592 + i * 512, 512, "g", i))
    return blks


def stage_A(P, nc, xw, cs, w_in, ident, identb, g1b, qgb, kgb, qT_s, kT_s, v_s, prw, sg_s):
    hT = P.sb("hT", [128, 16, 16, 128], BF16)
    xt = [P.sb("xt%d" % i, [128, D], F32) for i in range(2)]
    ht = [P.sb("ht%d" % i, [128, D], F32) for i in range(2)]
    sq = P.sb("sqjunk", [128, D], BF16)
    st = [P.sb("st%d" % i, [128, 4], F32) for i in range(2)]
    wst = P.sb("wst", [128, 16, 512], F32)
    wbf = [P.sb("wbf%d" % i, [128, 16, 512], BF16) for i in range(2)]
    pT = [P.ps("pT%d" % i, [128, 4, 128], F32) for i in range(2)]
    pacc = [P.ps("pacc%d" % i, [128, 512], F32) for i in range(2)]
    pTb = [P.ps("pTb%d" % i, [128, 4, 128], BF16) for i in range(2)]
    ev = [P.sb("ev%d" % i, [128, 512], F32) for i in range(2)]
    evb = [P.sb("evb%d" % i, [128, 512], BF16) for i in range(2)]
    qn = [P.sb("qn%d" % i, [128, 512], F32) for i in range(2)]
    qr = [P.sb("qr%d" % i, [128, 512], BF16) for i in range(2)]
    qs = [P.sb("qs%d" % i, [128, 16], F32) for i in range(2)]
    cst = [P.sb("cst%d" % i, [128, 16], F32) for i in range(2)]
    tmp8 = [P.sb("tmp8_%d" % i, [128, 8, 16], F32) for i in range(2)]
    qTb = [P.sb("qTb%d" % i, [128, 4, 128], BF16) for i in range(2)]
    blks = col_blocks()
    wload_i = [0]
    for grp in range(2):
        for t in range(16):
            gt = grp * 16 + t
            b = t % 2
            X, H, S = xt[b], ht[b], st[b]
            P.dma("sp", X[:], xw.ap()[gt * 128:(gt + 1) * 128, :], writes=["xt%d" % b])
            P.op("act", lambda e, X=X, S=S: e.activation(sq[:], X[:], AF.Square, accum_out=S[:, 0:1]),
                 reads=["xt%d" % b], writes=["sq", "st%d" % b])
            P.op("act", lambda e, S=S: e.activation(S[:, 1:2], S[:, 0:1], AF.Sqrt, bias=EPSB[0][:, 0:1], scale=1.0 / D),
                 reads=["st%d" % b, "epsb"], writes=["st%d" % b])
            P.op("dve", lambda e, S=S: e.reciprocal(S[:, 2:3], S[:, 1:2]), reads=["st%d" % b], writes=["st%d" % b])
            P.op("dve", lambda e, X=X, H=H, S=S: e.scalar_tensor_tensor(out=H[:], in0=X[:], scalar=S[:, 2:3], in1=g1b[:],
                                                                      op0=ALU.mult, op1=ALU.mult),
                 reads=["xt%d" % b, "st%d" % b, "g1b"], writes=["ht%d" % b])
            for c4 in range(4):
                pb = c4 % 2
                for j in range(4):
                    kc = c4 * 4 + j
                    P.op("pe", lambda e, H=H, kc=kc, pb=pb, j=j: e.transpose(pT[pb][:, j, :], H[:, kc * 128:(kc + 1) * 128], ident[:]),
                         reads=["ht%d" % b, "ident"], writes=["pT%d" % pb])
                eng = "act" if c4 % 2 == 0 else "dve"
                if eng == "act":
                    P.op("act", lambda e, t=t, c4=c4, pb=pb: e.copy(hT[:, t, c4 * 4:(c4 + 1) * 4, :], pT[pb][:]),
                         reads=["pT%d" % pb], writes=["hT%d_%d" % (t, c4)])
                else:
                    P.op("dve", lambda e, t=t, c4=c4, pb=pb: e.tensor_copy(hT[:, t, c4 * 4:(c4 + 1) * 4, :], pT[pb][:]),
                         reads=["pT%d" % pb], writes=["hT%d_%d" % (t, c4)])
        for wblk, (c0, cw, kind, bi) in enumerate(blks):
            if grp == 0 and kind in ("q", "g"):
                continue
            wi = wload_i[0]
            wload_i[0] += 1
            wb = wbf[wi % 2]
            wk = "wbf%d" % (wi % 2)
            P.dma("sp", wst[:, :, 0:cw], w_in[wblk].ap().rearrange("(k p) c -> p k c", p=128),
                  writes=["wst"])
            for hh in range(2):
                eng = "pool" if hh == 0 else "dve"
                if eng == "pool":
                    P.op("pool", lambda e, wb=wb, hh=hh, cw=cw: e.tensor_copy(wb[:, hh * 8:(hh + 1) * 8, 0:cw], wst[:, hh * 8:(hh + 1) * 8, 0:cw]),
                         reads=["wst"], writes=[wk + "_%d" % hh])
                else:
                    P.op("dve", lambda e, wb=wb, hh=hh, cw=cw: e.tensor_copy(wb[:, hh * 8:(hh + 1) * 8, 0:cw], wst[:, hh * 8:(hh + 1) * 8, 0:cw]),
                         reads=["wst"], writes=[wk + "_%d" % hh])
            for t in range(16):
                gt = grp * 16 + t
                tok0 = gt * 128
                pa = pacc[t % 2]
                pk = "pacc%d" % (t % 2)
                for kc in range(16):
                    P.op("pe", lambda e, pa=pa, t=t, kc=kc, wb=wb, cw=cw: e.matmul(pa[:, 0:cw], hT[:, t, kc, :], wb[:, kc, 0:cw],
                                                                              start=(kc == 0), stop=(kc == 15)),
                         reads=["hT%d_%d" % (t, kc // 4), wk + "_%d" % (kc // 8)], writes=[pk])
                b = t % 2
                if kind == "v":
                    P.op("act", lambda e, pa=pa, b=b: e.copy(evb[b][:], pa[:]), reads=[pk], writes=["evb%d" % b])
                    P.dma("sp", v_s.ap()[tok0:tok0 + 128, bi * 512:(bi + 1) * 512], evb[b][:], reads=["evb%d" % b], writes=["v_s:%d:%d" % (gt, bi)])
                elif kind == "rw":
                    P.op("act", lambda e, pa=pa, b=b, cw=cw: e.copy(ev[b][:, 0:cw], pa[:, 0:cw]), reads=[pk], writes=["ev%d" % b])
                    P.dma("sp", prw.ap()[1 + tok0:1 + tok0 + 128, bi * 512:bi * 512 + cw], ev[b][:, 0:cw], reads=["ev%d" % b], writes=["prw:%d:%d" % (gt, bi)])
                elif kind == "g":
                    P.op("act", lambda e, pa=pa, b=b: e.activation(evb[b][:], pa[:], AF.Sigmoid), reads=[pk], writes=["evb%d" % b])
                    P.dma("sp", sg_s.ap()[t * 128:(t + 1) * 128, bi * 512:(bi + 1) * 512], evb[b][:], reads=["evb%d" % b], writes=["sg_s:%d:%d" % (t, bi)])
                else:
                    gb_ = qgb if kind == "q" else kgb
                    QN, QS, QR, CS, T8 = qn[b], qs[b], qr[b], cst[b], tmp8[b]
                    P.dma("pool", CS[:], cs.ap()[tok0:tok0 + 128, :], writes=["cst%d" % b])
                    P.op("act", lambda e, pa=pa, QN=QN: e.activation(QN[:], pa[:], AF.Square), reads=[pk], writes=["qn%d" % b])
                    P.op("dve", lambda e, QN=QN, QS=QS: e.tensor_reduce(QS[:, 0:8], QN[:].rearrange("p (g d) -> p g d", d=64), AX.X, ALU.add),
                         reads=["qn%d" % b], writes=["qs%d" % b])
                    P.op("act", lambda e, QS=QS: e.activation(QS[:, 0:8], QS[:, 0:8], AF.Sqrt, bias=EPSB[0][:, 0:1], scale=1.0 / 64),
                         reads=["qs%d" % b, "epsb"], writes=["qs%d" % b])
                    P.op("dve", lambda e, QS=QS: e.reciprocal(QS[:, 8:16], QS[:, 0:8]), reads=["qs%d" % b], writes=["qs%d" % b])
                    P.op("dve", lambda e, pa=pa, QN=QN, QS=QS: e.tensor_tensor(
                        QN[:].rearrange("p (g d) -> p g d", d=64), pa[:].rearrange("p (g d) -> p g d", d=64),
                        AP(QS, 8, [[16, 128], [1, 8], [0, 64]]), ALU.mult),
                        reads=[pk, "qs%d" % b], writes=["qn%d" % b])
                    P.op("dve", lambda e, QN=QN, gb_=gb_: e.tensor_tensor(QN[:], QN[:], gb_[:], ALU.mult),
                         reads=["qn%d" % b, "qgb", "kgb"], writes=["qn%d" % b])
                    x1 = AP(QN, 0, [[512, 128], [64, 8], [1, 8]])
                    x2 = AP(QN, 8, [[512, 128], [64, 8], [1, 8]])
                    cosb = AP(CS, 0, [[16, 128], [0, 8], [1, 8]])
                    sinb = AP(CS, 8, [[16, 128], [0, 8], [1, 8]])
                    t_a = AP(T8, 0, [[128, 128], [16, 8], [1, 8]])
                    t_b = AP(T8, 8, [[128, 128], [16, 8], [1, 8]])
                    P.op("dve", lambda e, t_a=t_a, x2=x2, sinb=sinb: e.tensor_tensor(t_a, x2, sinb, ALU.mult), reads=["qn%d" % b, "cst%d" % b], writes=["tmp8_%d" % b])
                    P.op("dve", lambda e, t_b=t_b, x1=x1, sinb=sinb: e.tensor_tensor(t_b, x1, sinb, ALU.mult), reads=["qn%d" % b, "cst%d" % b], writes=["tmp8_%d" % b])
                    P.op("dve", lambda e, x1=x1, cosb=cosb: e.tensor_tensor(x1, x1, cosb, ALU.mult), reads=["qn%d" % b, "cst%d" % b], writes=["qn%d" % b])
                    P.op("dve", lambda e, x2=x2, cosb=cosb: e.tensor_tensor(x2, x2, cosb, ALU.mult), reads=["qn%d" % b, "cst%d" % b], writes=["qn%d" % b])
                    P.op("dve", lambda e, x1=x1, t_a=t_a: e.tensor_tensor(x1, x1, t_a, ALU.subtract), reads=["qn%d" % b, "tmp8_%d" % b], writes=["qn%d" % b])
                    P.op("dve", lambda e, x2=x2, t_b=t_b: e.tensor_tensor(x2, x2, t_b, ALU.add), reads=["qn%d" % b, "tmp8_%d" % b], writes=["qn%d" % b])
                    P.op("act", lambda e, QN=QN, QR=QR: e.copy(QR[:], QN[:]), reads=["qn%d" % b], writes=["qr%d" % b])
                    for j in range(4):
                        P.op("pe", lambda e, QR=QR, j=j, b=b: e.transpose(pTb[b][:, j, :], QR[:, j * 128:(j + 1) * 128], identb[:]),
                             reads=["qr%d" % b, "identb"], writes=["pTb%d" % b])
                    P.op("act", lambda e, b=b: e.copy(qTb[b][:], pTb[b][:]), reads=["pTb%d" % b], writes=["qTb%d" % b])
                    if kind == "q":
                        dst = qT_s.ap()[bi * 4:(bi + 1) * 4, :, t * 128:(t + 1) * 128].rearrange("h p t -> p h t")
                        P.dma("sp", dst, qTb[b][:], reads=["qTb%d" % b], writes=["qT_s:%d:%d" % (t, bi)])
                    else:
                        dst = kT_s.ap()[bi * 4:(bi + 1) * 4, :, tok0:tok0 + 128].rearrange("h p t -> p h t")
                        P.dma("sp", dst, qTb[b][:], reads=["qTb%d" % b], writes=["kT_s:%d:%d" % (gt, bi)])


def stage_B(P, nc, qT_s, kT_s, v_s, validT, tri_d, lamv, subln_g, ya_s):
    LAM_INIT = 0.2
    trif = P.sb("trif", [128, 128], F32)
    tri = P.sb("tri", [128, 128], BF16)
    P.dma("sp", trif[:], tri_d.ap(), writes=["trif"])
    P.op("dve", lambda e: e.tensor_copy(tri[:], trif[:]), reads=["trif"], writes=["tri"])
    validc = P.sb("validc", [128, 32], F32)
    P.dma("sp", validc[:], validT.ap(), writes=["validc"])
    sgb = P.sb("sgb", [128, 128], F32)
    P.dma("sp", sgb[:], AP(subln_g, 0, [[0, 128], [1, 128]]), writes=["sgb"])
    P.op("dve", lambda e: e.tensor_scalar(sgb[:], sgb[:], 1.0 - LAM_INIT, None, ALU.mult), reads=["sgb"], writes=["sgb"])
    lv = P.sb("lv", [1, 4, 64], F32)
    for i in range(4):
        P.dma("sp", lv[:, i, :], lamv[i].ap(), writes=["lv"])
    lp = P.sb("lp", [1, 2, 64], F32)
    ls = P.sb("ls", [1, 8], F32)
    ones1 = P.sb("ones1", [1, 128], F32)
    nlamb = P.sb("nlamb", [128, 1], F32)
    plam = P.ps("plam", [128, 2], F32)
    P.op("dve", lambda e: e.memset(ones1[:], 1.0), writes=["ones1"])
    P.op("dve", lambda e: e.memset(ls[:], 0.0), writes=["ls"])
    P.op("dve", lambda e: e.tensor_tensor(lp[:, 0, :], lv[:, 0, :], lv[:, 1, :], ALU.mult), reads=["lv"], writes=["lp"])
    P.op("dve", lambda e: e.tensor_tensor(lp[:, 1, :], lv[:, 2, :], lv[:, 3, :], ALU.mult), reads=["lv"], writes=["lp"])
    P.op("dve", lambda e: e.tensor_reduce(ls[:, 0:2], lp[:], AX.X, ALU.add), reads=["lp"], writes=["ls"])
    P.op("act", lambda e: e.activation(ls[:, 2:4], ls[:, 0:2], AF.Exp), reads=["ls"], writes=["ls"])
    P.op("dve", lambda e: e.tensor_tensor(ls[:, 4:5], ls[:, 3:4], ls[:, 2:3], ALU.subtract), reads=["ls"], writes=["ls"])
    P.op("dve", lambda e: e.tensor_scalar(ls[:, 6:8], ls[:, 4:6], -LAM_INIT, None, ALU.add), reads=["ls"], writes=["ls"])
    P.op("pe", lambda e: e.matmul(plam[:, 0:2], ones1[:], ls[:, 6:8], start=True, stop=True), reads=["ones1", "ls"], writes=["plam"])
    P.op("dve", lambda e: e.tensor_copy(nlamb[:], plam[:, 0:1]), reads=["plam"], writes=["nlamb"])

    qT = [P.sb("aqT%d" % i, [128, NTOK], BF16) for i in range(2)]
    kT = [P.sb("akT%d" % i, [128, WTOK], BF16) for i in range(2)]
    Vh = [P.sb("aVh%d" % i, [128, 32, 130], BF16) for i in range(2)]
    psS = [P.ps("psS%d" % i, [128, 512], F32) for i in range(2)]
    Oacc = [P.ps("Oacc%d" % i, [128, 2, 130], F32) for i in range(2)]
    PT = [P.sb("aPT%d" % i, [128, 512], BF16) for i in range(3)]
    Oc = [P.sb("aOc%d" % i, [128, 4, 130], F32) for i in range(2)]
    rl = P.sb("arl", [128, 8], F32)
    ssq[0] = P.sb("assq", [128, 4], F32)
    otmp = P.sb("aotmp", [128, 128], F32)
    obuf = P.sb("aobuf", [128, 128], F32)
    osq = P.sb("aosq", [128, 128], BF16)
    yat = [P.sb("ayat%d" % i, [128, 128], F32) for i in range(2)]
    nexp = 0
    nya = 0
    for h in range(8):
        hb = h % 2
        Q, Kt, V = qT[hb], kT[hb], Vh[hb]
        P.dma("sp", Q[:], qT_s.ap()[h], reads=["qT_s:%d:%d" % (t, h // 4) for t in range(16)], writes=["aqT%d" % hb])
        P.dma("sp", Kt[:], kT_s.ap()[h], reads=["kT_s:%d:%d" % (t, h // 4) for t in range(32)], writes=["akT%d" % hb])
        P.dma("sp", V[:, :, 0:128], v_s.ap()[:, h * 128:(h + 1) * 128].rearrange("(kt p) d -> p kt d", p=128),
              reads=["v_s:%d:%d" % (t, h // 4) for t in range(32)], writes=["aVh%d" % hb])
        P.op("dve", lambda e, V=V: e.tensor_copy(V[:, :, 128:129], validc[:].rearrange("p (k o) -> p k o", o=1)),
             reads=["validc"], writes=["aVh%d" % hb])
        for G in range(4):
            for c in range(2):
                nkt = 16 + 4 * G + 4
                for kt in range(nkt):
                    sb_ = kt % 2
                    P.op("pe", lambda e, sb_=sb_, c=c, kt=kt, G=G, Q=Q, Kt=Kt: e.matmul(
                        psS[sb_][:], Kt[c * 64:(c + 1) * 64, kt * 128:(kt + 1) * 128], Q[c * 64:(c + 1) * 64, G * 512:(G + 1) * 512],
                        start=True, stop=True), reads=["aqT%d" % hb, "akT%d" % hb], writes=["psS%d" % sb_])
                    pb = nexp % 3
                    nexp += 1
                    pt = PT[pb]
                    P.op("act", lambda e, pt=pt, sb_=sb_: e.activation(pt[:], psS[sb_][:], AF.Exp), reads=["psS%d" % sb_], writes=["aPT%d" % pb])
                    rel = kt - (16 + 4 * G)
                    for j in range(4):
                        if rel > j:
                            continue
                        if rel == j:
                            P.op("dve", lambda e, pt=pt, j=j: e.tensor_tensor(pt[:, j * 128:(j + 1) * 128], pt[:, j * 128:(j + 1) * 128], tri[:], ALU.mult),
                                 reads=["aPT%d" % pb, "tri"], writes=["aPT%d" % pb])
                        last = (kt == 16 + 4 * G + j)
                        P.op("pe", lambda e, pt=pt, j=j, kt=kt, V=V, last=last: e.matmul(
                            Oacc[j // 2][:, j % 2, 0:129], pt[:, j * 128:(j + 1) * 128], V[:, kt, 0:129],
                            start=(kt == 0 and j % 2 == 0), stop=last, skip_group_check=True),
                            reads=["aPT%d" % pb, "aVh%d" % hb], writes=["Oacc%d" % (j // 2)])
                for a in range(2):
                    P.op("act", lambda e, a=a, c=c: e.copy(Oc[c][:, 2 * a:2 * a + 2, :], Oacc[a][:]), reads=["Oacc%d" % a], writes=["aOc%d" % c])
            P.op("dve", lambda e: e.reciprocal(rl[:, 0:4], Oc[0][:, :, 128]), reads=["aOc0"], writes=["arl"])
            P.op("dve", lambda e: e.reciprocal(rl[:, 4:8], Oc[1][:, :, 128]), reads=["aOc1"], writes=["arl"])
            P.op("dve", lambda e: e.tensor_scalar(rl[:, 4:8], rl[:, 4:8], nlamb[:, 0:1], None, ALU.mult), reads=["arl", "nlamb"], writes=["arl"])
            for j in range(4):
                yb_ = nya % 2
                nya += 1
                Y = yat[yb_]
                P.op("dve", lambda e, j=j: e.tensor_scalar(otmp[:], Oc[0][:, j, 0:128], rl[:, j:j + 1], None, ALU.mult), reads=["aOc0", "arl"], writes=["aotmp"])
                P.op("dve", lambda e, j=j: e.scalar_tensor_tensor(out=obuf[:], in0=Oc[1][:, j, 0:128], scalar=rl[:, 4 + j:5 + j], in1=otmp[:],
                                                                  op0=ALU.mult, op1=ALU.add), reads=["aOc1", "arl", "aotmp"], writes=["aobuf"])
                P.op("act", lambda e: e.activation(osq[:], obuf[:], AF.Square, accum_out=ssq[0][:, 0:1]),
                     reads=["aobuf"], writes=["aosq", "assq"])
                P.op("act", lambda e: e.activation(ssq[0][:, 1:2], ssq[0][:, 0:1], AF.Sqrt, bias=EPSB[0][:, 1:2], scale=1.0 / 128), reads=["assq", "epsb"], writes=["assq"])
                P.op("dve", lambda e: e.reciprocal(ssq[0][:, 2:3], ssq[0][:, 1:2]), reads=["assq"], writes=["assq"])
                P.op("dve", lambda e, Y=Y: e.scalar_tensor_tensor(out=Y[:], in0=obuf[:], scalar=ssq[0][:, 2:3], in1=sgb[:], op0=ALU.mult, op1=ALU.mult),
                     reads=["aobuf", "assq", "sgb"], writes=["ayat%d" % yb_])
                qt = 4 * G + j
                P.dma("sp", ya_s.ap()[qt * 128:(qt + 1) * 128, h * 128:(h + 1) * 128], Y[:], reads=["ayat%d" % yb_], writes=["ya_s:%d:%d" % (qt, h)])


ssq = [None]


EPSB = [None]
LASTP = [None]


def rope_table():
    inv_freq = (500000.0 ** (-np.arange(0, 16, 2, dtype=np.float32) / 16)).astype(np.float32)
    ang = np.arange(4096, dtype=np.float32)[:, None] * inv_freq[None, :]
    return np.cos(ang).astype(np.float32), np.sin(ang).astype(np.float32)


def make_in_maps(I, ncores=NCORES, with_experts=True):
    cos, sin = rope_table()
    cst = host_consts()
    maps = []
    for c in range(ncores):
        b, sh = c // 2, c % 2
        x = I["x"][b]
        xwin = np.zeros((WTOK, D), np.float32)
        cswin = np.zeros((WTOK, 16), np.float32)
        if sh == 0:
            xwin[NTOK:] = x[:NTOK]
            cswin[NTOK:, 0:8] = cos[:NTOK]
            cswin[NTOK:, 8:16] = sin[:NTOK]
            cswin[:NTOK, 0:8] = 1.0
        else:
            xwin[:] = x
            cswin[:, 0:8] = cos
            cswin[:, 8:16] = sin
        valid = np.ones((WTOK,), np.float32)
        if sh == 0:
            valid[:NTOK] = 0.0
        m = {"xw": xwin, "cs": cswin, "validT": np.ascontiguousarray(valid.reshape(32, 128).T)}
        for k in ("norm1_g", "q_norm_g", "k_norm_g", "lam_q1", "lam_k1", "lam_q2", "lam_k2", "subln_g"):
            m[k] = np.ascontiguousarray(I[k]).reshape(1, -1)
        for k in ("shift_mu", "w0", "a0", "k_k", "k_a"):
            m[k] = np.ascontiguousarray(I[k]).reshape(1, -1)
        for k in ("r_k", "lnx_g", "lnx_b"):
            m[k + "_c"] = np.ascontiguousarray(I[k].reshape(8, 128).T)
        for k in ("w_up", "a_up", "g_up"):
            m[k] = np.ascontiguousarray(I[k][0])
        m["proj_a"] = np.ascontiguousarray(I["proj_a"][0])
        m["proj_b"] = np.ascontiguousarray(I["proj_b"][0])
        for i in range(2):
            m["w_out_%d" % i] = np.ascontiguousarray(I["w_out"][0][i * 1024:(i + 1) * 1024])
        m["norm2_g"] = np.ascontiguousarray(I["norm2_g"]).reshape(1, -1)
        m["router_w"] = np.ascontiguousarray(np.concatenate([I["router_g"][0], I["router_e"][0]], axis=1))
        m["router_b"] = np.ascontiguousarray(np.concatenate([I["router_g_b"][0], I["router_e_b"][0]]).reshape(1, 36))
        m["ebase_h"] = (np.arange(32, dtype=np.float32) * CAP).reshape(1, 32)
        m["ts_h2"] = cst["ts_h"]
        if with_experts:
            for e in range(32):
                m["wg_%d" % e] = np.ascontiguousarray(I["w_gate_e"][0][e])
                m["wu_%d" % e] = np.ascontiguousarray(I["w_up_e"][0][e])
                m["wd_%d" % e] = np.ascontiguousarray(I["w_down_e"][0][e])
        for i, (c0, cw, kind, bi) in enumerate(col_blocks()):
            m["win_%d" % i] = np.ascontiguousarray(I["w_in"][0][:, c0:c0 + cw])
        m.update(cst)
        maps.append(m)
    return maps


def kernel(**inputs):
    I = {k: np.asarray(v) for k, v in inputs.items()}
    nc, onames = build(stages=("A", "B", "R", "O", "M"), debug=False)
    in_maps = make_in_maps(I, NCORES)
    res = run_bass_kernel_spmd(nc, in_maps, core_ids=list(range(NCORES)))
    out = np.empty((4, 4096, D), np.float32)
    for c in range(NCORES):
        b, sh = c // 2, c % 2
        out[b, sh * NTOK:(sh + 1) * NTOK] = np.asarray(res.results[c]["out"])
    return out


def rwkv_consts():
    s = np.arange(128)[:, None]
    t = np.arange(128)[None, :]
    c = {}
    c["cmat_h"] = ((s <= t).astype(np.float32) - (s <= 63).astype(np.float32))
    m2 = np.zeros((128, 2), np.float32)
    m2[:64, 0] = 1.0
    m2[64:, 1] = 1.0
    c["msk2_h"] = m2
    c["ts_h"] = (s < t).astype(np.float32)
    c["ti_h"] = (s <= t).astype(np.float32)
    c["tsl_h"] = (s > t).astype(np.float32)
    bd = np.zeros((128, 128), np.float32)
    bd[:64, :64] = 1.0
    bd[64:, 64:] = 1.0
    c["bd1_h"] = bd
    return c


def stage_R(P, nc, din, prw, ident, identb, ybT_s):
    shift_mu = din("shift_mu", [1, RWW])
    pv = {n: din(n, [1, 1024]) for n in ("w0", "a0", "k_k", "k_a")}
    colp = {n: din(n + "_c", [128, 8]) for n in ("r_k", "lnx_g", "lnx_b")}
    w_up = din("w_up", [96, 1024])
    a_up = din("a_up", [96, 1024])
    g_up = din("g_up", [256, 1024])
    cd = {n: din(n, [128, 2] if n == "msk2_h" else [128, 128]) for n in ("cmat_h", "msk2_h", "ts_h", "ti_h", "tsl_h", "bd1_h")}

    def ld(name, shape, src, dt=F32):
        t = P.sb(name, shape, dt)
        P.dma("sp", t[:], src, writes=[name])
        return t

    mu_b = ld("mu_b", [128, RWW], AP(shift_mu, 0, [[0, 128], [1, RWW]]))
    w0_b = ld("w0_b", [128, 1024], AP(pv["w0"], 0, [[0, 128], [1, 1024]]))
    a0_b = ld("a0_b", [128, 1024], AP(pv["a0"], 0, [[0, 128], [1, 1024]]))
    kk_b = ld("kk_b", [128, 1024], AP(pv["k_k"], 0, [[0, 128], [1, 1024]]))
    ka_b = ld("ka_b", [128, 1024], AP(pv["k_a"], 0, [[0, 128], [1, 1024]]))
    rk_c = ld("rk_c", [128, 8], colp["r_k"].ap())
    lg_c = ld("lg_c", [128, 8], colp["lnx_g"].ap())
    lb_c = ld("lb_c", [128, 8], colp["lnx_b"].ap())
    wup = P.sb("wup", [128, 1024], F32)
    aup = P.sb("aup", [128, 1024], F32)
    for t_, src_, nm_ in ((wup, w_up, "wup"), (aup, a_up, "aup")):
        P.op("dve", lambda e, t_=t_: e.memset(t_[:], 0.0), writes=[nm_])
        P.dma("sp", t_[0:96, :], src_.ap(), writes=[nm_])
    gup = ld("gup", [128, 2, 1024], g_up.ap().rearrange("(k p) c -> p k c", p=128))
    cmat = ld("cmat", [128, 128], cd["cmat_h"].ap())
    msk2 = ld("msk2", [128, 2], cd["msk2_h"].ap())
    bdf = ld("bdf", [128, 128], cd["bd1_h"].ap())
    tsf = ld("tsf", [128, 128], cd["ts_h"].ap())
    tif = ld("tif", [128, 128], cd["ti_h"].ap())
    tslf = ld("tslf", [128, 128], cd["tsl_h"].ap())
    TS = P.sb("TSb", [128, 128], BF16)
    TI = P.sb("TIb", [128, 128], BF16)
    TSL = P.sb("TSLb", [128, 128], BF16)
    bd1 = P.sb("bd1b", [128, 128], BF16)
    bd64 = P.sb("bd64", [128, 128], F32)
    P.op("dve", lambda e: e.tensor_copy(TS[:], tsf[:]), reads=["tsf"], writes=["TSb"])
    P.op("dve", lambda e: e.tensor_copy(TI[:], tif[:]), reads=["tif"], writes=["TIb"])
    P.op("dve", lambda e: e.tensor_copy(TSL[:], tslf[:]), reads=["tslf"], writes=["TSLb"])
    P.op("dve", lambda e: e.tensor_copy(bd1[:], bdf[:]), reads=["bdf"], writes=["bd1b"])
    P.op("dve", lambda e: e.tensor_scalar(bd64[:], bdf[:], 1.0 / 64, None, ALU.mult), reads=["bdf"], writes=["bd64"])

    P0 = P.sb("rP0", [128, RWW], F32)
    P1 = P.sb("rP1", [128, RWW], F32)
    LI = P.sb("rLI", [128, 512], F32)
    P.op("dve", lambda e: e.memset(LI[:], 0.0), writes=["rLI"])
    LIT = [P.sb("rLIT%d" % i, [128, 4, 128], F32) for i in range(2)]
    U = P.sb("rU", [128, 1024], F32)
    AS = P.sb("rAS", [128, 1024], F32)
    E1, E2, E3 = P1[:, 0:1024], P1[:, 1024:2048], P1[:, 2048:3072]
    KK = P.sb("rKK", [128, 1024], F32)
    T1 = P.sb("rT1", [128, 1024], F32)
    KM = P.sb("rKM", [128, 1024], F32)
    ss = P.sb("rss", [128, 48], F32)
    TM = [[P.sb("rTM%d_%d" % (k, i), [128, 1024], BF16) for i in range(1)] * 2 for k in range(5)]
    FF = [P.sb("rFF%d" % i, [128, 5, 8, 128], BF16) for i in range(1)] * 2
    SC = [P.sb("rSC%d" % i, [128, 8, 2], F32) for i in range(2)]
    psT = P.ps("rpsT", [128, 4, 128], F32)
    pb = [P.ps("rpb%d" % i, [128, 512], F32) for i in range(2)]
    psTb = P.ps("rpsTb", [128, 8, 128], BF16)
    slots = [P.ps("rsl%d" % i, [128, 4, 128], F32) for i in range(4)]
    St = P.sb("rSt", [128, 8, 64], F32)
    Stb = P.sb("rStb", [128, 8, 64], BF16)
    Y2 = P.sb("rY2", [128, 8, 128], F32)
    P.op("dve", lambda e: e.memset(St[:], 0.0), writes=["rSt%d" % h for h in range(16)])
    P.op("dve", lambda e: e.memset(Stb[:], 0.0), writes=["rStb%d" % h for h in range(16)])
    NL = 4
    lane = []
    for l in range(NL):
        d = {}
        for n in ("Nab", "NabT", "Ma", "MaT", "Mb", "MbT", "Pm", "Nka", "Mbr", "Mkr", "AX", "WU", "E", "Pp"):
            d[n] = P.sb("rl%d_%s" % (l, n), [128, 128], BF16)
        for n in ("Yl", "Qp", "AXf"):
            d[n] = P.sb("rl%d_%s" % (l, n), [128, 128], F32)
        d["n"] = 0
        lane.append(d)
    fin = {n: P.sb("rf_" + n, [128, 128], F32) for n in ("YC", "SQ", "SD", "YN", "BN")}
    finb = {n: P.sb("rf_" + n, [128, 128], BF16) for n in ("RK",)}
    YB = [P.sb("rf_YB%d" % i, [128, 128], BF16) for i in range(2)]
    evq = [0]

    def slot(l):
        d = lane[l]
        i = d["n"] % 4
        d["n"] += 1
        return slots[l][:, i, :], "rsl%d" % l

    def ev_eng():
        evq[0] += 1
        return "dve" if evq[0] % 3 else "act"

    def mm(out, okey, lhsT, rhs, reads, start=True, stop=True):
        P.op("pe", lambda e: e.matmul(out, lhsT, rhs, start=start, stop=stop, skip_group_check=True), reads=reads, writes=[okey])

    _lo, _hi = (int(v) for v in os.environ.get('RCHUNKS', '0,32').split(','))
    for c in range(_lo, _hi):
        own = c >= 16
        t0 = c * 128
        cb = c % 2
        F, S_, Lt = FF[cb], SC[cb], LIT[cb]
        tmA, tmR, tmB, tmK, tmV = (TM[k][cb] for k in range(5))
        kA, kR, kB, kK, kV = ("rTM%d_0" % k for k in range(5))
        kF, kS, kL = "rFF0", "rSC%d" % cb, "rLIT%d" % cb
        rd = ["prw:%d:%d" % (c, b) for b in range(7)] + (["prw:%d:%d" % (c - 1, b) for b in range(7)] if c else ["prw"])
        P.dma("sp", P1[:], prw.ap()[1 + t0:1 + t0 + 128, :], reads=rd, writes=["rP1", "rE1", "rE2", "rE3"])
        P.dma("sp", P0[:], prw.ap()[t0:t0 + 128, :], reads=rd, writes=["rP0"])
        P.op("dve", lambda e: e.tensor_tensor(P0[:], P0[:], P1[:], ALU.subtract), reads=["rP0", "rP1"], writes=["rP0"])
        P.op("pool", lambda e: e.tensor_tensor(P0[:], P0[:], mu_b[:], ALU.mult), reads=["rP0", "mu_b"], writes=["rP0"])
        P.op("dve", lambda e: e.tensor_tensor(P0[:], P0[:], P1[:], ALU.add), reads=["rP0", "rP1"], writes=["rP0", "rE1", "rE2", "rE3"])
        Z = P0
        zr, zk, zv = Z[:, 0:1024], Z[:, 1024:2048], Z[:, 2048:3072]
        P.op("act", lambda e: e.activation(LI[:, 0:96], Z[:, 3072:3168], AF.Tanh), reads=["rP0"], writes=["rLI"])
        P.op("act", lambda e: e.copy(LI[:, 128:224], Z[:, 3168:3264]), reads=["rP0"], writes=["rLI"])
        P.op("act", lambda e: e.activation(LI[:, 256:512], Z[:, 3264:3520], AF.Sigmoid), reads=["rP0"], writes=["rLI"])
        for j_ in range(4):
            P.op("pe", lambda e, j_=j_: e.transpose(psT[:, j_, :], LI[:, j_ * 128:(j_ + 1) * 128], ident[:]), reads=["rLI", "ident"], writes=["rpsT"])
        P.op("act", lambda e, Lt=Lt: e.copy(Lt[:], psT[:]), reads=["rpsT"], writes=[kL])
        for hf in range(2):
            mm(pb[hf][:], "rpb%d" % hf, Lt[:, 0, :], wup[:, hf * 512:(hf + 1) * 512], [kL, "wup"])
            P.op("dve", lambda e, hf=hf: e.tensor_tensor(U[:, hf * 512:(hf + 1) * 512], pb[hf][:], w0_b[:, hf * 512:(hf + 1) * 512], ALU.add),
                 reads=["rpb%d" % hf, "w0_b"], writes=["rU"])
        P.op("act", lambda e: e.activation(U[:], U[:], AF.Sigmoid), reads=["rU"], writes=["rU"])
        P.op("dve", lambda e: e.tensor_scalar(U[:], U[:], -0.6065306597126334, None, ALU.mult), reads=["rU"], writes=["rU"])
        for hf in range(2):
            mm(pb[hf][:], "rpb%d" % hf, Lt[:, 1, :], aup[:, hf * 512:(hf + 1) * 512], [kL, "aup"])
            P.op("dve", lambda e, hf=hf: e.tensor_tensor(AS[:, hf * 512:(hf + 1) * 512], pb[hf][:], a0_b[:, hf * 512:(hf + 1) * 512], ALU.add),
                 reads=["rpb%d" % hf, "a0_b"], writes=["rAS"])
        P.op("act", lambda e: e.activation(AS[:], AS[:], AF.Sigmoid), reads=["rAS"], writes=["rAS"])
        for hf in range(2):
            hs_ = slice(hf * 512, (hf + 1) * 512)
            mm(pb[hf][:], "rpb%d" % hf, cmat[:], U[:, hs_], ["cmat", "rU"])
            P.op("act", lambda e, hf=hf, hs_=hs_: e.activation(E1[:, hs_], pb[hf][:], AF.Exp), reads=["rpb%d" % hf], writes=["rE1"])
            P.op("act", lambda e, hf=hf, hs_=hs_: e.activation(E2[:, hs_], pb[hf][:], AF.Exp, scale=-1.0), reads=["rpb%d" % hf], writes=["rE2"])
            P.op("dve", lambda e, hf=hf, hs_=hs_: e.tensor_tensor(E3[:, hs_], pb[hf][:], U[:, hs_], ALU.subtract), reads=["rpb%d" % hf, "rU"], writes=["rE3"])
        P.op("act", lambda e: e.activation(E3, E3, AF.Exp), reads=["rE3"], writes=["rE3"])
        for hp in range(8):
            P.op("pe", lambda e, hp=hp: e.matmul(psT[:, 0, 2 * hp:2 * hp + 2], U[:, hp * 128:(hp + 1) * 128], msk2[:], start=True, stop=True, skip_group_check=True),
                 reads=["rU", "msk2", kL], writes=["rpsT"])
        P.op("act", lambda e, S_=S_: e.activation(S_[:].rearrange("p h t -> p (h t)"), psT[:, 0, 0:16], AF.Exp), reads=["rpsT"], writes=[kS])
        P.op("pool", lambda e: e.tensor_tensor(KK[:], zk, kk_b[:], ALU.mult), reads=["rP0", "kk_b"], writes=["rKK"])
        P.op("pool", lambda e: e.tensor_tensor(T1[:], KK[:], KK[:], ALU.mult), reads=["rKK"], writes=["rT1"])
        P.op("dve", lambda e: e.tensor_reduce(ss[:, 0:16], T1[:].rearrange("p (h d) -> p h d", d=64), AX.X, ALU.add), reads=["rT1"], writes=["rss"])
        P.op("act", lambda e: e.activation(ss[:, 0:16], ss[:, 0:16], AF.Sqrt), reads=["rss"], writes=["rss"])
        P.op("dve", lambda e: e.tensor_scalar(ss[:, 0:16], ss[:, 0:16], 1e-12, None, ALU.max), reads=["rss"], writes=["rss"])
        P.op("dve", lambda e: e.reciprocal(ss[:, 16:32], ss[:, 0:16]), reads=["rss"], writes=["rss"])
        P.op("dve", lambda e: e.tensor_tensor(KK[:].rearrange("p (h d) -> p h d", d=64), KK[:].rearrange("p (h d) -> p h d", d=64),
                                              AP(ss, 16, [[48, 128], [1, 16], [0, 64]]), ALU.mult), reads=["rKK", "rss"], writes=["rKK"])
        P.op("dve", lambda e: e.scalar_tensor_tensor(out=T1[:], in0=AS[:], scalar=-1.0, in1=ka_b[:], op0=ALU.add, op1=ALU.mult),
             reads=["rAS", "ka_b"], writes=["rT1"])
        P.op("dve", lambda e: e.scalar_tensor_tensor(out=KM[:], in0=T1[:], scalar=1.0, in1=zk, op0=ALU.add, op1=ALU.mult),
             reads=["rT1", "rP0"], writes=["rKM"])
        P.op("dve", lambda e, o=tmA: e.scalar_tensor_tensor(out=o[:], in0=KK[:], scalar=-1.0, in1=E3, op0=ALU.mult, op1=ALU.mult),
             reads=["rKK", "rE3"], writes=[kA])
        P.op("pool", lambda e: e.tensor_tensor(T1[:], KK[:], AS[:], ALU.mult), reads=["rKK", "rAS"], writes=["rT1"])
        P.op("dve", lambda e, o=tmB: e.tensor_tensor(o[:], T1[:], E2, ALU.mult), reads=["rT1", "rE2"], writes=[kB])
        P.op("pool", lambda e, o=tmR: e.tensor_tensor(o[:], zr, E1, ALU.mult), reads=["rP0", "rE1"], writes=[kR])
        P.op("dve", lambda e, o=tmK: e.tensor_tensor(o[:], KM[:], E2, ALU.mult), reads=["rKM", "rE2"], writes=[kK])
        P.op("act", lambda e, o=tmV: e.copy(o[:], zv), reads=["rP0"], writes=[kV])
        for kind, (tm, kk_) in enumerate(((tmA, kA), (tmR, kR), (tmB, kB), (tmK, kK), (tmV, kV))):
            if kind == 1 and not own:
                continue
            if kind == 4 and not own:
                continue
            for hp in range(8):
                P.op("pe", lambda e, tm=tm, hp=hp: e.transpose(psTb[:, hp, :], tm[:, hp * 128:(hp + 1) * 128], identb[:]),
                     reads=[kk_, "identb"], writes=["rpsTb"])
            eng = "act" if kind % 2 else "dve"
            if eng == "act":
                P.op("act", lambda e, F=F, kind=kind: e.copy(F[:, kind, :, :], psTb[:]), reads=["rpsTb"], writes=[kF + "_%d" % kind])
            else:
                P.op("dve", lambda e, F=F, kind=kind: e.tensor_copy(F[:, kind, :, :], psTb[:]), reads=["rpsTb"], writes=[kF + "_%d" % kind])

        def head_gen(h, l, S_=S_, own=own, kS=kS):
            d = lane[l]
            par, hp = h % 2, h // 2
            eo = par * 64
            hs = slice(h * 64, (h + 1) * 64)
            es = slice(eo, eo + 64)
            kn = lambda n: "rl%d_%s" % (l, n)
            Af, Rf, Bf, Kf = (F[es, k, hp, :] for k in range(4))
            fk = [kF + "_%d" % k for k in range(5)]

            def evac(dst, dkey, src, skey, mask=None, mkey=None):
                eng = ev_eng() if mask is None else "dve"
                if mask is not None:
                    P.op("dve", lambda e: e.tensor_tensor(dst, src, mask, ALU.mult), reads=[skey, mkey], writes=[dkey])
                elif eng == "act":
                    P.op("act", lambda e: e.copy(dst, src), reads=[skey], writes=[dkey])
                else:
                    P.op("dve", lambda e: e.tensor_copy(dst, src), reads=[skey], writes=[dkey])

            o, ok = slot(l)
            mm(o, ok, Bf, Af, [fk[2], fk[0]])
            evac(d["Nab"][:], kn("Nab"), o, ok, TS[:], "TSb")
            o, ok = slot(l)
            mm(o, ok, Af, Bf, [fk[0], fk[2]])
            evac(d["NabT"][:], kn("NabT"), o, ok, TSL[:], "TSLb")
            P.op("pool", lambda e: e.tensor_tensor(d["Pm"][:], d["Nab"][:], identb[:], ALU.add), reads=[kn("Nab"), "identb"], writes=[kn("Pm")])
            yield
            if RSTOP < 2:
                return
            o, ok = slot(l)
            mm(o, ok, Kf, Af, [fk[3], fk[0]])
            evac(d["Nka"][:], kn("Nka"), o, ok, TS[:], "TSb")
            if own:
                o, ok = slot(l)
                mm(o, ok, Bf, Rf, [fk[2], fk[1]])
                evac(d["Mbr"][:], kn("Mbr"), o, ok, TI[:], "TIb")
                o, ok = slot(l)
                mm(o, ok, Kf, Rf, [fk[3], fk[1]])
                evac(d["Mkr"][:], kn("Mkr"), o, ok, TI[:], "TIb")
            yield
            if RSTOP < 3:
                return
            M, MT, kM, kMT = d["Nab"], d["NabT"], kn("Nab"), kn("NabT")
            for lev in range(1, 7):
                nM, nMT = (d["Ma"], d["MaT"]) if lev % 2 else (d["Mb"], d["MbT"])
                knM, knMT = (kn("Ma"), kn("MaT")) if lev % 2 else (kn("Mb"), kn("MbT"))
                if lev < 6:
                    o, ok = slot(l)
                    mm(o, ok, MT[:], M[:], [kMT, kM])
                    evac(nM[:], knM, o, ok)
                o, ok = slot(l)
                mm(o, ok, M[:], MT[:], [kM, kMT])
                evac(nMT[:], knMT, o, ok)
                yield
                o, ok = slot(l)
                mm(o, ok, nMT[:], d["Pm"][:], [knMT, kn("Pm")])
                P.op("dve", lambda e, o=o: e.tensor_tensor(d["Pm"][:], o, d["Pm"][:], ALU.add), reads=[ok, kn("Pm")], writes=[kn("Pm")])
                M, MT, kM, kMT = nM, nMT, knM, knMT
                yield
            if RSTOP < 4:
                return
            o, ok = slot(l)
            mm(o[:, 0:64], ok, d["Nka"][:], tmV[:, hs], [kn("Nka"), kV])
            evac(d["AX"][:, 64:128], kn("AX"), o[:, 0:64], ok)
            P.op("pool", lambda e: e.tensor_copy(d["AX"][:, 0:64], tmA[:, hs]), reads=[kA], writes=[kn("AX")])
            yield
            o, ok = slot(l)
            mm(o, ok, d["Pm"][:], d["AX"][:], [kn("Pm"), kn("AX")])
            evac(d["WU"][:], kn("WU"), o, ok)
            yield
            WT, UT = d["WU"][:, 0:64], d["WU"][:, 64:128]
            if RSTOP < 5:
                return
            o, ok = slot(l)
            mm(o[es, 0:64], ok, WT, tmB[:, hs], [kn("WU"), kB])
            P.op("dve", lambda e, o=o: e.tensor_tensor(d["Qp"][es, 64:128], o[es, 0:64], ident[es, es], ALU.add), reads=[ok, "ident"], writes=[kn("Qp") + "t"])
            P.op("dve", lambda e: e.tensor_scalar(d["Pp"][es, 0:64], d["Qp"][es, 64:128], S_[es, hp, 0:1], None, ALU.mult),
                 reads=[kn("Qp") + "t", kS], writes=[kn("Pp")])
            o, ok = slot(l)
            mm(o[es, 0:64], ok, tmB[:, hs], UT, [kB, kn("WU")], start=True, stop=False)
            mm(o[es, 0:64], ok, tmK[:, hs], tmV[:, hs], [kK, kV], start=False, stop=True)
            P.op("dve", lambda e, o=o: e.tensor_scalar(d["Qp"][es, 0:64], o[es, 0:64], S_[es, hp, 1:2], None, ALU.mult),
                 reads=[ok, kS], writes=[kn("Qp")])
            if own:
                o, ok = slot(l)
                mm(o[es, :], ok, WT, d["Mbr"][:], [kn("WU"), kn("Mbr")])
                P.op("dve", lambda e, o=o: e.tensor_tensor(d["Yl"][es, :], o[es, :], Rf, ALU.add), reads=[ok, fk[1]], writes=[kn("Yl") + "e"])
                P.op("dve", lambda e: e.tensor_scalar(d["E"][es, :], d["Yl"][es, :], S_[es, hp, 0:1], None, ALU.mult),
                     reads=[kn("Yl") + "e", kS], writes=[kn("E")])
                o, ok = slot(l)
                mm(o[es, :], ok, UT, d["Mbr"][:], [kn("WU"), kn("Mbr")], start=True, stop=False)
                mm(o[es, :], ok, tmV[:, hs], d["Mkr"][:], [kV, kn("Mkr")], start=False, stop=True)
                P.op("act", lambda e, o=o: e.copy(d["AXf"][es, :], o[es, :]), reads=[ok], writes=[kn("AXf")])
            yield
            if RSTOP < 6:
                return
            if own:
                o, ok = slot(l)
                mm(o[es, :], ok, Stb[es, hp, :], d["E"][es, :], ["rStb%d" % h, kn("E")])
                P.op("dve", lambda e, o=o: e.tensor_tensor(Y2[es, hp, :], o[es, :], d["AXf"][es, :], ALU.add), reads=[ok, kn("AXf")], writes=["rY2_%d" % h])
            o, ok = slot(l)
            mm(o[es, 0:64], ok, d["Pp"][es, 0:64], Stb[es, hp, :], [kn("Pp"), "rStb%d" % h])
            P.op("dve", lambda e, o=o: e.scalar_tensor_tensor(out=St[es, hp, :], in0=o[es, 0:64], scalar=S_[es, hp, 1:2], in1=d["Qp"][es, 0:64],
                                                             op0=ALU.mult, op1=ALU.add), reads=[ok, kS, kn("Qp")], writes=["rSt%d" % h])
            P.op("act", lambda e: e.copy(Stb[es, hp, :], St[es, hp, :]), reads=["rSt%d" % h], writes=["rStb%d" % h])
            yield

        for grp in range(4 if RSTOP >= 1 else 0):
            gens = [head_gen(grp * NL + l, l) for l in range(NL)]
            alive = list(gens)
            while alive:
                nxt = []
                for g in alive:
                    try:
                        next(g)
                        nxt.append(g)
                    except StopIteration:
                        pass
                alive = nxt
        if own and RSTOP >= 7:
            for hp in range(8):
                l = hp % NL
                yk = ["rY2_%d" % (2 * hp), "rY2_%d" % (2 * hp + 1)]
                o, ok = slot(l)
                mm(o, ok, bd64[:], Y2[:, hp, :], ["bd64"] + yk)
                P.op("dve", lambda e, o=o, hp=hp: e.tensor_tensor(fin["YC"][:], Y2[:, hp, :], o, ALU.subtract), reads=yk + [ok], writes=["rfYC"])
                P.op("act", lambda e: e.activation(fin["SQ"][:], fin["YC"][:], AF.Square), reads=["rfYC"], writes=["rfSQ"])
                o, ok = slot(l)
                mm(o, ok, bd64[:], fin["SQ"][:], ["bd64", "rfSQ"])
                P.op("act", lambda e, o=o: e.activation(fin["SD"][:], o, AF.Sqrt, bias=EPSB[0][:, 2:3], scale=1.0), reads=[ok, "epsb"], writes=["rfSD"])
                P.op("dve", lambda e: e.reciprocal(fin["SD"][:], fin["SD"][:]), reads=["rfSD"], writes=["rfSD"])
                P.op("dve", lambda e: e.tensor_tensor(fin["YN"][:], fin["YC"][:], fin["SD"][:], ALU.mult), reads=["rfYC", "rfSD"], writes=["rfYN"])
                P.op("dve", lambda e, hp=hp: e.tensor_scalar(fin["YN"][:], fin["YN"][:], lg_c[:, hp:hp + 1], lb_c[:, hp:hp + 1], ALU.mult, ALU.add),
                     reads=["rfYN", "lg_c", "lb_c"], writes=["rfYN"])
                P.op("dve", lambda e, hp=hp: e.scalar_tensor_tensor(out=finb["RK"][:], in0=F[:, 1, hp, :], scalar=rk_c[:, hp:hp + 1], in1=F[:, 3, hp, :],
                                                                    op0=ALU.mult, op1=ALU.mult), reads=[kF + "_1", kF + "_3", "rk_c"], writes=["rfRK"])
                o, ok = slot(l)
                mm(o, ok, bd1[:], finb["RK"][:], ["bd1b", "rfRK"])
                P.op("dve", lambda e, o=o, hp=hp: e.tensor_tensor(fin["BN"][:], o, F[:, 4, hp, :], ALU.mult), reads=[ok, kF + "_4"], writes=["rfBN"])
                P.op("dve", lambda e: e.tensor_tensor(fin["YN"][:], fin["YN"][:], fin["BN"][:], ALU.add), reads=["rfYN", "rfBN"], writes=["rfYN"])
                o, ok = slot(l)
                mm(o, ok, gup[:, 0, hp * 128:(hp + 1) * 128], Lt[:, 2, :], ["gup", kL], start=True, stop=False)
                mm(o, ok, gup[:, 1, hp * 128:(hp + 1) * 128], Lt[:, 3, :], ["gup", kL], start=False, stop=True)
                yb = YB[hp % 2]
                P.op("dve", lambda e, o=o, yb=yb: e.tensor_tensor(yb[:], fin["YN"][:], o, ALU.mult), reads=["rfYN", ok], writes=["rfYB%d" % (hp % 2)])
                q0 = (c - 16) * 128
                P.dma("sp", ybT_s.ap()[hp * 128:(hp + 1) * 128, q0:q0 + 128], yb[:], reads=["rfYB%d" % (hp % 2)], writes=["ybT:%d:%d" % (c - 16, hp)])


def stage_M(P, nc, din, identb, xs_d, y_d, x1_s, slot_s, gate_s, out_d):
    wg = [din("wg_%d" % e, [D, 1024]) for e in range(32)]
    wu = [din("wu_%d" % e, [D, 1024]) for e in range(32)]
    wd = [din("wd_%d" % e, [1024, D]) for e in range(32)]
    NS = CAP // 128
    xs = [P.sb("mxs%d" % i, [128, D], BF16) for i in range(2)]
    xT = P.sb("mxT", [128, 16, CAP], BF16)
    wst = [P.sb("mwst%d" % i, [128, 16, 256], F32) for i in range(2)]
    wgb = [P.sb("mwgb%d" % i, [128, 16, 256], BF16) for i in range(2)]
    wub = [P.sb("mwub%d" % i, [128, 16, 256], BF16) for i in range(2)]
    wdb = [P.sb("mwdb%d" % i, [128, 8, 512], BF16) for i in range(2)]
    hT = P.sb("mhT", [128, 8, CAP], BF16)
    sl = P.sb("msl", [128, CAP], F32)
    yo = [P.sb("myo%d" % i, [128, 512], F32) for i in range(2)]
    psTb = P.ps("mpsTb", [128, 8, 128], BF16)
    psG = [P.ps("mpsG%d" % i, [128, CAP], F32) for i in range(2)]
    psU = [P.ps("mpsU%d" % i, [128, CAP], F32) for i in range(2)]
    psY = [P.ps("mpsY%d" % i, [128, 512], F32) for i in range(2)]
    zr = P.sb("mzr", [1, D], F32)
    P.op("dve", lambda e: e.memset(zr[:], 0.0), writes=["mzr"])
    P.dma("sp", y_d.ap()[NSLOT:NSLOT + 1, :], zr[:], reads=["mzr"], writes=["y_dump"])
    nw = [0]
    ncast = [0]

    def cast(dst, src, rk, wk):
        ncast[0] += 1
        eng = ("dve", "pool", "act")[ncast[0] % 3]
        if eng == "act":
            P.op("act", lambda e: e.copy(dst, src), reads=[rk], writes=[wk])
        else:
            P.op(eng, lambda e: e.tensor_copy(dst, src), reads=[rk], writes=[wk])

    for ex in range(32):
        for s_ in range(NS):
            X = xs[s_ % 2]
            r0 = ex * CAP + s_ * 128
            P.dma("sp", X[:], xs_d.ap()[r0:r0 + 128, :], reads=["xs_all"], writes=["mxs%d" % (s_ % 2)])
            for half in range(2):
                for k in range(8):
                    kc = half * 8 + k
                    P.op("pe", lambda e, X=X, k=k, kc=kc: e.transpose(psTb[:, k, :], X[:, kc * 128:(kc + 1) * 128], identb[:]),
                         reads=["mxs%d" % (s_ % 2), "identb"], writes=["mpsTb"])
                P.op("dve", lambda e, half=half, s_=s_: e.tensor_copy(xT[:, half * 8:(half + 1) * 8, s_ * 128:(s_ + 1) * 128], psTb[:]),
                     reads=["mpsTb"], writes=["mxT_%d_%d" % (s_, half)])
        xk = ["mxT_%d_%d" % (s_, half) for s_ in range(NS) for half in range(2)]
        for cb in range(4):
            bb = nw[0] % 2
            nw[0] += 1
            for (wsrc, wdst, nm) in ((wg[ex], wgb[bb], "mwgb%d" % bb), (wu[ex], wub[bb], "mwub%d" % bb)):
                st_i = ncast[0] % 2
                W = wst[st_i]
                P.dma("sp", W[:], wsrc.ap()[:, cb * 256:(cb + 1) * 256].rearrange("(k p) c -> p k c", p=128), writes=["mwst%d" % st_i])
                cast(wdst[:], W[:], "mwst%d" % st_i, nm)
            for hc in range(2):
                pg, pu = psG[hc], psU[hc]
                for k in range(16):
                    P.op("pe", lambda e, pg=pg, k=k, hc=hc, bb=bb: e.matmul(pg[:], wgb[bb][:, k, hc * 128:(hc + 1) * 128], xT[:, k, :], start=(k == 0), stop=(k == 15)),
                         reads=["mwgb%d" % bb] + xk, writes=["mpsG%d" % hc])
                for k in range(16):
                    P.op("pe", lambda e, pu=pu, k=k, hc=hc, bb=bb: e.matmul(pu[:], wub[bb][:, k, hc * 128:(hc + 1) * 128], xT[:, k, :], start=(k == 0), stop=(k == 15)),
                         reads=["mwub%d" % bb] + xk, writes=["mpsU%d" % hc])
                hi = cb * 2 + hc
                P.op("act", lambda e, pg=pg: e.activation(sl[:], pg[:], AF.Silu), reads=["mpsG%d" % hc], writes=["msl"])
                P.op("dve", lambda e, pu=pu, hi=hi: e.tensor_tensor(hT[:, hi, :], sl[:], pu[:], ALU.mult), reads=["msl", "mpsU%d" % hc], writes=["mhT_%d" % hi])
        hk = ["mhT_%d" % i for i in range(8)]
        for cb in range(4):
            bb = cb % 2
            st_i = ncast[0] % 2
            W = wst[st_i]
            Wv = W[:].rearrange("p (a k) c -> p a (k c)", a=8)
            P.dma("sp", Wv, wd[ex].ap()[:, cb * 512:(cb + 1) * 512].rearrange("(k p) c -> p k c", p=128), writes=["mwst%d" % st_i])
            cast(wdb[bb][:], Wv, "mwst%d" % st_i, "mwdb%d" % bb)
            for s_ in range(NS):
                py = psY[s_ % 2]
                for k in range(8):
                    P.op("pe", lambda e, py=py, k=k, s_=s_, bb=bb: e.matmul(py[:], hT[:, k, s_ * 128:(s_ + 1) * 128], wdb[bb][:, k, :], start=(k == 0), stop=(k == 7)),
                         reads=hk + ["mwdb%d" % bb], writes=["mpsY%d" % (s_ % 2)])
                Y = yo[s_ % 2]
                P.op("act", lambda e, py=py, Y=Y: e.copy(Y[:], py[:]), reads=["mpsY%d" % (s_ % 2)], writes=["myo%d" % (s_ % 2)])
                r0 = ex * CAP + s_ * 128
                P.dma("sp", y_d.ap()[r0:r0 + 128, cb * 512:(cb + 1) * 512], Y[:], reads=["myo%d" % (s_ % 2)], writes=["y_all"])
    P.barrier()
    x1 = [P.sb("mx1_%d" % i, [128, D], F32) for i in range(2)]
    y1 = [P.sb("my1_%d" % i, [128, D], F32) for i in range(2)]
    y2 = [P.sb("my2_%d" % i, [128, D], F32) for i in range(2)]
    si = [P.sb("msi%d" % i, [128, 2], I32) for i in range(2)]
    gt = [P.sb("mgt%d" % i, [128, 2], F32) for i in range(2)]
    for t in range(16):
        b = t % 2
        P.dma("sp", si[b][:], slot_s.ap()[t], writes=["msi%d" % b])
        P.dma("sp", gt[b][:], gate_s.ap()[t], writes=["mgt%d" % b])
        P.dma("sp", x1[b][:], x1_s.ap()[t * 128:(t + 1) * 128, :], writes=["mx1_%d" % b])
        P.idma_gather(y1[b][:], y_d, si[b][:, 0:1], reads=["msi%d" % b], writes=["my1_%d" % b])
        P.idma_gather(y2[b][:], y_d, si[b][:, 1:2], reads=["msi%d" % b], writes=["my2_%d" % b])
        P.op("dve", lambda e, b=b: e.scalar_tensor_tensor(out=x1[b][:], in0=y1[b][:], scalar=gt[b][:, 0:1], in1=x1[b][:], op0=ALU.mult, op1=ALU.add),
             reads=["my1_%d" % b, "mgt%d" % b, "mx1_%d" % b], writes=["mx1_%d" % b])
        P.op("dve", lambda e, b=b: e.scalar_tensor_tensor(out=x1[b][:], in0=y2[b][:], scalar=gt[b][:, 1:2], in1=x1[b][:], op0=ALU.mult, op1=ALU.add),
             reads=["my2_%d" % b, "mgt%d" % b, "mx1_%d" % b], writes=["mx1_%d" % b])
        P.dma("sp", out_d.ap()[t * 128:(t + 1) * 128, :], x1[b][:], reads=["mx1_%d" % b], writes=["out:%d" % t])


CAP = 256
NSLOT = 32 * CAP


def stage_O1(P, nc, din, identb, ya_s, ybT_s, sg_s, mgT_s):
    proj_a = din("proj_a", [1024, D])
    proj_b = din("proj_b", [1024, D])
    PA = P.sb("oPA", [128, 8, D], BF16)
    PB = P.sb("oPB", [128, 8, D], BF16)
    wst = P.sb("owst", [128, 8, 512], F32)
    for wi, (src, dst, nm) in enumerate(((proj_a, PA, "oPA"), (proj_b, PB, "oPB"))):
        for cb in range(4):
            P.dma("sp", wst[:], src.ap()[:, cb * 512:(cb + 1) * 512].rearrange("(k p) c -> p k c", p=128), writes=["owst"])
            eng = "dve" if cb % 2 else "pool"
            P.op(eng, lambda e, dst=dst, cb=cb: e.tensor_copy(dst[:, :, cb * 512:(cb + 1) * 512], wst[:]), reads=["owst"], writes=[nm + "_%d" % cb])
    yat = [P.sb("oya%d" % i, [128, 1024], F32) for i in range(2)]
    yab = P.sb("oyab", [128, 1024], BF16)
    yaT = P.sb("oyaT", [128, 8, 128], BF16)
    ybT = [P.sb("oybT%d" % i, [128, 8, 128], BF16) for i in range(2)]
    sg = [P.sb("osg%d" % i, [128, 4096], BF16) for i in range(2)]
    m1 = P.sb("om1", [128, 512], F32)
    m2 = P.sb("om2", [128, 512], F32)
    MG = P.sb("oMG", [128, D], BF16)
    mgT = [P.sb("omgT%d" % i, [128, 16, 128], BF16) for i in range(2)]
    psTb = P.ps("opsTb", [128, 8, 128], BF16)
    psA = [P.ps("opsA%d" % i, [128, 512], F32) for i in range(2)]
    psB = [P.ps("opsB%d" % i, [128, 512], F32) for i in range(2)]
    for t in range(16):
        b = t % 2
        P.dma("sp", yat[b][:], ya_s.ap()[t * 128:(t + 1) * 128, :], reads=["ya_s:%d:%d" % (t, h) for h in range(8)], writes=["oya%d" % b])
        P.dma("sp", ybT[b][:], ybT_s.ap()[:, t * 128:(t + 1) * 128].rearrange("(k p) t -> p k t", p=128),
              reads=["ybT:%d:%d" % (t, h) for h in range(8)], writes=["oybT%d" % b])
        P.dma("sp", sg[b][:], sg_s.ap()[t * 128:(t + 1) * 128, :], reads=["sg_s:%d:%d" % (t, i) for i in range(8)], writes=["osg%d" % b])
        P.op("act", lambda e, b=b: e.copy(yab[:], yat[b][:]), reads=["oya%d" % b], writes=["oyab"])
        for k in range(8):
            P.op("pe", lambda e, k=k: e.transpose(psTb[:, k, :], yab[:, k * 128:(k + 1) * 128], identb[:]), reads=["oyab", "identb"], writes=["opsTb"])
        P.op("dve", lambda e: e.tensor_copy(yaT[:], psTb[:]), reads=["opsTb"], writes=["oyaT"])
        for cb in range(4):
            cs_ = slice(cb * 512, (cb + 1) * 512)
            pa, pbb = psA[cb % 2], psB[cb % 2]
            ka, kb_ = "opsA%d" % (cb % 2), "opsB%d" % (cb % 2)
            for k in range(8):
                P.op("pe", lambda e, pa=pa, k=k, cs_=cs_: e.matmul(pa[:], yaT[:, k, :], PA[:, k, cs_], start=(k == 0), stop=(k == 7)),
                     reads=["oyaT", "oPA_%d" % cb], writes=[ka])
            for k in range(8):
                P.op("pe", lambda e, pbb=pbb, k=k, cs_=cs_, b=b: e.matmul(pbb[:], ybT[b][:, k, :], PB[:, k, cs_], start=(k == 0), stop=(k == 7)),
                     reads=["oybT%d" % b, "oPB_%d" % cb], writes=[kb_])
            P.op("dve", lambda e, pa=pa, cs_=cs_, b=b: e.tensor_tensor(m1[:], pa[:], sg[b][:, cs_], ALU.mult), reads=[ka, "osg%d" % b], writes=["om1"])
            P.op("dve", lambda e, pbb=pbb, cb=cb, b=b: e.tensor_tensor(m2[:], pbb[:], sg[b][:, 2048 + cb * 512:2048 + (cb + 1) * 512], ALU.mult),
                 reads=[kb_, "osg%d" % b], writes=["om2"])
            P.op("pool", lambda e, cs_=cs_: e.tensor_tensor(MG[:, cs_], m1[:], m2[:], ALU.add), reads=["om1", "om2"], writes=["oMG_%d" % cb])
        for half in range(2):
            for k in range(8):
                kc = half * 8 + k
                P.op("pe", lambda e, k=k, kc=kc: e.transpose(psTb[:, k, :], MG[:, kc * 128:(kc + 1) * 128], identb[:]),
                     reads=["oMG_%d" % (kc // 4), "identb"], writes=["opsTb"])
            P.op("act", lambda e, half=half, b=b: e.copy(mgT[b][:, half * 8:(half + 1) * 8, :], psTb[:]), reads=["opsTb"], writes=["omgT%d_%d" % (b, half)])
        P.dma("sp", mgT_s.ap()[t], mgT[b][:], reads=["omgT%d_0" % b, "omgT%d_1" % b], writes=["mgT_s:%d" % t])


def stage_O2(P, nc, din, ident, xw, mgT_s, x1_s, xs_d, slot_s, gate_s):
    w_out = [din("w_out_%d" % i, [1024, D]) for i in range(2)]
    norm2_g = din("norm2_g", [1, D])
    rw_d = din("router_w", [D, 36])
    rb_d = din("router_b", [1, 36])
    ebase_d = din("ebase_h", [1, 32])
    tsf_d = din("ts_h2", [128, 128])
    WO = P.sb("oWO", [128, 16, D], BF16)
    wst = P.sb("o2wst", [128, 8, 512], F32)
    for i in range(2):
        for cb in range(4):
            P.dma("sp", wst[:], w_out[i].ap()[:, cb * 512:(cb + 1) * 512].rearrange("(k p) c -> p k c", p=128), writes=["o2wst"])
            eng = "dve" if cb % 2 else "pool"
            P.op(eng, lambda e, i=i, cb=cb: e.tensor_copy(WO[:, i * 8:(i + 1) * 8, cb * 512:(cb + 1) * 512], wst[:]), reads=["o2wst"], writes=["oWO_%d_%d" % (i, cb)])
    g2b = P.sb("og2b", [128, D], F32)
    P.dma("sp", g2b[:], AP(norm2_g, 0, [[0, 128], [1, D]]), writes=["og2b"])
    RW = P.sb("oRW", [128, 16, 36], F32)
    P.dma("sp", RW[:], rw_d.ap().rearrange("(k p) c -> p k c", p=128), writes=["oRW"])
    rbb = P.sb("orbb", [128, 36], F32)
    P.dma("sp", rbb[:], AP(rb_d, 0, [[0, 128], [1, 36]]), writes=["orbb"])
    ebase = P.sb("oebase", [128, 32], F32)
    P.dma("sp", ebase[:], AP(ebase_d, 0, [[0, 128], [1, 32]]), writes=["oebase"])
    tsf = P.sb("otsf", [128, 128], F32)
    P.dma("sp", tsf[:], tsf_d.ap(), writes=["otsf"])
    onesf = P.sb("oones", [128, 128], F32)
    P.op("dve", lambda e: e.memset(onesf[:], 1.0), writes=["oones"])
    carry = P.sb("ocarry", [128, 32], F32)
    P.op("dve", lambda e: e.memset(carry[:], 0.0), writes=["ocarry"])
    zxs = P.sb("ozxs", [128, D], BF16)
    P.op("dve", lambda e: e.memset(zxs[:], 0.0), writes=["ozxs"])
    zk_ = []
    for i_ in range(NSLOT // 128):
        P.dma("sp", xs_d.ap()[i_ * 128:(i_ + 1) * 128, :], zxs[:], reads=["ozxs"], writes=["xsz:%d" % i_])
        zk_.append("xsz:%d" % i_)
    P.dma("sp", xs_d.ap()[NSLOT:NSLOT + 1, :], zxs[0:1, :], reads=["ozxs"], writes=["xsz:d"])
    zk_.append("xsz:d")
    P.wait_all("pool", zk_)
    mgT = [P.sb("o2mgT%d" % i, [128, 16, 128], BF16) for i in range(2)]
    xt = [P.sb("o2x%d" % i, [128, D], F32) for i in range(2)]
    X1 = P.sb("oX1", [128, D], F32)
    H2 = P.sb("oH2", [128, D], F32)
    H2b = [P.sb("oH2b%d" % i, [128, D], BF16) for i in range(2)]
    sq = P.sb("o2sq", [128, D], BF16)
    h2T = P.sb("oh2T", [128, 16, 128], F32)
    r = P.sb("or", [128, 256], F32)
    slot_i = [P.sb("oslot%d" % i, [128, 2], I32) for i in range(2)]
    gate2 = [P.sb("ogate%d" % i, [128, 2], F32) for i in range(2)]
    psX = [P.ps("opsX%d" % i, [128, 512], F32) for i in range(2)]
    psT = P.ps("o2psT", [128, 4, 128], F32)
    psR = P.ps("opsR", [128, 512], F32)
    psP = P.ps("opsP", [128, 512], F32)

    def R(a, b_):
        return r[:, a:b_]

    for t in range(16):
        b = t % 2
        P.dma("sp", mgT[b][:], mgT_s.ap()[t], reads=["mgT_s:%d" % t], writes=["o2mgT%d" % b])
        P.dma("sp", xt[b][:], xw.ap()[NTOK + t * 128:NTOK + (t + 1) * 128, :], writes=["o2x%d" % b])
        for cb in range(4):
            cs_ = slice(cb * 512, (cb + 1) * 512)
            px, kx = psX[cb % 2], "opsX%d" % (cb % 2)
            for k in range(16):
                P.op("pe", lambda e, px=px, k=k, cs_=cs_, b=b: e.matmul(px[:], mgT[b][:, k, :], WO[:, k, cs_], start=(k == 0), stop=(k == 15)),
                     reads=["o2mgT%d" % b, "oWO_%d_%d" % (k // 8, cb)], writes=[kx])
            P.op("dve", lambda e, px=px, cs_=cs_, b=b: e.tensor_tensor(X1[:, cs_], px[:], xt[b][:, cs_], ALU.add), reads=[kx, "o2x%d" % b], writes=["oX1_%d" % cb])
        x1k = ["oX1_%d" % cb for cb in range(4)]
        P.dma("sp", x1_s.ap()[t * 128:(t + 1) * 128, :], X1[:], reads=x1k, writes=["x1_s:%d" % t])
        P.op("act", lambda e: e.activation(sq[:], X1[:], AF.Square, accum_out=R(0, 1)), reads=x1k, writes=["o2sq", "or"])
        P.op("act", lambda e: e.activation(R(1, 2), R(0, 1), AF.Sqrt, bias=EPSB[0][:, 0:1], scale=1.0 / D), reads=["or", "epsb"], writes=["or"])
        P.op("dve", lambda e: e.reciprocal(R(2, 3), R(1, 2)), reads=["or"], writes=["or"])
        P.op("dve", lambda e: e.scalar_tensor_tensor(out=H2[:], in0=X1[:], scalar=R(2, 3), in1=g2b[:], op0=ALU.mult, op1=ALU.mult),
             reads=x1k + ["or", "og2b"], writes=["oH2"])
        P.op("act", lambda e, b=b: e.copy(H2b[b][:], H2[:]), reads=["oH2"], writes=["oH2b%d" % b])
        for c4 in range(4):
            for j in range(4):
                kc = c4 * 4 + j
                P.op("pe", lambda e, j=j, kc=kc: e.transpose(psT[:, j, :], H2[:, kc * 128:(kc + 1) * 128], ident[:]), reads=["oH2", "ident"], writes=["o2psT"])
            eng = "act" if c4 % 2 else "dve"
            if eng == "act":
                P.op("act", lambda e, c4=c4: e.copy(h2T[:, c4 * 4:(c4 + 1) * 4, :], psT[:]), reads=["o2psT"], writes=["oh2T_%d" % c4])
            else:
                P.op("dve", lambda e, c4=c4: e.tensor_copy(h2T[:, c4 * 4:(c4 + 1) * 4, :], psT[:]), reads=["o2psT"], writes=["oh2T_%d" % c4])
        for k in range(16):
            P.op("pe", lambda e, k=k: e.matmul(psR[:, 0:36], h2T[:, k, :], RW[:, k, :], start=(k == 0), stop=(k == 15)),
                 reads=["oh2T_%d" % (k // 4), "oRW"], writes=["opsR"])
        P.op("dve", lambda e: e.tensor_tensor(R(8, 12), psR[:, 0:4], rbb[:, 0:4], ALU.add), reads=["opsR", "orbb"], writes=["or"])
        P.op("dve", lambda e: e.tensor_tensor(R(16, 48), psR[:, 4:36], rbb[:, 4:36], ALU.add), reads=["opsR", "orbb"], writes=["or"])
        P.op("dve", lambda e: e.tensor_reduce(R(48, 49), R(8, 12), AX.X, ALU.max), reads=["or"], writes=["or"])
        P.op("dve", lambda e: e.tensor_scalar(R(52, 56), R(8, 12), R(48, 49), None, ALU.is_equal), reads=["or"], writes=["or"])
        P.op("dve", lambda e: e.tensor_scalar(R(56, 60), R(8, 12), R(48, 49), None, ALU.subtract), reads=["or"], writes=["or"])
        P.op("act", lambda e: e.activation(R(56, 60), R(56, 60), AF.Exp, accum_out=R(60, 61)), reads=["or"], writes=["or"])
        P.op("dve", lambda e: e.reciprocal(R(61, 62), R(60, 61)), reads=["or"], writes=["or"])
        P.op("dve", lambda e: e.tensor_scalar(R(64, 72), R(16, 24), R(52, 53), None, ALU.mult), reads=["or"], writes=["or"])
        for g in range(1, 4):
            P.op("dve", lambda e, g=g: e.scalar_tensor_tensor(out=R(64, 72), in0=R(16 + 8 * g, 24 + 8 * g), scalar=R(52 + g, 53 + g), in1=R(64, 72),
                                                              op0=ALU.mult, op1=ALU.add), reads=["or"], writes=["or"])
        P.op("dve", lambda e: e.max(R(72, 80), R(64, 72)), reads=["or"], writes=["or"])
        P.op("dve", lambda e: e.tensor_scalar(R(80, 88), R(64, 72), R(72, 73), None, ALU.is_equal), reads=["or"], writes=["or"])
        P.op("dve", lambda e: e.tensor_scalar(R(88, 96), R(64, 72), R(73, 74), None, ALU.is_equal), reads=["or"], writes=["or"])
        P.op("dve", lambda e: e.tensor_tensor(R(96, 97), R(73, 74), R(72, 73), ALU.subtract), reads=["or"], writes=["or"])
        P.op("act", lambda e: e.activation(R(97, 98), R(96, 97), AF.Exp), reads=["or"], writes=["or"])
        P.op("dve", lambda e: e.tensor_scalar(R(98, 99), R(97, 98), 1.0, None, ALU.add), reads=["or"], writes=["or"])
        P.op("dve", lambda e: e.reciprocal(R(98, 99), R(98, 99)), reads=["or"], writes=["or"])
        P.op("dve", lambda e: e.tensor_tensor(R(99, 100), R(61, 62), R(98, 99), ALU.mult), reads=["or"], writes=["or"])
        P.op("dve", lambda e: e.tensor_tensor(R(100, 101), R(61, 62), R(99, 100), ALU.subtract), reads=["or"], writes=["or"])
        for g in range(4):
            P.op("dve", lambda e, g=g: e.tensor_scalar(R(104 + 8 * g, 112 + 8 * g), R(80, 88), R(52 + g, 53 + g), None, ALU.mult), reads=["or"], writes=["or"])
            P.op("dve", lambda e, g=g: e.tensor_scalar(R(136 + 8 * g, 144 + 8 * g), R(88, 96), R(52 + g, 53 + g), None, ALU.mult), reads=["or"], writes=["or"])
        P.op("dve", lambda e: e.tensor_tensor(R(200, 232), R(104, 136), R(136, 168), ALU.add), reads=["or"], writes=["or"])
        P.op("pe", lambda e: e.matmul(psP[:, 0:32], tsf[:], R(200, 232), start=True, stop=True, skip_group_check=True), reads=["otsf", "or"], writes=["opsP"])
        P.op("pe", lambda e: e.matmul(psP[:, 32:64], onesf[:], R(200, 232), start=True, stop=True, skip_group_check=True), reads=["oones", "or"], writes=["opsP"])
        P.op("dve", lambda e: e.tensor_tensor(R(168, 200), psP[:, 0:32], carry[:], ALU.add), reads=["opsP", "ocarry"], writes=["or"])
        P.op("dve", lambda e: e.tensor_tensor(carry[:], carry[:], psP[:, 32:64], ALU.add), reads=["opsP", "ocarry"], writes=["ocarry"])
        S, Gt = slot_i[b], gate2[b]
        for k, (oh0, gcol) in enumerate(((104, 99), (136, 100))):
            P.op("dve", lambda e, oh0=oh0: e.tensor_tensor(R(200, 232), R(oh0, oh0 + 32), R(168, 200), ALU.mult), reads=["or"], writes=["or"])
            P.op("dve", lambda e: e.tensor_reduce(R(232, 233), R(200, 232), AX.X, ALU.add), reads=["or"], writes=["or"])
            P.op("dve", lambda e, oh0=oh0: e.tensor_tensor(R(200, 232), R(oh0, oh0 + 32), ebase[:], ALU.mult), reads=["or", "oebase"], writes=["or"])
            P.op("dve", lambda e: e.tensor_reduce(R(233, 234), R(200, 232), AX.X, ALU.add), reads=["or"], writes=["or"])
            P.op("dve", lambda e: e.tensor_scalar(R(234, 235), R(232, 233), float(CAP), None, ALU.is_lt), reads=["or"], writes=["or"])
            P.op("dve", lambda e: e.tensor_tensor(R(235, 236), R(232, 233), R(233, 234), ALU.add), reads=["or"], writes=["or"])
            P.op("dve", lambda e: e.tensor_scalar(R(235, 236), R(235, 236), float(NSLOT), None, ALU.subtract), reads=["or"], writes=["or"])
            P.op("dve", lambda e: e.tensor_tensor(R(236, 237), R(235, 236), R(234, 235), ALU.mult), reads=["or"], writes=["or"])
            P.op("dve", lambda e: e.tensor_scalar(R(236, 237), R(236, 237), float(NSLOT), None, ALU.add), reads=["or"], writes=["or"])
            P.op("dve", lambda e, S=S, k=k: e.tensor_copy(S[:, k:k + 1], R(236, 237)), reads=["or"], writes=["oslot%d" % b])
            P.op("dve", lambda e, Gt=Gt, k=k, gcol=gcol: e.tensor_tensor(Gt[:, k:k + 1], R(gcol, gcol + 1), R(234, 235), ALU.mult), reads=["or"], writes=["ogate%d" % b])
        for k in range(2):
            P.idma_scatter(xs_d, S[:, k:k + 1], H2b[b][:], reads=["oslot%d" % b, "oH2b%d" % b], writes=["xs:%d:%d" % (t, k)])
        P.dma("sp", slot_s.ap()[t], S[:], reads=["oslot%d" % b], writes=["slot_s:%d" % t])
        P.dma("sp", gate_s.ap()[t], Gt[:], reads=["ogate%d" % b], writes=["gate_s:%d" % t])
```

```python
import numpy as np
from contextlib import ExitStack
import concourse.bass as bass
import concourse.mybir as mybir
from concourse.bass_utils import run_bass_kernel_spmd

F32 = mybir.dt.float32
F32R = mybir.dt.float32r
BF16 = mybir.dt.bfloat16
I32 = mybir.dt.int32
U32 = mybir.dt.uint32
AF = mybir.ActivationFunctionType
ALU = mybir.AluOpType
AX = mybir.AxisListType

D = 2048
NTOK = 2048
WTOK = 4096
INW = 10688
RWW = 3520
NCORES = 8
import os
RSTOP = int(os.environ.get('RSTOP', '9'))


class Prog:
    ENG = ("pe", "act", "dve", "pool", "sp")

    def __init__(self, nc, es):
        self.nc = nc
        self.es = es
        self.q = {e: [] for e in self.ENG}
        self.sem = {e: es.enter_context(nc.semaphore("c_" + e)) for e in ("pe", "act", "dve", "pool")}
        self.cnt = {e: 0 for e in self.ENG}
        self.NR = 24
        self.ring = {e: [es.enter_context(nc.semaphore("r_%s%d" % (e, i))) for i in range(self.NR)]
                     for e in ("sp", "pool")}
        self.ring_n = {e: 0 for e in ("sp", "pool")}
        self.ring_val = {e: [0] * self.NR for e in ("sp", "pool")}
        self.seen = {e: {} for e in self.ENG}
        self.lastw = {}
        self.reads = {}
        self.cur = es
        self.stack = []

    def sb(self, name, shape, dt):
        return self.cur.enter_context(self.nc.sbuf_tensor(name, list(shape), dt))

    def ps(self, name, shape, dt=F32):
        return self.cur.enter_context(self.nc.psum_tensor(name, list(shape), dt))

    def push(self):
        self.stack.append(self.cur)
        self.cur = ExitStack()

    def pop(self):
        self.barrier()
        self.cur.close()
        self.cur = self.stack.pop()

    def barrier(self):
        for eng in self.ENG:
            for e2 in ("pe", "act", "dve", "pool"):
                if self.cnt[e2]:
                    self._wait(eng, (self.sem[e2], self.cnt[e2], "bar"))
            for qn in self.ring:
                for i in range(self.NR):
                    if self.ring_val[qn][i]:
                        self._wait(eng, (self.ring[qn][i], self.ring_val[qn][i], "dma"))

    def dram(self, name, shape, dt, kind="Internal"):
        return self.nc.dram_tensor(name, list(shape), dt, kind=kind)

    def _wait(self, eng, ev):
        sem, val, src = ev
        if eng == "pe" and src == "pe":
            return
        key = id(sem)
        if self.seen[eng].get(key, 0) >= val:
            return
        self.seen[eng][key] = val
        self.q[eng].append(lambda e, sem=sem, val=val: e.wait_ge(sem, val))

    def _deps(self, eng, reads, writes):
        for k in reads:
            ev = self.lastw.get(k)
            if ev is not None:
                self._wait(eng, ev)
        for k in writes:
            ev = self.lastw.get(k)
            if ev is not None:
                self._wait(eng, ev)
            for ev in self.reads.get(k, ()):
                self._wait(eng, ev)

    def _commit(self, ev, reads, writes):
        for k in writes:
            self.lastw[k] = ev
            self.reads[k] = []
        for k in reads:
            self.reads.setdefault(k, []).append(ev)
            if len(self.reads[k]) > 64:
                self.reads[k] = self.reads[k][-64:]

    def op(self, eng, fn, reads=(), writes=(), signal=True):
        self._deps(eng, reads, writes)
        sem = self.sem[eng]
        if eng == "pe" and not signal:
            self.q[eng].append(lambda e, fn=fn: fn(e))
            self._commit((sem, self.cnt[eng] + 1, eng), reads, writes)
            return
        self.cnt[eng] += 1
        n = self.cnt[eng]
        self.q[eng].append(lambda e, fn=fn, sem=sem: fn(e).then_inc(sem, 1))
        self._commit((sem, n, eng), reads, writes)

    def dma(self, eng, out, in_, reads=(), writes=(), **kw):
        self._deps(eng, reads, writes)
        i = self.ring_n[eng] % self.NR
        self.ring_n[eng] += 1
        sem = self.ring[eng][i]
        prev = self.ring_val[eng][i]
        if prev:
            self._wait(eng, (sem, prev, "dma"))
        val = prev + 16
        self.ring_val[eng][i] = val
        self.q[eng].append(lambda e, out=out, in_=in_, sem=sem, kw=kw: e.dma_start(out=out, in_=in_, **kw).then_inc(sem, 16))
        ev = (sem, val, "dma")
        self._commit(ev, reads, writes)
        return ev

    def _idma(self, mk, reads, writes):
        eng = "pool"
        self._deps(eng, reads, writes)
        i = self.ring_n[eng] % self.NR
        self.ring_n[eng] += 1
        sem = self.ring[eng][i]
        prev = self.ring_val[eng][i]
        if prev:
            self._wait(eng, (sem, prev, "dma"))
        val = prev + 16
        self.ring_val[eng][i] = val
        self.q[eng].append(lambda e, mk=mk, sem=sem: mk(e).then_inc(sem, 16))
        self._commit((sem, val, "dma"), reads, writes)

    def idma_scatter(self, dram, idx, src, reads=(), writes=()):
        nrow = dram.shape[0]
        self._idma(lambda e: e.indirect_dma_start(out=dram[:, :], out_offset=bass.IndirectOffsetOnAxis(ap=idx, axis=0), in_=src,
                                                  in_offset=None), reads, writes)

    def idma_gather(self, dst, dram, idx, reads=(), writes=()):
        nrow = dram.shape[0]
        self._idma(lambda e: e.indirect_dma_start(out=dst, out_offset=None, in_=dram[:, :],
                                                  in_offset=bass.IndirectOffsetOnAxis(ap=idx, axis=0)), reads, writes)

    def wait_all(self, eng, keys):
        for k in keys:
            ev = self.lastw.get(k)
            if ev is not None:
                self._wait(eng, ev)

    def emit(self):
        nc = self.nc
        with nc.Block() as blk:
            @blk.tensor
            def _(e):
                for f in self.q["pe"]:
                    f(e)

            @blk.scalar
            def _(e):
                for f in self.q["act"]:
                    f(e)

            @blk.vector
            def _(e):
                for f in self.q["dve"]:
                    f(e)

            @blk.gpsimd
            def _(e):
                for f in self.q["pool"]:
                    f(e)

            @blk.sync
            def _(e):
                for f in self.q["sp"]:
                    f(e)


def AP(t, off, dims):
    return bass.AP(t, off, [list(d) for d in dims])


def host_consts():
    c = {}
    c["ident_h"] = np.eye(128, dtype=np.float32)
    c["tri_h"] = np.triu(np.ones((128, 128), np.float32))
    c.update(rwkv_consts())
    return c


def build(stages=("A",), debug=False):
    nc = bass.Bass("TRN2", target_bir_lowering=False)
    es = ExitStack()
    P = Prog(nc, es)
    dr = {}

    def din(name, shape, dt=F32):
        dr[name] = nc.dram_tensor(name, list(shape), dt, kind="ExternalInput")
        return dr[name]

    xw = din("xw", [WTOK, D])
    cs = din("cs", [WTOK, 16])
    norm1_g = din("norm1_g", [1, D])
    w_in = [din("win_%d" % i, [D, cw]) for i, (c0, cw, kind, bi) in enumerate(col_blocks())]
    q_norm_g = din("q_norm_g", [1, 64])
    k_norm_g = din("k_norm_g", [1, 64])
    ident_d = din("ident_h", [128, 128])
    qT_s = P.dram("qT_s", [8, 128, NTOK], BF16)
    kT_s = P.dram("kT_s", [8, 128, WTOK], BF16)
    v_s = P.dram("v_s", [WTOK, 1024], BF16)
    prw = P.dram("prw", [WTOK + 1, RWW], F32)
    sg_s = P.dram("sg_s", [NTOK, 4096], BF16)
    outs = {}
    ya_s = P.dram("ya_s", [NTOK, 1024], F32)
    ybT_s = P.dram("ybT_s", [1024, NTOK], BF16)
    if debug:
        dbg = debug if isinstance(debug, (list, tuple, set)) else ("qT", "kT", "v", "prw", "sg")
        shp = {"qT": ([8, 128, NTOK], BF16), "kT": ([8, 128, WTOK], BF16), "v": ([WTOK, 1024], BF16),
               "prw": ([WTOK + 1, RWW], F32), "sg": ([NTOK, 4096], BF16), "ya": ([NTOK, 1024], F32), "ybT": ([1024, NTOK], BF16),
               "x1": ([NTOK, D], F32), "slot": ([16, 128, 2], I32), "gate": ([16, 128, 2], F32)}
        for k in dbg:
            outs["dbg_" + k] = nc.dram_tensor("dbg_" + k, shp[k][0], shp[k][1], kind="ExternalOutput")
        qT_s = outs.get("dbg_qT", qT_s); kT_s = outs.get("dbg_kT", kT_s); v_s = outs.get("dbg_v", v_s)
        prw = outs.get("dbg_prw", prw); sg_s = outs.get("dbg_sg", sg_s); ya_s = outs.get("dbg_ya", ya_s)
        ybT_s = outs.get("dbg_ybT", ybT_s)
    if not outs and "M" not in stages:
        outs["dummy_out"] = nc.dram_tensor("dummy_out", [1, 64], F32, kind="ExternalOutput")
    validT = din("validT", [128, 32])
    tri_d = din("tri_h", [128, 128])
    lamv = [din(n, [1, 64]) for n in ("lam_q1", "lam_k1", "lam_q2", "lam_k2")]
    subln_g = din("subln_g", [1, 128])

    ident = P.sb("ident", [128, 128], F32)
    identb = P.sb("identb", [128, 128], BF16)
    g1b = P.sb("g1b", [128, D], F32)
    qgb = P.sb("qgb", [128, 512], F32)
    kgb = P.sb("kgb", [128, 512], F32)
    zrow = P.sb("zrow", [1, RWW], F32)
    epsb = P.sb("epsb", [128, 4], F32)
    EPSB[0] = epsb
    P.op("dve", lambda e: e.memset(epsb[:, 0:1], 1e-6), writes=["epsb"])
    P.op("dve", lambda e: e.memset(epsb[:, 1:2], 1e-5), writes=["epsb"])
    P.op("dve", lambda e: e.memset(epsb[:, 2:3], 64e-5), writes=["epsb"])
    P.op("dve", lambda e: e.memset(epsb[:, 3:4], 1.0), writes=["epsb"])
    P.dma("sp", ident[:], ident_d.ap(), writes=["ident"])
    P.op("dve", lambda e: e.tensor_copy(identb[:], ident[:]), reads=["ident"], writes=["identb"])
    P.dma("sp", g1b[:], AP(norm1_g, 0, [[0, 128], [1, D]]), writes=["g1b"])
    for j in range(8):
        P.dma("sp", qgb[:, j * 64:(j + 1) * 64], AP(q_norm_g, 0, [[0, 128], [1, 64]]), writes=["qgb"])
        P.dma("sp", kgb[:, j * 64:(j + 1) * 64], AP(k_norm_g, 0, [[0, 128], [1, 64]]), writes=["kgb"])
    P.op("dve", lambda e: e.tensor_scalar(qgb[:], qgb[:], 0.125, None, ALU.mult), reads=["qgb"], writes=["qgb"])
    P.op("dve", lambda e: e.memset(zrow[:], 0.0), writes=["zrow"])
    P.dma("sp", prw.ap()[0:1, :], zrow[:], reads=["zrow"], writes=["prw"])

    if "A" in stages:
        P.push()
        stage_A(P, nc, xw, cs, w_in, ident, identb, g1b, qgb, kgb, qT_s, kT_s, v_s, prw, sg_s)
        P.pop()
    if "B" in stages:
        P.push()
        stage_B(P, nc, qT_s, kT_s, v_s, validT, tri_d, lamv, subln_g, ya_s)
        P.pop()

    if "R" in stages:
        P.push()
        stage_R(P, nc, din, prw, ident, identb, ybT_s)
        P.pop()

    if "O" in stages:
        mgT_s = P.dram("mgT_s", [16, 128, 16, 128], BF16)
        x1_s = outs.get("dbg_x1") or P.dram("x1_s", [NTOK, D], F32)
        xs_d = P.dram("xs_d", [NSLOT + 1, D], BF16)
        slot_s = outs.get("dbg_slot") or P.dram("slot_s", [16, 128, 2], I32)
        gate_s = outs.get("dbg_gate") or P.dram("gate_s", [16, 128, 2], F32)
        P.push()
        stage_O1(P, nc, din, identb, ya_s, ybT_s, sg_s, mgT_s)
        P.pop()
        P.push()
        stage_O2(P, nc, din, ident, xw, mgT_s, x1_s, xs_d, slot_s, gate_s)
        P.pop()
    if "M" in stages:
        y_d = P.dram("y_d", [NSLOT + 1, D], F32)
        out_d = nc.dram_tensor("out", [NTOK, D], F32, kind="ExternalOutput")
        outs["out"] = out_d
        P.push()
        stage_M(P, nc, din, identb, xs_d, y_d, x1_s, slot_s, gate_s, out_d)
        P.pop()

    P.wait_all("sp", list(P.lastw.keys()))
    P.emit()
    es.close()
    LASTP[0] = P
    return nc, list(outs.keys())


def col_blocks():
    blks = []
    for i in range(2):
        blks.append((i * 512, 512, "q", i))
    for i in range(2):
        blks.append((1024 + i * 512, 512, "k", i))
    for i in range(2):
        blks.append((2048 + i * 512, 512, "v", i))
    for i in range(7):
        w = 512 if i < 6 else RWW - 6 * 512
        blks.append((3072 + i * 512, w, "rw", i))
    for i in range(8):
        blks.append((6592 + i * 512, 512, "g", i))
    return blks


def stage_A(P, nc, xw, cs, w_in, ident, identb, g1b, qgb, kgb, qT_s, kT_s, v_s, prw, sg_s):
    hT = P.sb("hT", [128, 16, 16, 128], BF16)
    xt = [P.sb("xt%d" % i, [128, D], F32) for i in range(2)]
    ht = [P.sb("ht%d" % i, [128, D], F32) for i in range(2)]
    sq = P.sb("sqjunk", [128, D], BF16)
    st = [P.sb("st%d" % i, [128, 4], F32) for i in range(2)]
    wst = P.sb("wst", [128, 16, 512], F32)
    wbf = [P.sb("wbf%d" % i, [128, 16, 512], BF16) for i in range(2)]
    pT = [P.ps("pT%d" % i, [128, 4, 128], F32) for i in range(2)]
    pacc = [P.ps("pacc%d" % i, [128, 512], F32) for i in range(2)]
    pTb = [P.ps("pTb%d" % i, [128, 4, 128], BF16) for i in range(2)]
    ev = [P.sb("ev%d" % i, [128, 512], F32) for i in range(2)]
    evb = [P.sb("evb%d" % i, [128, 512], BF16) for i in range(2)]
    qn = [P.sb("qn%d" % i, [128, 512], F32) for i in range(2)]
    qr = [P.sb("qr%d" % i, [128, 512], BF16) for i in range(2)]
    qs = [P.sb("qs%d" % i, [128, 16], F32) for i in range(2)]
    cst = [P.sb("cst%d" % i, [128, 16], F32) for i in range(2)]
    tmp8 = [P.sb("tmp8_%d" % i, [128, 8, 16], F32) for i in range(2)]
    qTb = [P.sb("qTb%d" % i, [128, 4, 128], BF16) for i in range(2)]
    blks = col_blocks()
    wload_i = [0]
    for grp in range(2):
        for t in range(16):
            gt = grp * 16 + t
            b = t % 2
            X, H, S = xt[b], ht[b], st[b]
            P.dma("sp", X[:], xw.ap()[gt * 128:(gt + 1) * 128, :], writes=["xt%d" % b])
            P.op("act", lambda e, X=X, S=S: e.activation(sq[:], X[:], AF.Square, accum_out=S[:, 0:1]),
                 reads=["xt%d" % b], writes=["sq", "st%d" % b])
            P.op("act", lambda e, S=S: e.activation(S[:, 1:2], S[:, 0:1], AF.Sqrt, bias=EPSB[0][:, 0:1], scale=1.0 / D),
                 reads=["st%d" % b, "epsb"], writes=["st%d" % b])
            P.op("dve", lambda e, S=S: e.reciprocal(S[:, 2:3], S[:, 1:2]), reads=["st%d" % b], writes=["st%d" % b])
            P.op("dve", lambda e, X=X, H=H, S=S: e.scalar_tensor_tensor(out=H[:], in0=X[:], scalar=S[:, 2:3], in1=g1b[:],
                                                                      op0=ALU.mult, op1=ALU.mult),
                 reads=["xt%d" % b, "st%d" % b, "g1b"], writes=["ht%d" % b])
            for c4 in range(4):
                pb = c4 % 2
                for j in range(4):
                    kc = c4 * 4 + j
                    P.op("pe", lambda e, H=H, kc=kc, pb=pb, j=j: e.transpose(pT[pb][:, j, :], H[:, kc * 128:(kc + 1) * 128], ident[:]),
                         reads=["ht%d" % b, "ident"], writes=["pT%d" % pb], signal=(j == 3))
                eng = "act" if c4 % 2 == 0 else "dve"
                if eng == "act":
                    P.op("act", lambda e, t=t, c4=c4, pb=pb: e.copy(hT[:, t, c4 * 4:(c4 + 1) * 4, :], pT[pb][:]),
                         reads=["pT%d" % pb], writes=["hT%d_%d" % (t, c4)])
                else:
                    P.op("dve", lambda e, t=t, c4=c4, pb=pb: e.tensor_copy(hT[:, t, c4 * 4:(c4 + 1) * 4, :], pT[pb][:]),
                         reads=["pT%d" % pb], writes=["hT%d_%d" % (t, c4)])
        for wblk, (c0, cw, kind, bi) in enumerate(blks):
            if grp == 0 and kind in ("q", "g"):
                continue
            wi = wload_i[0]
            wload_i[0] += 1
            wb = wbf[wi % 2]
            wk = "wbf%d" % (wi % 2)
            P.dma("sp", wst[:, :, 0:cw], w_in[wblk].ap().rearrange("(k p) c -> p k c", p=128),
                  writes=["wst"])
            for hh in range(2):
                eng = "pool" if hh == 0 else "dve"
                if eng == "pool":
                    P.op("pool", lambda e, wb=wb, hh=hh, cw=cw: e.tensor_copy(wb[:, hh * 8:(hh + 1) * 8, 0:cw], wst[:, hh * 8:(hh + 1) * 8, 0:cw]),
                         reads=["wst"], writes=[wk + "_%d" % hh])
                else:
                    P.op("dve", lambda e, wb=wb, hh=hh, cw=cw: e.tensor_copy(wb[:, hh * 8:(hh + 1) * 8, 0:cw], wst[:, hh * 8:(hh + 1) * 8, 0:cw]),
                         reads=["wst"], writes=[wk + "_%d" % hh])
            for t in range(16):
                gt = grp * 16 + t
                tok0 = gt * 128
                pa = pacc[t % 2]
                pk = "pacc%d" % (t % 2)
                for kc in range(16):
                    P.op("pe", lambda e, pa=pa, t=t, kc=kc, wb=wb, cw=cw: e.matmul(pa[:, 0:cw], hT[:, t, kc, :], wb[:, kc, 0:cw],
                                                                              start=(kc == 0), stop=(kc == 15)),
                         reads=["hT%d_%d" % (t, kc // 4), wk + "_%d" % (kc // 8)], writes=[pk], signal=(kc == 15))
                b = t % 2
                if kind == "v":
                    P.op("act", lambda e, pa=pa, b=b: e.copy(evb[b][:], pa[:]), reads=[pk], writes=["evb%d" % b])
                    P.dma("sp", v_s.ap()[tok0:tok0 + 128, bi * 512:(bi + 1) * 512], evb[b][:], reads=["evb%d" % b], writes=["v_s:%d:%d" % (gt, bi)])
                elif kind == "rw":
                    P.op("act", lambda e, pa=pa, b=b, cw=cw: e.copy(ev[b][:, 0:cw], pa[:, 0:cw]), reads=[pk], writes=["ev%d" % b])
                    P.dma("sp", prw.ap()[1 + tok0:1 + tok0 + 128, bi * 512:bi * 512 + cw], ev[b][:, 0:cw], reads=["ev%d" % b], writes=["prw:%d:%d" % (gt, bi)])
                elif kind == "g":
                    P.op("act", lambda e, pa=pa, b=b: e.activation(evb[b][:], pa[:], AF.Sigmoid), reads=[pk], writes=["evb%d" % b])
                    P.dma("sp", sg_s.ap()[t * 128:(t + 1) * 128, bi * 512:(bi + 1) * 512], evb[b][:], reads=["evb%d" % b], writes=["sg_s:%d:%d" % (t, bi)])
                else:
                    gb_ = qgb if kind == "q" else kgb
                    QN, QS, QR, CS, T8 = qn[b], qs[b], qr[b], cst[b], tmp8[b]
                    P.dma("pool", CS[:], cs.ap()[tok0:tok0 + 128, :], writes=["cst%d" % b])
                    P.op("act", lambda e, pa=pa, QN=QN: e.activation(QN[:], pa[:], AF.Square), reads=[pk], writes=["qn%d" % b])
                    P.op("dve", lambda e, QN=QN, QS=QS: e.tensor_reduce(QS[:, 0:8], QN[:].rearrange("p (g d) -> p g d", d=64), AX.X, ALU.add),
                         reads=["qn%d" % b], writes=["qs%d" % b])
                    P.op("act", lambda e, QS=QS: e.activation(QS[:, 0:8], QS[:, 0:8], AF.Sqrt, bias=EPSB[0][:, 0:1], scale=1.0 / 64),
                         reads=["qs%d" % b, "epsb"], writes=["qs%d" % b])
                    P.op("dve", lambda e, QS=QS: e.reciprocal(QS[:, 8:16], QS[:, 0:8]), reads=["qs%d" % b], writes=["qs%d" % b])
                    P.op("dve", lambda e, pa=pa, QN=QN, QS=QS: e.tensor_tensor(
                        QN[:].rearrange("p (g d) -> p g d", d=64), pa[:].rearrange("p (g d) -> p g d", d=64),
                        AP(QS, 8, [[16, 128], [1, 8], [0, 64]]), ALU.mult),
                        reads=[pk, "qs%d" % b], writes=["qn%d" % b])
                    P.op("dve", lambda e, QN=QN, gb_=gb_: e.tensor_tensor(QN[:], QN[:], gb_[:], ALU.mult),
                         reads=["qn%d" % b, "qgb", "kgb"], writes=["qn%d" % b])
                    x1 = AP(QN, 0, [[512, 128], [64, 8], [1, 8]])
                    x2 = AP(QN, 8, [[512, 128], [64, 8], [1, 8]])
                    cosb = AP(CS, 0, [[16, 128], [0, 8], [1, 8]])
                    sinb = AP(CS, 8, [[16, 128], [0, 8], [1, 8]])
                    t_a = AP(T8, 0, [[128, 128], [16, 8], [1, 8]])
                    t_b = AP(T8, 8, [[128, 128], [16, 8], [1, 8]])
                    P.op("dve", lambda e, t_a=t_a, x2=x2, sinb=sinb: e.tensor_tensor(t_a, x2, sinb, ALU.mult), reads=["qn%d" % b, "cst%d" % b], writes=["tmp8_%d" % b])
                    P.op("dve", lambda e, t_b=t_b, x1=x1, sinb=sinb: e.tensor_tensor(t_b, x1, sinb, ALU.mult), reads=["qn%d" % b, "cst%d" % b], writes=["tmp8_%d" % b])
                    P.op("dve", lambda e, x1=x1, cosb=cosb: e.tensor_tensor(x1, x1, cosb, ALU.mult), reads=["qn%d" % b, "cst%d" % b], writes=["qn%d" % b])
                    P.op("dve", lambda e, x2=x2, cosb=cosb: e.tensor_tensor(x2, x2, cosb, ALU.mult), reads=["qn%d" % b, "cst%d" % b], writes=["qn%d" % b])
                    P.op("dve", lambda e, x1=x1, t_a=t_a: e.tensor_tensor(x1, x1, t_a, ALU.subtract), reads=["qn%d" % b, "tmp8_%d" % b], writes=["qn%d" % b])
                    P.op("dve", lambda e, x2=x2, t_b=t_b: e.tensor_tensor(x2, x2, t_b, ALU.add), reads=["qn%d" % b, "tmp8_%d" % b], writes=["qn%d" % b])
                    P.op("act", lambda e, QN=QN, QR=QR: e.copy(QR[:], QN[:]), reads=["qn%d" % b], writes=["qr%d" % b])
                    for j in range(4):
                        P.op("pe", lambda e, QR=QR, j=j, b=b: e.transpose(pTb[b][:, j, :], QR[:, j * 128:(j + 1) * 128], identb[:]),
                             reads=["qr%d" % b, "identb"], writes=["pTb%d" % b], signal=(j == 3))
                    P.op("act", lambda e, b=b: e.copy(qTb[b][:], pTb[b][:]), reads=["pTb%d" % b], writes=["qTb%d" % b])
                    if kind == "q":
                        dst = qT_s.ap()[bi * 4:(bi + 1) * 4, :, t * 128:(t + 1) * 128].rearrange("h p t -> p h t")
                        P.dma("sp", dst, qTb[b][:], reads=["qTb%d" % b], writes=["qT_s:%d:%d" % (t, bi)])
                    else:
                        dst = kT_s.ap()[bi * 4:(bi + 1) * 4, :, tok0:tok0 + 128].rearrange("h p t -> p h t")
                        P.dma("sp", dst, qTb[b][:], reads=["qTb%d" % b], writes=["kT_s:%d:%d" % (gt, bi)])


def stage_B(P, nc, qT_s, kT_s, v_s, validT, tri_d, lamv, subln_g, ya_s):
    LAM_INIT = 0.2
    trif = P.sb("trif", [128, 128], F32)
    tri = P.sb("tri", [128, 128], BF16)
    P.dma("sp", trif[:], tri_d.ap(), writes=["trif"])
    P.op("dve", lambda e: e.tensor_copy(tri[:], trif[:]), reads=["trif"], writes=["tri"])
    validc = P.sb("validc", [128, 32], F32)
    P.dma("sp", validc[:], validT.ap(), writes=["validc"])
    sgb = P.sb("sgb", [128, 128], F32)
    P.dma("sp", sgb[:], AP(subln_g, 0, [[0, 128], [1, 128]]), writes=["sgb"])
    P.op("dve", lambda e: e.tensor_scalar(sgb[:], sgb[:], 1.0 - LAM_INIT, None, ALU.mult), reads=["sgb"], writes=["sgb"])
    lv = P.sb("lv", [1, 4, 64], F32)
    for i in range(4):
        P.dma("sp", lv[:, i, :], lamv[i].ap(), writes=["lv"])
    lp = P.sb("lp", [1, 2, 64], F32)
    ls = P.sb("ls", [1, 8], F32)
    ones1 = P.sb("ones1", [1, 128], F32)
    nlamb = P.sb("nlamb", [128, 1], F32)
    plam = P.ps("plam", [128, 2], F32)
    P.op("dve", lambda e: e.memset(ones1[:], 1.0), writes=["ones1"])
    P.op("dve", lambda e: e.memset(ls[:], 0.0), writes=["ls"])
    P.op("dve", lambda e: e.tensor_tensor(lp[:, 0, :], lv[:, 0, :], lv[:, 1, :], ALU.mult), reads=["lv"], writes=["lp"])
    P.op("dve", lambda e: e.tensor_tensor(lp[:, 1, :], lv[:, 2, :], lv[:, 3, :], ALU.mult), reads=["lv"], writes=["lp"])
    P.op("dve", lambda e: e.tensor_reduce(ls[:, 0:2], lp[:], AX.X, ALU.add), reads=["lp"], writes=["ls"])
    P.op("act", lambda e: e.activation(ls[:, 2:4], ls[:, 0:2], AF.Exp), reads=["ls"], writes=["ls"])
    P.op("dve", lambda e: e.tensor_tensor(ls[:, 4:5], ls[:, 3:4], ls[:, 2:3], ALU.subtract), reads=["ls"], writes=["ls"])
    P.op("dve", lambda e: e.tensor_scalar(ls[:, 6:8], ls[:, 4:6], -LAM_INIT, None, ALU.add), reads=["ls"], writes=["ls"])
    P.op("pe", lambda e: e.matmul(plam[:, 0:2], ones1[:], ls[:, 6:8], start=True, stop=True), reads=["ones1", "ls"], writes=["plam"])
    P.op("dve", lambda e: e.tensor_copy(nlamb[:], plam[:, 0:1]), reads=["plam"], writes=["nlamb"])

    qT = [P.sb("aqT%d" % i, [128, NTOK], BF16) for i in range(2)]
    kT = [P.sb("akT%d" % i, [128, WTOK], BF16) for i in range(2)]
    Vh = [P.sb("aVh%d" % i, [128, 32, 130], BF16) for i in range(2)]
    psS = [P.ps("psS%d" % i, [128, 512], F32) for i in range(2)]
    Oacc = [P.ps("Oacc%d" % i, [128, 2, 130], F32) for i in range(2)]
    PT = [P.sb("aPT%d" % i, [128, 512], BF16) for i in range(3)]
    Oc = [P.sb("aOc%d" % i, [128, 4, 130], F32) for i in range(2)]
    rl = P.sb("arl", [128, 8], F32)
    ssq[0] = P.sb("assq", [128, 4], F32)
    otmp = P.sb("aotmp", [128, 128], F32)
    obuf = P.sb("aobuf", [128, 128], F32)
    osq = P.sb("aosq", [128, 128], BF16)
    yat = [P.sb("ayat%d" % i, [128, 128], F32) for i in range(2)]
    nexp = 0
    nya = 0
    for h in range(8):
        hb = h % 2
        Q, Kt, V = qT[hb], kT[hb], Vh[hb]
        P.dma("sp", Q[:], qT_s.ap()[h], reads=["qT_s:%d:%d" % (t, h // 4) for t in range(16)], writes=["aqT%d" % hb])
        P.dma("sp", Kt[:], kT_s.ap()[h], reads=["kT_s:%d:%d" % (t, h // 4) for t in range(32)], writes=["akT%d" % hb])
        P.dma("sp", V[:, :, 0:128], v_s.ap()[:, h * 128:(h + 1) * 128].rearrange("(kt p) d -> p kt d", p=128),
              reads=["v_s:%d:%d" % (t, h // 4) for t in range(32)], writes=["aVh%d" % hb])
        P.op("dve", lambda e, V=V: e.tensor_copy(V[:, :, 128:129], validc[:].rearrange("p (k o) -> p k o", o=1)),
             reads=["validc"], writes=["aVh%d" % hb])
        for G in range(4):
            for c in range(2):
                nkt = 16 + 4 * G + 4
                for kt in range(nkt):
                    sb_ = kt % 2
                    P.op("pe", lambda e, sb_=sb_, c=c, kt=kt, G=G, Q=Q, Kt=Kt: e.matmul(
                        psS[sb_][:], Kt[c * 64:(c + 1) * 64, kt * 128:(kt + 1) * 128], Q[c * 64:(c + 1) * 64, G * 512:(G + 1) * 512],
                        start=True, stop=True), reads=["aqT%d" % hb, "akT%d" % hb], writes=["psS%d" % sb_])
                    pb = nexp % 3
                    nexp += 1
                    pt = PT[pb]
                    P.op("act", lambda e, pt=pt, sb_=sb_: e.activation(pt[:], psS[sb_][:], AF.Exp), reads=["psS%d" % sb_], writes=["aPT%d" % pb])
                    rel = kt - (16 + 4 * G)
                    for j in range(4):
                        if rel > j:
                            continue
                        if rel == j:
                            P.op("dve", lambda e, pt=pt, j=j: e.tensor_tensor(pt[:, j * 128:(j + 1) * 128], pt[:, j * 128:(j + 1) * 128], tri[:], ALU.mult),
                                 reads=["aPT%d" % pb, "tri"], writes=["aPT%d" % pb])
                        last = (kt == 16 + 4 * G + j)
                        P.op("pe", lambda e, pt=pt, j=j, kt=kt, V=V, last=last: e.matmul(
                            Oacc[j // 2][:, j % 2, 0:129], pt[:, j * 128:(j + 1) * 128], V[:, kt, 0:129],
                            start=(kt == 0 and j % 2 == 0), stop=last, skip_group_check=True),
                            reads=["aPT%d" % pb, "aVh%d" % hb], writes=["Oacc%d" % (j // 2)], signal=last)
                for a in range(2):
                    P.op("act", lambda e, a=a, c=c: e.copy(Oc[c][:, 2 * a:2 * a + 2, :], Oacc[a][:]), reads=["Oacc%d" % a], writes=["aOc%d" % c])
            P.op("dve", lambda e: e.reciprocal(rl[:, 0:4], Oc[0][:, :, 128]), reads=["aOc0"], writes=["arl"])
            P.op("dve", lambda e: e.reciprocal(rl[:, 4:8], Oc[1][:, :, 128]), reads=["aOc1"], writes=["arl"])
            P.op("dve", lambda e: e.tensor_scalar(rl[:, 4:8], rl[:, 4:8], nlamb[:, 0:1], None, ALU.mult), reads=["arl", "nlamb"], writes=["arl"])
            for j in range(4):
                yb_ = nya % 2
                nya += 1
                Y = yat[yb_]
                P.op("dve", lambda e, j=j: e.tensor_scalar(otmp[:], Oc[0][:, j, 0:128], rl[:, j:j + 1], None, ALU.mult), reads=["aOc0", "arl"], writes=["aotmp"])
                P.op("dve", lambda e, j=j: e.scalar_tensor_tensor(out=obuf[:], in0=Oc[1][:, j, 0:128], scalar=rl[:, 4 + j:5 + j], in1=otmp[:],
                                                                  op0=ALU.mult, op1=ALU.add), reads=["aOc1", "arl", "aotmp"], writes=["aobuf"])
                P.op("act", lambda e: e.activation(osq[:], obuf[:], AF.Square, accum_out=ssq[0][:, 0:1]),
                     reads=["aobuf"], writes=["aosq", "assq"])
                P.op("act", lambda e: e.activation(ssq[0][:, 1:2], ssq[0][:, 0:1], AF.Sqrt, bias=EPSB[0][:, 1:2], scale=1.0 / 128), reads=["assq", "epsb"], writes=["assq"])
                P.op("dve", lambda e: e.reciprocal(ssq[0][:, 2:3], ssq[0][:, 1:2]), reads=["assq"], writes=["assq"])
                P.op("dve", lambda e, Y=Y: e.scalar_tensor_tensor(out=Y[:], in0=obuf[:], scalar=ssq[0][:, 2:3], in1=sgb[:], op0=ALU.mult, op1=ALU.mult),
                     reads=["aobuf", "assq", "sgb"], writes=["ayat%d" % yb_])
                qt = 4 * G + j
                P.dma("sp", ya_s.ap()[qt * 128:(qt + 1) * 128, h * 128:(h + 1) * 128], Y[:], reads=["ayat%d" % yb_], writes=["ya_s:%d:%d" % (qt, h)])


ssq = [None]


EPSB = [None]
LASTP = [None]


def rope_table():
    inv_freq = (500000.0 ** (-np.arange(0, 16, 2, dtype=np.float32) / 16)).astype(np.float32)
    ang = np.arange(4096, dtype=np.float32)[:, None] * inv_freq[None, :]
    return np.cos(ang).astype(np.float32), np.sin(ang).astype(np.float32)


def make_in_maps(I, ncores=NCORES, with_experts=True):
    cos, sin = rope_table()
    cst = host_consts()
    maps = []
    for c in range(ncores):
        b, sh = c // 2, c % 2
        x = I["x"][b]
        xwin = np.zeros((WTOK, D), np.float32)
        cswin = np.zeros((WTOK, 16), np.float32)
        if sh == 0:
            xwin[NTOK:] = x[:NTOK]
            cswin[NTOK:, 0:8] = cos[:NTOK]
            cswin[NTOK:, 8:16] = sin[:NTOK]
            cswin[:NTOK, 0:8] = 1.0
        else:
            xwin[:] = x
            cswin[:, 0:8] = cos
            cswin[:, 8:16] = sin
        valid = np.ones((WTOK,), np.float32)
        if sh == 0:
            valid[:NTOK] = 0.0
        m = {"xw": xwin, "cs": cswin, "validT": np.ascontiguousarray(valid.reshape(32, 128).T)}
        for k in ("norm1_g", "q_norm_g", "k_norm_g", "lam_q1", "lam_k1", "lam_q2", "lam_k2", "subln_g"):
            m[k] = np.ascontiguousarray(I[k]).reshape(1, -1)
        for k in ("shift_mu", "w0", "a0", "k_k", "k_a"):
            m[k] = np.ascontiguousarray(I[k]).reshape(1, -1)
        for k in ("r_k", "lnx_g", "lnx_b"):
            m[k + "_c"] = np.ascontiguousarray(I[k].reshape(8, 128).T)
        for k in ("w_up", "a_up", "g_up"):
            m[k] = np.ascontiguousarray(I[k][0])
        m["proj_a"] = np.ascontiguousarray(I["proj_a"][0])
        m["proj_b"] = np.ascontiguousarray(I["proj_b"][0])
        for i in range(2):
            m["w_out_%d" % i] = np.ascontiguousarray(I["w_out"][0][i * 1024:(i + 1) * 1024])
        m["norm2_g"] = np.ascontiguousarray(I["norm2_g"]).reshape(1, -1)
        m["router_w"] = np.ascontiguousarray(np.concatenate([I["router_g"][0], I["router_e"][0]], axis=1))
        m["router_b"] = np.ascontiguousarray(np.concatenate([I["router_g_b"][0], I["router_e_b"][0]]).reshape(1, 36))
        m["ebase_h"] = (np.arange(32, dtype=np.float32) * CAP).reshape(1, 32)
        m["ts_h2"] = cst["ts_h"]
        if with_experts:
            for e in range(32):
                m["wg_%d" % e] = np.ascontiguousarray(I["w_gate_e"][0][e])
                m["wu_%d" % e] = np.ascontiguousarray(I["w_up_e"][0][e])
                m["wd_%d" % e] = np.ascontiguousarray(I["w_down_e"][0][e])
        for i, (c0, cw, kind, bi) in enumerate(col_blocks()):
            m["win_%d" % i] = np.ascontiguousarray(I["w_in"][0][:, c0:c0 + cw])
        m.update(cst)
        maps.append(m)
    return maps


def kernel(**inputs):
    I = {k: np.asarray(v) for k, v in inputs.items()}
    nc, onames = build(stages=("A", "B", "R", "O", "M"), debug=False)
    in_maps = make_in_maps(I, NCORES)
    res = run_bass_kernel_spmd(nc, in_maps, core_ids=list(range(NCORES)))
    out = np.empty((4, 4096, D), np.float32)
    for c in range(NCORES):
        b, sh = c // 2, c % 2
        out[b, sh * NTOK:(sh + 1) * NTOK] = np.asarray(res.results[c]["out"])
    return out


def rwkv_consts():
    s = np.arange(128)[:, None]
    t = np.arange(128)[None, :]
    c = {}
    c["cmat_h"] = ((s <= t).astype(np.float32) - (s <= 63).astype(np.float32))
    m2 = np.zeros((128, 2), np.float32)
    m2[:64, 0] = 1.0
    m2[64:, 1] = 1.0
    c["msk2_h"] = m2
    c["ts_h"] = (s < t).astype(np.float32)
    c["ti_h"] = (s <= t).astype(np.float32)
    c["tsl_h"] = (s > t).astype(np.float32)
    bd = np.zeros((128, 128), np.float32)
    bd[:64, :64] = 1.0
    bd[64:, 64:] = 1.0
    c["bd1_h"] = bd
    return c


def stage_R(P, nc, din, prw, ident, identb, ybT_s):
    shift_mu = din("shift_mu", [1, RWW])
    pv = {n: din(n, [1, 1024]) for n in ("w0", "a0", "k_k", "k_a")}
    colp = {n: din(n + "_c", [128, 8]) for n in ("r_k", "lnx_g", "lnx_b")}
    w_up = din("w_up", [96, 1024])
    a_up = din("a_up", [96, 1024])
    g_up = din("g_up", [256, 1024])
    cd = {n: din(n, [128, 2] if n == "msk2_h" else [128, 128]) for n in ("cmat_h", "msk2_h", "ts_h", "ti_h", "tsl_h", "bd1_h")}

    def ld(name, shape, src, dt=F32):
        t = P.sb(name, shape, dt)
        P.dma("sp", t[:], src, writes=[name])
        return t

    mu_b = ld("mu_b", [128, RWW], AP(shift_mu, 0, [[0, 128], [1, RWW]]))
    w0_b = ld("w0_b", [128, 1024], AP(pv["w0"], 0, [[0, 128], [1, 1024]]))
    a0_b = ld("a0_b", [128, 1024], AP(pv["a0"], 0, [[0, 128], [1, 1024]]))
    kk_b = ld("kk_b", [128, 1024], AP(pv["k_k"], 0, [[0, 128], [1, 1024]]))
    ka_b = ld("ka_b", [128, 1024], AP(pv["k_a"], 0, [[0, 128], [1, 1024]]))
    rk_c = ld("rk_c", [128, 8], colp["r_k"].ap())
    lg_c = ld("lg_c", [128, 8], colp["lnx_g"].ap())
    lb_c = ld("lb_c", [128, 8], colp["lnx_b"].ap())
    wup = P.sb("wup", [128, 1024], F32)
    aup = P.sb("aup", [128, 1024], F32)
    for t_, src_, nm_ in ((wup, w_up, "wup"), (aup, a_up, "aup")):
        P.op("dve", lambda e, t_=t_: e.memset(t_[:], 0.0), writes=[nm_])
        P.dma("sp", t_[0:96, :], src_.ap(), writes=[nm_])
    gup = ld("gup", [128, 2, 1024], g_up.ap().rearrange("(k p) c -> p k c", p=128))
    cmat = ld("cmat", [128, 128], cd["cmat_h"].ap())
    msk2 = ld("msk2", [128, 2], cd["msk2_h"].ap())
    bdf = ld("bdf", [128, 128], cd["bd1_h"].ap())
    tsf = ld("tsf", [128, 128], cd["ts_h"].ap())
    tif = ld("tif", [128, 128], cd["ti_h"].ap())
    tslf = ld("tslf", [128, 128], cd["tsl_h"].ap())
    TS = P.sb("TSb", [128, 128], BF16)
    TI = P.sb("TIb", [128, 128], BF16)
    TSL = P.sb("TSLb", [128, 128], BF16)
    bd1 = P.sb("bd1b", [128, 128], BF16)
    bd64 = P.sb("bd64", [128, 128], F32)
    P.op("dve", lambda e: e.tensor_copy(TS[:], tsf[:]), reads=["tsf"], writes=["TSb"])
    P.op("dve", lambda e: e.tensor_copy(TI[:], tif[:]), reads=["tif"], writes=["TIb"])
    P.op("dve", lambda e: e.tensor_copy(TSL[:], tslf[:]), reads=["tslf"], writes=["TSLb"])
    P.op("dve", lambda e: e.tensor_copy(bd1[:], bdf[:]), reads=["bdf"], writes=["bd1b"])
    P.op("dve", lambda e: e.tensor_scalar(bd64[:], bdf[:], 1.0 / 64, None, ALU.mult), reads=["bdf"], writes=["bd64"])

    P0 = P.sb("rP0", [128, RWW], F32)
    P1 = P.sb("rP1", [128, RWW], F32)
    LI = P.sb("rLI", [128, 512], F32)
    P.op("dve", lambda e: e.memset(LI[:], 0.0), writes=["rLI"])
    LIT = [P.sb("rLIT%d" % i, [128, 4, 128], F32) for i in range(2)]
    U = P.sb("rU", [128, 1024], F32)
    AS = P.sb("rAS", [128, 1024], F32)
    E1, E2, E3 = P1[:, 0:1024], P1[:, 1024:2048], P1[:, 2048:3072]
    KK = P.sb("rKK", [128, 1024], F32)
    T1 = P.sb("rT1", [128, 1024], F32)
    KM = P.sb("rKM", [128, 1024], F32)
    ss = P.sb("rss", [128, 48], F32)
    TM = [[P.sb("rTM%d_%d" % (k, i), [128, 1024], BF16) for i in range(1)] * 2 for k in range(5)]
    FF = [P.sb("rFF%d" % i, [128, 5, 8, 128], BF16) for i in range(1)] * 2
    SC = [P.sb("rSC%d" % i, [128, 8, 2], F32) for i in range(2)]
    psT = P.ps("rpsT", [128, 4, 128], F32)
    pb = [P.ps("rpb%d" % i, [128, 512], F32) for i in range(2)]
    psTb = P.ps("rpsTb", [128, 8, 128], BF16)
    slots = [P.ps("rsl%d" % i, [128, 4, 128], F32) for i in range(4)]
    St = P.sb("rSt", [128, 8, 64], F32)
    Stb = P.sb("rStb", [128, 8, 64], BF16)
    Y2 = P.sb("rY2", [128, 8, 128], F32)
    P.op("dve", lambda e: e.memset(St[:], 0.0), writes=["rSt%d" % h for h in range(16)])
    P.op("dve", lambda e: e.memset(Stb[:], 0.0), writes=["rStb%d" % h for h in range(16)])
    NL = 4
    lane = []
    for l in range(NL):
        d = {}
        for n in ("Nab", "NabT", "Ma", "MaT", "Mb", "MbT", "Pm", "Nka", "Mbr", "Mkr", "AX", "WU", "E", "Pp"):
            d[n] = P.sb("rl%d_%s" % (l, n), [128, 128], BF16)
        for n in ("Yl", "Qp", "AXf"):
            d[n] = P.sb("rl%d_%s" % (l, n), [128, 128], F32)
        d["n"] = 0
        lane.append(d)
    fin = {n: P.sb("rf_" + n, [128, 128], F32) for n in ("YC", "SQ", "SD", "YN", "BN")}
    finb = {n: P.sb("rf_" + n, [128, 128], BF16) for n in ("RK",)}
    YB = [P.sb("rf_YB%d" % i, [128, 128], BF16) for i in range(2)]
    evq = [0]

    def slot(l):
        d = lane[l]
        i = d["n"] % 4
        d["n"] += 1
        return slots[l][:, i, :], "rsl%d" % l

    def ev_eng():
        evq[0] += 1
        return "dve" if evq[0] % 3 else "act"

    def mm(out, okey, lhsT, rhs, reads, start=True, stop=True):
        P.op("pe", lambda e: e.matmul(out, lhsT, rhs, start=start, stop=stop, skip_group_check=True), reads=reads, writes=[okey])

    _lo, _hi = (int(v) for v in os.environ.get('RCHUNKS', '0,32').split(','))
    for c in range(_lo, _hi):
        own = c >= 16
        t0 = c * 128
        cb = c % 2
        F, S_, Lt = FF[cb], SC[cb], LIT[cb]
        tmA, tmR, tmB, tmK, tmV = (TM[k][cb] for k in range(5))
        kA, kR, kB, kK, kV = ("rTM%d_0" % k for k in range(5))
        kF, kS, kL = "rFF0", "rSC%d" % cb, "rLIT%d" % cb
        rd = ["prw:%d:%d" % (c, b) for b in range(7)] + (["prw:%d:%d" % (c - 1, b) for b in range(7)] if c else ["prw"])
        P.dma("sp", P1[:], prw.ap()[1 + t0:1 + t0 + 128, :], reads=rd, writes=["rP1", "rE1", "rE2", "rE3"])
        P.dma("sp", P0[:], prw.ap()[t0:t0 + 128, :], reads=rd, writes=["rP0"])
        P.op("dve", lambda e: e.tensor_tensor(P0[:], P0[:], P1[:], ALU.subtract), reads=["rP0", "rP1"], writes=["rP0"])
        P.op("pool", lambda e: e.tensor_tensor(P0[:], P0[:], mu_b[:], ALU.mult), reads=["rP0", "mu_b"], writes=["rP0"])
        P.op("dve", lambda e: e.tensor_tensor(P0[:], P0[:], P1[:], ALU.add), reads=["rP0", "rP1"], writes=["rP0", "rE1", "rE2", "rE3"])
        Z = P0
        zr, zk, zv = Z[:, 0:1024], Z[:, 1024:2048], Z[:, 2048:3072]
        P.op("act", lambda e: e.activation(LI[:, 0:96], Z[:, 3072:3168], AF.Tanh), reads=["rP0"], writes=["rLI"])
        P.op("act", lambda e: e.copy(LI[:, 128:224], Z[:, 3168:3264]), reads=["rP0"], writes=["rLI"])
        P.op("act", lambda e: e.activation(LI[:, 256:512], Z[:, 3264:3520], AF.Sigmoid), reads=["rP0"], writes=["rLI"])
        for j_ in range(4):
            P.op("pe", lambda e, j_=j_: e.transpose(psT[:, j_, :], LI[:, j_ * 128:(j_ + 1) * 128], ident[:]), reads=["rLI", "ident"], writes=["rpsT"])
        P.op("act", lambda e, Lt=Lt: e.copy(Lt[:], psT[:]), reads=["rpsT"], writes=[kL])
        for hf in range(2):
            mm(pb[hf][:], "rpb%d" % hf, Lt[:, 0, :], wup[:, hf * 512:(hf + 1) * 512], [kL, "wup"])
            P.op("dve", lambda e, hf=hf: e.tensor_tensor(U[:, hf * 512:(hf + 1) * 512], pb[hf][:], w0_b[:, hf * 512:(hf + 1) * 512], ALU.add),
                 reads=["rpb%d" % hf, "w0_b"], writes=["rU"])
        P.op("act", lambda e: e.activation(U[:], U[:], AF.Sigmoid), reads=["rU"], writes=["rU"])
        P.op("dve", lambda e: e.tensor_scalar(U[:], U[:], -0.6065306597126334, None, ALU.mult), reads=["rU"], writes=["rU"])
        for hf in range(2):
            mm(pb[hf][:], "rpb%d" % hf, Lt[:, 1, :], aup[:, hf * 512:(hf + 1) * 512], [kL, "aup"])
            P.op("dve", lambda e, hf=hf: e.tensor_tensor(AS[:, hf * 512:(hf + 1) * 512], pb[hf][:], a0_b[:, hf * 512:(hf + 1) * 512], ALU.add),
                 reads=["rpb%d" % hf, "a0_b"], writes=["rAS"])
        P.op("act", lambda e: e.activation(AS[:], AS[:], AF.Sigmoid), reads=["rAS"], writes=["rAS"])
        for hf in range(2):
            hs_ = slice(hf * 512, (hf + 1) * 512)
            mm(pb[hf][:], "rpb%d" % hf, cmat[:], U[:, hs_], ["cmat", "rU"])
            P.op("act", lambda e, hf=hf, hs_=hs_: e.activation(E1[:, hs_], pb[hf][:], AF.Exp), reads=["rpb%d" % hf], writes=["rE1"])
            P.op("act", lambda e, hf=hf, hs_=hs_: e.activation(E2[:, hs_], pb[hf][:], AF.Exp, scale=-1.0), reads=["rpb%d" % hf], writes=["rE2"])
            P.op("dve", lambda e, hf=hf, hs_=hs_: e.tensor_tensor(E3[:, hs_], pb[hf][:], U[:, hs_], ALU.subtract), reads=["rpb%d" % hf, "rU"], writes=["rE3"])
        P.op("act", lambda e: e.activation(E3, E3, AF.Exp), reads=["rE3"], writes=["rE3"])
        for hp in range(8):
            P.op("pe", lambda e, hp=hp: e.matmul(psT[:, 0, 2 * hp:2 * hp + 2], U[:, hp * 128:(hp + 1) * 128], msk2[:], start=True, stop=True, skip_group_check=True),
                 reads=["rU", "msk2", kL], writes=["rpsT"])
        P.op("act", lambda e, S_=S_: e.activation(S_[:].rearrange("p h t -> p (h t)"), psT[:, 0, 0:16], AF.Exp), reads=["rpsT"], writes=[kS])
        P.op("pool", lambda e: e.tensor_tensor(KK[:], zk, kk_b[:], ALU.mult), reads=["rP0", "kk_b"], writes=["rKK"])
        P.op("pool", lambda e: e.tensor_tensor(T1[:], KK[:], KK[:], ALU.mult), reads=["rKK"], writes=["rT1"])
        P.op("dve", lambda e: e.tensor_reduce(ss[:, 0:16], T1[:].rearrange("p (h d) -> p h d", d=64), AX.X, ALU.add), reads=["rT1"], writes=["rss"])
        P.op("act", lambda e: e.activation(ss[:, 0:16], ss[:, 0:16], AF.Sqrt), reads=["rss"], writes=["rss"])
        P.op("dve", lambda e: e.tensor_scalar(ss[:, 0:16], ss[:, 0:16], 1e-12, None, ALU.max), reads=["rss"], writes=["rss"])
        P.op("dve", lambda e: e.reciprocal(ss[:, 16:32], ss[:, 0:16]), reads=["rss"], writes=["rss"])
        P.op("dve", lambda e: e.tensor_tensor(KK[:].rearrange("p (h d) -> p h d", d=64), KK[:].rearrange("p (h d) -> p h d", d=64),
                                              AP(ss, 16, [[48, 128], [1, 16], [0, 64]]), ALU.mult), reads=["rKK", "rss"], writes=["rKK"])
        P.op("dve", lambda e: e.scalar_tensor_tensor(out=T1[:], in0=AS[:], scalar=-1.0, in1=ka_b[:], op0=ALU.add, op1=ALU.mult),
             reads=["rAS", "ka_b"], writes=["rT1"])
        P.op("dve", lambda e: e.scalar_tensor_tensor(out=KM[:], in0=T1[:], scalar=1.0, in1=zk, op0=ALU.add, op1=ALU.mult),
             reads=["rT1", "rP0"], writes=["rKM"])
        P.op("dve", lambda e, o=tmA: e.scalar_tensor_tensor(out=o[:], in0=KK[:], scalar=-1.0, in1=E3, op0=ALU.mult, op1=ALU.mult),
             reads=["rKK", "rE3"], writes=[kA])
        P.op("pool", lambda e: e.tensor_tensor(T1[:], KK[:], AS[:], ALU.mult), reads=["rKK", "rAS"], writes=["rT1"])
        P.op("dve", lambda e, o=tmB: e.tensor_tensor(o[:], T1[:], E2, ALU.mult), reads=["rT1", "rE2"], writes=[kB])
        P.op("pool", lambda e, o=tmR: e.tensor_tensor(o[:], zr, E1, ALU.mult), reads=["rP0", "rE1"], writes=[kR])
        P.op("dve", lambda e, o=tmK: e.tensor_tensor(o[:], KM[:], E2, ALU.mult), reads=["rKM", "rE2"], writes=[kK])
        P.op("act", lambda e, o=tmV: e.copy(o[:], zv), reads=["rP0"], writes=[kV])
        for kind, (tm, kk_) in enumerate(((tmA, kA), (tmR, kR), (tmB, kB), (tmK, kK), (tmV, kV))):
            if kind == 1 and not own:
                continue
            if kind == 4 and not own:
                continue
            for hp in range(8):
                P.op("pe", lambda e, tm=tm, hp=hp: e.transpose(psTb[:, hp, :], tm[:, hp * 128:(hp + 1) * 128], identb[:]),
                     reads=[kk_, "identb"], writes=["rpsTb"], signal=(hp == 7))
            eng = "act" if kind % 2 else "dve"
            if eng == "act":
                P.op("act", lambda e, F=F, kind=kind: e.copy(F[:, kind, :, :], psTb[:]), reads=["rpsTb"], writes=[kF + "_%d" % kind])
            else:
                P.op("dve", lambda e, F=F, kind=kind: e.tensor_copy(F[:, kind, :, :], psTb[:]), reads=["rpsTb"], writes=[kF + "_%d" % kind])

        def head_gen(h, l, S_=S_, own=own, kS=kS):
            d = lane[l]
            par, hp = h % 2, h // 2
            eo = par * 64
            hs = slice(h * 64, (h + 1) * 64)
            es = slice(eo, eo + 64)
            kn = lambda n: "rl%d_%s" % (l, n)
            Af, Rf, Bf, Kf = (F[es, k, hp, :] for k in range(4))
            fk = [kF + "_%d" % k for k in range(5)]

            def evac(dst, dkey, src, skey, mask=None, mkey=None):
                eng = ev_eng() if mask is None else "dve"
                if mask is not None:
                    P.op("dve", lambda e: e.tensor_tensor(dst, src, mask, ALU.mult), reads=[skey, mkey], writes=[dkey])
                elif eng == "act":
                    P.op("act", lambda e: e.copy(dst, src), reads=[skey], writes=[dkey])
                else:
                    P.op("dve", lambda e: e.tensor_copy(dst, src), reads=[skey], writes=[dkey])

            o, ok = slot(l)
            mm(o, ok, Bf, Af, [fk[2], fk[0]])
            evac(d["Nab"][:], kn("Nab"), o, ok, TS[:], "TSb")
            o, ok = slot(l)
            mm(o, ok, Af, Bf, [fk[0], fk[2]])
            evac(d["NabT"][:], kn("NabT"), o, ok, TSL[:], "TSLb")
            P.op("pool", lambda e: e.tensor_tensor(d["Pm"][:], d["Nab"][:], identb[:], ALU.add), reads=[kn("Nab"), "identb"], writes=[kn("Pm")])
            yield
            if RSTOP < 2:
                return
            o, ok = slot(l)
            mm(o, ok, Kf, Af, [fk[3], fk[0]])
            evac(d["Nka"][:], kn("Nka"), o, ok, TS[:], "TSb")
            if own:
                o, ok = slot(l)
                mm(o, ok, Bf, Rf, [fk[2], fk[1]])
                evac(d["Mbr"][:], kn("Mbr"), o, ok, TI[:], "TIb")
                o, ok = slot(l)
                mm(o, ok, Kf, Rf, [fk[3], fk[1]])
                evac(d["Mkr"][:], kn("Mkr"), o, ok, TI[:], "TIb")
            yield
            if RSTOP < 3:
                return
            M, MT, kM, kMT = d["Nab"], d["NabT"], kn("Nab"), kn("NabT")
            for lev in range(1, 7):
                nM, nMT = (d["Ma"], d["MaT"]) if lev % 2 else (d["Mb"], d["MbT"])
                knM, knMT = (kn("Ma"), kn("MaT")) if lev % 2 else (kn("Mb"), kn("MbT"))
                if lev < 6:
                    o, ok = slot(l)
                    mm(o, ok, MT[:], M[:], [kMT, kM])
                    evac(nM[:], knM, o, ok)
                o, ok = slot(l)
                mm(o, ok, M[:], MT[:], [kM, kMT])
                evac(nMT[:], knMT, o, ok)
                yield
                o, ok = slot(l)
                mm(o, ok, nMT[:], d["Pm"][:], [knMT, kn("Pm")])
                P.op("dve", lambda e, o=o: e.tensor_tensor(d["Pm"][:], o, d["Pm"][:], ALU.add), reads=[ok, kn("Pm")], writes=[kn("Pm")])
                M, MT, kM, kMT = nM, nMT, knM, knMT
                yield
            if RSTOP < 4:
                return
            o, ok = slot(l)
            mm(o[:, 0:64], ok, d["Nka"][:], tmV[:, hs], [kn("Nka"), kV])
            evac(d["AX"][:, 64:128], kn("AX"), o[:, 0:64], ok)
            P.op("pool", lambda e: e.tensor_copy(d["AX"][:, 0:64], tmA[:, hs]), reads=[kA], writes=[kn("AX")])
            yield
            o, ok = slot(l)
            mm(o, ok, d["Pm"][:], d["AX"][:], [kn("Pm"), kn("AX")])
            evac(d["WU"][:], kn("WU"), o, ok)
            yield
            WT, UT = d["WU"][:, 0:64], d["WU"][:, 64:128]
            if RSTOP < 5:
                return
            o, ok = slot(l)
            mm(o[es, 0:64], ok, WT, tmB[:, hs], [kn("WU"), kB])
            P.op("dve", lambda e, o=o: e.tensor_tensor(d["Qp"][es, 64:128], o[es, 0:64], ident[es, es], ALU.add), reads=[ok, "ident"], writes=[kn("Qp") + "t"])
            P.op("dve", lambda e: e.tensor_scalar(d["Pp"][es, 0:64], d["Qp"][es, 64:128], S_[es, hp, 0:1], None, ALU.mult),
                 reads=[kn("Qp") + "t", kS], writes=[kn("Pp")])
            o, ok = slot(l)
            mm(o[es, 0:64], ok, tmB[:, hs], UT, [kB, kn("WU")], start=True, stop=False)
            mm(o[es, 0:64], ok, tmK[:, hs], tmV[:, hs], [kK, kV], start=False, stop=True)
            P.op("dve", lambda e, o=o: e.tensor_scalar(d["Qp"][es, 0:64], o[es, 0:64], S_[es, hp, 1:2], None, ALU.mult),
                 reads=[ok, kS], writes=[kn("Qp")])
            if own:
                o, ok = slot(l)
                mm(o[es, :], ok, WT, d["Mbr"][:], [kn("WU"), kn("Mbr")])
                P.op("dve", lambda e, o=o: e.tensor_tensor(d["Yl"][es, :], o[es, :], Rf, ALU.add), reads=[ok, fk[1]], writes=[kn("Yl") + "e"])
                P.op("dve", lambda e: e.tensor_scalar(d["E"][es, :], d["Yl"][es, :], S_[es, hp, 0:1], None, ALU.mult),
                     reads=[kn("Yl") + "e", kS], writes=[kn("E")])
                o, ok = slot(l)
                mm(o[es, :], ok, UT, d["Mbr"][:], [kn("WU"), kn("Mbr")], start=True, stop=False)
                mm(o[es, :], ok, tmV[:, hs], d["Mkr"][:], [kV, kn("Mkr")], start=False, stop=True)
                P.op("act", lambda e, o=o: e.copy(d["AXf"][es, :], o[es, :]), reads=[ok], writes=[kn("AXf")])
            yield
            if RSTOP < 6:
                return
            if own:
                o, ok = slot(l)
                mm(o[es, :], ok, Stb[es, hp, :], d["E"][es, :], ["rStb%d" % h, kn("E")])
                P.op("dve", lambda e, o=o: e.tensor_tensor(Y2[es, hp, :], o[es, :], d["AXf"][es, :], ALU.add), reads=[ok, kn("AXf")], writes=["rY2_%d" % h])
            o, ok = slot(l)
            mm(o[es, 0:64], ok, d["Pp"][es, 0:64], Stb[es, hp, :], [kn("Pp"), "rStb%d" % h])
            P.op("dve", lambda e, o=o: e.scalar_tensor_tensor(out=St[es, hp, :], in0=o[es, 0:64], scalar=S_[es, hp, 1:2], in1=d["Qp"][es, 0:64],
                                                             op0=ALU.mult, op1=ALU.add), reads=[ok, kS, kn("Qp")], writes=["rSt%d" % h])
            P.op("act", lambda e: e.copy(Stb[es, hp, :], St[es, hp, :]), reads=["rSt%d" % h], writes=["rStb%d" % h])
            yield

        for grp in range(4 if RSTOP >= 1 else 0):
            gens = [head_gen(grp * NL + l, l) for l in range(NL)]
            alive = list(gens)
            while alive:
                nxt = []
                for g in alive:
                    try:
                        next(g)
                        nxt.append(g)
                    except StopIteration:
                        pass
                alive = nxt
        if own and RSTOP >= 7:
            for hp in range(8):
                l = hp % NL
                yk = ["rY2_%d" % (2 * hp), "rY2_%d" % (2 * hp + 1)]
                o, ok = slot(l)
                mm(o, ok, bd64[:], Y2[:, hp, :], ["bd64"] + yk)
                P.op("dve", lambda e, o=o, hp=hp: e.tensor_tensor(fin["YC"][:], Y2[:, hp, :], o, ALU.subtract), reads=yk + [ok], writes=["rfYC"])
                P.op("act", lambda e: e.activation(fin["SQ"][:], fin["YC"][:], AF.Square), reads=["rfYC"], writes=["rfSQ"])
                o, ok = slot(l)
                mm(o, ok, bd64[:], fin["SQ"][:], ["bd64", "rfSQ"])
                P.op("act", lambda e, o=o: e.activation(fin["SD"][:], o, AF.Sqrt, bias=EPSB[0][:, 2:3], scale=1.0), reads=[ok, "epsb"], writes=["rfSD"])
                P.op("dve", lambda e: e.reciprocal(fin["SD"][:], fin["SD"][:]), reads=["rfSD"], writes=["rfSD"])
                P.op("dve", lambda e: e.tensor_tensor(fin["YN"][:], fin["YC"][:], fin["SD"][:], ALU.mult), reads=["rfYC", "rfSD"], writes=["rfYN"])
                P.op("dve", lambda e, hp=hp: e.tensor_scalar(fin["YN"][:], fin["YN"][:], lg_c[:, hp:hp + 1], lb_c[:, hp:hp + 1], ALU.mult, ALU.add),
                     reads=["rfYN", "lg_c", "lb_c"], writes=["rfYN"])
                P.op("dve", lambda e, hp=hp: e.scalar_tensor_tensor(out=finb["RK"][:], in0=F[:, 1, hp, :], scalar=rk_c[:, hp:hp + 1], in1=F[:, 3, hp, :],
                                                                    op0=ALU.mult, op1=ALU.mult), reads=[kF + "_1", kF + "_3", "rk_c"], writes=["rfRK"])
                o, ok = slot(l)
                mm(o, ok, bd1[:], finb["RK"][:], ["bd1b", "rfRK"])
                P.op("dve", lambda e, o=o, hp=hp: e.tensor_tensor(fin["BN"][:], o, F[:, 4, hp, :], ALU.mult), reads=[ok, kF + "_4"], writes=["rfBN"])
                P.op("dve", lambda e: e.tensor_tensor(fin["YN"][:], fin["YN"][:], fin["BN"][:], ALU.add), reads=["rfYN", "rfBN"], writes=["rfYN"])
                o, ok = slot(l)
                mm(o, ok, gup[:, 0, hp * 128:(hp + 1) * 128], Lt[:, 2, :], ["gup", kL], start=True, stop=False)
                mm(o, ok, gup[:, 1, hp * 128:(hp + 1) * 128], Lt[:, 3, :], ["gup", kL], start=False, stop=True)
                yb = YB[hp % 2]
                P.op("dve", lambda e, o=o, yb=yb: e.tensor_tensor(yb[:], fin["YN"][:], o, ALU.mult), reads=["rfYN", ok], writes=["rfYB%d" % (hp % 2)])
                q0 = (c - 16) * 128
                P.dma("sp", ybT_s.ap()[hp * 128:(hp + 1) * 128, q0:q0 + 128], yb[:], reads=["rfYB%d" % (hp % 2)], writes=["ybT:%d:%d" % (c - 16, hp)])


def stage_M(P, nc, din, identb, xs_d, y_d, x1_s, slot_s, gate_s, out_d):
    wg = [din("wg_%d" % e, [D, 1024]) for e in range(32)]
    wu = [din("wu_%d" % e, [D, 1024]) for e in range(32)]
    wd = [din("wd_%d" % e, [1024, D]) for e in range(32)]
    NS = CAP // 128
    xs = [P.sb("mxs%d" % i, [128, D], BF16) for i in range(2)]
    xT = P.sb("mxT", [128, 16, CAP], BF16)
    wst = [P.sb("mwst%d" % i, [128, 16, 256], F32) for i in range(2)]
    wgb = [P.sb("mwgb%d" % i, [128, 16, 256], BF16) for i in range(2)]
    wub = [P.sb("mwub%d" % i, [128, 16, 256], BF16) for i in range(2)]
    wdb = [P.sb("mwdb%d" % i, [128, 8, 512], BF16) for i in range(2)]
    hT = P.sb("mhT", [128, 8, CAP], BF16)
    sl = P.sb("msl", [128, CAP], F32)
    yo = [P.sb("myo%d" % i, [128, 512], F32) for i in range(2)]
    psTb = P.ps("mpsTb", [128, 8, 128], BF16)
    psG = [P.ps("mpsG%d" % i, [128, CAP], F32) for i in range(2)]
    psU = [P.ps("mpsU%d" % i, [128, CAP], F32) for i in range(2)]
    psY = [P.ps("mpsY%d" % i, [128, 512], F32) for i in range(2)]
    zr = P.sb("mzr", [1, D], F32)
    P.op("dve", lambda e: e.memset(zr[:], 0.0), writes=["mzr"])
    P.dma("sp", y_d.ap()[NSLOT:NSLOT + 1, :], zr[:], reads=["mzr"], writes=["y_dump"])
    nw = [0]
    ncast = [0]

    def cast(dst, src, rk, wk):
        ncast[0] += 1
        eng = ("dve", "pool", "act")[ncast[0] % 3]
        if eng == "act":
            P.op("act", lambda e: e.copy(dst, src), reads=[rk], writes=[wk])
        else:
            P.op(eng, lambda e: e.tensor_copy(dst, src), reads=[rk], writes=[wk])

    for ex in range(32):
        for s_ in range(NS):
            X = xs[s_ % 2]
            r0 = ex * CAP + s_ * 128
            P.dma("sp", X[:], xs_d.ap()[r0:r0 + 128, :], reads=["xs_all"], writes=["mxs%d" % (s_ % 2)])
            for half in range(2):
                for k in range(8):
                    kc = half * 8 + k
                    P.op("pe", lambda e, X=X, k=k, kc=kc: e.transpose(psTb[:, k, :], X[:, kc * 128:(kc + 1) * 128], identb[:]),
                         reads=["mxs%d" % (s_ % 2), "identb"], writes=["mpsTb"], signal=(k == 7))
                P.op("dve", lambda e, half=half, s_=s_: e.tensor_copy(xT[:, half * 8:(half + 1) * 8, s_ * 128:(s_ + 1) * 128], psTb[:]),
                     reads=["mpsTb"], writes=["mxT_%d_%d" % (s_, half)])
        xk = ["mxT_%d_%d" % (s_, half) for s_ in range(NS) for half in range(2)]
        for cb in range(4):
            bb = nw[0] % 2
            nw[0] += 1
            for (wsrc, wdst, nm) in ((wg[ex], wgb[bb], "mwgb%d" % bb), (wu[ex], wub[bb], "mwub%d" % bb)):
                st_i = ncast[0] % 2
                W = wst[st_i]
                P.dma("sp", W[:], wsrc.ap()[:, cb * 256:(cb + 1) * 256].rearrange("(k p) c -> p k c", p=128), writes=["mwst%d" % st_i])
                cast(wdst[:], W[:], "mwst%d" % st_i, nm)
            for hc in range(2):
                pg, pu = psG[hc], psU[hc]
                for k in range(16):
                    P.op("pe", lambda e, pg=pg, k=k, hc=hc, bb=bb: e.matmul(pg[:], wgb[bb][:, k, hc * 128:(hc + 1) * 128], xT[:, k, :], start=(k == 0), stop=(k == 15)),
                         reads=["mwgb%d" % bb] + xk, writes=["mpsG%d" % hc], signal=(k == 15))
                for k in range(16):
                    P.op("pe", lambda e, pu=pu, k=k, hc=hc, bb=bb: e.matmul(pu[:], wub[bb][:, k, hc * 128:(hc + 1) * 128], xT[:, k, :], start=(k == 0), stop=(k == 15)),
                         reads=["mwub%d" % bb] + xk, writes=["mpsU%d" % hc], signal=(k == 15))
                hi = cb * 2 + hc
                P.op("act", lambda e, pg=pg: e.activation(sl[:], pg[:], AF.Silu), reads=["mpsG%d" % hc], writes=["msl"])
                P.op("dve", lambda e, pu=pu, hi=hi: e.tensor_tensor(hT[:, hi, :], sl[:], pu[:], ALU.mult), reads=["msl", "mpsU%d" % hc], writes=["mhT_%d" % hi])
        hk = ["mhT_%d" % i for i in range(8)]
        for cb in range(4):
            bb = cb % 2
            st_i = ncast[0] % 2
            W = wst[st_i]
            Wv = W[:].rearrange("p (a k) c -> p a (k c)", a=8)
            P.dma("sp", Wv, wd[ex].ap()[:, cb * 512:(cb + 1) * 512].rearrange("(k p) c -> p k c", p=128), writes=["mwst%d" % st_i])
            cast(wdb[bb][:], Wv, "mwst%d" % st_i, "mwdb%d" % bb)
            for s_ in range(NS):
                py = psY[s_ % 2]
                for k in range(8):
                    P.op("pe", lambda e, py=py, k=k, s_=s_, bb=bb: e.matmul(py[:], hT[:, k, s_ * 128:(s_ + 1) * 128], wdb[bb][:, k, :], start=(k == 0), stop=(k == 7)),
                         reads=hk + ["mwdb%d" % bb], writes=["mpsY%d" % (s_ % 2)], signal=(k == 7))
                Y = yo[s_ % 2]
                P.op("act", lambda e, py=py, Y=Y: e.copy(Y[:], py[:]), reads=["mpsY%d" % (s_ % 2)], writes=["myo%d" % (s_ % 2)])
                r0 = ex * CAP + s_ * 128
                P.dma("sp", y_d.ap()[r0:r0 + 128, cb * 512:(cb + 1) * 512], Y[:], reads=["myo%d" % (s_ % 2)], writes=["y_all"])
    P.barrier()
    x1 = [P.sb("mx1_%d" % i, [128, D], F32) for i in range(2)]
    y1 = [P.sb("my1_%d" % i, [128, D], F32) for i in range(2)]
    y2 = [P.sb("my2_%d" % i, [128, D], F32) for i in range(2)]
    si = [P.sb("msi%d" % i, [128, 2], I32) for i in range(2)]
    gt = [P.sb("mgt%d" % i, [128, 2], F32) for i in range(2)]
    for t in range(16):
        b = t % 2
        P.dma("sp", si[b][:], slot_s.ap()[t], writes=["msi%d" % b])
        P.dma("sp", gt[b][:], gate_s.ap()[t], writes=["mgt%d" % b])
        P.dma("sp", x1[b][:], x1_s.ap()[t * 128:(t + 1) * 128, :], writes=["mx1_%d" % b])
        P.idma_gather(y1[b][:], y_d, si[b][:, 0:1], reads=["msi%d" % b], writes=["my1_%d" % b])
        P.idma_gather(y2[b][:], y_d, si[b][:, 1:2], reads=["msi%d" % b], writes=["my2_%d" % b])
        P.op("dve", lambda e, b=b: e.scalar_tensor_tensor(out=x1[b][:], in0=y1[b][:], scalar=gt[b][:, 0:1], in1=x1[b][:], op0=ALU.mult, op1=ALU.add),
             reads=["my1_%d" % b, "mgt%d" % b, "mx1_%d" % b], writes=["mx1_%d" % b])
        P.op("dve", lambda e, b=b: e.scalar_tensor_tensor(out=x1[b][:], in0=y2[b][:], scalar=gt[b][:, 1:2], in1=x1[b][:], op0=ALU.mult, op1=ALU.add),
             reads=["my2_%d" % b, "mgt%d" % b, "mx1_%d" % b], writes=["mx1_%d" % b])
        P.dma("sp", out_d.ap()[t * 128:(t + 1) * 128, :], x1[b][:], reads=["mx1_%d" % b], writes=["out:%d" % t])


CAP = 256
NSLOT = 32 * CAP


def stage_O1(P, nc, din, identb, ya_s, ybT_s, sg_s, mgT_s):
    proj_a = din("proj_a", [1024, D])
    proj_b = din("proj_b", [1024, D])
    PA = P.sb("oPA", [128, 8, D], BF16)
    PB = P.sb("oPB", [128, 8, D], BF16)
    wst = P.sb("owst", [128, 8, 512], F32)
    for wi, (src, dst, nm) in enumerate(((proj_a, PA, "oPA"), (proj_b, PB, "oPB"))):
        for cb in range(4):
            P.dma("sp", wst[:], src.ap()[:, cb * 512:(cb + 1) * 512].rearrange("(k p) c -> p k c", p=128), writes=["owst"])
            eng = "dve" if cb % 2 else "pool"
            P.op(eng, lambda e, dst=dst, cb=cb: e.tensor_copy(dst[:, :, cb * 512:(cb + 1) * 512], wst[:]), reads=["owst"], writes=[nm + "_%d" % cb])
    yat = [P.sb("oya%d" % i, [128, 1024], F32) for i in range(2)]
    yab = P.sb("oyab", [128, 1024], BF16)
    yaT = P.sb("oyaT", [128, 8, 128], BF16)
    ybT = [P.sb("oybT%d" % i, [128, 8, 128], BF16) for i in range(2)]
    sg = [P.sb("osg%d" % i, [128, 4096], BF16) for i in range(2)]
    m1 = P.sb("om1", [128, 512], F32)
    m2 = P.sb("om2", [128, 512], F32)
    MG = P.sb("oMG", [128, D], BF16)
    mgT = [P.sb("omgT%d" % i, [128, 16, 128], BF16) for i in range(2)]
    psTb = P.ps("opsTb", [128, 8, 128], BF16)
    psA = [P.ps("opsA%d" % i, [128, 512], F32) for i in range(2)]
    psB = [P.ps("opsB%d" % i, [128, 512], F32) for i in range(2)]
    for t in range(16):
        b = t % 2
        P.dma("sp", yat[b][:], ya_s.ap()[t * 128:(t + 1) * 128, :], reads=["ya_s:%d:%d" % (t, h) for h in range(8)], writes=["oya%d" % b])
        P.dma("sp", ybT[b][:], ybT_s.ap()[:, t * 128:(t + 1) * 128].rearrange("(k p) t -> p k t", p=128),
              reads=["ybT:%d:%d" % (t, h) for h in range(8)], writes=["oybT%d" % b])
        P.dma("sp", sg[b][:], sg_s.ap()[t * 128:(t + 1) * 128, :], reads=["sg_s:%d:%d" % (t, i) for i in range(8)], writes=["osg%d" % b])
        P.op("act", lambda e, b=b: e.copy(yab[:], yat[b][:]), reads=["oya%d" % b], writes=["oyab"])
        for k in range(8):
            P.op("pe", lambda e, k=k: e.transpose(psTb[:, k, :], yab[:, k * 128:(k + 1) * 128], identb[:]), reads=["oyab", "identb"], writes=["opsTb"], signal=(k == 7))
        P.op("dve", lambda e: e.tensor_copy(yaT[:], psTb[:]), reads=["opsTb"], writes=["oyaT"])
        for cb in range(4):
            cs_ = slice(cb * 512, (cb + 1) * 512)
            pa, pbb = psA[cb % 2], psB[cb % 2]
            ka, kb_ = "opsA%d" % (cb % 2), "opsB%d" % (cb % 2)
            for k in range(8):
                P.op("pe", lambda e, pa=pa, k=k, cs_=cs_: e.matmul(pa[:], yaT[:, k, :], PA[:, k, cs_], start=(k == 0), stop=(k == 7)),
                     reads=["oyaT", "oPA_%d" % cb], writes=[ka], signal=(k == 7))
            for k in range(8):
                P.op("pe", lambda e, pbb=pbb, k=k, cs_=cs_, b=b: e.matmul(pbb[:], ybT[b][:, k, :], PB[:, k, cs_], start=(k == 0), stop=(k == 7)),
                     reads=["oybT%d" % b, "oPB_%d" % cb], writes=[kb_], signal=(k == 7))
            P.op("dve", lambda e, pa=pa, cs_=cs_, b=b: e.tensor_tensor(m1[:], pa[:], sg[b][:, cs_], ALU.mult), reads=[ka, "osg%d" % b], writes=["om1"])
            P.op("dve", lambda e, pbb=pbb, cb=cb, b=b: e.tensor_tensor(m2[:], pbb[:], sg[b][:, 2048 + cb * 512:2048 + (cb + 1) * 512], ALU.mult),
                 reads=[kb_, "osg%d" % b], writes=["om2"])
            P.op("pool", lambda e, cs_=cs_: e.tensor_tensor(MG[:, cs_], m1[:], m2[:], ALU.add), reads=["om1", "om2"], writes=["oMG_%d" % cb])
        for half in range(2):
            for k in range(8):
                kc = half * 8 + k
                P.op("pe", lambda e, k=k, kc=kc: e.transpose(psTb[:, k, :], MG[:, kc * 128:(kc + 1) * 128], identb[:]),
                     reads=["oMG_%d" % (kc // 4), "identb"], writes=["opsTb"], signal=(k == 7))
            P.op("act", lambda e, half=half, b=b: e.copy(mgT[b][:, half * 8:(half + 1) * 8, :], psTb[:]), reads=["opsTb"], writes=["omgT%d_%d" % (b, half)])
        P.dma("sp", mgT_s.ap()[t], mgT[b][:], reads=["omgT%d_0" % b, "omgT%d_1" % b], writes=["mgT_s:%d" % t])


def stage_O2(P, nc, din, ident, xw, mgT_s, x1_s, xs_d, slot_s, gate_s):
    w_out = [din("w_out_%d" % i, [1024, D]) for i in range(2)]
    norm2_g = din("norm2_g", [1, D])
    rw_d = din("router_w", [D, 36])
    rb_d = din("router_b", [1, 36])
    ebase_d = din("ebase_h", [1, 32])
    tsf_d = din("ts_h2", [128, 128])
    WO = P.sb("oWO", [128, 16, D], BF16)
    wst = P.sb("o2wst", [128, 8, 512], F32)
    for i in range(2):
        for cb in range(4):
            P.dma("sp", wst[:], w_out[i].ap()[:, cb * 512:(cb + 1) * 512].rearrange("(k p) c -> p k c", p=128), writes=["o2wst"])
            eng = "dve" if cb % 2 else "pool"
            P.op(eng, lambda e, i=i, cb=cb: e.tensor_copy(WO[:, i * 8:(i + 1) * 8, cb * 512:(cb + 1) * 512], wst[:]), reads=["o2wst"], writes=["oWO_%d_%d" % (i, cb)])
    g2b = P.sb("og2b", [128, D], F32)
    P.dma("sp", g2b[:], AP(norm2_g, 0, [[0, 128], [1, D]]), writes=["og2b"])
    RW = P.sb("oRW", [128, 16, 36], F32)
    P.dma("sp", RW[:], rw_d.ap().rearrange("(k p) c -> p k c", p=128), writes=["oRW"])
    rbb = P.sb("orbb", [128, 36], F32)
    P.dma("sp", rbb[:], AP(rb_d, 0, [[0, 128], [1, 36]]), writes=["orbb"])
    ebase = P.sb("oebase", [128, 32], F32)
    P.dma("sp", ebase[:], AP(ebase_d, 0, [[0, 128], [1, 32]]), writes=["oebase"])
    tsf = P.sb("otsf", [128, 128], F32)
    P.dma("sp", tsf[:], tsf_d.ap(), writes=["otsf"])
    onesf = P.sb("oones", [128, 128], F32)
    P.op("dve", lambda e: e.memset(onesf[:], 1.0), writes=["oones"])
    carry = P.sb("ocarry", [128, 32], F32)
    P.op("dve", lambda e: e.memset(carry[:], 0.0), writes=["ocarry"])
    zxs = P.sb("ozxs", [128, D], BF16)
    P.op("dve", lambda e: e.memset(zxs[:], 0.0), writes=["ozxs"])
    zk_ = []
    for i_ in range(NSLOT // 128):
        P.dma("sp", xs_d.ap()[i_ * 128:(i_ + 1) * 128, :], zxs[:], reads=["ozxs"], writes=["xsz:%d" % i_])
        zk_.append("xsz:%d" % i_)
    P.dma("sp", xs_d.ap()[NSLOT:NSLOT + 1, :], zxs[0:1, :], reads=["ozxs"], writes=["xsz:d"])
    zk_.append("xsz:d")
    P.wait_all("pool", zk_)
    mgT = [P.sb("o2mgT%d" % i, [128, 16, 128], BF16) for i in range(2)]
    xt = [P.sb("o2x%d" % i, [128, D], F32) for i in range(2)]
    X1 = P.sb("oX1", [128, D], F32)
    H2 = P.sb("oH2", [128, D], F32)
    H2b = [P.sb("oH2b%d" % i, [128, D], BF16) for i in range(2)]
    sq = P.sb("o2sq", [128, D], BF16)
    h2T = P.sb("oh2T", [128, 16, 128], F32)
    r = P.sb("or", [128, 256], F32)
    slot_i = [P.sb("oslot%d" % i, [128, 2], I32) for i in range(2)]
    gate2 = [P.sb("ogate%d" % i, [128, 2], F32) for i in range(2)]
    psX = [P.ps("opsX%d" % i, [128, 512], F32) for i in range(2)]
    psT = P.ps("o2psT", [128, 4, 128], F32)
    psR = P.ps("opsR", [128, 512], F32)
    psP = P.ps("opsP", [128, 512], F32)

    def R(a, b_):
        return r[:, a:b_]

    for t in range(16):
        b = t % 2
        P.dma("sp", mgT[b][:], mgT_s.ap()[t], reads=["mgT_s:%d" % t], writes=["o2mgT%d" % b])
        P.dma("sp", xt[b][:], xw.ap()[NTOK + t * 128:NTOK + (t + 1) * 128, :], writes=["o2x%d" % b])
        for cb in range(4):
            cs_ = slice(cb * 512, (cb + 1) * 512)
            px, kx = psX[cb % 2], "opsX%d" % (cb % 2)
            for k in range(16):
                P.op("pe", lambda e, px=px, k=k, cs_=cs_, b=b: e.matmul(px[:], mgT[b][:, k, :], WO[:, k, cs_], start=(k == 0), stop=(k == 15)),
                     reads=["o2mgT%d" % b, "oWO_%d_%d" % (k // 8, cb)], writes=[kx], signal=(k == 15))
            P.op("dve", lambda e, px=px, cs_=cs_, b=b: e.tensor_tensor(X1[:, cs_], px[:], xt[b][:, cs_], ALU.add), reads=[kx, "o2x%d" % b], writes=["oX1_%d" % cb])
        x1k = ["oX1_%d" % cb for cb in range(4)]
        P.dma("sp", x1_s.ap()[t * 128:(t + 1) * 128, :], X1[:], reads=x1k, writes=["x1_s:%d" % t])
        P.op("act", lambda e: e.activation(sq[:], X1[:], AF.Square, accum_out=R(0, 1)), reads=x1k, writes=["o2sq", "or"])
        P.op("act", lambda e: e.activation(R(1, 2), R(0, 1), AF.Sqrt, bias=EPSB[0][:, 0:1], scale=1.0 / D), reads=["or", "epsb"], writes=["or"])
        P.op("dve", lambda e: e.reciprocal(R(2, 3), R(1, 2)), reads=["or"], writes=["or"])
        P.op("dve", lambda e: e.scalar_tensor_tensor(out=H2[:], in0=X1[:], scalar=R(2, 3), in1=g2b[:], op0=ALU.mult, op1=ALU.mult),
             reads=x1k + ["or", "og2b"], writes=["oH2"])
        P.op("act", lambda e, b=b: e.copy(H2b[b][:], H2[:]), reads=["oH2"], writes=["oH2b%d" % b])
        for c4 in range(4):
            for j in range(4):
                kc = c4 * 4 + j
                P.op("pe", lambda e, j=j, kc=kc: e.transpose(psT[:, j, :], H2[:, kc * 128:(kc + 1) * 128], ident[:]), reads=["oH2", "ident"], writes=["o2psT"], signal=(j == 3))
            eng = "act" if c4 % 2 else "dve"
            if eng == "act":
                P.op("act", lambda e, c4=c4: e.copy(h2T[:, c4 * 4:(c4 + 1) * 4, :], psT[:]), reads=["o2psT"], writes=["oh2T_%d" % c4])
            else:
                P.op("dve", lambda e, c4=c4: e.tensor_copy(h2T[:, c4 * 4:(c4 + 1) * 4, :], psT[:]), reads=["o2psT"], writes=["oh2T_%d" % c4])
        for k in range(16):
            P.op("pe", lambda e, k=k: e.matmul(psR[:, 0:36], h2T[:, k, :], RW[:, k, :], start=(k == 0), stop=(k == 15)),
                 reads=["oh2T_%d" % (k // 4), "oRW"], writes=["opsR"], signal=(k == 15))
        P.op("dve", lambda e: e.tensor_tensor(R(8, 12), psR[:, 0:4], rbb[:, 0:4], ALU.add), reads=["opsR", "orbb"], writes=["or"])
        P.op("dve", lambda e: e.tensor_tensor(R(16, 48), psR[:, 4:36], rbb[:, 4:36], ALU.add), reads=["opsR", "orbb"], writes=["or"])
        P.op("dve", lambda e: e.tensor_reduce(R(48, 49), R(8, 12), AX.X, ALU.max), reads=["or"], writes=["or"])
        P.op("dve", lambda e: e.tensor_scalar(R(52, 56), R(8, 12), R(48, 49), None, ALU.is_equal), reads=["or"], writes=["or"])
        P.op("dve", lambda e: e.tensor_scalar(R(56, 60), R(8, 12), R(48, 49), None, ALU.subtract), reads=["or"], writes=["or"])
        P.op("act", lambda e: e.activation(R(56, 60), R(56, 60), AF.Exp, accum_out=R(60, 61)), reads=["or"], writes=["or"])
        P.op("dve", lambda e: e.reciprocal(R(61, 62), R(60, 61)), reads=["or"], writes=["or"])
        P.op("dve", lambda e: e.tensor_scalar(R(64, 72), R(16, 24), R(52, 53), None, ALU.mult), reads=["or"], writes=["or"])
        for g in range(1, 4):
            P.op("dve", lambda e, g=g: e.scalar_tensor_tensor(out=R(64, 72), in0=R(16 + 8 * g, 24 + 8 * g), scalar=R(52 + g, 53 + g), in1=R(64, 72),
                                                              op0=ALU.mult, op1=ALU.add), reads=["or"], writes=["or"])
        P.op("dve", lambda e: e.max(R(72, 80), R(64, 72)), reads=["or"], writes=["or"])
        P.op("dve", lambda e: e.tensor_scalar(R(80, 88), R(64, 72), R(72, 73), None, ALU.is_equal), reads=["or"], writes=["or"])
        P.op("dve", lambda e: e.tensor_scalar(R(88, 96), R(64, 72), R(73, 74), None, ALU.is_equal), reads=["or"], writes=["or"])
        P.op("dve", lambda e: e.tensor_tensor(R(96, 97), R(73, 74), R(72, 73), ALU.subtract), reads=["or"], writes=["or"])
        P.op("act", lambda e: e.activation(R(97, 98), R(96, 97), AF.Exp), reads=["or"], writes=["or"])
        P.op("dve", lambda e: e.tensor_scalar(R(98, 99), R(97, 98), 1.0, None, ALU.add), reads=["or"], writes=["or"])
        P.op("dve", lambda e: e.reciprocal(R(98, 99), R(98, 99)), reads=["or"], writes=["or"])
        P.op("dve", lambda e: e.tensor_tensor(R(99, 100), R(61, 62), R(98, 99), ALU.mult), reads=["or"], writes=["or"])
        P.op("dve", lambda e: e.tensor_tensor(R(100, 101), R(61, 62), R(99, 100), ALU.subtract), reads=["or"], writes=["or"])
        for g in range(4):
            P.op("dve", lambda e, g=g: e.tensor_scalar(R(104 + 8 * g, 112 + 8 * g), R(80, 88), R(52 + g, 53 + g), None, ALU.mult), reads=["or"], writes=["or"])
            P.op("dve", lambda e, g=g: e.tensor_scalar(R(136 + 8 * g, 144 + 8 * g), R(88, 96), R(52 + g, 53 + g), None, ALU.mult), reads=["or"], writes=["or"])
        P.op("dve", lambda e: e.tensor_tensor(R(200, 232), R(104, 136), R(136, 168), ALU.add), reads=["or"], writes=["or"])
        P.op("pe", lambda e: e.matmul(psP[:, 0:32], tsf[:], R(200, 232), start=True, stop=True, skip_group_check=True), reads=["otsf", "or"], writes=["opsP"])
        P.op("pe", lambda e: e.matmul(psP[:, 32:64], onesf[:], R(200, 232), start=True, stop=True, skip_group_check=True), reads=["oones", "or"], writes=["opsP"])
        P.op("dve", lambda e: e.tensor_tensor(R(168, 200), psP[:, 0:32], carry[:], ALU.add), reads=["opsP", "ocarry"], writes=["or"])
        P.op("dve", lambda e: e.tensor_tensor(carry[:], carry[:], psP[:, 32:64], ALU.add), reads=["opsP", "ocarry"], writes=["ocarry"])
        S, Gt = slot_i[b], gate2[b]
        for k, (oh0, gcol) in enumerate(((104, 99), (136, 100))):
            P.op("dve", lambda e, oh0=oh0: e.tensor_tensor(R(200, 232), R(oh0, oh0 + 32), R(168, 200), ALU.mult), reads=["or"], writes=["or"])
            P.op("dve", lambda e: e.tensor_reduce(R(232, 233), R(200, 232), AX.X, ALU.add), reads=["or"], writes=["or"])
            P.op("dve", lambda e, oh0=oh0: e.tensor_tensor(R(200, 232), R(oh0, oh0 + 32), ebase[:], ALU.mult), reads=["or", "oebase"], writes=["or"])
            P.op("dve", lambda e: e.tensor_reduce(R(233, 234), R(200, 232), AX.X, ALU.add), reads=["or"], writes=["or"])
            P.op("dve", lambda e: e.tensor_scalar(R(234, 235), R(232, 233), float(CAP), None, ALU.is_lt), reads=["or"], writes=["or"])
            P.op("dve", lambda e: e.tensor_tensor(R(235, 236), R(232, 233), R(233, 234), ALU.add), reads=["or"], writes=["or"])
            P.op("dve", lambda e: e.tensor_scalar(R(235, 236), R(235, 236), float(NSLOT), None, ALU.subtract), reads=["or"], writes=["or"])
            P.op("dve", lambda e: e.tensor_tensor(R(236, 237), R(235, 236), R(234, 235), ALU.mult), reads=["or"], writes=["or"])
            P.op("dve", lambda e: e.tensor_scalar(R(236, 237), R(236, 237), float(NSLOT), None, ALU.add), reads=["or"], writes=["or"])
            P.op("dve", lambda e, S=S, k=k: e.tensor_copy(S[:, k:k + 1], R(236, 237)), reads=["or"], writes=["oslot%d" % b])
            P.op("dve", lambda e, Gt=Gt, k=k, gcol=gcol: e.tensor_tensor(Gt[:, k:k + 1], R(gcol, gcol + 1), R(234, 235), ALU.mult), reads=["or"], writes=["ogate%d" % b])
        for k in range(2):
            P.idma_scatter(xs_d, S[:, k:k + 1], H2b[b][:], reads=["oslot%d" % b, "oH2b%d" % b], writes=["xs:%d:%d" % (t, k)])
        P.dma("sp", slot_s.ap()[t], S[:], reads=["oslot%d" % b], writes=["slot_s:%d" % t])
        P.dma("sp", gate_s.ap()[t], Gt[:], reads=["ogate%d" % b], writes=["gate_s:%d" % t])
```

```python
import numpy as np
from contextlib import ExitStack
import concourse.bass as bass
import concourse.mybir as mybir
from concourse.bass_utils import run_bass_kernel_spmd

F32 = mybir.dt.float32
F32R = mybir.dt.float32r
BF16 = mybir.dt.bfloat16
I32 = mybir.dt.int32
U32 = mybir.dt.uint32
AF = mybir.ActivationFunctionType
ALU = mybir.AluOpType
AX = mybir.AxisListType

D = 2048
NTOK = 2048
WTOK = 4096
INW = 10688
RWW = 3520
NCORES = 8
import os
RSTOP = int(os.environ.get('RSTOP', '9'))


class Prog:
    ENG = ("pe", "act", "dve", "pool", "sp")

    def __init__(self, nc, es):
        self.nc = nc
        self.es = es
        self.q = {e: [] for e in self.ENG}
        self.sem = {e: es.enter_context(nc.semaphore("c_" + e)) for e in ("pe", "act", "dve", "pool")}
        self.cnt = {e: 0 for e in self.ENG}
        self.NR = 24
        self.ring = {e: [es.enter_context(nc.semaphore("r_%s%d" % (e, i))) for i in range(self.NR)]
                     for e in ("sp", "pool", "act")}
        self.ring_n = {e: 0 for e in ("sp", "pool", "act")}
        self.ring_val = {e: [0] * self.NR for e in ("sp", "pool", "act")}
        self.seen = {e: {} for e in self.ENG}
        self.lastw = {}
        self.reads = {}
        self.cur = es
        self.stack = []

    def sb(self, name, shape, dt):
        return self.cur.enter_context(self.nc.sbuf_tensor(name, list(shape), dt))

    def ps(self, name, shape, dt=F32):
        return self.cur.enter_context(self.nc.psum_tensor(name, list(shape), dt))

    def push(self):
        self.stack.append(self.cur)
        self.cur = ExitStack()

    def pop(self):
        self.barrier()
        self.cur.close()
        self.cur = self.stack.pop()

    def barrier(self):
        for eng in self.ENG:
            for e2 in ("pe", "act", "dve", "pool"):
                if self.cnt[e2]:
                    self._wait(eng, (self.sem[e2], self.cnt[e2], "bar"))
            for qn in self.ring:
                for i in range(self.NR):
                    if self.ring_val[qn][i]:
                        self._wait(eng, (self.ring[qn][i], self.ring_val[qn][i], "dma"))

    def dram(self, name, shape, dt, kind="Internal"):
        return self.nc.dram_tensor(name, list(shape), dt, kind=kind)

    def _wait(self, eng, ev):
        sem, val, src = ev
        if eng == "pe" and src == "pe":
            return
        key = id(sem)
        if self.seen[eng].get(key, 0) >= val:
            return
        self.seen[eng][key] = val
        self.q[eng].append(lambda e, sem=sem, val=val: e.wait_ge(sem, val))

    def _deps(self, eng, reads, writes):
        for k in reads:
            ev = self.lastw.get(k)
            if ev is not None:
                self._wait(eng, ev)
        for k in writes:
            ev = self.lastw.get(k)
            if ev is not None:
                self._wait(eng, ev)
            for ev in self.reads.get(k, ()):
                self._wait(eng, ev)

    def _commit(self, ev, reads, writes):
        for k in writes:
            self.lastw[k] = ev
            self.reads[k] = []
        for k in reads:
            self.reads.setdefault(k, []).append(ev)
            if len(self.reads[k]) > 64:
                self.reads[k] = self.reads[k][-64:]

    def op(self, eng, fn, reads=(), writes=(), signal=True):
        self._deps(eng, reads, writes)
        sem = self.sem[eng]
        if eng == "pe" and not signal:
            self.q[eng].append(lambda e, fn=fn: fn(e))
            self._commit((sem, self.cnt[eng] + 1, eng), reads, writes)
            return
        self.cnt[eng] += 1
        n = self.cnt[eng]
        self.q[eng].append(lambda e, fn=fn, sem=sem: fn(e).then_inc(sem, 1))
        self._commit((sem, n, eng), reads, writes)

    def dma(self, eng, out, in_, reads=(), writes=(), **kw):
        self._deps(eng, reads, writes)
        i = self.ring_n[eng] % self.NR
        self.ring_n[eng] += 1
        sem = self.ring[eng][i]
        prev = self.ring_val[eng][i]
        if prev:
            self._wait(eng, (sem, prev, "dma"))
        val = prev + 16
        self.ring_val[eng][i] = val
        self.q[eng].append(lambda e, out=out, in_=in_, sem=sem, kw=kw: e.dma_start(out=out, in_=in_, **kw).then_inc(sem, 16))
        ev = (sem, val, "dma")
        self._commit(ev, reads, writes)
        return ev

    def _idma(self, mk, reads, writes):
        eng = "pool"
        self._deps(eng, reads, writes)
        i = self.ring_n[eng] % self.NR
        self.ring_n[eng] += 1
        sem = self.ring[eng][i]
        prev = self.ring_val[eng][i]
        if prev:
            self._wait(eng, (sem, prev, "dma"))
        val = prev + 16
        self.ring_val[eng][i] = val
        self.q[eng].append(lambda e, mk=mk, sem=sem: mk(e).then_inc(sem, 16))
        self._commit((sem, val, "dma"), reads, writes)

    def idma_scatter(self, dram, idx, src, reads=(), writes=()):
        nrow = dram.shape[0]
        self._idma(lambda e: e.indirect_dma_start(out=dram[:, :], out_offset=bass.IndirectOffsetOnAxis(ap=idx, axis=0), in_=src,
                                                  in_offset=None), reads, writes)

    def idma_gather(self, dst, dram, idx, reads=(), writes=()):
        nrow = dram.shape[0]
        self._idma(lambda e: e.indirect_dma_start(out=dst, out_offset=None, in_=dram[:, :],
                                                  in_offset=bass.IndirectOffsetOnAxis(ap=idx, axis=0)), reads, writes)

    def wait_all(self, eng, keys):
        for k in keys:
            ev = self.lastw.get(k)
            if ev is not None:
                self._wait(eng, ev)

    def emit(self):
        nc = self.nc
        with nc.Block() as blk:
            @blk.tensor
            def _(e):
                for f in self.q["pe"]:
                    f(e)

            @blk.scalar
            def _(e):
                for f in self.q["act"]:
                    f(e)

            @blk.vector
            def _(e):
                for f in self.q["dve"]:
                    f(e)

            @blk.gpsimd
            def _(e):
                for f in self.q["pool"]:
                    f(e)

            @blk.sync
            def _(e):
                for f in self.q["sp"]:
                    f(e)


def AP(t, off, dims):
    return bass.AP(t, off, [list(d) for d in dims])


def host_consts():
    c = {}
    c["ident_h"] = np.eye(128, dtype=np.float32)
    c["tri_h"] = np.triu(np.ones((128, 128), np.float32))
    c.update(rwkv_consts())
    return c


def build(stages=("A",), debug=False):
    nc = bass.Bass("TRN2", target_bir_lowering=False)
    es = ExitStack()
    P = Prog(nc, es)
    dr = {}

    def din(name, shape, dt=F32):
        dr[name] = nc.dram_tensor(name, list(shape), dt, kind="ExternalInput")
        return dr[name]

    xw = din("xw", [WTOK, D])
    cs = din("cs", [WTOK, 16])
    norm1_g = din("norm1_g", [1, D])
    w_in = [din("win_%d" % i, [D, cw]) for i, (c0, cw, kind, bi) in enumerate(col_blocks())]
    q_norm_g = din("q_norm_g", [1, 64])
    k_norm_g = din("k_norm_g", [1, 64])
    ident_d = din("ident_h", [128, 128])
    qT_s = P.dram("qT_s", [8, 128, NTOK], BF16)
    kT_s = P.dram("kT_s", [8, 128, WTOK], BF16)
    v_s = P.dram("v_s", [WTOK, 1024], BF16)
    prw = P.dram("prw", [WTOK + 1, RWW], F32)
    sg_s = P.dram("sg_s", [NTOK, 4096], BF16)
    outs = {}
    ya_s = P.dram("ya_s", [NTOK, 1024], F32)
    ybT_s = P.dram("ybT_s", [1024, NTOK], BF16)
    if debug:
        dbg = debug if isinstance(debug, (list, tuple, set)) else ("qT", "kT", "v", "prw", "sg")
        shp = {"qT": ([8, 128, NTOK], BF16), "kT": ([8, 128, WTOK], BF16), "v": ([WTOK, 1024], BF16),
               "prw": ([WTOK + 1, RWW], F32), "sg": ([NTOK, 4096], BF16), "ya": ([NTOK, 1024], F32), "ybT": ([1024, NTOK], BF16),
               "x1": ([NTOK, D], F32), "slot": ([16, 128, 2], I32), "gate": ([16, 128, 2], F32)}
        for k in dbg:
            outs["dbg_" + k] = nc.dram_tensor("dbg_" + k, shp[k][0], shp[k][1], kind="ExternalOutput")
        qT_s = outs.get("dbg_qT", qT_s); kT_s = outs.get("dbg_kT", kT_s); v_s = outs.get("dbg_v", v_s)
        prw = outs.get("dbg_prw", prw); sg_s = outs.get("dbg_sg", sg_s); ya_s = outs.get("dbg_ya", ya_s)
        ybT_s = outs.get("dbg_ybT", ybT_s)
    if not outs and "M" not in stages:
        outs["dummy_out"] = nc.dram_tensor("dummy_out", [1, 64], F32, kind="ExternalOutput")
    validT = din("validT", [128, 32])
    tri_d = din("tri_h", [128, 128])
    lamv = [din(n, [1, 64]) for n in ("lam_q1", "lam_k1", "lam_q2", "lam_k2")]
    subln_g = din("subln_g", [1, 128])

    ident = P.sb("ident", [128, 128], F32)
    identb = P.sb("identb", [128, 128], BF16)
    g1b = P.sb("g1b", [128, D], F32)
    qgb = P.sb("qgb", [128, 512], F32)
    kgb = P.sb("kgb", [128, 512], F32)
    zrow = P.sb("zrow", [1, RWW], F32)
    epsb = P.sb("epsb", [128, 4], F32)
    EPSB[0] = epsb
    P.op("dve", lambda e: e.memset(epsb[:, 0:1], 1e-6), writes=["epsb"])
    P.op("dve", lambda e: e.memset(epsb[:, 1:2], 1e-5), writes=["epsb"])
    P.op("dve", lambda e: e.memset(epsb[:, 2:3], 64e-5), writes=["epsb"])
    P.op("dve", lambda e: e.memset(epsb[:, 3:4], 1.0), writes=["epsb"])
    P.dma("sp", ident[:], ident_d.ap(), writes=["ident"])
    P.op("dve", lambda e: e.tensor_copy(identb[:], ident[:]), reads=["ident"], writes=["identb"])
    P.dma("sp", g1b[:], AP(norm1_g, 0, [[0, 128], [1, D]]), writes=["g1b"])
    for j in range(8):
        P.dma("sp", qgb[:, j * 64:(j + 1) * 64], AP(q_norm_g, 0, [[0, 128], [1, 64]]), writes=["qgb"])
        P.dma("sp", kgb[:, j * 64:(j + 1) * 64], AP(k_norm_g, 0, [[0, 128], [1, 64]]), writes=["kgb"])
    P.op("dve", lambda e: e.tensor_scalar(qgb[:], qgb[:], 0.125, None, ALU.mult), reads=["qgb"], writes=["qgb"])
    P.op("dve", lambda e: e.memset(zrow[:], 0.0), writes=["zrow"])
    P.dma("sp", prw.ap()[0:1, :], zrow[:], reads=["zrow"], writes=["prw"])

    if "A" in stages:
        P.push()
        stage_A(P, nc, xw, cs, w_in, ident, identb, g1b, qgb, kgb, qT_s, kT_s, v_s, prw, sg_s)
        P.pop()
    if "B" in stages:
        P.push()
        stage_B(P, nc, qT_s, kT_s, v_s, validT, tri_d, lamv, subln_g, ya_s)
        P.pop()

    if "R" in stages:
        P.push()
        stage_R(P, nc, din, prw, ident, identb, ybT_s)
        P.pop()

    if "O" in stages:
        mgT_s = P.dram("mgT_s", [16, 128, 16, 128], BF16)
        x1_s = outs.get("dbg_x1") or P.dram("x1_s", [NTOK, D], F32)
        xs_d = P.dram("xs_d", [NSLOT + 1, D], BF16)
        slot_s = outs.get("dbg_slot") or P.dram("slot_s", [16, 128, 2], I32)
        gate_s = outs.get("dbg_gate") or P.dram("gate_s", [16, 128, 2], F32)
        P.push()
        stage_O1(P, nc, din, identb, ya_s, ybT_s, sg_s, mgT_s)
        P.pop()
        P.push()
        stage_O2(P, nc, din, ident, xw, mgT_s, x1_s, xs_d, slot_s, gate_s)
        P.pop()
    if "M" in stages:
        y_d = P.dram("y_d", [NSLOT + 1, D], F32)
        out_d = nc.dram_tensor("out", [NTOK, D], F32, kind="ExternalOutput")
        outs["out"] = out_d
        P.push()
        stage_M(P, nc, din, identb, xs_d, y_d, x1_s, slot_s, gate_s, out_d)
        P.pop()
        P.push()
        stage_M2(P, nc, y_d, x1_s, slot_s, gate_s, out_d)
        P.pop()

    P.wait_all("sp", list(P.lastw.keys()))
    P.emit()
    es.close()
    LASTP[0] = P
    return nc, list(outs.keys())


def col_blocks():
    blks = []
    for i in range(2):
        blks.append((i * 512, 512, "q", i))
    for i in range(2):
        blks.append((1024 + i * 512, 512, "k", i))
    for i in range(2):
        blks.append((2048 + i * 512, 512, "v", i))
    for i in range(7):
        w = 512 if i < 6 else RWW - 6 * 512
        blks.append((3072 + i * 512, w, "rw", i))
    for i in range(8):
        blks.append((6592 + i * 512, 512, "g", i))
    return blks


def stage_A(P, nc, xw, cs, w_in, ident, identb, g1b, qgb, kgb, qT_s, kT_s, v_s, prw, sg_s):
    hT = P.sb("hT", [128, 16, 16, 128], BF16)
    xt = [P.sb("xt%d" % i, [128, D], F32) for i in range(2)]
    ht = [P.sb("ht%d" % i, [128, D], F32) for i in range(2)]
    sq = P.sb("sqjunk", [128, D], BF16)
    st = [P.sb("st%d" % i, [128, 4], F32) for i in range(2)]
    wst = P.sb("wst", [128, 16, 512], F32)
    wbf = [P.sb("wbf%d" % i, [128, 16, 512], BF16) for i in range(2)]
    pT = [P.ps("pT%d" % i, [128, 4, 128], F32) for i in range(2)]
    pacc = [P.ps("pacc%d" % i, [128, 512], F32) for i in range(2)]
    pTb = [P.ps("pTb%d" % i, [128, 4, 128], BF16) for i in range(2)]
    ev = [P.sb("ev%d" % i, [128, 512], F32) for i in range(2)]
    evb = [P.sb("evb%d" % i, [128, 512], BF16) for i in range(2)]
    qn = [P.sb("qn%d" % i, [128, 512], F32) for i in range(2)]
    qr = [P.sb("qr%d" % i, [128, 512], BF16) for i in range(2)]
    qs = [P.sb("qs%d" % i, [128, 16], F32) for i in range(2)]
    cst = [P.sb("cst%d" % i, [128, 16], F32) for i in range(2)]
    tmp8 = [P.sb("tmp8_%d" % i, [128, 8, 16], F32) for i in range(2)]
    qTb = [P.sb("qTb%d" % i, [128, 4, 128], BF16) for i in range(2)]
    blks = col_blocks()
    items = [(g_, wblk) + blk for g_ in range(2) for wblk, blk in enumerate(blks) if not (g_ == 0 and blk[2] in ("q", "g"))]

    def issue_w(idx):
        g_, wblk, c0, cw, kind, bi = items[idx]
        wb = wbf[idx % 2]
        wk = "wbf%d" % (idx % 2)
        P.dma("sp", wst[:, :, 0:cw], w_in[wblk].ap().rearrange("(k p) c -> p k c", p=128), writes=["wst"])
        for hh in range(2):
            eng = "pool" if hh == 0 else "dve"
            P.op(eng, lambda e, wb=wb, hh=hh, cw=cw: e.tensor_copy(wb[:, hh * 8:(hh + 1) * 8, 0:cw], wst[:, hh * 8:(hh + 1) * 8, 0:cw]),
                 reads=["wst"], writes=[wk + "_%d" % hh])
    for grp in range(2):
        for t in range(16):
            gt = grp * 16 + t
            b = t % 2
            X, H, S = xt[b], ht[b], st[b]
            P.dma("sp", X[:], xw.ap()[gt * 128:(gt + 1) * 128, :], writes=["xt%d" % b])
            P.op("act", lambda e, X=X, S=S: e.activation(sq[:], X[:], AF.Square, accum_out=S[:, 0:1]),
                 reads=["xt%d" % b], writes=["sq", "st%d" % b])
            P.op("act", lambda e, S=S: e.activation(S[:, 1:2], S[:, 0:1], AF.Sqrt, bias=EPSB[0][:, 0:1], scale=1.0 / D),
                 reads=["st%d" % b, "epsb"], writes=["st%d" % b])
            P.op("dve", lambda e, S=S: e.reciprocal(S[:, 2:3], S[:, 1:2]), reads=["st%d" % b], writes=["st%d" % b])
            P.op("dve", lambda e, X=X, H=H, S=S: e.scalar_tensor_tensor(out=H[:], in0=X[:], scalar=S[:, 2:3], in1=g1b[:],
                                                                      op0=ALU.mult, op1=ALU.mult),
                 reads=["xt%d" % b, "st%d" % b, "g1b"], writes=["ht%d" % b])
            for c4 in range(4):
                pb = c4 % 2
                for j in range(4):
                    kc = c4 * 4 + j
                    P.op("pe", lambda e, H=H, kc=kc, pb=pb, j=j: e.transpose(pT[pb][:, j, :], H[:, kc * 128:(kc + 1) * 128], ident[:]),
                         reads=["ht%d" % b, "ident"], writes=["pT%d" % pb], signal=(j == 3))
                eng = "act" if c4 % 2 == 0 else "dve"
                if eng == "act":
                    P.op("act", lambda e, t=t, c4=c4, pb=pb: e.copy(hT[:, t, c4 * 4:(c4 + 1) * 4, :], pT[pb][:]),
                         reads=["pT%d" % pb], writes=["hT%d_%d" % (t, c4)])
                else:
                    P.op("dve", lambda e, t=t, c4=c4, pb=pb: e.tensor_copy(hT[:, t, c4 * 4:(c4 + 1) * 4, :], pT[pb][:]),
                         reads=["pT%d" % pb], writes=["hT%d_%d" % (t, c4)])
        for wblk, (c0, cw, kind, bi) in enumerate(blks):
            if grp == 0 and kind in ("q", "g"):
                continue
            idx = [i_ for i_, it in enumerate(items) if it[0] == grp and it[1] == wblk][0]
            if idx == 0:
                issue_w(0)
            if idx + 1 < len(items):
                issue_w(idx + 1)
            wb = wbf[idx % 2]
            wk = "wbf%d" % (idx % 2)
            for t in range(16):
                gt = grp * 16 + t
                tok0 = gt * 128
                pa = pacc[t % 2]
                pk = "pacc%d" % (t % 2)
                for kc in range(16):
                    P.op("pe", lambda e, pa=pa, t=t, kc=kc, wb=wb, cw=cw: e.matmul(pa[:, 0:cw], hT[:, t, kc, :], wb[:, kc, 0:cw],
                                                                              start=(kc == 0), stop=(kc == 15)),
                         reads=["hT%d_%d" % (t, kc // 4), wk + "_%d" % (kc // 8)], writes=[pk], signal=(kc == 15))
                b = t % 2
                if kind == "v":
                    P.op("act", lambda e, pa=pa, b=b: e.copy(evb[b][:], pa[:]), reads=[pk], writes=["evb%d" % b])
                    P.dma("act", v_s.ap()[tok0:tok0 + 128, bi * 512:(bi + 1) * 512], evb[b][:], reads=["evb%d" % b], writes=["v_s:%d:%d" % (gt, bi)])
                elif kind == "rw":
                    P.op("act", lambda e, pa=pa, b=b, cw=cw: e.copy(ev[b][:, 0:cw], pa[:, 0:cw]), reads=[pk], writes=["ev%d" % b])
                    P.dma("act", prw.ap()[1 + tok0:1 + tok0 + 128, bi * 512:bi * 512 + cw], ev[b][:, 0:cw], reads=["ev%d" % b], writes=["prw:%d:%d" % (gt, bi)])
                elif kind == "g":
                    P.op("act", lambda e, pa=pa, b=b: e.activation(evb[b][:], pa[:], AF.Sigmoid), reads=[pk], writes=["evb%d" % b])
                    P.dma("act", sg_s.ap()[t * 128:(t + 1) * 128, bi * 512:(bi + 1) * 512], evb[b][:], reads=["evb%d" % b], writes=["sg_s:%d:%d" % (t, bi)])
                else:
                    gb_ = qgb if kind == "q" else kgb
                    QN, QS, QR, CS, T8 = qn[b], qs[b], qr[b], cst[b], tmp8[b]
                    P.dma("pool", CS[:], cs.ap()[tok0:tok0 + 128, :], writes=["cst%d" % b])
                    P.op("act", lambda e, pa=pa, QN=QN: e.activation(QN[:], pa[:], AF.Square), reads=[pk], writes=["qn%d" % b])
                    P.op("dve", lambda e, QN=QN, QS=QS: e.tensor_reduce(QS[:, 0:8], QN[:].rearrange("p (g d) -> p g d", d=64), AX.X, ALU.add),
                         reads=["qn%d" % b], writes=["qs%d" % b])
                    P.op("act", lambda e, QS=QS: e.activation(QS[:, 0:8], QS[:, 0:8], AF.Sqrt, bias=EPSB[0][:, 0:1], scale=1.0 / 64),
                         reads=["qs%d" % b, "epsb"], writes=["qs%d" % b])
                    P.op("dve", lambda e, QS=QS: e.reciprocal(QS[:, 8:16], QS[:, 0:8]), reads=["qs%d" % b], writes=["qs%d" % b])
                    P.op("dve", lambda e, pa=pa, QN=QN, QS=QS: e.tensor_tensor(
                        QN[:].rearrange("p (g d) -> p g d", d=64), pa[:].rearrange("p (g d) -> p g d", d=64),
                        AP(QS, 8, [[16, 128], [1, 8], [0, 64]]), ALU.mult),
                        reads=[pk, "qs%d" % b], writes=["qn%d" % b])
                    P.op("dve", lambda e, QN=QN, gb_=gb_: e.tensor_tensor(QN[:], QN[:], gb_[:], ALU.mult),
                         reads=["qn%d" % b, "qgb", "kgb"], writes=["qn%d" % b])
                    x1 = AP(QN, 0, [[512, 128], [64, 8], [1, 8]])
                    x2 = AP(QN, 8, [[512, 128], [64, 8], [1, 8]])
                    cosb = AP(CS, 0, [[16, 128], [0, 8], [1, 8]])
                    sinb = AP(CS, 8, [[16, 128], [0, 8], [1, 8]])
                    t_a = AP(T8, 0, [[128, 128], [16, 8], [1, 8]])
                    t_b = AP(T8, 8, [[128, 128], [16, 8], [1, 8]])
                    P.op("dve", lambda e, t_a=t_a, x2=x2, sinb=sinb: e.tensor_tensor(t_a, x2, sinb, ALU.mult), reads=["qn%d" % b, "cst%d" % b], writes=["tmp8_%d" % b])
                    P.op("dve", lambda e, t_b=t_b, x1=x1, sinb=sinb: e.tensor_tensor(t_b, x1, sinb, ALU.mult), reads=["qn%d" % b, "cst%d" % b], writes=["tmp8_%d" % b])
                    P.op("dve", lambda e, x1=x1, cosb=cosb: e.tensor_tensor(x1, x1, cosb, ALU.mult), reads=["qn%d" % b, "cst%d" % b], writes=["qn%d" % b])
                    P.op("dve", lambda e, x2=x2, cosb=cosb: e.tensor_tensor(x2, x2, cosb, ALU.mult), reads=["qn%d" % b, "cst%d" % b], writes=["qn%d" % b])
                    P.op("dve", lambda e, x1=x1, t_a=t_a: e.tensor_tensor(x1, x1, t_a, ALU.subtract), reads=["qn%d" % b, "tmp8_%d" % b], writes=["qn%d" % b])
                    P.op("dve", lambda e, x2=x2, t_b=t_b: e.tensor_tensor(x2, x2, t_b, ALU.add), reads=["qn%d" % b, "tmp8_%d" % b], writes=["qn%d" % b])
                    P.op("act", lambda e, QN=QN, QR=QR: e.copy(QR[:], QN[:]), reads=["qn%d" % b], writes=["qr%d" % b])
                    for j in range(4):
                        P.op("pe", lambda e, QR=QR, j=j, b=b: e.transpose(pTb[b][:, j, :], QR[:, j * 128:(j + 1) * 128], identb[:]),
                             reads=["qr%d" % b, "identb"], writes=["pTb%d" % b], signal=(j == 3))
                    P.op("act", lambda e, b=b: e.copy(qTb[b][:], pTb[b][:]), reads=["pTb%d" % b], writes=["qTb%d" % b])
                    if kind == "q":
                        dst = qT_s.ap()[bi * 4:(bi + 1) * 4, :, t * 128:(t + 1) * 128].rearrange("h p t -> p h t")
                        P.dma("act", dst, qTb[b][:], reads=["qTb%d" % b], writes=["qT_s:%d:%d" % (t, bi)])
                    else:
                        dst = kT_s.ap()[bi * 4:(bi + 1) * 4, :, tok0:tok0 + 128].rearrange("h p t -> p h t")
                        P.dma("act", dst, qTb[b][:], reads=["qTb%d" % b], writes=["kT_s:%d:%d" % (gt, bi)])


def stage_B(P, nc, qT_s, kT_s, v_s, validT, tri_d, lamv, subln_g, ya_s):
    LAM_INIT = 0.2
    trif = P.sb("trif", [128, 128], F32)
    tri = P.sb("tri", [128, 128], BF16)
    P.dma("sp", trif[:], tri_d.ap(), writes=["trif"])
    P.op("dve", lambda e: e.tensor_copy(tri[:], trif[:]), reads=["trif"], writes=["tri"])
    validc = P.sb("validc", [128, 32], F32)
    P.dma("sp", validc[:], validT.ap(), writes=["validc"])
    sgb = P.sb("sgb", [128, 128], F32)
    P.dma("sp", sgb[:], AP(subln_g, 0, [[0, 128], [1, 128]]), writes=["sgb"])
    P.op("dve", lambda e: e.tensor_scalar(sgb[:], sgb[:], 1.0 - LAM_INIT, None, ALU.mult), reads=["sgb"], writes=["sgb"])
    lv = P.sb("lv", [1, 4, 64], F32)
    for i in range(4):
        P.dma("sp", lv[:, i, :], lamv[i].ap(), writes=["lv"])
    lp = P.sb("lp", [1, 2, 64], F32)
    ls = P.sb("ls", [1, 8], F32)
    ones1 = P.sb("ones1", [1, 128], F32)
    nlamb = P.sb("nlamb", [128, 1], F32)
    plam = P.ps("plam", [128, 2], F32)
    P.op("dve", lambda e: e.memset(ones1[:], 1.0), writes=["ones1"])
    P.op("dve", lambda e: e.memset(ls[:], 0.0), writes=["ls"])
    P.op("dve", lambda e: e.tensor_tensor(lp[:, 0, :], lv[:, 0, :], lv[:, 1, :], ALU.mult), reads=["lv"], writes=["lp"])
    P.op("dve", lambda e: e.tensor_tensor(lp[:, 1, :], lv[:, 2, :], lv[:, 3, :], ALU.mult), reads=["lv"], writes=["lp"])
    P.op("dve", lambda e: e.tensor_reduce(ls[:, 0:2], lp[:], AX.X, ALU.add), reads=["lp"], writes=["ls"])
    P.op("act", lambda e: e.activation(ls[:, 2:4], ls[:, 0:2], AF.Exp), reads=["ls"], writes=["ls"])
    P.op("dve", lambda e: e.tensor_tensor(ls[:, 4:5], ls[:, 3:4], ls[:, 2:3], ALU.subtract), reads=["ls"], writes=["ls"])
    P.op("dve", lambda e: e.tensor_scalar(ls[:, 6:8], ls[:, 4:6], -LAM_INIT, None, ALU.add), reads=["ls"], writes=["ls"])
    P.op("pe", lambda e: e.matmul(plam[:, 0:2], ones1[:], ls[:, 6:8], start=True, stop=True), reads=["ones1", "ls"], writes=["plam"])
    P.op("dve", lambda e: e.tensor_copy(nlamb[:], plam[:, 0:1]), reads=["plam"], writes=["nlamb"])

    qT = [P.sb("aqT%d" % i, [128, NTOK], BF16) for i in range(2)]
    kT = [P.sb("akT%d" % i, [128, WTOK], BF16) for i in range(2)]
    Vh = [P.sb("aVh%d" % i, [128, 32, 130], BF16) for i in range(2)]
    psS = [P.ps("psS%d" % i, [128, 512], F32) for i in range(2)]
    Oacc = [P.ps("Oacc%d" % i, [128, 2, 130], F32) for i in range(2)]
    PT = [P.sb("aPT%d" % i, [128, 512], BF16) for i in range(3)]
    Oc = [P.sb("aOc%d" % i, [128, 4, 130], F32) for i in range(2)]
    rl = P.sb("arl", [128, 8], F32)
    ssq[0] = P.sb("assq", [128, 4], F32)
    otmp = P.sb("aotmp", [128, 128], F32)
    obuf = P.sb("aobuf", [128, 128], F32)
    osq = P.sb("aosq", [128, 128], BF16)
    yat = [P.sb("ayat%d" % i, [128, 128], F32) for i in range(2)]
    nexp = 0
    nya = 0
    for h in range(8):
        hb = h % 2
        Q, Kt, V = qT[hb], kT[hb], Vh[hb]
        P.dma("sp", Q[:], qT_s.ap()[h], reads=["qT_s:%d:%d" % (t, h // 4) for t in range(16)], writes=["aqT%d" % hb])
        P.dma("sp", Kt[:], kT_s.ap()[h], reads=["kT_s:%d:%d" % (t, h // 4) for t in range(32)], writes=["akT%d" % hb])
        P.dma("sp", V[:, :, 0:128], v_s.ap()[:, h * 128:(h + 1) * 128].rearrange("(kt p) d -> p kt d", p=128),
              reads=["v_s:%d:%d" % (t, h // 4) for t in range(32)], writes=["aVh%d" % hb])
        P.op("dve", lambda e, V=V: e.tensor_copy(V[:, :, 128:129], validc[:].rearrange("p (k o) -> p k o", o=1)),
             reads=["validc"], writes=["aVh%d" % hb])
        for G in range(4):
            for c in range(2):
                nkt = 16 + 4 * G + 4
                for kt in range(nkt):
                    sb_ = kt % 2
                    P.op("pe", lambda e, sb_=sb_, c=c, kt=kt, G=G, Q=Q, Kt=Kt: e.matmul(
                        psS[sb_][:], Kt[c * 64:(c + 1) * 64, kt * 128:(kt + 1) * 128], Q[c * 64:(c + 1) * 64, G * 512:(G + 1) * 512],
                        start=True, stop=True), reads=["aqT%d" % hb, "akT%d" % hb], writes=["psS%d" % sb_])
                    pb = nexp % 3
                    nexp += 1
                    pt = PT[pb]
                    P.op("act", lambda e, pt=pt, sb_=sb_: e.activation(pt[:], psS[sb_][:], AF.Exp), reads=["psS%d" % sb_], writes=["aPT%d" % pb])
                    rel = kt - (16 + 4 * G)
                    for j in range(4):
                        if rel > j:
                            continue
                        if rel == j:
                            P.op("dve", lambda e, pt=pt, j=j: e.tensor_tensor(pt[:, j * 128:(j + 1) * 128], pt[:, j * 128:(j + 1) * 128], tri[:], ALU.mult),
                                 reads=["aPT%d" % pb, "tri"], writes=["aPT%d" % pb])
                        last = (kt == 16 + 4 * G + j)
                        P.op("pe", lambda e, pt=pt, j=j, kt=kt, V=V, last=last: e.matmul(
                            Oacc[j // 2][:, j % 2, 0:129], pt[:, j * 128:(j + 1) * 128], V[:, kt, 0:129],
                            start=(kt == 0 and j % 2 == 0), stop=last, skip_group_check=True),
                            reads=["aPT%d" % pb, "aVh%d" % hb], writes=["Oacc%d" % (j // 2)], signal=last)
                for a in range(2):
                    P.op("act", lambda e, a=a, c=c: e.copy(Oc[c][:, 2 * a:2 * a + 2, :], Oacc[a][:]), reads=["Oacc%d" % a], writes=["aOc%d" % c])
            P.op("dve", lambda e: e.reciprocal(rl[:, 0:4], Oc[0][:, :, 128]), reads=["aOc0"], writes=["arl"])
            P.op("dve", lambda e: e.reciprocal(rl[:, 4:8], Oc[1][:, :, 128]), reads=["aOc1"], writes=["arl"])
            P.op("dve", lambda e: e.tensor_scalar(rl[:, 4:8], rl[:, 4:8], nlamb[:, 0:1], None, ALU.mult), reads=["arl", "nlamb"], writes=["arl"])
            for j in range(4):
                yb_ = nya % 2
                nya += 1
                Y = yat[yb_]
                P.op("dve", lambda e, j=j: e.tensor_scalar(otmp[:], Oc[0][:, j, 0:128], rl[:, j:j + 1], None, ALU.mult), reads=["aOc0", "arl"], writes=["aotmp"])
                P.op("dve", lambda e, j=j: e.scalar_tensor_tensor(out=obuf[:], in0=Oc[1][:, j, 0:128], scalar=rl[:, 4 + j:5 + j], in1=otmp[:],
                                                                  op0=ALU.mult, op1=ALU.add), reads=["aOc1", "arl", "aotmp"], writes=["aobuf"])
                P.op("act", lambda e: e.activation(osq[:], obuf[:], AF.Square, accum_out=ssq[0][:, 0:1]),
                     reads=["aobuf"], writes=["aosq", "assq"])
                P.op("act", lambda e: e.activation(ssq[0][:, 1:2], ssq[0][:, 0:1], AF.Sqrt, bias=EPSB[0][:, 1:2], scale=1.0 / 128), reads=["assq", "epsb"], writes=["assq"])
                P.op("dve", lambda e: e.reciprocal(ssq[0][:, 2:3], ssq[0][:, 1:2]), reads=["assq"], writes=["assq"])
                P.op("dve", lambda e, Y=Y: e.scalar_tensor_tensor(out=Y[:], in0=obuf[:], scalar=ssq[0][:, 2:3], in1=sgb[:], op0=ALU.mult, op1=ALU.mult),
                     reads=["aobuf", "assq", "sgb"], writes=["ayat%d" % yb_])
                qt = 4 * G + j
                P.dma("sp", ya_s.ap()[qt * 128:(qt + 1) * 128, h * 128:(h + 1) * 128], Y[:], reads=["ayat%d" % yb_], writes=["ya_s:%d:%d" % (qt, h)])


ssq = [None]


EPSB = [None]
LASTP = [None]


def rope_table():
    inv_freq = (500000.0 ** (-np.arange(0, 16, 2, dtype=np.float32) / 16)).astype(np.float32)
    ang = np.arange(4096, dtype=np.float32)[:, None] * inv_freq[None, :]
    return np.cos(ang).astype(np.float32), np.sin(ang).astype(np.float32)


def make_in_maps(I, ncores=NCORES, with_experts=True):
    cos, sin = rope_table()
    cst = host_consts()
    maps = []
    for c in range(ncores):
        b, sh = c // 2, c % 2
        x = I["x"][b]
        xwin = np.zeros((WTOK, D), np.float32)
        cswin = np.zeros((WTOK, 16), np.float32)
        if sh == 0:
            xwin[NTOK:] = x[:NTOK]
            cswin[NTOK:, 0:8] = cos[:NTOK]
            cswin[NTOK:, 8:16] = sin[:NTOK]
            cswin[:NTOK, 0:8] = 1.0
        else:
            xwin[:] = x
            cswin[:, 0:8] = cos
            cswin[:, 8:16] = sin
        valid = np.ones((WTOK,), np.float32)
        if sh == 0:
            valid[:NTOK] = 0.0
        m = {"xw": xwin, "cs": cswin, "validT": np.ascontiguousarray(valid.reshape(32, 128).T)}
        for k in ("norm1_g", "q_norm_g", "k_norm_g", "lam_q1", "lam_k1", "lam_q2", "lam_k2", "subln_g"):
            m[k] = np.ascontiguousarray(I[k]).reshape(1, -1)
        for k in ("shift_mu", "w0", "a0", "k_k", "k_a"):
            m[k] = np.ascontiguousarray(I[k]).reshape(1, -1)
        for k in ("r_k", "lnx_g", "lnx_b"):
            m[k + "_c"] = np.ascontiguousarray(I[k].reshape(8, 128).T)
        for k in ("w_up", "a_up", "g_up"):
            m[k] = np.ascontiguousarray(I[k][0])
        m["proj_a"] = np.ascontiguousarray(I["proj_a"][0])
        m["proj_b"] = np.ascontiguousarray(I["proj_b"][0])
        for i in range(2):
            m["w_out_%d" % i] = np.ascontiguousarray(I["w_out"][0][i * 1024:(i + 1) * 1024])
        m["norm2_g"] = np.ascontiguousarray(I["norm2_g"]).reshape(1, -1)
        m["router_w"] = np.ascontiguousarray(np.concatenate([I["router_g"][0], I["router_e"][0]], axis=1))
        m["router_b"] = np.ascontiguousarray(np.concatenate([I["router_g_b"][0], I["router_e_b"][0]]).reshape(1, 36))
        m["ebase_h"] = (np.arange(32, dtype=np.float32) * CAP).reshape(1, 32)
        m["ts_h2"] = cst["ts_h"]
        if with_experts:
            for e in range(32):
                m["wg_%d" % e] = np.ascontiguousarray(I["w_gate_e"][0][e])
                m["wu_%d" % e] = np.ascontiguousarray(I["w_up_e"][0][e])
                m["wd_%d" % e] = np.ascontiguousarray(I["w_down_e"][0][e])
        for i, (c0, cw, kind, bi) in enumerate(col_blocks()):
            m["win_%d" % i] = np.ascontiguousarray(I["w_in"][0][:, c0:c0 + cw])
        m.update(cst)
        maps.append(m)
    return maps


def kernel(**inputs):
    I = {k: np.asarray(v) for k, v in inputs.items()}
    nc, onames = build(stages=("A", "B", "R", "O", "M"), debug=False)
    in_maps = make_in_maps(I, NCORES)
    res = run_bass_kernel_spmd(nc, in_maps, core_ids=list(range(NCORES)))
    out = np.empty((4, 4096, D), np.float32)
    for c in range(NCORES):
        b, sh = c // 2, c % 2
        out[b, sh * NTOK:(sh + 1) * NTOK] = np.asarray(res.results[c]["out"])
    return out


def rwkv_consts():
    s = np.arange(128)[:, None]
    t = np.arange(128)[None, :]
    c = {}
    c["cmat_h"] = ((s <= t).astype(np.float32) - (s <= 63).astype(np.float32))
    m2 = np.zeros((128, 2), np.float32)
    m2[:64, 0] = 1.0
    m2[64:, 1] = 1.0
    c["msk2_h"] = m2
    c["ts_h"] = (s < t).astype(np.float32)
    c["ti_h"] = (s <= t).astype(np.float32)
    c["tsl_h"] = (s > t).astype(np.float32)
    bd = np.zeros((128, 128), np.float32)
    bd[:64, :64] = 1.0
    bd[64:, 64:] = 1.0
    c["bd1_h"] = bd
    return c


def stage_R(P, nc, din, prw, ident, identb, ybT_s):
    shift_mu = din("shift_mu", [1, RWW])
    pv = {n: din(n, [1, 1024]) for n in ("w0", "a0", "k_k", "k_a")}
    colp = {n: din(n + "_c", [128, 8]) for n in ("r_k", "lnx_g", "lnx_b")}
    w_up = din("w_up", [96, 1024])
    a_up = din("a_up", [96, 1024])
    g_up = din("g_up", [256, 1024])
    cd = {n: din(n, [128, 2] if n == "msk2_h" else [128, 128]) for n in ("cmat_h", "msk2_h", "ts_h", "ti_h", "tsl_h", "bd1_h")}

    def ld(name, shape, src, dt=F32):
        t = P.sb(name, shape, dt)
        P.dma("sp", t[:], src, writes=[name])
        return t

    mu_b = ld("mu_b", [128, RWW], AP(shift_mu, 0, [[0, 128], [1, RWW]]))
    w0_b = ld("w0_b", [128, 1024], AP(pv["w0"], 0, [[0, 128], [1, 1024]]))
    a0_b = ld("a0_b", [128, 1024], AP(pv["a0"], 0, [[0, 128], [1, 1024]]))
    kk_b = ld("kk_b", [128, 1024], AP(pv["k_k"], 0, [[0, 128], [1, 1024]]))
    ka_b = ld("ka_b", [128, 1024], AP(pv["k_a"], 0, [[0, 128], [1, 1024]]))
    rk_c = ld("rk_c", [128, 8], colp["r_k"].ap())
    lg_c = ld("lg_c", [128, 8], colp["lnx_g"].ap())
    lb_c = ld("lb_c", [128, 8], colp["lnx_b"].ap())
    wup = P.sb("wup", [128, 1024], F32)
    aup = P.sb("aup", [128, 1024], F32)
    for t_, src_, nm_ in ((wup, w_up, "wup"), (aup, a_up, "aup")):
        P.op("dve", lambda e, t_=t_: e.memset(t_[:], 0.0), writes=[nm_])
        P.dma("sp", t_[0:96, :], src_.ap(), writes=[nm_])
    gup = ld("gup", [128, 2, 1024], g_up.ap().rearrange("(k p) c -> p k c", p=128))
    cmat = ld("cmat", [128, 128], cd["cmat_h"].ap())
    msk2 = ld("msk2", [128, 2], cd["msk2_h"].ap())
    bdf = ld("bdf", [128, 128], cd["bd1_h"].ap())
    tsf = ld("tsf", [128, 128], cd["ts_h"].ap())
    tif = ld("tif", [128, 128], cd["ti_h"].ap())
    tslf = ld("tslf", [128, 128], cd["tsl_h"].ap())
    TS = P.sb("TSb", [128, 128], BF16)
    TI = P.sb("TIb", [128, 128], BF16)
    TSL = P.sb("TSLb", [128, 128], BF16)
    bd1 = P.sb("bd1b", [128, 128], BF16)
    bd64 = P.sb("bd64", [128, 128], F32)
    P.op("dve", lambda e: e.tensor_copy(TS[:], tsf[:]), reads=["tsf"], writes=["TSb"])
    P.op("dve", lambda e: e.tensor_copy(TI[:], tif[:]), reads=["tif"], writes=["TIb"])
    P.op("dve", lambda e: e.tensor_copy(TSL[:], tslf[:]), reads=["tslf"], writes=["TSLb"])
    P.op("dve", lambda e: e.tensor_copy(bd1[:], bdf[:]), reads=["bdf"], writes=["bd1b"])
    P.op("dve", lambda e: e.tensor_scalar(bd64[:], bdf[:], 1.0 / 64, None, ALU.mult), reads=["bdf"], writes=["bd64"])

    P0 = P.sb("rP0", [128, RWW], F32)
    P1 = P.sb("rP1", [128, RWW], F32)
    LI = P.sb("rLI", [128, 512], F32)
    P.op("dve", lambda e: e.memset(LI[:], 0.0), writes=["rLI"])
    LIT = [P.sb("rLIT%d" % i, [128, 4, 128], F32) for i in range(2)]
    U = P.sb("rU", [128, 1024], F32)
    AS = P.sb("rAS", [128, 1024], F32)
    E1, E2, E3 = P1[:, 0:1024], P1[:, 1024:2048], P1[:, 2048:3072]
    KK = P.sb("rKK", [128, 1024], F32)
    T1 = P.sb("rT1", [128, 1024], F32)
    KM = P.sb("rKM", [128, 1024], F32)
    ss = P.sb("rss", [128, 48], F32)
    TM = [[P.sb("rTM%d_%d" % (k, i), [128, 1024], BF16) for i in range(1)] * 2 for k in range(5)]
    FF = [P.sb("rFF%d" % i, [128, 5, 8, 128], BF16) for i in range(1)] * 2
    SC = [P.sb("rSC%d" % i, [128, 8, 2], F32) for i in range(2)]
    psT = P.ps("rpsT", [128, 4, 128], F32)
    pb = [P.ps("rpb%d" % i, [128, 512], F32) for i in range(2)]
    psTb = P.ps("rpsTb", [128, 8, 128], BF16)
    slots = [P.ps("rsl%d" % i, [128, 4, 128], F32) for i in range(4)]
    St = P.sb("rSt", [128, 8, 64], F32)
    Stb = P.sb("rStb", [128, 8, 64], BF16)
    Y2 = P.sb("rY2", [128, 8, 128], F32)
    P.op("dve", lambda e: e.memset(St[:], 0.0), writes=["rSt%d" % h for h in range(16)])
    P.op("dve", lambda e: e.memset(Stb[:], 0.0), writes=["rStb%d" % h for h in range(16)])
    NL = 4
    lane = []
    for l in range(NL):
        d = {}
        for n in ("Nab", "NabT", "Ma", "MaT", "Mb", "MbT", "Pm", "Nka", "Mbr", "Mkr", "AX", "WU", "E", "Pp"):
            d[n] = P.sb("rl%d_%s" % (l, n), [128, 128], BF16)
        for n in ("Yl", "Qp", "AXf"):
            d[n] = P.sb("rl%d_%s" % (l, n), [128, 128], F32)
        d["n"] = 0
        lane.append(d)
    fin = {n: P.sb("rf_" + n, [128, 128], F32) for n in ("YC", "SQ", "SD", "YN", "BN")}
    finb = {n: P.sb("rf_" + n, [128, 128], BF16) for n in ("RK",)}
    YB = [P.sb("rf_YB%d" % i, [128, 128], BF16) for i in range(2)]
    evq = [0]

    def slot(l):
        d = lane[l]
        i = d["n"] % 4
        d["n"] += 1
        return slots[l][:, i, :], "rsl%d" % l

    def ev_eng():
        evq[0] += 1
        return "dve" if evq[0] % 3 else "act"

    def mm(out, okey, lhsT, rhs, reads, start=True, stop=True):
        P.op("pe", lambda e: e.matmul(out, lhsT, rhs, start=start, stop=stop, skip_group_check=True), reads=reads, writes=[okey])

    _lo, _hi = (int(v) for v in os.environ.get('RCHUNKS', '0,32').split(','))
    for c in range(_lo, _hi):
        own = c >= 16
        t0 = c * 128
        cb = c % 2
        F, S_, Lt = FF[cb], SC[cb], LIT[cb]
        tmA, tmR, tmB, tmK, tmV = (TM[k][cb] for k in range(5))
        kA, kR, kB, kK, kV = ("rTM%d_0" % k for k in range(5))
        kF, kS, kL = "rFF0", "rSC%d" % cb, "rLIT%d" % cb
        rd = ["prw:%d:%d" % (c, b) for b in range(7)] + (["prw:%d:%d" % (c - 1, b) for b in range(7)] if c else ["prw"])
        P.dma("sp", P1[:], prw.ap()[1 + t0:1 + t0 + 128, :], reads=rd, writes=["rP1", "rE1", "rE2", "rE3"])
        P.dma("sp", P0[:], prw.ap()[t0:t0 + 128, :], reads=rd, writes=["rP0"])
        P.op("dve", lambda e: e.tensor_tensor(P0[:], P0[:], P1[:], ALU.subtract), reads=["rP0", "rP1"], writes=["rP0"])
        P.op("pool", lambda e: e.tensor_tensor(P0[:], P0[:], mu_b[:], ALU.mult), reads=["rP0", "mu_b"], writes=["rP0"])
        P.op("dve", lambda e: e.tensor_tensor(P0[:], P0[:], P1[:], ALU.add), reads=["rP0", "rP1"], writes=["rP0", "rE1", "rE2", "rE3"])
        Z = P0
        zr, zk, zv = Z[:, 0:1024], Z[:, 1024:2048], Z[:, 2048:3072]
        P.op("act", lambda e: e.activation(LI[:, 0:96], Z[:, 3072:3168], AF.Tanh), reads=["rP0"], writes=["rLI"])
        P.op("act", lambda e: e.copy(LI[:, 128:224], Z[:, 3168:3264]), reads=["rP0"], writes=["rLI"])
        P.op("act", lambda e: e.activation(LI[:, 256:512], Z[:, 3264:3520], AF.Sigmoid), reads=["rP0"], writes=["rLI"])
        for j_ in range(4):
            P.op("pe", lambda e, j_=j_: e.transpose(psT[:, j_, :], LI[:, j_ * 128:(j_ + 1) * 128], ident[:]), reads=["rLI", "ident"], writes=["rpsT"])
        P.op("act", lambda e, Lt=Lt: e.copy(Lt[:], psT[:]), reads=["rpsT"], writes=[kL])
        for hf in range(2):
            mm(pb[hf][:], "rpb%d" % hf, Lt[:, 0, :], wup[:, hf * 512:(hf + 1) * 512], [kL, "wup"])
            P.op("dve", lambda e, hf=hf: e.tensor_tensor(U[:, hf * 512:(hf + 1) * 512], pb[hf][:], w0_b[:, hf * 512:(hf + 1) * 512], ALU.add),
                 reads=["rpb%d" % hf, "w0_b"], writes=["rU"])
        P.op("act", lambda e: e.activation(U[:], U[:], AF.Sigmoid), reads=["rU"], writes=["rU"])
        P.op("dve", lambda e: e.tensor_scalar(U[:], U[:], -0.6065306597126334, None, ALU.mult), reads=["rU"], writes=["rU"])
        for hf in range(2):
            mm(pb[hf][:], "rpb%d" % hf, Lt[:, 1, :], aup[:, hf * 512:(hf + 1) * 512], [kL, "aup"])
            P.op("dve", lambda e, hf=hf: e.tensor_tensor(AS[:, hf * 512:(hf + 1) * 512], pb[hf][:], a0_b[:, hf * 512:(hf + 1) * 512], ALU.add),
                 reads=["rpb%d" % hf, "a0_b"], writes=["rAS"])
        P.op("act", lambda e: e.activation(AS[:], AS[:], AF.Sigmoid), reads=["rAS"], writes=["rAS"])
        for hf in range(2):
            hs_ = slice(hf * 512, (hf + 1) * 512)
            mm(pb[hf][:], "rpb%d" % hf, cmat[:], U[:, hs_], ["cmat", "rU"])
            P.op("act", lambda e, hf=hf, hs_=hs_: e.activation(E1[:, hs_], pb[hf][:], AF.Exp), reads=["rpb%d" % hf], writes=["rE1"])
            P.op("act", lambda e, hf=hf, hs_=hs_: e.activation(E2[:, hs_], pb[hf][:], AF.Exp, scale=-1.0), reads=["rpb%d" % hf], writes=["rE2"])
            P.op("dve", lambda e, hf=hf, hs_=hs_: e.tensor_tensor(E3[:, hs_], pb[hf][:], U[:, hs_], ALU.subtract), reads=["rpb%d" % hf, "rU"], writes=["rE3"])
        P.op("act", lambda e: e.activation(E3, E3, AF.Exp), reads=["rE3"], writes=["rE3"])
        for hp in range(8):
            P.op("pe", lambda e, hp=hp: e.matmul(psT[:, 0, 2 * hp:2 * hp + 2], U[:, hp * 128:(hp + 1) * 128], msk2[:], start=True, stop=True, skip_group_check=True),
                 reads=["rU", "msk2", kL], writes=["rpsT"])
        P.op("act", lambda e, S_=S_: e.activation(S_[:].rearrange("p h t -> p (h t)"), psT[:, 0, 0:16], AF.Exp), reads=["rpsT"], writes=[kS])
        P.op("pool", lambda e: e.tensor_tensor(KK[:], zk, kk_b[:], ALU.mult), reads=["rP0", "kk_b"], writes=["rKK"])
        P.op("pool", lambda e: e.tensor_tensor(T1[:], KK[:], KK[:], ALU.mult), reads=["rKK"], writes=["rT1"])
        P.op("dve", lambda e: e.tensor_reduce(ss[:, 0:16], T1[:].rearrange("p (h d) -> p h d", d=64), AX.X, ALU.add), reads=["rT1"], writes=["rss"])
        P.op("act", lambda e: e.activation(ss[:, 0:16], ss[:, 0:16], AF.Sqrt), reads=["rss"], writes=["rss"])
        P.op("dve", lambda e: e.tensor_scalar(ss[:, 0:16], ss[:, 0:16], 1e-12, None, ALU.max), reads=["rss"], writes=["rss"])
        P.op("dve", lambda e: e.reciprocal(ss[:, 16:32], ss[:, 0:16]), reads=["rss"], writes=["rss"])
        P.op("dve", lambda e: e.tensor_tensor(KK[:].rearrange("p (h d) -> p h d", d=64), KK[:].rearrange("p (h d) -> p h d", d=64),
                                              AP(ss, 16, [[48, 128], [1, 16], [0, 64]]), ALU.mult), reads=["rKK", "rss"], writes=["rKK"])
        P.op("dve", lambda e: e.scalar_tensor_tensor(out=T1[:], in0=AS[:], scalar=-1.0, in1=ka_b[:], op0=ALU.add, op1=ALU.mult),
             reads=["rAS", "ka_b"], writes=["rT1"])
        P.op("dve", lambda e: e.scalar_tensor_tensor(out=KM[:], in0=T1[:], scalar=1.0, in1=zk, op0=ALU.add, op1=ALU.mult),
             reads=["rT1", "rP0"], writes=["rKM"])
        P.op("dve", lambda e, o=tmA: e.scalar_tensor_tensor(out=o[:], in0=KK[:], scalar=-1.0, in1=E3, op0=ALU.mult, op1=ALU.mult),
             reads=["rKK", "rE3"], writes=[kA])
        P.op("pool", lambda e: e.tensor_tensor(T1[:], KK[:], AS[:], ALU.mult), reads=["rKK", "rAS"], writes=["rT1"])
        P.op("dve", lambda e, o=tmB: e.tensor_tensor(o[:], T1[:], E2, ALU.mult), reads=["rT1", "rE2"], writes=[kB])
        P.op("pool", lambda e, o=tmR: e.tensor_tensor(o[:], zr, E1, ALU.mult), reads=["rP0", "rE1"], writes=[kR])
        P.op("dve", lambda e, o=tmK: e.tensor_tensor(o[:], KM[:], E2, ALU.mult), reads=["rKM", "rE2"], writes=[kK])
        P.op("act", lambda e, o=tmV: e.copy(o[:], zv), reads=["rP0"], writes=[kV])
        for kind, (tm, kk_) in enumerate(((tmA, kA), (tmR, kR), (tmB, kB), (tmK, kK), (tmV, kV))):
            if kind == 1 and not own:
                continue
            if kind == 4 and not own:
                continue
            for hp in range(8):
                P.op("pe", lambda e, tm=tm, hp=hp: e.transpose(psTb[:, hp, :], tm[:, hp * 128:(hp + 1) * 128], identb[:]),
                     reads=[kk_, "identb"], writes=["rpsTb"], signal=(hp == 7))
            eng = "act" if kind % 2 else "dve"
            if eng == "act":
                P.op("act", lambda e, F=F, kind=kind: e.copy(F[:, kind, :, :], psTb[:]), reads=["rpsTb"], writes=[kF + "_%d" % kind])
            else:
                P.op("dve", lambda e, F=F, kind=kind: e.tensor_copy(F[:, kind, :, :], psTb[:]), reads=["rpsTb"], writes=[kF + "_%d" % kind])

        def head_gen(h, l, S_=S_, own=own, kS=kS):
            d = lane[l]
            par, hp = h % 2, h // 2
            eo = par * 64
            hs = slice(h * 64, (h + 1) * 64)
            es = slice(eo, eo + 64)
            kn = lambda n: "rl%d_%s" % (l, n)
            Af, Rf, Bf, Kf = (F[es, k, hp, :] for k in range(4))
            fk = [kF + "_%d" % k for k in range(5)]

            def evac(dst, dkey, src, skey, mask=None, mkey=None):
                eng = ev_eng() if mask is None else "dve"
                if mask is not None:
                    P.op("dve", lambda e: e.tensor_tensor(dst, src, mask, ALU.mult), reads=[skey, mkey], writes=[dkey])
                elif eng == "act":
                    P.op("act", lambda e: e.copy(dst, src), reads=[skey], writes=[dkey])
                else:
                    P.op("dve", lambda e: e.tensor_copy(dst, src), reads=[skey], writes=[dkey])

            o, ok = slot(l)
            mm(o, ok, Bf, Af, [fk[2], fk[0]])
            evac(d["Nab"][:], kn("Nab"), o, ok, TS[:], "TSb")
            o, ok = slot(l)
            mm(o, ok, Af, Bf, [fk[0], fk[2]])
            evac(d["NabT"][:], kn("NabT"), o, ok, TSL[:], "TSLb")
            P.op("pool", lambda e: e.tensor_tensor(d["Pm"][:], d["Nab"][:], identb[:], ALU.add), reads=[kn("Nab"), "identb"], writes=[kn("Pm")])
            yield
            if RSTOP < 2:
                return
            o, ok = slot(l)
            mm(o, ok, Kf, Af, [fk[3], fk[0]])
            evac(d["Nka"][:], kn("Nka"), o, ok, TS[:], "TSb")
            if own:
                o, ok = slot(l)
                mm(o, ok, Bf, Rf, [fk[2], fk[1]])
                evac(d["Mbr"][:], kn("Mbr"), o, ok, TI[:], "TIb")
                o, ok = slot(l)
                mm(o, ok, Kf, Rf, [fk[3], fk[1]])
                evac(d["Mkr"][:], kn("Mkr"), o, ok, TI[:], "TIb")
            yield
            if RSTOP < 3:
                return
            M, MT, kM, kMT = d["Nab"], d["NabT"], kn("Nab"), kn("NabT")
            for lev in range(1, 7):
                nM, nMT = (d["Ma"], d["MaT"]) if lev % 2 else (d["Mb"], d["MbT"])
                knM, knMT = (kn("Ma"), kn("MaT")) if lev % 2 else (kn("Mb"), kn("MbT"))
                if lev < 6:
                    o, ok = slot(l)
                    mm(o, ok, MT[:], M[:], [kMT, kM])
                    evac(nM[:], knM, o, ok)
                o, ok = slot(l)
                mm(o, ok, M[:], MT[:], [kM, kMT])
                evac(nMT[:], knMT, o, ok)
                yield
                o, ok = slot(l)
                mm(o, ok, nMT[:], d["Pm"][:], [knMT, kn("Pm")])
                P.op("dve", lambda e, o=o: e.tensor_tensor(d["Pm"][:], o, d["Pm"][:], ALU.add), reads=[ok, kn("Pm")], writes=[kn("Pm")])
                M, MT, kM, kMT = nM, nMT, knM, knMT
                yield
            if RSTOP < 4:
                return
            o, ok = slot(l)
            mm(o[:, 0:64], ok, d["Nka"][:], tmV[:, hs], [kn("Nka"), kV])
            evac(d["AX"][:, 64:128], kn("AX"), o[:, 0:64], ok)
            P.op("pool", lambda e: e.tensor_copy(d["AX"][:, 0:64], tmA[:, hs]), reads=[kA], writes=[kn("AX")])
            yield
            o, ok = slot(l)
            mm(o, ok, d["Pm"][:], d["AX"][:], [kn("Pm"), kn("AX")])
            evac(d["WU"][:], kn("WU"), o, ok)
            yield
            WT, UT = d["WU"][:, 0:64], d["WU"][:, 64:128]
            if RSTOP < 5:
                return
            o, ok = slot(l)
            mm(o[es, 0:64], ok, WT, tmB[:, hs], [kn("WU"), kB])
            P.op("dve", lambda e, o=o: e.tensor_tensor(d["Qp"][es, 64:128], o[es, 0:64], ident[es, es], ALU.add), reads=[ok, "ident"], writes=[kn("Qp") + "t"])
            P.op("dve", lambda e: e.tensor_scalar(d["Pp"][es, 0:64], d["Qp"][es, 64:128], S_[es, hp, 0:1], None, ALU.mult),
                 reads=[kn("Qp") + "t", kS], writes=[kn("Pp")])
            o, ok = slot(l)
            mm(o[es, 0:64], ok, tmB[:, hs], UT, [kB, kn("WU")], start=True, stop=False)
            mm(o[es, 0:64], ok, tmK[:, hs], tmV[:, hs], [kK, kV], start=False, stop=True)
            P.op("dve", lambda e, o=o: e.tensor_scalar(d["Qp"][es, 0:64], o[es, 0:64], S_[es, hp, 1:2], None, ALU.mult),
                 reads=[ok, kS], writes=[kn("Qp")])
            if own:
                o, ok = slot(l)
                mm(o[es, :], ok, WT, d["Mbr"][:], [kn("WU"), kn("Mbr")])
                P.op("dve", lambda e, o=o: e.tensor_tensor(d["Yl"][es, :], o[es, :], Rf, ALU.add), reads=[ok, fk[1]], writes=[kn("Yl") + "e"])
                P.op("dve", lambda e: e.tensor_scalar(d["E"][es, :], d["Yl"][es, :], S_[es, hp, 0:1], None, ALU.mult),
                     reads=[kn("Yl") + "e", kS], writes=[kn("E")])
                o, ok = slot(l)
                mm(o[es, :], ok, UT, d["Mbr"][:], [kn("WU"), kn("Mbr")], start=True, stop=False)
                mm(o[es, :], ok, tmV[:, hs], d["Mkr"][:], [kV, kn("Mkr")], start=False, stop=True)
                P.op("act", lambda e, o=o: e.copy(d["AXf"][es, :], o[es, :]), reads=[ok], writes=[kn("AXf")])
            yield
            if RSTOP < 6:
                return
            if own:
                o, ok = slot(l)
                mm(o[es, :], ok, Stb[es, hp, :], d["E"][es, :], ["rStb%d" % h, kn("E")])
                P.op("dve", lambda e, o=o: e.tensor_tensor(Y2[es, hp, :], o[es, :], d["AXf"][es, :], ALU.add), reads=[ok, kn("AXf")], writes=["rY2_%d" % h])
            o, ok = slot(l)
            mm(o[es, 0:64], ok, d["Pp"][es, 0:64], Stb[es, hp, :], [kn("Pp"), "rStb%d" % h])
            P.op("dve", lambda e, o=o: e.scalar_tensor_tensor(out=St[es, hp, :], in0=o[es, 0:64], scalar=S_[es, hp, 1:2], in1=d["Qp"][es, 0:64],
                                                             op0=ALU.mult, op1=ALU.add), reads=[ok, kS, kn("Qp")], writes=["rSt%d" % h])
            P.op("act", lambda e: e.copy(Stb[es, hp, :], St[es, hp, :]), reads=["rSt%d" % h], writes=["rStb%d" % h])
            yield

        for grp in range(4 if RSTOP >= 1 else 0):
            gens = [head_gen(grp * NL + l, l) for l in range(NL)]
            alive = list(gens)
            while alive:
                nxt = []
                for g in alive:
                    try:
                        next(g)
                        nxt.append(g)
                    except StopIteration:
                        pass
                alive = nxt
        if own and RSTOP >= 7:
            for hp in range(8):
                l = hp % NL
                yk = ["rY2_%d" % (2 * hp), "rY2_%d" % (2 * hp + 1)]
                o, ok = slot(l)
                mm(o, ok, bd64[:], Y2[:, hp, :], ["bd64"] + yk)
                P.op("dve", lambda e, o=o, hp=hp: e.tensor_tensor(fin["YC"][:], Y2[:, hp, :], o, ALU.subtract), reads=yk + [ok], writes=["rfYC"])
                P.op("act", lambda e: e.activation(fin["SQ"][:], fin["YC"][:], AF.Square), reads=["rfYC"], writes=["rfSQ"])
                o, ok = slot(l)
                mm(o, ok, bd64[:], fin["SQ"][:], ["bd64", "rfSQ"])
                P.op("act", lambda e, o=o: e.activation(fin["SD"][:], o, AF.Sqrt, bias=EPSB[0][:, 2:3], scale=1.0), reads=[ok, "epsb"], writes=["rfSD"])
                P.op("dve", lambda e: e.reciprocal(fin["SD"][:], fin["SD"][:]), reads=["rfSD"], writes=["rfSD"])
                P.op("dve", lambda e: e.tensor_tensor(fin["YN"][:], fin["YC"][:], fin["SD"][:], ALU.mult), reads=["rfYC", "rfSD"], writes=["rfYN"])
                P.op("dve", lambda e, hp=hp: e.tensor_scalar(fin["YN"][:], fin["YN"][:], lg_c[:, hp:hp + 1], lb_c[:, hp:hp + 1], ALU.mult, ALU.add),
                     reads=["rfYN", "lg_c", "lb_c"], writes=["rfYN"])
                P.op("dve", lambda e, hp=hp: e.scalar_tensor_tensor(out=finb["RK"][:], in0=F[:, 1, hp, :], scalar=rk_c[:, hp:hp + 1], in1=F[:, 3, hp, :],
                                                                    op0=ALU.mult, op1=ALU.mult), reads=[kF + "_1", kF + "_3", "rk_c"], writes=["rfRK"])
                o, ok = slot(l)
                mm(o, ok, bd1[:], finb["RK"][:], ["bd1b", "rfRK"])
                P.op("dve", lambda e, o=o, hp=hp: e.tensor_tensor(fin["BN"][:], o, F[:, 4, hp, :], ALU.mult), reads=[ok, kF + "_4"], writes=["rfBN"])
                P.op("dve", lambda e: e.tensor_tensor(fin["YN"][:], fin["YN"][:], fin["BN"][:], ALU.add), reads=["rfYN", "rfBN"], writes=["rfYN"])
                o, ok = slot(l)
                mm(o, ok, gup[:, 0, hp * 128:(hp + 1) * 128], Lt[:, 2, :], ["gup", kL], start=True, stop=False)
                mm(o, ok, gup[:, 1, hp * 128:(hp + 1) * 128], Lt[:, 3, :], ["gup", kL], start=False, stop=True)
                yb = YB[hp % 2]
                P.op("dve", lambda e, o=o, yb=yb: e.tensor_tensor(yb[:], fin["YN"][:], o, ALU.mult), reads=["rfYN", ok], writes=["rfYB%d" % (hp % 2)])
                q0 = (c - 16) * 128
                P.dma("sp", ybT_s.ap()[hp * 128:(hp + 1) * 128, q0:q0 + 128], yb[:], reads=["rfYB%d" % (hp % 2)], writes=["ybT:%d:%d" % (c - 16, hp)])


def stage_M(P, nc, din, identb, xs_d, y_d, x1_s, slot_s, gate_s, out_d):
    wg = [din("wg_%d" % e, [D, 1024]) for e in range(32)]
    wu = [din("wu_%d" % e, [D, 1024]) for e in range(32)]
    wd = [din("wd_%d" % e, [1024, D]) for e in range(32)]
    NS = CAP // 128
    xs = [P.sb("mxs%d" % i, [128, D], BF16) for i in range(2 * NS)]
    xT = P.sb("mxT", [128, 16, CAP], BF16)
    NST = 4
    wst = [P.sb("mwst%d" % i, [128, 16, 256], F32) for i in range(NST)]
    wgb = [P.sb("mwgb%d" % i, [128, 16, 256], BF16) for i in range(2)]
    wub = [P.sb("mwub%d" % i, [128, 16, 256], BF16) for i in range(2)]
    wdb = [P.sb("mwdb%d" % i, [128, 8, 512], BF16) for i in range(2)]
    hT = P.sb("mhT", [128, 8, CAP], BF16)
    sl = P.sb("msl", [128, CAP], F32)
    yo = [P.sb("myo%d" % i, [128, 512], F32) for i in range(2)]
    psTb = P.ps("mpsTb", [128, 8, 128], BF16)
    psG = [P.ps("mpsG%d" % i, [128, 512], F32) for i in range(2)]
    psU = [P.ps("mpsU%d" % i, [128, 512], F32) for i in range(2)]
    psY = [P.ps("mpsY%d" % i, [128, 512], F32) for i in range(2)]
    zr = P.sb("mzr", [1, D], F32)
    P.op("dve", lambda e: e.memset(zr[:], 0.0), writes=["mzr"])
    P.dma("sp", y_d.ap()[NSLOT:NSLOT + 1, :], zr[:], reads=["mzr"], writes=["y_dump"])
    ncast = [0]
    nst = [0]

    def cast(dst, src, rk, wk):
        ncast[0] += 1
        eng = ("dve", "pool", "act")[ncast[0] % 3]
        if eng == "act":
            P.op("act", lambda e: e.copy(dst, src), reads=[rk], writes=[wk])
        else:
            P.op(eng, lambda e: e.tensor_copy(dst, src), reads=[rk], writes=[wk])

    items = []
    for ex in range(32):
        items += [("gu", ex, cb) for cb in range(4)] + [("dn", ex, cb) for cb in range(4)]

    def issue_w(i):
        kind, ex, cb = items[i]
        bb = i % 2
        if kind == "gu":
            for (wsrc, wdst, nm) in ((wg[ex], wgb[bb], "mwgb%d" % bb), (wu[ex], wub[bb], "mwub%d" % bb)):
                st_i = nst[0] % NST
                nst[0] += 1
                W = wst[st_i]
                P.dma("sp", W[:], wsrc.ap()[:, cb * 256:(cb + 1) * 256].rearrange("(k p) c -> p k c", p=128), writes=["mwst%d" % st_i])
                cast(wdst[:], W[:], "mwst%d" % st_i, nm)
        else:
            st_i = nst[0] % NST
            nst[0] += 1
            Wv = wst[st_i][:].rearrange("p (a k) c -> p a (k c)", a=8)
            P.dma("sp", Wv, wd[ex].ap()[:, cb * 512:(cb + 1) * 512].rearrange("(k p) c -> p k c", p=128), writes=["mwst%d" % st_i])
            cast(wdb[bb][:], Wv, "mwst%d" % st_i, "mwdb%d" % bb)

    def load_xs(ex):
        for s_ in range(NS):
            xi = (ex * NS + s_) % (2 * NS)
            r0 = ex * CAP + s_ * 128
            P.dma("sp", xs[xi][:], xs_d.ap()[r0:r0 + 128, :], reads=["xs_all"], writes=["mxs%d" % xi])

    load_xs(0)
    issue_w(0)
    xk = ["mxT_%d_%d" % (s_, half) for s_ in range(NS) for half in range(2)]
    hk = ["mhT_%d" % i for i in range(8)]
    for i, (kind, ex, cb) in enumerate(items):
        bb = i % 2
        if i + 1 < len(items):
            issue_w(i + 1)
        if kind == "gu" and cb == 0:
            for s_ in range(NS):
                xi = (ex * NS + s_) % (2 * NS)
                X = xs[xi]
                for half in range(2):
                    for k in range(8):
                        kc = half * 8 + k
                        P.op("pe", lambda e, X=X, k=k, kc=kc: e.transpose(psTb[:, k, :], X[:, kc * 128:(kc + 1) * 128], identb[:]),
                             reads=["mxs%d" % xi, "identb"], writes=["mpsTb"], signal=(k == 7))
                    P.op("dve", lambda e, half=half, s_=s_: e.tensor_copy(xT[:, half * 8:(half + 1) * 8, s_ * 128:(s_ + 1) * 128], psTb[:]),
                         reads=["mpsTb"], writes=["mxT_%d_%d" % (s_, half)])
            if ex + 1 < 32:
                load_xs(ex + 1)
        if kind == "gu":
            for hc in range(2):
                pg, pu = psG[hc], psU[hc]
                for k in range(16):
                    P.op("pe", lambda e, pg=pg, k=k, hc=hc, bb=bb: e.matmul(pg[:, 0:CAP], wgb[bb][:, k, hc * 128:(hc + 1) * 128], xT[:, k, :], start=(k == 0), stop=(k == 15)),
                         reads=["mwgb%d" % bb] + xk, writes=["mpsG%d" % hc], signal=(k == 15))
                for k in range(16):
                    P.op("pe", lambda e, pu=pu, k=k, hc=hc, bb=bb: e.matmul(pu[:, 0:CAP], wub[bb][:, k, hc * 128:(hc + 1) * 128], xT[:, k, :], start=(k == 0), stop=(k == 15)),
                         reads=["mwub%d" % bb] + xk, writes=["mpsU%d" % hc], signal=(k == 15))
                hi = cb * 2 + hc
                P.op("act", lambda e, pg=pg: e.activation(sl[:], pg[:, 0:CAP], AF.Silu), reads=["mpsG%d" % hc], writes=["msl"])
                P.op("dve", lambda e, pu=pu, hi=hi: e.tensor_tensor(hT[:, hi, :], sl[:], pu[:, 0:CAP], ALU.mult), reads=["msl", "mpsU%d" % hc], writes=["mhT_%d" % hi])
        else:
            for s_ in range(NS):
                py = psY[s_ % 2]
                for k in range(8):
                    P.op("pe", lambda e, py=py, k=k, s_=s_, bb=bb: e.matmul(py[:], hT[:, k, s_ * 128:(s_ + 1) * 128], wdb[bb][:, k, :], start=(k == 0), stop=(k == 7)),
                         reads=hk + ["mwdb%d" % bb], writes=["mpsY%d" % (s_ % 2)], signal=(k == 7))
                Y = yo[s_ % 2]
                P.op("act", lambda e, py=py, Y=Y: e.copy(Y[:], py[:]), reads=["mpsY%d" % (s_ % 2)], writes=["myo%d" % (s_ % 2)])
                r0 = ex * CAP + s_ * 128
                P.dma("act", y_d.ap()[r0:r0 + 128, cb * 512:(cb + 1) * 512], Y[:], reads=["myo%d" % (s_ % 2)], writes=["y_all"])


def stage_M2(P, nc, y_d, x1_s, slot_s, gate_s, out_d):
    x1 = [P.sb("mx1_%d" % i, [128, D], F32) for i in range(2)]
    y1 = [P.sb("my1_%d" % i, [128, D], F32) for i in range(2)]
    y2 = [P.sb("my2_%d" % i, [128, D], F32) for i in range(2)]
    si = [P.sb("msi%d" % i, [128, 2], I32) for i in range(2)]
    gt = [P.sb("mgt%d" % i, [128, 2], F32) for i in range(2)]
    for t in range(16):
        b = t % 2
        P.dma("sp", si[b][:], slot_s.ap()[t], writes=["msi%d" % b])
        P.dma("sp", gt[b][:], gate_s.ap()[t], writes=["mgt%d" % b])
        P.dma("sp", x1[b][:], x1_s.ap()[t * 128:(t + 1) * 128, :], writes=["mx1_%d" % b])
        P.idma_gather(y1[b][:], y_d, si[b][:, 0:1], reads=["msi%d" % b], writes=["my1_%d" % b])
        P.idma_gather(y2[b][:], y_d, si[b][:, 1:2], reads=["msi%d" % b], writes=["my2_%d" % b])
        P.op("dve", lambda e, b=b: e.scalar_tensor_tensor(out=x1[b][:], in0=y1[b][:], scalar=gt[b][:, 0:1], in1=x1[b][:], op0=ALU.mult, op1=ALU.add),
             reads=["my1_%d" % b, "mgt%d" % b, "mx1_%d" % b], writes=["mx1_%d" % b])
        P.op("dve", lambda e, b=b: e.scalar_tensor_tensor(out=x1[b][:], in0=y2[b][:], scalar=gt[b][:, 1:2], in1=x1[b][:], op0=ALU.mult, op1=ALU.add),
             reads=["my2_%d" % b, "mgt%d" % b, "mx1_%d" % b], writes=["mx1_%d" % b])
        P.dma("sp", out_d.ap()[t * 128:(t + 1) * 128, :], x1[b][:], reads=["mx1_%d" % b], writes=["out:%d" % t])


CAP = 256
NSLOT = 32 * CAP


def stage_O1(P, nc, din, identb, ya_s, ybT_s, sg_s, mgT_s):
    proj_a = din("proj_a", [1024, D])
    proj_b = din("proj_b", [1024, D])
    PA = P.sb("oPA", [128, 8, D], BF16)
    PB = P.sb("oPB", [128, 8, D], BF16)
    wst = P.sb("owst", [128, 8, 512], F32)
    for wi, (src, dst, nm) in enumerate(((proj_a, PA, "oPA"), (proj_b, PB, "oPB"))):
        for cb in range(4):
            P.dma("sp", wst[:], src.ap()[:, cb * 512:(cb + 1) * 512].rearrange("(k p) c -> p k c", p=128), writes=["owst"])
            eng = "dve" if cb % 2 else "pool"
            P.op(eng, lambda e, dst=dst, cb=cb: e.tensor_copy(dst[:, :, cb * 512:(cb + 1) * 512], wst[:]), reads=["owst"], writes=[nm + "_%d" % cb])
    yat = [P.sb("oya%d" % i, [128, 1024], F32) for i in range(2)]
    yab = P.sb("oyab", [128, 1024], BF16)
    yaT = P.sb("oyaT", [128, 8, 128], BF16)
    ybT = [P.sb("oybT%d" % i, [128, 8, 128], BF16) for i in range(2)]
    sg = [P.sb("osg%d" % i, [128, 4096], BF16) for i in range(2)]
    m1 = P.sb("om1", [128, 512], F32)
    m2 = P.sb("om2", [128, 512], F32)
    MG = P.sb("oMG", [128, D], BF16)
    mgT = [P.sb("omgT%d" % i, [128, 16, 128], BF16) for i in range(2)]
    psTb = P.ps("opsTb", [128, 8, 128], BF16)
    psA = [P.ps("opsA%d" % i, [128, 512], F32) for i in range(2)]
    psB = [P.ps("opsB%d" % i, [128, 512], F32) for i in range(2)]
    for t in range(16):
        b = t % 2
        P.dma("sp", yat[b][:], ya_s.ap()[t * 128:(t + 1) * 128, :], reads=["ya_s:%d:%d" % (t, h) for h in range(8)], writes=["oya%d" % b])
        P.dma("sp", ybT[b][:], ybT_s.ap()[:, t * 128:(t + 1) * 128].rearrange("(k p) t -> p k t", p=128),
              reads=["ybT:%d:%d" % (t, h) for h in range(8)], writes=["oybT%d" % b])
        P.dma("sp", sg[b][:], sg_s.ap()[t * 128:(t + 1) * 128, :], reads=["sg_s:%d:%d" % (t, i) for i in range(8)], writes=["osg%d" % b])
        P.op("act", lambda e, b=b: e.copy(yab[:], yat[b][:]), reads=["oya%d" % b], writes=["oyab"])
        for k in range(8):
            P.op("pe", lambda e, k=k: e.transpose(psTb[:, k, :], yab[:, k * 128:(k + 1) * 128], identb[:]), reads=["oyab", "identb"], writes=["opsTb"], signal=(k == 7))
        P.op("dve", lambda e: e.tensor_copy(yaT[:], psTb[:]), reads=["opsTb"], writes=["oyaT"])
        for cb in range(4):
            cs_ = slice(cb * 512, (cb + 1) * 512)
            pa, pbb = psA[cb % 2], psB[cb % 2]
            ka, kb_ = "opsA%d" % (cb % 2), "opsB%d" % (cb % 2)
            for k in range(8):
                P.op("pe", lambda e, pa=pa, k=k, cs_=cs_: e.matmul(pa[:], yaT[:, k, :], PA[:, k, cs_], start=(k == 0), stop=(k == 7)),
                     reads=["oyaT", "oPA_%d" % cb], writes=[ka], signal=(k == 7))
            for k in range(8):
                P.op("pe", lambda e, pbb=pbb, k=k, cs_=cs_, b=b: e.matmul(pbb[:], ybT[b][:, k, :], PB[:, k, cs_], start=(k == 0), stop=(k == 7)),
                     reads=["oybT%d" % b, "oPB_%d" % cb], writes=[kb_], signal=(k == 7))
            P.op("dve", lambda e, pa=pa, cs_=cs_, b=b: e.tensor_tensor(m1[:], pa[:], sg[b][:, cs_], ALU.mult), reads=[ka, "osg%d" % b], writes=["om1"])
            P.op("dve", lambda e, pbb=pbb, cb=cb, b=b: e.tensor_tensor(m2[:], pbb[:], sg[b][:, 2048 + cb * 512:2048 + (cb + 1) * 512], ALU.mult),
                 reads=[kb_, "osg%d" % b], writes=["om2"])
            P.op("pool", lambda e, cs_=cs_: e.tensor_tensor(MG[:, cs_], m1[:], m2[:], ALU.add), reads=["om1", "om2"], writes=["oMG_%d" % cb])
        for half in range(2):
            for k in range(8):
                kc = half * 8 + k
                P.op("pe", lambda e, k=k, kc=kc: e.transpose(psTb[:, k, :], MG[:, kc * 128:(kc + 1) * 128], identb[:]),
                     reads=["oMG_%d" % (kc // 4), "identb"], writes=["opsTb"], signal=(k == 7))
            P.op("act", lambda e, half=half, b=b: e.copy(mgT[b][:, half * 8:(half + 1) * 8, :], psTb[:]), reads=["opsTb"], writes=["omgT%d_%d" % (b, half)])
        P.dma("sp", mgT_s.ap()[t], mgT[b][:], reads=["omgT%d_0" % b, "omgT%d_1" % b], writes=["mgT_s:%d" % t])


def stage_O2(P, nc, din, ident, xw, mgT_s, x1_s, xs_d, slot_s, gate_s):
    w_out = [din("w_out_%d" % i, [1024, D]) for i in range(2)]
    norm2_g = din("norm2_g", [1, D])
    rw_d = din("router_w", [D, 36])
    rb_d = din("router_b", [1, 36])
    ebase_d = din("ebase_h", [1, 32])
    tsf_d = din("ts_h2", [128, 128])
    WO = P.sb("oWO", [128, 16, D], BF16)
    wst = P.sb("o2wst", [128, 8, 512], F32)
    for i in range(2):
        for cb in range(4):
            P.dma("sp", wst[:], w_out[i].ap()[:, cb * 512:(cb + 1) * 512].rearrange("(k p) c -> p k c", p=128), writes=["o2wst"])
            eng = "dve" if cb % 2 else "pool"
            P.op(eng, lambda e, i=i, cb=cb: e.tensor_copy(WO[:, i * 8:(i + 1) * 8, cb * 512:(cb + 1) * 512], wst[:]), reads=["o2wst"], writes=["oWO_%d_%d" % (i, cb)])
    g2b = P.sb("og2b", [128, D], F32)
    P.dma("sp", g2b[:], AP(norm2_g, 0, [[0, 128], [1, D]]), writes=["og2b"])
    RW = P.sb("oRW", [128, 16, 36], F32)
    P.dma("sp", RW[:], rw_d.ap().rearrange("(k p) c -> p k c", p=128), writes=["oRW"])
    rbb = P.sb("orbb", [128, 36], F32)
    P.dma("sp", rbb[:], AP(rb_d, 0, [[0, 128], [1, 36]]), writes=["orbb"])
    ebase = P.sb("oebase", [128, 32], F32)
    P.dma("sp", ebase[:], AP(ebase_d, 0, [[0, 128], [1, 32]]), writes=["oebase"])
    tsf = P.sb("otsf", [128, 128], F32)
    P.dma("sp", tsf[:], tsf_d.ap(), writes=["otsf"])
    onesf = P.sb("oones", [128, 128], F32)
    P.op("dve", lambda e: e.memset(onesf[:], 1.0), writes=["oones"])
    carry = P.sb("ocarry", [128, 32], F32)
    P.op("dve", lambda e: e.memset(carry[:], 0.0), writes=["ocarry"])
    zxs = P.sb("ozxs", [128, D], BF16)
    P.op("dve", lambda e: e.memset(zxs[:], 0.0), writes=["ozxs"])
    zk_ = []
    for i_ in range(NSLOT // 128):
        P.dma("sp", xs_d.ap()[i_ * 128:(i_ + 1) * 128, :], zxs[:], reads=["ozxs"], writes=["xsz:%d" % i_])
        zk_.append("xsz:%d" % i_)
    P.dma("sp", xs_d.ap()[NSLOT:NSLOT + 1, :], zxs[0:1, :], reads=["ozxs"], writes=["xsz:d"])
    zk_.append("xsz:d")
    P.wait_all("pool", zk_)
    mgT = [P.sb("o2mgT%d" % i, [128, 16, 128], BF16) for i in range(2)]
    xt = [P.sb("o2x%d" % i, [128, D], F32) for i in range(2)]
    X1 = P.sb("oX1", [128, D], F32)
    H2 = P.sb("oH2", [128, D], F32)
    H2b = [P.sb("oH2b%d" % i, [128, D], BF16) for i in range(2)]
    sq = P.sb("o2sq", [128, D], BF16)
    h2T = P.sb("oh2T", [128, 16, 128], F32)
    r = P.sb("or", [128, 256], F32)
    slot_i = [P.sb("oslot%d" % i, [128, 2], I32) for i in range(2)]
    gate2 = [P.sb("ogate%d" % i, [128, 2], F32) for i in range(2)]
    psX = [P.ps("opsX%d" % i, [128, 512], F32) for i in range(2)]
    psT = P.ps("o2psT", [128, 4, 128], F32)
    psR = P.ps("opsR", [128, 512], F32)
    psP = P.ps("opsP", [128, 512], F32)

    def R(a, b_):
        return r[:, a:b_]

    for t in range(16):
        b = t % 2
        P.dma("sp", mgT[b][:], mgT_s.ap()[t], reads=["mgT_s:%d" % t], writes=["o2mgT%d" % b])
        P.dma("sp", xt[b][:], xw.ap()[NTOK + t * 128:NTOK + (t + 1) * 128, :], writes=["o2x%d" % b])
        for cb in range(4):
            cs_ = slice(cb * 512, (cb + 1) * 512)
            px, kx = psX[cb % 2], "opsX%d" % (cb % 2)
            for k in range(16):
                P.op("pe", lambda e, px=px, k=k, cs_=cs_, b=b: e.matmul(px[:], mgT[b][:, k, :], WO[:, k, cs_], start=(k == 0), stop=(k == 15)),
                     reads=["o2mgT%d" % b, "oWO_%d_%d" % (k // 8, cb)], writes=[kx], signal=(k == 15))
            P.op("dve", lambda e, px=px, cs_=cs_, b=b: e.tensor_tensor(X1[:, cs_], px[:], xt[b][:, cs_], ALU.add), reads=[kx, "o2x%d" % b], writes=["oX1_%d" % cb])
        x1k = ["oX1_%d" % cb for cb in range(4)]
        P.dma("sp", x1_s.ap()[t * 128:(t + 1) * 128, :], X1[:], reads=x1k, writes=["x1_s:%d" % t])
        P.op("act", lambda e: e.activation(sq[:], X1[:], AF.Square, accum_out=R(0, 1)), reads=x1k, writes=["o2sq", "or"])
        P.op("act", lambda e: e.activation(R(1, 2), R(0, 1), AF.Sqrt, bias=EPSB[0][:, 0:1], scale=1.0 / D), reads=["or", "epsb"], writes=["or"])
        P.op("dve", lambda e: e.reciprocal(R(2, 3), R(1, 2)), reads=["or"], writes=["or"])
        P.op("dve", lambda e: e.scalar_tensor_tensor(out=H2[:], in0=X1[:], scalar=R(2, 3), in1=g2b[:], op0=ALU.mult, op1=ALU.mult),
             reads=x1k + ["or", "og2b"], writes=["oH2"])
        P.op("act", lambda e, b=b: e.copy(H2b[b][:], H2[:]), reads=["oH2"], writes=["oH2b%d" % b])
        for c4 in range(4):
            for j in range(4):
                kc = c4 * 4 + j
                P.op("pe", lambda e, j=j, kc=kc: e.transpose(psT[:, j, :], H2[:, kc * 128:(kc + 1) * 128], ident[:]), reads=["oH2", "ident"], writes=["o2psT"], signal=(j == 3))
            eng = "act" if c4 % 2 else "dve"
            if eng == "act":
                P.op("act", lambda e, c4=c4: e.copy(h2T[:, c4 * 4:(c4 + 1) * 4, :], psT[:]), reads=["o2psT"], writes=["oh2T_%d" % c4])
            else:
                P.op("dve", lambda e, c4=c4: e.tensor_copy(h2T[:, c4 * 4:(c4 + 1) * 4, :], psT[:]), reads=["o2psT"], writes=["oh2T_%d" % c4])
        for k in range(16):
            P.op("pe", lambda e, k=k: e.matmul(psR[:, 0:36], h2T[:, k, :], RW[:, k, :], start=(k == 0), stop=(k == 15)),
                 reads=["oh2T_%d" % (k // 4), "oRW"], writes=["opsR"], signal=(k == 15))
        P.op("dve", lambda e: e.tensor_tensor(R(8, 12), psR[:, 0:4], rbb[:, 0:4], ALU.add), reads=["opsR", "orbb"], writes=["or"])
        P.op("dve", lambda e: e.tensor_tensor(R(16, 48), psR[:, 4:36], rbb[:, 4:36], ALU.add), reads=["opsR", "orbb"], writes=["or"])
        P.op("dve", lambda e: e.tensor_reduce(R(48, 49), R(8, 12), AX.X, ALU.max), reads=["or"], writes=["or"])
        P.op("dve", lambda e: e.tensor_scalar(R(52, 56), R(8, 12), R(48, 49), None, ALU.is_equal), reads=["or"], writes=["or"])
        P.op("dve", lambda e: e.tensor_scalar(R(56, 60), R(8, 12), R(48, 49), None, ALU.subtract), reads=["or"], writes=["or"])
        P.op("act", lambda e: e.activation(R(56, 60), R(56, 60), AF.Exp, accum_out=R(60, 61)), reads=["or"], writes=["or"])
        P.op("dve", lambda e: e.reciprocal(R(61, 62), R(60, 61)), reads=["or"], writes=["or"])
        P.op("dve", lambda e: e.tensor_scalar(R(64, 72), R(16, 24), R(52, 53), None, ALU.mult), reads=["or"], writes=["or"])
        for g in range(1, 4):
            P.op("dve", lambda e, g=g: e.scalar_tensor_tensor(out=R(64, 72), in0=R(16 + 8 * g, 24 + 8 * g), scalar=R(52 + g, 53 + g), in1=R(64, 72),
                                                              op0=ALU.mult, op1=ALU.add), reads=["or"], writes=["or"])
        P.op("dve", lambda e: e.max(R(72, 80), R(64, 72)), reads=["or"], writes=["or"])
        P.op("dve", lambda e: e.tensor_scalar(R(80, 88), R(64, 72), R(72, 73), None, ALU.is_equal), reads=["or"], writes=["or"])
        P.op("dve", lambda e: e.tensor_scalar(R(88, 96), R(64, 72), R(73, 74), None, ALU.is_equal), reads=["or"], writes=["or"])
        P.op("dve", lambda e: e.tensor_tensor(R(96, 97), R(73, 74), R(72, 73), ALU.subtract), reads=["or"], writes=["or"])
        P.op("act", lambda e: e.activation(R(97, 98), R(96, 97), AF.Exp), reads=["or"], writes=["or"])
        P.op("dve", lambda e: e.tensor_scalar(R(98, 99), R(97, 98), 1.0, None, ALU.add), reads=["or"], writes=["or"])
        P.op("dve", lambda e: e.reciprocal(R(98, 99), R(98, 99)), reads=["or"], writes=["or"])
        P.op("dve", lambda e: e.tensor_tensor(R(99, 100), R(61, 62), R(98, 99), ALU.mult), reads=["or"], writes=["or"])
        P.op("dve", lambda e: e.tensor_tensor(R(100, 101), R(61, 62), R(99, 100), ALU.subtract), reads=["or"], writes=["or"])
        for g in range(4):
            P.op("dve", lambda e, g=g: e.tensor_scalar(R(104 + 8 * g, 112 + 8 * g), R(80, 88), R(52 + g, 53 + g), None, ALU.mult), reads=["or"], writes=["or"])
            P.op("dve", lambda e, g=g: e.tensor_scalar(R(136 + 8 * g, 144 + 8 * g), R(88, 96), R(52 + g, 53 + g), None, ALU.mult), reads=["or"], writes=["or"])
        P.op("dve", lambda e: e.tensor_tensor(R(200, 232), R(104, 136), R(136, 168), ALU.add), reads=["or"], writes=["or"])
        P.op("pe", lambda e: e.matmul(psP[:, 0:32], tsf[:], R(200, 232), start=True, stop=True, skip_group_check=True), reads=["otsf", "or"], writes=["opsP"])
        P.op("pe", lambda e: e.matmul(psP[:, 32:64], onesf[:], R(200, 232), start=True, stop=True, skip_group_check=True), reads=["oones", "or"], writes=["opsP"])
        P.op("dve", lambda e: e.tensor_tensor(R(168, 200), psP[:, 0:32], carry[:], ALU.add), reads=["opsP", "ocarry"], writes=["or"])
        P.op("dve", lambda e: e.tensor_tensor(carry[:], carry[:], psP[:, 32:64], ALU.add), reads=["opsP", "ocarry"], writes=["ocarry"])
        S, Gt = slot_i[b], gate2[b]
        for k, (oh0, gcol) in enumerate(((104, 99), (136, 100))):
            P.op("dve", lambda e, oh0=oh0: e.tensor_tensor(R(200, 232), R(oh0, oh0 + 32), R(168, 200), ALU.mult), reads=["or"], writes=["or"])
            P.op("dve", lambda e: e.tensor_reduce(R(232, 233), R(200, 232), AX.X, ALU.add), reads=["or"], writes=["or"])
            P.op("dve", lambda e, oh0=oh0: e.tensor_tensor(R(200, 232), R(oh0, oh0 + 32), ebase[:], ALU.mult), reads=["or", "oebase"], writes=["or"])
            P.op("dve", lambda e: e.tensor_reduce(R(233, 234), R(200, 232), AX.X, ALU.add), reads=["or"], writes=["or"])
            P.op("dve", lambda e: e.tensor_scalar(R(234, 235), R(232, 233), float(CAP), None, ALU.is_lt), reads=["or"], writes=["or"])
            P.op("dve", lambda e: e.tensor_tensor(R(235, 236), R(232, 233), R(233, 234), ALU.add), reads=["or"], writes=["or"])
            P.op("dve", lambda e: e.tensor_scalar(R(235, 236), R(235, 236), float(NSLOT), None, ALU.subtract), reads=["or"], writes=["or"])
            P.op("dve", lambda e: e.tensor_tensor(R(236, 237), R(235, 236), R(234, 235), ALU.mult), reads=["or"], writes=["or"])
            P.op("dve", lambda e: e.tensor_scalar(R(236, 237), R(236, 237), float(NSLOT), None, ALU.add), reads=["or"], writes=["or"])
            P.op("dve", lambda e, S=S, k=k: e.tensor_copy(S[:, k:k + 1], R(236, 237)), reads=["or"], writes=["oslot%d" % b])
            P.op("dve", lambda e, Gt=Gt, k=k, gcol=gcol: e.tensor_tensor(Gt[:, k:k + 1], R(gcol, gcol + 1), R(234, 235), ALU.mult), reads=["or"], writes=["ogate%d" % b])
        for k in range(2):
            P.idma_scatter(xs_d, S[:, k:k + 1], H2b[b][:], reads=["oslot%d" % b, "oH2b%d" % b], writes=["xs:%d:%d" % (t, k)])
        P.dma("sp", slot_s.ap()[t], S[:], reads=["oslot%d" % b], writes=["slot_s:%d" % t])
        P.dma("sp", gate_s.ap()[t], Gt[:], reads=["ogate%d" % b], writes=["gate_s:%d" % t])
```

```python
import numpy as np
from contextlib import ExitStack
import concourse.bass as bass
import concourse.mybir as mybir
from concourse.bass_utils import run_bass_kernel_spmd

F32 = mybir.dt.float32
F32R = mybir.dt.float32r
BF16 = mybir.dt.bfloat16
I32 = mybir.dt.int32
U32 = mybir.dt.uint32
AF = mybir.ActivationFunctionType
ALU = mybir.AluOpType
AX = mybir.AxisListType

D = 2048
NTOK = 2048
WTOK = 4096
INW = 10688
RWW = 3520
NCORES = 8
import os
RSTOP = int(os.environ.get('RSTOP', '9'))


class Prog:
    ENG = ("pe", "act", "dve", "pool", "sp")

    def __init__(self, nc, es):
        self.nc = nc
        self.es = es
        self.q = {e: [] for e in self.ENG}
        self.sem = {e: es.enter_context(nc.semaphore("c_" + e)) for e in ("pe", "act", "dve", "pool")}
        self.cnt = {e: 0 for e in self.ENG}
        self.NR = 24
        self.ring = {e: [es.enter_context(nc.semaphore("r_%s%d" % (e, i))) for i in range(self.NR)]
                     for e in ("sp", "pool", "act")}
        self.ring_n = {e: 0 for e in ("sp", "pool", "act")}
        self.ring_val = {e: [0] * self.NR for e in ("sp", "pool", "act")}
        self.seen = {e: {} for e in self.ENG}
        self.lastw = {}
        self.reads = {}
        self.cur = es
        self.stack = []

    def sb(self, name, shape, dt):
        return self.cur.enter_context(self.nc.sbuf_tensor(name, list(shape), dt))

    def ps(self, name, shape, dt=F32):
        return self.cur.enter_context(self.nc.psum_tensor(name, list(shape), dt))

    def push(self):
        self.stack.append(self.cur)
        self.cur = ExitStack()

    def pop(self):
        self.barrier()
        self.cur.close()
        self.cur = self.stack.pop()

    def barrier(self):
        for eng in self.ENG:
            for e2 in ("pe", "act", "dve", "pool"):
                if self.cnt[e2]:
                    self._wait(eng, (self.sem[e2], self.cnt[e2], "bar"))
            for qn in self.ring:
                for i in range(self.NR):
                    if self.ring_val[qn][i]:
                        self._wait(eng, (self.ring[qn][i], self.ring_val[qn][i], "dma"))

    def dram(self, name, shape, dt, kind="Internal"):
        return self.nc.dram_tensor(name, list(shape), dt, kind=kind)

    def _wait(self, eng, ev):
        sem, val, src = ev
        if eng == "pe" and src == "pe":
            return
        key = id(sem)
        if self.seen[eng].get(key, 0) >= val:
            return
        self.seen[eng][key] = val
        self.q[eng].append(lambda e, sem=sem, val=val: e.wait_ge(sem, val))

    def _deps(self, eng, reads, writes):
        for k in reads:
            ev = self.lastw.get(k)
            if ev is not None:
                self._wait(eng, ev)
        for k in writes:
            ev = self.lastw.get(k)
            if ev is not None:
                self._wait(eng, ev)
            for ev in self.reads.get(k, ()):
                self._wait(eng, ev)

    def _commit(self, ev, reads, writes):
        for k in writes:
            self.lastw[k] = ev
            self.reads[k] = []
        for k in reads:
            self.reads.setdefault(k, []).append(ev)
            if len(self.reads[k]) > 64:
                self.reads[k] = self.reads[k][-64:]

    def op(self, eng, fn, reads=(), writes=(), signal=True):
        self._deps(eng, reads, writes)
        sem = self.sem[eng]
        if eng == "pe" and not signal:
            self.q[eng].append(lambda e, fn=fn: fn(e))
            self._commit((sem, self.cnt[eng] + 1, eng), reads, writes)
            return
        self.cnt[eng] += 1
        n = self.cnt[eng]
        self.q[eng].append(lambda e, fn=fn, sem=sem: fn(e).then_inc(sem, 1))
        self._commit((sem, n, eng), reads, writes)

    def dma(self, eng, out, in_, reads=(), writes=(), **kw):
        self._deps(eng, reads, writes)
        i = self.ring_n[eng] % self.NR
        self.ring_n[eng] += 1
        sem = self.ring[eng][i]
        prev = self.ring_val[eng][i]
        if prev:
            self._wait(eng, (sem, prev, "dma"))
        val = prev + 16
        self.ring_val[eng][i] = val
        self.q[eng].append(lambda e, out=out, in_=in_, sem=sem, kw=kw: e.dma_start(out=out, in_=in_, **kw).then_inc(sem, 16))
        ev = (sem, val, "dma")
        self._commit(ev, reads, writes)
        return ev

    def _idma(self, mk, reads, writes):
        eng = "pool"
        self._deps(eng, reads, writes)
        i = self.ring_n[eng] % self.NR
        self.ring_n[eng] += 1
        sem = self.ring[eng][i]
        prev = self.ring_val[eng][i]
        if prev:
            self._wait(eng, (sem, prev, "dma"))
        val = prev + 16
        self.ring_val[eng][i] = val
        self.q[eng].append(lambda e, mk=mk, sem=sem: mk(e).then_inc(sem, 16))
        self._commit((sem, val, "dma"), reads, writes)

    def idma_scatter(self, dram, idx, src, reads=(), writes=()):
        nrow = dram.shape[0]
        self._idma(lambda e: e.indirect_dma_start(out=dram[:, :], out_offset=bass.IndirectOffsetOnAxis(ap=idx, axis=0), in_=src,
                                                  in_offset=None), reads, writes)

    def idma_gather(self, dst, dram, idx, reads=(), writes=()):
        nrow = dram.shape[0]
        self._idma(lambda e: e.indirect_dma_start(out=dst, out_offset=None, in_=dram[:, :],
                                                  in_offset=bass.IndirectOffsetOnAxis(ap=idx, axis=0)), reads, writes)

    def wait_all(self, eng, keys):
        for k in keys:
            ev = self.lastw.get(k)
            if ev is not None:
                self._wait(eng, ev)

    def emit(self):
        nc = self.nc
        with nc.Block() as blk:
            @blk.tensor
            def _(e):
                for f in self.q["pe"]:
                    f(e)

            @blk.scalar
            def _(e):
                for f in self.q["act"]:
                    f(e)

            @blk.vector
            def _(e):
                for f in self.q["dve"]:
                    f(e)

            @blk.gpsimd
            def _(e):
                for f in self.q["pool"]:
                    f(e)

            @blk.sync
            def _(e):
                for f in self.q["sp"]:
                    f(e)


def AP(t, off, dims):
    return bass.AP(t, off, [list(d) for d in dims])


def host_consts():
    c = {}
    c["ident_h"] = np.eye(128, dtype=np.float32)
    c["tri_h"] = np.triu(np.ones((128, 128), np.float32))
    c.update(rwkv_consts())
    return c


def build(stages=("A",), debug=False):
    nc = bass.Bass("TRN2", target_bir_lowering=False)
    es = ExitStack()
    P = Prog(nc, es)
    dr = {}

    def din(name, shape, dt=F32):
        dr[name] = nc.dram_tensor(name, list(shape), dt, kind="ExternalInput")
        return dr[name]

    xw = din("xw", [WTOK, D])
    cs = din("cs", [WTOK, 16])
    norm1_g = din("norm1_g", [1, D])
    w_in = [din("win_%d" % i, [D, cw]) for i, (c0, cw, kind, bi) in enumerate(col_blocks())]
    q_norm_g = din("q_norm_g", [1, 64])
    k_norm_g = din("k_norm_g", [1, 64])
    ident_d = din("ident_h", [128, 128])
    qT_s = P.dram("qT_s", [8, 128, NTOK], BF16)
    kT_s = P.dram("kT_s", [8, 128, WTOK], BF16)
    v_s = P.dram("v_s", [WTOK, 1024], BF16)
    prw = P.dram("prw", [WTOK + 1, RWW], F32)
    sg_s = P.dram("sg_s", [NTOK, 4096], BF16)
    outs = {}
    ya_s = P.dram("ya_s", [NTOK, 1024], F32)
    ybT_s = P.dram("ybT_s", [1024, NTOK], BF16)
    if debug:
        dbg = debug if isinstance(debug, (list, tuple, set)) else ("qT", "kT", "v", "prw", "sg")
        shp = {"qT": ([8, 128, NTOK], BF16), "kT": ([8, 128, WTOK], BF16), "v": ([WTOK, 1024], BF16),
               "prw": ([WTOK + 1, RWW], F32), "sg": ([NTOK, 4096], BF16), "ya": ([NTOK, 1024], F32), "ybT": ([1024, NTOK], BF16),
               "x1": ([NTOK, D], F32), "slot": ([16, 128, 2], I32), "gate": ([16, 128, 2], F32)}
        for k in dbg:
            outs["dbg_" + k] = nc.dram_tensor("dbg_" + k, shp[k][0], shp[k][1], kind="ExternalOutput")
        qT_s = outs.get("dbg_qT", qT_s); kT_s = outs.get("dbg_kT", kT_s); v_s = outs.get("dbg_v", v_s)
        prw = outs.get("dbg_prw", prw); sg_s = outs.get("dbg_sg", sg_s); ya_s = outs.get("dbg_ya", ya_s)
        ybT_s = outs.get("dbg_ybT", ybT_s)
    if not outs and "M" not in stages:
        outs["dummy_out"] = nc.dram_tensor("dummy_out", [1, 64], F32, kind="ExternalOutput")
    validT = din("validT", [128, 32])
    tri_d = din("tri_h", [128, 128])
    lamv = [din(n, [1, 64]) for n in ("lam_q1", "lam_k1", "lam_q2", "lam_k2")]
    subln_g = din("subln_g", [1, 128])

    ident = P.sb("ident", [128, 128], F32)
    identb = P.sb("identb", [128, 128], BF16)
    epsb = P.sb("epsb", [128, 4], F32)
    EPSB[0] = epsb
    P.op("dve", lambda e: e.memset(epsb[:, 0:1], 1e-6), writes=["epsb"])
    P.op("dve", lambda e: e.memset(epsb[:, 1:2], 1e-5), writes=["epsb"])
    P.op("dve", lambda e: e.memset(epsb[:, 2:3], 64e-5), writes=["epsb"])
    P.op("dve", lambda e: e.memset(epsb[:, 3:4], 1.0), writes=["epsb"])
    P.dma("sp", ident[:], ident_d.ap(), writes=["ident"])
    P.op("dve", lambda e: e.tensor_copy(identb[:], ident[:]), reads=["ident"], writes=["identb"])
    P.push()
    zrow = P.sb("zrow", [1, RWW], F32)
    P.op("dve", lambda e: e.memset(zrow[:], 0.0), writes=["zrow"])
    P.dma("sp", prw.ap()[0:1, :], zrow[:], reads=["zrow"], writes=["prw"])
    P.pop()

    if "A" in stages:
        P.push()
        g1b = P.sb("g1b", [128, D], F32)
        qgb = P.sb("qgb", [128, 512], F32)
        kgb = P.sb("kgb", [128, 512], F32)
        P.dma("sp", g1b[:], AP(norm1_g, 0, [[0, 128], [1, D]]), writes=["g1b"])
        for j in range(8):
            P.dma("sp", qgb[:, j * 64:(j + 1) * 64], AP(q_norm_g, 0, [[0, 128], [1, 64]]), writes=["qgb"])
            P.dma("sp", kgb[:, j * 64:(j + 1) * 64], AP(k_norm_g, 0, [[0, 128], [1, 64]]), writes=["kgb"])
        P.op("dve", lambda e: e.tensor_scalar(qgb[:], qgb[:], 0.125, None, ALU.mult), reads=["qgb"], writes=["qgb"])
        stage_A(P, nc, xw, cs, w_in, ident, identb, g1b, qgb, kgb, qT_s, kT_s, v_s, prw, sg_s)
        P.pop()
    if "B" in stages:
        P.push()
        stage_B(P, nc, qT_s, kT_s, v_s, validT, tri_d, lamv, subln_g, ya_s)
        P.pop()

    if "R" in stages:
        P.push()
        stage_R(P, nc, din, prw, ident, identb, ybT_s)
        P.pop()

    if "O" in stages:
        mgT_s = P.dram("mgT_s", [16, 128, 16, 128], BF16)
        x1_s = outs.get("dbg_x1") or P.dram("x1_s", [NTOK, D], F32)
        xs_d = P.dram("xs_d", [NSLOT + 1, D], BF16)
        slot_s = outs.get("dbg_slot") or P.dram("slot_s", [16, 128, 2], I32)
        gate_s = outs.get("dbg_gate") or P.dram("gate_s", [16, 128, 2], F32)
        P.push()
        stage_O1(P, nc, din, identb, ya_s, ybT_s, sg_s, mgT_s)
        P.pop()
        P.push()
        stage_O2(P, nc, din, ident, xw, mgT_s, x1_s, xs_d, slot_s, gate_s)
        P.pop()
    if "M" in stages:
        y_d = P.dram("y_d", [NSLOT + 1, D], F32)
        out_d = nc.dram_tensor("out", [NTOK, D], F32, kind="ExternalOutput")
        outs["out"] = out_d
        P.push()
        stage_M(P, nc, din, identb, xs_d, y_d, x1_s, slot_s, gate_s, out_d)
        P.pop()
        P.push()
        stage_M2(P, nc, y_d, x1_s, slot_s, gate_s, out_d)
        P.pop()

    P.wait_all("sp", list(P.lastw.keys()))
    P.emit()
    es.close()
    LASTP[0] = P
    return nc, list(outs.keys())


def col_blocks():
    blks = []
    for i in range(2):
        blks.append((i * 512, 512, "q", i))
    for i in range(2):
        blks.append((1024 + i * 512, 512, "k", i))
    for i in range(2):
        blks.append((2048 + i * 512, 512, "v", i))
    for i in range(7):
        w = 512 if i < 6 else RWW - 6 * 512
        blks.append((3072 + i * 512, w, "rw", i))
    for i in range(8):
        blks.append((6592 + i * 512, 512, "g", i))
    return blks


def stage_A(P, nc, xw, cs, w_in, ident, identb, g1b, qgb, kgb, qT_s, kT_s, v_s, prw, sg_s):
    hT = P.sb("hT", [128, 16, 16, 128], BF16)
    xt = [P.sb("xt%d" % i, [128, D], F32) for i in range(2)]
    ht = [P.sb("ht%d" % i, [128, D], F32) for i in range(2)]
    sq = P.sb("sqjunk", [128, D], BF16)
    st = [P.sb("st%d" % i, [128, 4], F32) for i in range(2)]
    wst = P.sb("wst", [128, 16, 512], F32)
    wbf = [P.sb("wbf%d" % i, [128, 16, 512], BF16) for i in range(2)]
    pT = [P.ps("pT%d" % i, [128, 4, 128], F32) for i in range(2)]
    pacc = [P.ps("pacc%d" % i, [128, 512], F32) for i in range(2)]
    pTb = [P.ps("pTb%d" % i, [128, 4, 128], BF16) for i in range(2)]
    ev = [P.sb("ev%d" % i, [128, 512], F32) for i in range(2)]
    evb = [P.sb("evb%d" % i, [128, 512], BF16) for i in range(2)]
    qn = [P.sb("qn%d" % i, [128, 512], F32) for i in range(2)]
    qr = [P.sb("qr%d" % i, [128, 512], BF16) for i in range(2)]
    qs = [P.sb("qs%d" % i, [128, 16], F32) for i in range(2)]
    cst = [P.sb("cst%d" % i, [128, 16], F32) for i in range(2)]
    tmp8 = [P.sb("tmp8_%d" % i, [128, 8, 16], F32) for i in range(2)]
    qTb = [P.sb("qTb%d" % i, [128, 4, 128], BF16) for i in range(2)]
    blks = col_blocks()
    items = [(g_, wblk) + blk for g_ in range(2) for wblk, blk in enumerate(blks) if not (g_ == 0 and blk[2] in ("q", "g"))]

    def issue_w(idx):
        g_, wblk, c0, cw, kind, bi = items[idx]
        wb = wbf[idx % 2]
        wk = "wbf%d" % (idx % 2)
        P.dma("sp", wst[:, :, 0:cw], w_in[wblk].ap().rearrange("(k p) c -> p k c", p=128), writes=["wst"])
        for hh in range(2):
            eng = "pool" if hh == 0 else "dve"
            P.op(eng, lambda e, wb=wb, hh=hh, cw=cw: e.tensor_copy(wb[:, hh * 8:(hh + 1) * 8, 0:cw], wst[:, hh * 8:(hh + 1) * 8, 0:cw]),
                 reads=["wst"], writes=[wk + "_%d" % hh])
    for grp in range(2):
        for t in range(16):
            gt = grp * 16 + t
            b = t % 2
            X, H, S = xt[b], ht[b], st[b]
            P.dma("sp", X[:], xw.ap()[gt * 128:(gt + 1) * 128, :], writes=["xt%d" % b])
            P.op("act", lambda e, X=X, S=S: e.activation(sq[:], X[:], AF.Square, accum_out=S[:, 0:1]),
                 reads=["xt%d" % b], writes=["sq", "st%d" % b])
            P.op("act", lambda e, S=S: e.activation(S[:, 1:2], S[:, 0:1], AF.Sqrt, bias=EPSB[0][:, 0:1], scale=1.0 / D),
                 reads=["st%d" % b, "epsb"], writes=["st%d" % b])
            P.op("dve", lambda e, S=S: e.reciprocal(S[:, 2:3], S[:, 1:2]), reads=["st%d" % b], writes=["st%d" % b])
            P.op("dve", lambda e, X=X, H=H, S=S: e.scalar_tensor_tensor(out=H[:], in0=X[:], scalar=S[:, 2:3], in1=g1b[:],
                                                                      op0=ALU.mult, op1=ALU.mult),
                 reads=["xt%d" % b, "st%d" % b, "g1b"], writes=["ht%d" % b])
            for c4 in range(4):
                pb = c4 % 2
                for j in range(4):
                    kc = c4 * 4 + j
                    P.op("pe", lambda e, H=H, kc=kc, pb=pb, j=j: e.transpose(pT[pb][:, j, :], H[:, kc * 128:(kc + 1) * 128], ident[:]),
                         reads=["ht%d" % b, "ident"], writes=["pT%d" % pb], signal=(j == 3))
                eng = "act" if c4 % 2 == 0 else "dve"
                if eng == "act":
                    P.op("act", lambda e, t=t, c4=c4, pb=pb: e.copy(hT[:, t, c4 * 4:(c4 + 1) * 4, :], pT[pb][:]),
                         reads=["pT%d" % pb], writes=["hT%d_%d" % (t, c4)])
                else:
                    P.op("dve", lambda e, t=t, c4=c4, pb=pb: e.tensor_copy(hT[:, t, c4 * 4:(c4 + 1) * 4, :], pT[pb][:]),
                         reads=["pT%d" % pb], writes=["hT%d_%d" % (t, c4)])
        for wblk, (c0, cw, kind, bi) in enumerate(blks):
            if grp == 0 and kind in ("q", "g"):
                continue
            idx = [i_ for i_, it in enumerate(items) if it[0] == grp and it[1] == wblk][0]
            if idx == 0:
                issue_w(0)
            if idx + 1 < len(items):
                issue_w(idx + 1)
            wb = wbf[idx % 2]
            wk = "wbf%d" % (idx % 2)
            for t in range(16):
                gt = grp * 16 + t
                tok0 = gt * 128
                pa = pacc[t % 2]
                pk = "pacc%d" % (t % 2)
                for kc in range(16):
                    P.op("pe", lambda e, pa=pa, t=t, kc=kc, wb=wb, cw=cw: e.matmul(pa[:, 0:cw], hT[:, t, kc, :], wb[:, kc, 0:cw],
                                                                              start=(kc == 0), stop=(kc == 15)),
                         reads=["hT%d_%d" % (t, kc // 4), wk + "_%d" % (kc // 8)], writes=[pk], signal=(kc == 15))
                b = t % 2
                if kind == "v":
                    P.op("act", lambda e, pa=pa, b=b: e.copy(evb[b][:], pa[:]), reads=[pk], writes=["evb%d" % b])
                    P.dma("act", v_s.ap()[tok0:tok0 + 128, bi * 512:(bi + 1) * 512], evb[b][:], reads=["evb%d" % b], writes=["v_s:%d:%d" % (gt, bi)])
                elif kind == "rw":
                    P.op("act", lambda e, pa=pa, b=b, cw=cw: e.copy(ev[b][:, 0:cw], pa[:, 0:cw]), reads=[pk], writes=["ev%d" % b])
                    P.dma("act", prw.ap()[1 + tok0:1 + tok0 + 128, bi * 512:bi * 512 + cw], ev[b][:, 0:cw], reads=["ev%d" % b], writes=["prw:%d:%d" % (gt, bi)])
                elif kind == "g":
                    P.op("act", lambda e, pa=pa, b=b: e.activation(evb[b][:], pa[:], AF.Sigmoid), reads=[pk], writes=["evb%d" % b])
                    P.dma("act", sg_s.ap()[t * 128:(t + 1) * 128, bi * 512:(bi + 1) * 512], evb[b][:], reads=["evb%d" % b], writes=["sg_s:%d:%d" % (t, bi)])
                else:
                    gb_ = qgb if kind == "q" else kgb
                    QN, QS, QR, CS, T8 = qn[b], qs[b], qr[b], cst[b], tmp8[b]
                    P.dma("pool", CS[:], cs.ap()[tok0:tok0 + 128, :], writes=["cst%d" % b])
                    P.op("act", lambda e, pa=pa, QN=QN: e.activation(QN[:], pa[:], AF.Square), reads=[pk], writes=["qn%d" % b])
                    P.op("dve", lambda e, QN=QN, QS=QS: e.tensor_reduce(QS[:, 0:8], QN[:].rearrange("p (g d) -> p g d", d=64), AX.X, ALU.add),
                         reads=["qn%d" % b], writes=["qs%d" % b])
                    P.op("act", lambda e, QS=QS: e.activation(QS[:, 0:8], QS[:, 0:8], AF.Sqrt, bias=EPSB[0][:, 0:1], scale=1.0 / 64),
                         reads=["qs%d" % b, "epsb"], writes=["qs%d" % b])
                    P.op("dve", lambda e, QS=QS: e.reciprocal(QS[:, 8:16], QS[:, 0:8]), reads=["qs%d" % b], writes=["qs%d" % b])
                    P.op("dve", lambda e, pa=pa, QN=QN, QS=QS: e.tensor_tensor(
                        QN[:].rearrange("p (g d) -> p g d", d=64), pa[:].rearrange("p (g d) -> p g d", d=64),
                        AP(QS, 8, [[16, 128], [1, 8], [0, 64]]), ALU.mult),
                        reads=[pk, "qs%d" % b], writes=["qn%d" % b])
                    P.op("dve", lambda e, QN=QN, gb_=gb_: e.tensor_tensor(QN[:], QN[:], gb_[:], ALU.mult),
                         reads=["qn%d" % b, "qgb", "kgb"], writes=["qn%d" % b])
                    x1 = AP(QN, 0, [[512, 128], [64, 8], [1, 8]])
                    x2 = AP(QN, 8, [[512, 128], [64, 8], [1, 8]])
                    cosb = AP(CS, 0, [[16, 128], [0, 8], [1, 8]])
                    sinb = AP(CS, 8, [[16, 128], [0, 8], [1, 8]])
                    t_a = AP(T8, 0, [[128, 128], [16, 8], [1, 8]])
                    t_b = AP(T8, 8, [[128, 128], [16, 8], [1, 8]])
                    P.op("dve", lambda e, t_a=t_a, x2=x2, sinb=sinb: e.tensor_tensor(t_a, x2, sinb, ALU.mult), reads=["qn%d" % b, "cst%d" % b], writes=["tmp8_%d" % b])
                    P.op("dve", lambda e, t_b=t_b, x1=x1, sinb=sinb: e.tensor_tensor(t_b, x1, sinb, ALU.mult), reads=["qn%d" % b, "cst%d" % b], writes=["tmp8_%d" % b])
                    P.op("dve", lambda e, x1=x1, cosb=cosb: e.tensor_tensor(x1, x1, cosb, ALU.mult), reads=["qn%d" % b, "cst%d" % b], writes=["qn%d" % b])
                    P.op("dve", lambda e, x2=x2, cosb=cosb: e.tensor_tensor(x2, x2, cosb, ALU.mult), reads=["qn%d" % b, "cst%d" % b], writes=["qn%d" % b])
                    P.op("dve", lambda e, x1=x1, t_a=t_a: e.tensor_tensor(x1, x1, t_a, ALU.subtract), reads=["qn%d" % b, "tmp8_%d" % b], writes=["qn%d" % b])
                    P.op("dve", lambda e, x2=x2, t_b=t_b: e.tensor_tensor(x2, x2, t_b, ALU.add), reads=["qn%d" % b, "tmp8_%d" % b], writes=["qn%d" % b])
                    P.op("act", lambda e, QN=QN, QR=QR: e.copy(QR[:], QN[:]), reads=["qn%d" % b], writes=["qr%d" % b])
                    for j in range(4):
                        P.op("pe", lambda e, QR=QR, j=j, b=b: e.transpose(pTb[b][:, j, :], QR[:, j * 128:(j + 1) * 128], identb[:]),
                             reads=["qr%d" % b, "identb"], writes=["pTb%d" % b], signal=(j == 3))
                    P.op("act", lambda e, b=b: e.copy(qTb[b][:], pTb[b][:]), reads=["pTb%d" % b], writes=["qTb%d" % b])
                    if kind == "q":
                        dst = qT_s.ap()[bi * 4:(bi + 1) * 4, :, t * 128:(t + 1) * 128].rearrange("h p t -> p h t")
                        P.dma("act", dst, qTb[b][:], reads=["qTb%d" % b], writes=["qT_s:%d:%d" % (t, bi)])
                    else:
                        dst = kT_s.ap()[bi * 4:(bi + 1) * 4, :, tok0:tok0 + 128].rearrange("h p t -> p h t")
                        P.dma("act", dst, qTb[b][:], reads=["qTb%d" % b], writes=["kT_s:%d:%d" % (gt, bi)])


def stage_B(P, nc, qT_s, kT_s, v_s, validT, tri_d, lamv, subln_g, ya_s):
    LAM_INIT = 0.2
    trif = P.sb("trif", [128, 128], F32)
    tri = P.sb("tri", [128, 128], BF16)
    P.dma("sp", trif[:], tri_d.ap(), writes=["trif"])
    P.op("dve", lambda e: e.tensor_copy(tri[:], trif[:]), reads=["trif"], writes=["tri"])
    validc = P.sb("validc", [128, 32], F32)
    P.dma("sp", validc[:], validT.ap(), writes=["validc"])
    sgb = P.sb("sgb", [128, 128], F32)
    P.dma("sp", sgb[:], AP(subln_g, 0, [[0, 128], [1, 128]]), writes=["sgb"])
    P.op("dve", lambda e: e.tensor_scalar(sgb[:], sgb[:], 1.0 - LAM_INIT, None, ALU.mult), reads=["sgb"], writes=["sgb"])
    lv = P.sb("lv", [1, 4, 64], F32)
    for i in range(4):
        P.dma("sp", lv[:, i, :], lamv[i].ap(), writes=["lv"])
    lp = P.sb("lp", [1, 2, 64], F32)
    ls = P.sb("ls", [1, 8], F32)
    ones1 = P.sb("ones1", [1, 128], F32)
    nlamb = P.sb("nlamb", [128, 1], F32)
    plam = P.ps("plam", [128, 2], F32)
    P.op("dve", lambda e: e.memset(ones1[:], 1.0), writes=["ones1"])
    P.op("dve", lambda e: e.memset(ls[:], 0.0), writes=["ls"])
    P.op("dve", lambda e: e.tensor_tensor(lp[:, 0, :], lv[:, 0, :], lv[:, 1, :], ALU.mult), reads=["lv"], writes=["lp"])
    P.op("dve", lambda e: e.tensor_tensor(lp[:, 1, :], lv[:, 2, :], lv[:, 3, :], ALU.mult), reads=["lv"], writes=["lp"])
    P.op("dve", lambda e: e.tensor_reduce(ls[:, 0:2], lp[:], AX.X, ALU.add), reads=["lp"], writes=["ls"])
    P.op("act", lambda e: e.activation(ls[:, 2:4], ls[:, 0:2], AF.Exp), reads=["ls"], writes=["ls"])
    P.op("dve", lambda e: e.tensor_tensor(ls[:, 4:5], ls[:, 3:4], ls[:, 2:3], ALU.subtract), reads=["ls"], writes=["ls"])
    P.op("dve", lambda e: e.tensor_scalar(ls[:, 6:8], ls[:, 4:6], -LAM_INIT, None, ALU.add), reads=["ls"], writes=["ls"])
    P.op("pe", lambda e: e.matmul(plam[:, 0:2], ones1[:], ls[:, 6:8], start=True, stop=True), reads=["ones1", "ls"], writes=["plam"])
    P.op("dve", lambda e: e.tensor_copy(nlamb[:], plam[:, 0:1]), reads=["plam"], writes=["nlamb"])

    qT = [P.sb("aqT%d" % i, [128, NTOK], BF16) for i in range(2)]
    kT = [P.sb("akT%d" % i, [128, WTOK], BF16) for i in range(2)]
    Vh = [P.sb("aVh%d" % i, [128, 32, 130], BF16) for i in range(2)]
    psS = [P.ps("psS%d" % i, [128, 512], F32) for i in range(2)]
    Oacc = [P.ps("Oacc%d" % i, [128, 2, 130], F32) for i in range(2)]
    PT = [P.sb("aPT%d" % i, [128, 512], BF16) for i in range(3)]
    Oc = [P.sb("aOc%d" % i, [128, 4, 130], F32) for i in range(2)]
    rl = P.sb("arl", [128, 8], F32)
    ssq[0] = P.sb("assq", [128, 4], F32)
    otmp = P.sb("aotmp", [128, 128], F32)
    obuf = P.sb("aobuf", [128, 128], F32)
    osq = P.sb("aosq", [128, 128], BF16)
    yat = [P.sb("ayat%d" % i, [128, 128], F32) for i in range(2)]
    nexp = 0
    nya = 0
    for h in range(8):
        hb = h % 2
        Q, Kt, V = qT[hb], kT[hb], Vh[hb]
        P.dma("sp", Q[:], qT_s.ap()[h], reads=["qT_s:%d:%d" % (t, h // 4) for t in range(16)], writes=["aqT%d" % hb])
        P.dma("sp", Kt[:], kT_s.ap()[h], reads=["kT_s:%d:%d" % (t, h // 4) for t in range(32)], writes=["akT%d" % hb])
        P.dma("sp", V[:, :, 0:128], v_s.ap()[:, h * 128:(h + 1) * 128].rearrange("(kt p) d -> p kt d", p=128),
              reads=["v_s:%d:%d" % (t, h // 4) for t in range(32)], writes=["aVh%d" % hb])
        P.op("dve", lambda e, V=V: e.tensor_copy(V[:, :, 128:129], validc[:].rearrange("p (k o) -> p k o", o=1)),
             reads=["validc"], writes=["aVh%d" % hb])
        for G in range(4):
            for c in range(2):
                nkt = 16 + 4 * G + 4
                for kt in range(nkt):
                    sb_ = kt % 2
                    P.op("pe", lambda e, sb_=sb_, c=c, kt=kt, G=G, Q=Q, Kt=Kt: e.matmul(
                        psS[sb_][:], Kt[c * 64:(c + 1) * 64, kt * 128:(kt + 1) * 128], Q[c * 64:(c + 1) * 64, G * 512:(G + 1) * 512],
                        start=True, stop=True), reads=["aqT%d" % hb, "akT%d" % hb], writes=["psS%d" % sb_])
                    pb = nexp % 3
                    nexp += 1
                    pt = PT[pb]
                    P.op("act", lambda e, pt=pt, sb_=sb_: e.activation(pt[:], psS[sb_][:], AF.Exp), reads=["psS%d" % sb_], writes=["aPT%d" % pb])
                    rel = kt - (16 + 4 * G)
                    for j in range(4):
                        if rel > j:
                            continue
                        if rel == j:
                            P.op("dve", lambda e, pt=pt, j=j: e.tensor_tensor(pt[:, j * 128:(j + 1) * 128], pt[:, j * 128:(j + 1) * 128], tri[:], ALU.mult),
                                 reads=["aPT%d" % pb, "tri"], writes=["aPT%d" % pb])
                        last = (kt == 16 + 4 * G + j)
                        P.op("pe", lambda e, pt=pt, j=j, kt=kt, V=V, last=last: e.matmul(
                            Oacc[j // 2][:, j % 2, 0:129], pt[:, j * 128:(j + 1) * 128], V[:, kt, 0:129],
                            start=(kt == 0 and j % 2 == 0), stop=last, skip_group_check=True),
                            reads=["aPT%d" % pb, "aVh%d" % hb], writes=["Oacc%d" % (j // 2)], signal=last)
                for a in range(2):
                    P.op("act", lambda e, a=a, c=c: e.copy(Oc[c][:, 2 * a:2 * a + 2, :], Oacc[a][:]), reads=["Oacc%d" % a], writes=["aOc%d" % c])
            P.op("dve", lambda e: e.reciprocal(rl[:, 0:4], Oc[0][:, :, 128]), reads=["aOc0"], writes=["arl"])
            P.op("dve", lambda e: e.reciprocal(rl[:, 4:8], Oc[1][:, :, 128]), reads=["aOc1"], writes=["arl"])
            P.op("dve", lambda e: e.tensor_scalar(rl[:, 4:8], rl[:, 4:8], nlamb[:, 0:1], None, ALU.mult), reads=["arl", "nlamb"], writes=["arl"])
            for j in range(4):
                yb_ = nya % 2
                nya += 1
                Y = yat[yb_]
                P.op("dve", lambda e, j=j: e.tensor_scalar(otmp[:], Oc[0][:, j, 0:128], rl[:, j:j + 1], None, ALU.mult), reads=["aOc0", "arl"], writes=["aotmp"])
                P.op("dve", lambda e, j=j: e.scalar_tensor_tensor(out=obuf[:], in0=Oc[1][:, j, 0:128], scalar=rl[:, 4 + j:5 + j], in1=otmp[:],
                                                                  op0=ALU.mult, op1=ALU.add), reads=["aOc1", "arl", "aotmp"], writes=["aobuf"])
                P.op("act", lambda e: e.activation(osq[:], obuf[:], AF.Square, accum_out=ssq[0][:, 0:1]),
                     reads=["aobuf"], writes=["aosq", "assq"])
                P.op("act", lambda e: e.activation(ssq[0][:, 1:2], ssq[0][:, 0:1], AF.Sqrt, bias=EPSB[0][:, 1:2], scale=1.0 / 128), reads=["assq", "epsb"], writes=["assq"])
                P.op("dve", lambda e: e.reciprocal(ssq[0][:, 2:3], ssq[0][:, 1:2]), reads=["assq"], writes=["assq"])
                P.op("dve", lambda e, Y=Y: e.scalar_tensor_tensor(out=Y[:], in0=obuf[:], scalar=ssq[0][:, 2:3], in1=sgb[:], op0=ALU.mult, op1=ALU.mult),
                     reads=["aobuf", "assq", "sgb"], writes=["ayat%d" % yb_])
                qt = 4 * G + j
                P.dma("sp", ya_s.ap()[qt * 128:(qt + 1) * 128, h * 128:(h + 1) * 128], Y[:], reads=["ayat%d" % yb_], writes=["ya_s:%d:%d" % (qt, h)])


ssq = [None]


EPSB = [None]
LASTP = [None]


def rope_table():
    inv_freq = (500000.0 ** (-np.arange(0, 16, 2, dtype=np.float32) / 16)).astype(np.float32)
    ang = np.arange(4096, dtype=np.float32)[:, None] * inv_freq[None, :]
    return np.cos(ang).astype(np.float32), np.sin(ang).astype(np.float32)


def make_in_maps(I, ncores=NCORES, with_experts=True):
    cos, sin = rope_table()
    cst = host_consts()
    maps = []
    for c in range(ncores):
        b, sh = c // 2, c % 2
        x = I["x"][b]
        xwin = np.zeros((WTOK, D), np.float32)
        cswin = np.zeros((WTOK, 16), np.float32)
        if sh == 0:
            xwin[NTOK:] = x[:NTOK]
            cswin[NTOK:, 0:8] = cos[:NTOK]
            cswin[NTOK:, 8:16] = sin[:NTOK]
            cswin[:NTOK, 0:8] = 1.0
        else:
            xwin[:] = x
            cswin[:, 0:8] = cos
            cswin[:, 8:16] = sin
        valid = np.ones((WTOK,), np.float32)
        if sh == 0:
            valid[:NTOK] = 0.0
        m = {"xw": xwin, "cs": cswin, "validT": np.ascontiguousarray(valid.reshape(32, 128).T)}
        for k in ("norm1_g", "q_norm_g", "k_norm_g", "lam_q1", "lam_k1", "lam_q2", "lam_k2", "subln_g"):
            m[k] = np.ascontiguousarray(I[k]).reshape(1, -1)
        for k in ("shift_mu", "w0", "a0", "k_k", "k_a"):
            m[k] = np.ascontiguousarray(I[k]).reshape(1, -1)
        for k in ("r_k", "lnx_g", "lnx_b"):
            m[k + "_c"] = np.ascontiguousarray(I[k].reshape(8, 128).T)
        for k in ("w_up", "a_up", "g_up"):
            m[k] = np.ascontiguousarray(I[k][0])
        m["proj_a"] = np.ascontiguousarray(I["proj_a"][0])
        m["proj_b"] = np.ascontiguousarray(I["proj_b"][0])
        for i in range(2):
            m["w_out_%d" % i] = np.ascontiguousarray(I["w_out"][0][i * 1024:(i + 1) * 1024])
        m["norm2_g"] = np.ascontiguousarray(I["norm2_g"]).reshape(1, -1)
        m["router_w"] = np.ascontiguousarray(np.concatenate([I["router_g"][0], I["router_e"][0]], axis=1))
        m["router_b"] = np.ascontiguousarray(np.concatenate([I["router_g_b"][0], I["router_e_b"][0]]).reshape(1, 36))
        m["ebase_h"] = (np.arange(32, dtype=np.float32) * CAP).reshape(1, 32)
        m["ts_h2"] = cst["ts_h"]
        if with_experts:
            for e in range(32):
                m["wg_%d" % e] = np.ascontiguousarray(I["w_gate_e"][0][e])
                m["wu_%d" % e] = np.ascontiguousarray(I["w_up_e"][0][e])
                m["wd_%d" % e] = np.ascontiguousarray(I["w_down_e"][0][e])
        for i, (c0, cw, kind, bi) in enumerate(col_blocks()):
            m["win_%d" % i] = np.ascontiguousarray(I["w_in"][0][:, c0:c0 + cw])
        m.update(cst)
        maps.append(m)
    return maps


def kernel(**inputs):
    I = {k: np.asarray(v) for k, v in inputs.items()}
    nc, onames = build(stages=("A", "B", "R", "O", "M"), debug=False)
    in_maps = make_in_maps(I, NCORES)
    res = run_bass_kernel_spmd(nc, in_maps, core_ids=list(range(NCORES)))
    out = np.empty((4, 4096, D), np.float32)
    for c in range(NCORES):
        b, sh = c // 2, c % 2
        out[b, sh * NTOK:(sh + 1) * NTOK] = np.asarray(res.results[c]["out"])
    return out


def rwkv_consts():
    s = np.arange(128)[:, None]
    t = np.arange(128)[None, :]
    c = {}
    c["cmat_h"] = ((s <= t).astype(np.float32) - (s <= 63).astype(np.float32))
    m2 = np.zeros((128, 2), np.float32)
    m2[:64, 0] = 1.0
    m2[64:, 1] = 1.0
    c["msk2_h"] = m2
    c["ts_h"] = (s < t).astype(np.float32)
    c["ti_h"] = (s <= t).astype(np.float32)
    c["tsl_h"] = (s > t).astype(np.float32)
    bd = np.zeros((128, 128), np.float32)
    bd[:64, :64] = 1.0
    bd[64:, 64:] = 1.0
    c["bd1_h"] = bd
    return c


def stage_R(P, nc, din, prw, ident, identb, ybT_s):
    shift_mu = din("shift_mu", [1, RWW])
    pv = {n: din(n, [1, 1024]) for n in ("w0", "a0", "k_k", "k_a")}
    colp = {n: din(n + "_c", [128, 8]) for n in ("r_k", "lnx_g", "lnx_b")}
    w_up = din("w_up", [96, 1024])
    a_up = din("a_up", [96, 1024])
    g_up = din("g_up", [256, 1024])
    cd = {n: din(n, [128, 2] if n == "msk2_h" else [128, 128]) for n in ("cmat_h", "msk2_h", "ts_h", "ti_h", "tsl_h", "bd1_h")}

    def ld(name, shape, src, dt=F32):
        t = P.sb(name, shape, dt)
        P.dma("sp", t[:], src, writes=[name])
        return t

    mu_b = ld("mu_b", [128, RWW], AP(shift_mu, 0, [[0, 128], [1, RWW]]))
    w0_b = ld("w0_b", [128, 1024], AP(pv["w0"], 0, [[0, 128], [1, 1024]]))
    a0_b = ld("a0_b", [128, 1024], AP(pv["a0"], 0, [[0, 128], [1, 1024]]))
    kk_b = ld("kk_b", [128, 1024], AP(pv["k_k"], 0, [[0, 128], [1, 1024]]))
    ka_b = ld("ka_b", [128, 1024], AP(pv["k_a"], 0, [[0, 128], [1, 1024]]))
    rk_c = ld("rk_c", [128, 8], colp["r_k"].ap())
    lg_c = ld("lg_c", [128, 8], colp["lnx_g"].ap())
    lb_c = ld("lb_c", [128, 8], colp["lnx_b"].ap())
    wup = P.sb("wup", [128, 1024], F32)
    aup = P.sb("aup", [128, 1024], F32)
    for t_, src_, nm_ in ((wup, w_up, "wup"), (aup, a_up, "aup")):
        P.op("dve", lambda e, t_=t_: e.memset(t_[:], 0.0), writes=[nm_])
        P.dma("sp", t_[0:96, :], src_.ap(), writes=[nm_])
    gup = ld("gup", [128, 2, 1024], g_up.ap().rearrange("(k p) c -> p k c", p=128))
    cmat = ld("cmat", [128, 128], cd["cmat_h"].ap())
    msk2 = ld("msk2", [128, 2], cd["msk2_h"].ap())
    bdf = ld("bdf", [128, 128], cd["bd1_h"].ap())
    tsf = ld("tsf", [128, 128], cd["ts_h"].ap())
    tif = ld("tif", [128, 128], cd["ti_h"].ap())
    tslf = ld("tslf", [128, 128], cd["tsl_h"].ap())
    TS = P.sb("TSb", [128, 128], BF16)
    TI = P.sb("TIb", [128, 128], BF16)
    TSL = P.sb("TSLb", [128, 128], BF16)
    bd1 = P.sb("bd1b", [128, 128], BF16)
    bd64 = P.sb("bd64", [128, 128], F32)
    P.op("dve", lambda e: e.tensor_copy(TS[:], tsf[:]), reads=["tsf"], writes=["TSb"])
    P.op("dve", lambda e: e.tensor_copy(TI[:], tif[:]), reads=["tif"], writes=["TIb"])
    P.op("dve", lambda e: e.tensor_copy(TSL[:], tslf[:]), reads=["tslf"], writes=["TSLb"])
    P.op("dve", lambda e: e.tensor_copy(bd1[:], bdf[:]), reads=["bdf"], writes=["bd1b"])
    P.op("dve", lambda e: e.tensor_scalar(bd64[:], bdf[:], 1.0 / 64, None, ALU.mult), reads=["bdf"], writes=["bd64"])

    P0 = P.sb("rP0", [128, RWW], F32)
    P1 = P.sb("rP1", [128, RWW], F32)
    LI = P.sb("rLI", [128, 512], F32)
    P.op("dve", lambda e: e.memset(LI[:], 0.0), writes=["rLI"])
    LIT = [P.sb("rLIT%d" % i, [128, 4, 128], F32) for i in range(2)]
    U = P.sb("rU", [128, 1024], F32)
    AS = P.sb("rAS", [128, 1024], F32)
    E1, E2, E3 = P1[:, 0:1024], P1[:, 1024:2048], P1[:, 2048:3072]
    KK = P.sb("rKK", [128, 1024], F32)
    T1 = P.sb("rT1", [128, 1024], F32)
    KM = P.sb("rKM", [128, 1024], F32)
    ss = P.sb("rss", [128, 48], F32)
    TM = [[P.sb("rTM%d_%d" % (k, i), [128, 1024], BF16) for i in range(1)] * 2 for k in range(5)]
    FF = [P.sb("rFF%d" % i, [128, 5, 8, 128], BF16) for i in range(1)] * 2
    SC = [P.sb("rSC%d" % i, [128, 8, 2], F32) for i in range(2)]
    slots = [P.ps("rsl%d" % i, [128, 4, 128], F32) for i in range(8)]
    psT = slots[4]
    pb = [slots[5][:].rearrange("p a c -> p (a c)"), slots[6][:].rearrange("p a c -> p (a c)")]
    psTb = slots[7].bitcast(BF16)[:].rearrange("p a (b c) -> p (a b) c", c=128)
    St = P.sb("rSt", [128, 8, 64], F32)
    Stb = P.sb("rStb", [128, 8, 64], BF16)
    Y2 = P.sb("rY2", [128, 8, 128], F32)
    P.op("dve", lambda e: e.memset(St[:], 0.0), writes=["rSt%d" % h for h in range(16)])
    P.op("dve", lambda e: e.memset(Stb[:], 0.0), writes=["rStb%d" % h for h in range(16)])
    NL = 8
    lane = []
    for l in range(NL):
        d = {}
        for n in ("Nab", "NabT", "Ma", "MaT", "Mb", "MbT", "Pm", "Nka", "Mbr", "Mkr", "AX", "WU", "E", "Pp"):
            d[n] = P.sb("rl%d_%s" % (l, n), [128, 128], BF16)
        for n in ("Yl", "Qp", "AXf"):
            d[n] = P.sb("rl%d_%s" % (l, n), [128, 128], F32)
        d["n"] = 0
        lane.append(d)
    fin = {n: P.sb("rf_" + n, [128, 128], F32) for n in ("YC", "SQ", "SD", "YN", "BN")}
    finb = {n: P.sb("rf_" + n, [128, 128], BF16) for n in ("RK",)}
    YB = [P.sb("rf_YB%d" % i, [128, 128], BF16) for i in range(2)]
    evq = [0]

    def slot(l):
        d = lane[l]
        i = d["n"] % 4
        d["n"] += 1
        return slots[l][:, i, :], "rsl%d" % l

    def ev_eng():
        evq[0] += 1
        return "dve" if evq[0] % 3 else "act"

    def mm(out, okey, lhsT, rhs, reads, start=True, stop=True):
        P.op("pe", lambda e: e.matmul(out, lhsT, rhs, start=start, stop=stop, skip_group_check=True), reads=reads, writes=[okey])

    _lo, _hi = (int(v) for v in os.environ.get('RCHUNKS', '0,32').split(','))
    for c in range(_lo, _hi):
        own = c >= 16
        t0 = c * 128
        cb = c % 2
        F, S_, Lt = FF[cb], SC[cb], LIT[cb]
        tmA, tmR, tmB, tmK, tmV = (TM[k][cb] for k in range(5))
        kA, kR, kB, kK, kV = ("rTM%d_0" % k for k in range(5))
        kF, kS, kL = "rFF0", "rSC%d" % cb, "rLIT%d" % cb
        rd = ["prw:%d:%d" % (c, b) for b in range(7)] + (["prw:%d:%d" % (c - 1, b) for b in range(7)] if c else ["prw"])
        P.dma("sp", P1[:], prw.ap()[1 + t0:1 + t0 + 128, :], reads=rd, writes=["rP1", "rE1", "rE2", "rE3"])
        P.dma("sp", P0[:], prw.ap()[t0:t0 + 128, :], reads=rd, writes=["rP0"])
        P.op("dve", lambda e: e.tensor_tensor(P0[:], P0[:], P1[:], ALU.subtract), reads=["rP0", "rP1"], writes=["rP0"])
        P.op("pool", lambda e: e.tensor_tensor(P0[:], P0[:], mu_b[:], ALU.mult), reads=["rP0", "mu_b"], writes=["rP0"])
        P.op("dve", lambda e: e.tensor_tensor(P0[:], P0[:], P1[:], ALU.add), reads=["rP0", "rP1"], writes=["rP0", "rE1", "rE2", "rE3"])
        Z = P0
        zr, zk, zv = Z[:, 0:1024], Z[:, 1024:2048], Z[:, 2048:3072]
        P.op("act", lambda e: e.activation(LI[:, 0:96], Z[:, 3072:3168], AF.Tanh), reads=["rP0"], writes=["rLI"])
        P.op("act", lambda e: e.copy(LI[:, 128:224], Z[:, 3168:3264]), reads=["rP0"], writes=["rLI"])
        P.op("act", lambda e: e.activation(LI[:, 256:512], Z[:, 3264:3520], AF.Sigmoid), reads=["rP0"], writes=["rLI"])
        for j_ in range(4):
            P.op("pe", lambda e, j_=j_: e.transpose(psT[:, j_, :], LI[:, j_ * 128:(j_ + 1) * 128], ident[:]), reads=["rLI", "ident"], writes=["rsl4"])
        P.op("act", lambda e, Lt=Lt: e.copy(Lt[:], psT[:]), reads=["rsl4"], writes=[kL])
        for hf in range(2):
            mm(pb[hf][:], "rsl%d" % (5 + hf), Lt[:, 0, :], wup[:, hf * 512:(hf + 1) * 512], [kL, "wup"])
            P.op("dve", lambda e, hf=hf: e.tensor_tensor(U[:, hf * 512:(hf + 1) * 512], pb[hf][:], w0_b[:, hf * 512:(hf + 1) * 512], ALU.add),
                 reads=["rsl%d" % (5 + hf), "w0_b"], writes=["rU"])
        P.op("act", lambda e: e.activation(U[:], U[:], AF.Sigmoid), reads=["rU"], writes=["rU"])
        P.op("dve", lambda e: e.tensor_scalar(U[:], U[:], -0.6065306597126334, None, ALU.mult), reads=["rU"], writes=["rU"])
        for hf in range(2):
            mm(pb[hf][:], "rsl%d" % (5 + hf), Lt[:, 1, :], aup[:, hf * 512:(hf + 1) * 512], [kL, "aup"])
            P.op("dve", lambda e, hf=hf: e.tensor_tensor(AS[:, hf * 512:(hf + 1) * 512], pb[hf][:], a0_b[:, hf * 512:(hf + 1) * 512], ALU.add),
                 reads=["rsl%d" % (5 + hf), "a0_b"], writes=["rAS"])
        P.op("act", lambda e: e.activation(AS[:], AS[:], AF.Sigmoid), reads=["rAS"], writes=["rAS"])
        for hf in range(2):
            hs_ = slice(hf * 512, (hf + 1) * 512)
            mm(pb[hf][:], "rsl%d" % (5 + hf), cmat[:], U[:, hs_], ["cmat", "rU"])
            P.op("act", lambda e, hf=hf, hs_=hs_: e.activation(E1[:, hs_], pb[hf][:], AF.Exp), reads=["rsl%d" % (5 + hf)], writes=["rE1"])
            P.op("act", lambda e, hf=hf, hs_=hs_: e.activation(E2[:, hs_], pb[hf][:], AF.Exp, scale=-1.0), reads=["rsl%d" % (5 + hf)], writes=["rE2"])
            P.op("dve", lambda e, hf=hf, hs_=hs_: e.tensor_tensor(E3[:, hs_], pb[hf][:], U[:, hs_], ALU.subtract), reads=["rsl%d" % (5 + hf), "rU"], writes=["rE3"])
        P.op("act", lambda e: e.activation(E3, E3, AF.Exp), reads=["rE3"], writes=["rE3"])
        for hp in range(8):
            P.op("pe", lambda e, hp=hp: e.matmul(psT[:, 0, 2 * hp:2 * hp + 2], U[:, hp * 128:(hp + 1) * 128], msk2[:], start=True, stop=True, skip_group_check=True),
                 reads=["rU", "msk2", kL], writes=["rsl4"])
        P.op("act", lambda e, S_=S_: e.activation(S_[:].rearrange("p h t -> p (h t)"), psT[:, 0, 0:16], AF.Exp), reads=["rsl4"], writes=[kS])
        P.op("pool", lambda e: e.tensor_tensor(KK[:], zk, kk_b[:], ALU.mult), reads=["rP0", "kk_b"], writes=["rKK"])
        P.op("pool", lambda e: e.tensor_tensor(T1[:], KK[:], KK[:], ALU.mult), reads=["rKK"], writes=["rT1"])
        P.op("dve", lambda e: e.tensor_reduce(ss[:, 0:16], T1[:].rearrange("p (h d) -> p h d", d=64), AX.X, ALU.add), reads=["rT1"], writes=["rss"])
        P.op("act", lambda e: e.activation(ss[:, 0:16], ss[:, 0:16], AF.Sqrt), reads=["rss"], writes=["rss"])
        P.op("dve", lambda e: e.tensor_scalar(ss[:, 0:16], ss[:, 0:16], 1e-12, None, ALU.max), reads=["rss"], writes=["rss"])
        P.op("dve", lambda e: e.reciprocal(ss[:, 16:32], ss[:, 0:16]), reads=["rss"], writes=["rss"])
        P.op("dve", lambda e: e.tensor_tensor(KK[:].rearrange("p (h d) -> p h d", d=64), KK[:].rearrange("p (h d) -> p h d", d=64),
                                              AP(ss, 16, [[48, 128], [1, 16], [0, 64]]), ALU.mult), reads=["rKK", "rss"], writes=["rKK"])
        P.op("dve", lambda e: e.scalar_tensor_tensor(out=T1[:], in0=AS[:], scalar=-1.0, in1=ka_b[:], op0=ALU.add, op1=ALU.mult),
             reads=["rAS", "ka_b"], writes=["rT1"])
        P.op("dve", lambda e: e.scalar_tensor_tensor(out=KM[:], in0=T1[:], scalar=1.0, in1=zk, op0=ALU.add, op1=ALU.mult),
             reads=["rT1", "rP0"], writes=["rKM"])
        P.op("dve", lambda e, o=tmA: e.scalar_tensor_tensor(out=o[:], in0=KK[:], scalar=-1.0, in1=E3, op0=ALU.mult, op1=ALU.mult),
             reads=["rKK", "rE3"], writes=[kA])
        P.op("pool", lambda e: e.tensor_tensor(T1[:], KK[:], AS[:], ALU.mult), reads=["rKK", "rAS"], writes=["rT1"])
        P.op("dve", lambda e, o=tmB: e.tensor_tensor(o[:], T1[:], E2, ALU.mult), reads=["rT1", "rE2"], writes=[kB])
        P.op("pool", lambda e, o=tmR: e.tensor_tensor(o[:], zr, E1, ALU.mult), reads=["rP0", "rE1"], writes=[kR])
        P.op("dve", lambda e, o=tmK: e.tensor_tensor(o[:], KM[:], E2, ALU.mult), reads=["rKM", "rE2"], writes=[kK])
        P.op("act", lambda e, o=tmV: e.copy(o[:], zv), reads=["rP0"], writes=[kV])
        for kind, (tm, kk_) in enumerate(((tmA, kA), (tmR, kR), (tmB, kB), (tmK, kK), (tmV, kV))):
            if kind == 1 and not own:
                continue
            if kind == 4 and not own:
                continue
            for hp in range(8):
                P.op("pe", lambda e, tm=tm, hp=hp: e.transpose(psTb[:, hp, :], tm[:, hp * 128:(hp + 1) * 128], identb[:]),
                     reads=[kk_, "identb"], writes=["rsl7"], signal=(hp == 7))
            eng = "act" if kind % 2 else "dve"
            if eng == "act":
                P.op("act", lambda e, F=F, kind=kind: e.copy(F[:, kind, :, :], psTb[:]), reads=["rsl7"], writes=[kF + "_%d" % kind])
            else:
                P.op("dve", lambda e, F=F, kind=kind: e.tensor_copy(F[:, kind, :, :], psTb[:]), reads=["rsl7"], writes=[kF + "_%d" % kind])

        def head_gen(h, l, S_=S_, own=own, kS=kS):
            d = lane[l]
            par, hp = h % 2, h // 2
            eo = par * 64
            hs = slice(h * 64, (h + 1) * 64)
            es = slice(eo, eo + 64)
            kn = lambda n: "rl%d_%s" % (l, n)
            Af, Rf, Bf, Kf = (F[es, k, hp, :] for k in range(4))
            fk = [kF + "_%d" % k for k in range(5)]

            def evac(dst, dkey, src, skey, mask=None, mkey=None):
                P.op("act", lambda e: e.copy(dst, src), reads=[skey], writes=[dkey])
                if mask is not None:
                    P.op("pool", lambda e: e.tensor_tensor(dst, dst, mask, ALU.mult), reads=[dkey, mkey], writes=[dkey])

            o, ok = slot(l)
            mm(o, ok, Bf, Af, [fk[2], fk[0]])
            evac(d["Nab"][:], kn("Nab"), o, ok, TS[:], "TSb")
            o, ok = slot(l)
            mm(o, ok, Af, Bf, [fk[0], fk[2]])
            evac(d["NabT"][:], kn("NabT"), o, ok, TSL[:], "TSLb")
            P.op("pool", lambda e: e.tensor_tensor(d["Pm"][:], d["Nab"][:], identb[:], ALU.add), reads=[kn("Nab"), "identb"], writes=[kn("Pm")])
            yield
            if RSTOP < 2:
                return
            o, ok = slot(l)
            mm(o, ok, Kf, Af, [fk[3], fk[0]])
            evac(d["Nka"][:], kn("Nka"), o, ok, TS[:], "TSb")
            if own:
                o, ok = slot(l)
                mm(o, ok, Bf, Rf, [fk[2], fk[1]])
                evac(d["Mbr"][:], kn("Mbr"), o, ok, TI[:], "TIb")
                o, ok = slot(l)
                mm(o, ok, Kf, Rf, [fk[3], fk[1]])
                evac(d["Mkr"][:], kn("Mkr"), o, ok, TI[:], "TIb")
            yield
            if RSTOP < 3:
                return
            M, MT, kM, kMT = d["Nab"], d["NabT"], kn("Nab"), kn("NabT")
            for lev in range(1, 7):
                nM, nMT = (d["Ma"], d["MaT"]) if lev % 2 else (d["Mb"], d["MbT"])
                knM, knMT = (kn("Ma"), kn("MaT")) if lev % 2 else (kn("Mb"), kn("MbT"))
                if lev < 6:
                    o, ok = slot(l)
                    mm(o, ok, MT[:], M[:], [kMT, kM])
                    evac(nM[:], knM, o, ok)
                o, ok = slot(l)
                mm(o, ok, M[:], MT[:], [kM, kMT])
                evac(nMT[:], knMT, o, ok)
                yield
                o, ok = slot(l)
                mm(o, ok, nMT[:], d["Pm"][:], [knMT, kn("Pm")])
                P.op("dve", lambda e, o=o: e.tensor_tensor(d["Pm"][:], o, d["Pm"][:], ALU.add), reads=[ok, kn("Pm")], writes=[kn("Pm")])
                M, MT, kM, kMT = nM, nMT, knM, knMT
                yield
            if RSTOP < 4:
                return
            o, ok = slot(l)
            mm(o[:, 0:64], ok, d["Nka"][:], tmV[:, hs], [kn("Nka"), kV])
            evac(d["AX"][:, 64:128], kn("AX"), o[:, 0:64], ok)
            P.op("pool", lambda e: e.tensor_copy(d["AX"][:, 0:64], tmA[:, hs]), reads=[kA], writes=[kn("AX")])
            yield
            o, ok = slot(l)
            mm(o, ok, d["Pm"][:], d["AX"][:], [kn("Pm"), kn("AX")])
            evac(d["WU"][:], kn("WU"), o, ok)
            yield
            WT, UT = d["WU"][:, 0:64], d["WU"][:, 64:128]
            if RSTOP < 5:
                return
            o, ok = slot(l)
            mm(o[es, 0:64], ok, WT, tmB[:, hs], [kn("WU"), kB])
            P.op("dve", lambda e, o=o: e.tensor_tensor(d["Qp"][es, 64:128], o[es, 0:64], ident[es, es], ALU.add), reads=[ok, "ident"], writes=[kn("Qp") + "t"])
            P.op("dve", lambda e: e.tensor_scalar(d["Pp"][es, 0:64], d["Qp"][es, 64:128], S_[es, hp, 0:1], None, ALU.mult),
                 reads=[kn("Qp") + "t", kS], writes=[kn("Pp")])
            o, ok = slot(l)
            mm(o[es, 0:64], ok, tmB[:, hs], UT, [kB, kn("WU")], start=True, stop=False)
            mm(o[es, 0:64], ok, tmK[:, hs], tmV[:, hs], [kK, kV], start=False, stop=True)
            P.op("dve", lambda e, o=o: e.tensor_scalar(d["Qp"][es, 0:64], o[es, 0:64], S_[es, hp, 1:2], None, ALU.mult),
                 reads=[ok, kS], writes=[kn("Qp")])
            if own:
                o, ok = slot(l)
                mm(o[es, :], ok, WT, d["Mbr"][:], [kn("WU"), kn("Mbr")])
                P.op("dve", lambda e, o=o: e.tensor_tensor(d["Yl"][es, :], o[es, :], Rf, ALU.add), reads=[ok, fk[1]], writes=[kn("Yl") + "e"])
                P.op("dve", lambda e: e.tensor_scalar(d["E"][es, :], d["Yl"][es, :], S_[es, hp, 0:1], None, ALU.mult),
                     reads=[kn("Yl") + "e", kS], writes=[kn("E")])
                o, ok = slot(l)
                mm(o[es, :], ok, UT, d["Mbr"][:], [kn("WU"), kn("Mbr")], start=True, stop=False)
                mm(o[es, :], ok, tmV[:, hs], d["Mkr"][:], [kV, kn("Mkr")], start=False, stop=True)
                P.op("act", lambda e, o=o: e.copy(d["AXf"][es, :], o[es, :]), reads=[ok], writes=[kn("AXf")])
            yield
            if RSTOP < 6:
                return
            if own:
                o, ok = slot(l)
                mm(o[es, :], ok, Stb[es, hp, :], d["E"][es, :], ["rStb%d" % h, kn("E")])
                P.op("dve", lambda e, o=o: e.tensor_tensor(Y2[es, hp, :], o[es, :], d["AXf"][es, :], ALU.add), reads=[ok, kn("AXf")], writes=["rY2_%d" % h])
            o, ok = slot(l)
            mm(o[es, 0:64], ok, d["Pp"][es, 0:64], Stb[es, hp, :], [kn("Pp"), "rStb%d" % h])
            P.op("dve", lambda e, o=o: e.scalar_tensor_tensor(out=St[es, hp, :], in0=o[es, 0:64], scalar=S_[es, hp, 1:2], in1=d["Qp"][es, 0:64],
                                                             op0=ALU.mult, op1=ALU.add), reads=[ok, kS, kn("Qp")], writes=["rSt%d" % h])
            P.op("act", lambda e: e.copy(Stb[es, hp, :], St[es, hp, :]), reads=["rSt%d" % h], writes=["rStb%d" % h])
            yield

        for grp in range(2 if RSTOP >= 1 else 0):
            gens = [head_gen(grp * NL + l, l) for l in range(NL)]
            alive = list(gens)
            while alive:
                nxt = []
                for g in alive:
                    try:
                        next(g)
                        nxt.append(g)
                    except StopIteration:
                        pass
                alive = nxt
        if own and RSTOP >= 7:
            for hp in range(8):
                l = hp % NL
                yk = ["rY2_%d" % (2 * hp), "rY2_%d" % (2 * hp + 1)]
                o, ok = slot(l)
                mm(o, ok, bd64[:], Y2[:, hp, :], ["bd64"] + yk)
                P.op("dve", lambda e, o=o, hp=hp: e.tensor_tensor(fin["YC"][:], Y2[:, hp, :], o, ALU.subtract), reads=yk + [ok], writes=["rfYC"])
                P.op("act", lambda e: e.activation(fin["SQ"][:], fin["YC"][:], AF.Square), reads=["rfYC"], writes=["rfSQ"])
                o, ok = slot(l)
                mm(o, ok, bd64[:], fin["SQ"][:], ["bd64", "rfSQ"])
                P.op("act", lambda e, o=o: e.activation(fin["SD"][:], o, AF.Sqrt, bias=EPSB[0][:, 2:3], scale=1.0), reads=[ok, "epsb"], writes=["rfSD"])
                P.op("dve", lambda e: e.reciprocal(fin["SD"][:], fin["SD"][:]), reads=["rfSD"], writes=["rfSD"])
                P.op("dve", lambda e: e.tensor_tensor(fin["YN"][:], fin["YC"][:], fin["SD"][:], ALU.mult), reads=["rfYC", "rfSD"], writes=["rfYN"])
                P.op("dve", lambda e, hp=hp: e.tensor_scalar(fin["YN"][:], fin["YN"][:], lg_c[:, hp:hp + 1], lb_c[:, hp:hp + 1], ALU.mult, ALU.add),
                     reads=["rfYN", "lg_c", "lb_c"], writes=["rfYN"])
                P.op("dve", lambda e, hp=hp: e.scalar_tensor_tensor(out=finb["RK"][:], in0=F[:, 1, hp, :], scalar=rk_c[:, hp:hp + 1], in1=F[:, 3, hp, :],
                                                                    op0=ALU.mult, op1=ALU.mult), reads=[kF + "_1", kF + "_3", "rk_c"], writes=["rfRK"])
                o, ok = slot(l)
                mm(o, ok, bd1[:], finb["RK"][:], ["bd1b", "rfRK"])
                P.op("dve", lambda e, o=o, hp=hp: e.tensor_tensor(fin["BN"][:], o, F[:, 4, hp, :], ALU.mult), reads=[ok, kF + "_4"], writes=["rfBN"])
                P.op("dve", lambda e: e.tensor_tensor(fin["YN"][:], fin["YN"][:], fin["BN"][:], ALU.add), reads=["rfYN", "rfBN"], writes=["rfYN"])
                o, ok = slot(l)
                mm(o, ok, gup[:, 0, hp * 128:(hp + 1) * 128], Lt[:, 2, :], ["gup", kL], start=True, stop=False)
                mm(o, ok, gup[:, 1, hp * 128:(hp + 1) * 128], Lt[:, 3, :], ["gup", kL], start=False, stop=True)
                yb = YB[hp % 2]
                P.op("dve", lambda e, o=o, yb=yb: e.tensor_tensor(yb[:], fin["YN"][:], o, ALU.mult), reads=["rfYN", ok], writes=["rfYB%d" % (hp % 2)])
                q0 = (c - 16) * 128
                P.dma("sp", ybT_s.ap()[hp * 128:(hp + 1) * 128, q0:q0 + 128], yb[:], reads=["rfYB%d" % (hp % 2)], writes=["ybT:%d:%d" % (c - 16, hp)])


def stage_M(P, nc, din, identb, xs_d, y_d, x1_s, slot_s, gate_s, out_d):
    wg = [din("wg_%d" % e, [D, 1024]) for e in range(32)]
    wu = [din("wu_%d" % e, [D, 1024]) for e in range(32)]
    wd = [din("wd_%d" % e, [1024, D]) for e in range(32)]
    NS = CAP // 128
    xs = [P.sb("mxs%d" % i, [128, D], BF16) for i in range(2 * NS)]
    xT = P.sb("mxT", [128, 16, CAP], BF16)
    NST = 4
    wst = [P.sb("mwst%d" % i, [128, 16, 256], F32) for i in range(NST)]
    wgb = [P.sb("mwgb%d" % i, [128, 16, 256], BF16) for i in range(2)]
    wub = [P.sb("mwub%d" % i, [128, 16, 256], BF16) for i in range(2)]
    wdb = [P.sb("mwdb%d" % i, [128, 8, 512], BF16) for i in range(2)]
    hT = P.sb("mhT", [128, 8, CAP], BF16)
    sl = P.sb("msl", [128, CAP], F32)
    yo = [P.sb("myo%d" % i, [128, 512], F32) for i in range(2)]
    psTb = P.ps("mpsTb", [128, 8, 128], BF16)
    psG = [P.ps("mpsG%d" % i, [128, 512], F32) for i in range(2)]
    psU = [P.ps("mpsU%d" % i, [128, 512], F32) for i in range(2)]
    psY = [P.ps("mpsY%d" % i, [128, 512], F32) for i in range(2)]
    zr = P.sb("mzr", [1, D], F32)
    P.op("dve", lambda e: e.memset(zr[:], 0.0), writes=["mzr"])
    P.dma("sp", y_d.ap()[NSLOT:NSLOT + 1, :], zr[:], reads=["mzr"], writes=["y_dump"])
    ncast = [0]
    nst = [0]

    def cast(dst, src, rk, wk):
        ncast[0] += 1
        eng = ("dve", "pool", "act")[ncast[0] % 3]
        if eng == "act":
            P.op("act", lambda e: e.copy(dst, src), reads=[rk], writes=[wk])
        else:
            P.op(eng, lambda e: e.tensor_copy(dst, src), reads=[rk], writes=[wk])

    items = []
    for ex in range(32):
        items += [("gu", ex, cb) for cb in range(4)] + [("dn", ex, cb) for cb in range(4)]

    def issue_w(i):
        kind, ex, cb = items[i]
        bb = i % 2
        if kind == "gu":
            for (wsrc, wdst, nm) in ((wg[ex], wgb[bb], "mwgb%d" % bb), (wu[ex], wub[bb], "mwub%d" % bb)):
                st_i = nst[0] % NST
                nst[0] += 1
                W = wst[st_i]
                P.dma("sp", W[:], wsrc.ap()[:, cb * 256:(cb + 1) * 256].rearrange("(k p) c -> p k c", p=128), writes=["mwst%d" % st_i])
                cast(wdst[:], W[:], "mwst%d" % st_i, nm)
        else:
            st_i = nst[0] % NST
            nst[0] += 1
            Wv = wst[st_i][:].rearrange("p (a k) c -> p a (k c)", a=8)
            P.dma("sp", Wv, wd[ex].ap()[:, cb * 512:(cb + 1) * 512].rearrange("(k p) c -> p k c", p=128), writes=["mwst%d" % st_i])
            cast(wdb[bb][:], Wv, "mwst%d" % st_i, "mwdb%d" % bb)

    def load_xs(ex):
        for s_ in range(NS):
            xi = (ex * NS + s_) % (2 * NS)
            r0 = ex * CAP + s_ * 128
            P.dma("sp", xs[xi][:], xs_d.ap()[r0:r0 + 128, :], reads=["xs_all"], writes=["mxs%d" % xi])

    load_xs(0)
    issue_w(0)
    xk = ["mxT_%d_%d" % (s_, half) for s_ in range(NS) for half in range(2)]
    hk = ["mhT_%d" % i for i in range(8)]
    for i, (kind, ex, cb) in enumerate(items):
        bb = i % 2
        if i + 1 < len(items):
            issue_w(i + 1)
        if kind == "gu" and cb == 0:
            for s_ in range(NS):
                xi = (ex * NS + s_) % (2 * NS)
                X = xs[xi]
                for half in range(2):
                    for k in range(8):
                        kc = half * 8 + k
                        P.op("pe", lambda e, X=X, k=k, kc=kc: e.transpose(psTb[:, k, :], X[:, kc * 128:(kc + 1) * 128], identb[:]),
                             reads=["mxs%d" % xi, "identb"], writes=["mpsTb"], signal=(k == 7))
                    P.op("dve", lambda e, half=half, s_=s_: e.tensor_copy(xT[:, half * 8:(half + 1) * 8, s_ * 128:(s_ + 1) * 128], psTb[:]),
                         reads=["mpsTb"], writes=["mxT_%d_%d" % (s_, half)])
            if ex + 1 < 32:
                load_xs(ex + 1)
        if kind == "gu":
            for hc in range(2):
                pg, pu = psG[hc], psU[hc]
                for k in range(16):
                    P.op("pe", lambda e, pg=pg, k=k, hc=hc, bb=bb: e.matmul(pg[:, 0:CAP], wgb[bb][:, k, hc * 128:(hc + 1) * 128], xT[:, k, :], start=(k == 0), stop=(k == 15)),
                         reads=["mwgb%d" % bb] + xk, writes=["mpsG%d" % hc], signal=(k == 15))
                for k in range(16):
                    P.op("pe", lambda e, pu=pu, k=k, hc=hc, bb=bb: e.matmul(pu[:, 0:CAP], wub[bb][:, k, hc * 128:(hc + 1) * 128], xT[:, k, :], start=(k == 0), stop=(k == 15)),
                         reads=["mwub%d" % bb] + xk, writes=["mpsU%d" % hc], signal=(k == 15))
                hi = cb * 2 + hc
                P.op("act", lambda e, pg=pg: e.activation(sl[:], pg[:, 0:CAP], AF.Silu), reads=["mpsG%d" % hc], writes=["msl"])
                P.op("dve", lambda e, pu=pu, hi=hi: e.tensor_tensor(hT[:, hi, :], sl[:], pu[:, 0:CAP], ALU.mult), reads=["msl", "mpsU%d" % hc], writes=["mhT_%d" % hi])
        else:
            for s_ in range(NS):
                py = psY[s_ % 2]
                for k in range(8):
                    P.op("pe", lambda e, py=py, k=k, s_=s_, bb=bb: e.matmul(py[:], hT[:, k, s_ * 128:(s_ + 1) * 128], wdb[bb][:, k, :], start=(k == 0), stop=(k == 7)),
                         reads=hk + ["mwdb%d" % bb], writes=["mpsY%d" % (s_ % 2)], signal=(k == 7))
                Y = yo[s_ % 2]
                P.op("act", lambda e, py=py, Y=Y: e.copy(Y[:], py[:]), reads=["mpsY%d" % (s_ % 2)], writes=["myo%d" % (s_ % 2)])
                r0 = ex * CAP + s_ * 128
                P.dma("act", y_d.ap()[r0:r0 + 128, cb * 512:(cb + 1) * 512], Y[:], reads=["myo%d" % (s_ % 2)], writes=["y_all"])


def stage_M2(P, nc, y_d, x1_s, slot_s, gate_s, out_d):
    x1 = [P.sb("mx1_%d" % i, [128, D], F32) for i in range(2)]
    y1 = [P.sb("my1_%d" % i, [128, D], F32) for i in range(2)]
    y2 = [P.sb("my2_%d" % i, [128, D], F32) for i in range(2)]
    si = [P.sb("msi%d" % i, [128, 2], I32) for i in range(2)]
    gt = [P.sb("mgt%d" % i, [128, 2], F32) for i in range(2)]
    for t in range(16):
        b = t % 2
        P.dma("sp", si[b][:], slot_s.ap()[t], writes=["msi%d" % b])
        P.dma("sp", gt[b][:], gate_s.ap()[t], writes=["mgt%d" % b])
        P.dma("sp", x1[b][:], x1_s.ap()[t * 128:(t + 1) * 128, :], writes=["mx1_%d" % b])
        P.idma_gather(y1[b][:], y_d, si[b][:, 0:1], reads=["msi%d" % b], writes=["my1_%d" % b])
        P.idma_gather(y2[b][:], y_d, si[b][:, 1:2], reads=["msi%d" % b], writes=["my2_%d" % b])
        P.op("dve", lambda e, b=b: e.scalar_tensor_tensor(out=x1[b][:], in0=y1[b][:], scalar=gt[b][:, 0:1], in1=x1[b][:], op0=ALU.mult, op1=ALU.add),
             reads=["my1_%d" % b, "mgt%d" % b, "mx1_%d" % b], writes=["mx1_%d" % b])
        P.op("dve", lambda e, b=b: e.scalar_tensor_tensor(out=x1[b][:], in0=y2[b][:], scalar=gt[b][:, 1:2], in1=x1[b][:], op0=ALU.mult, op1=ALU.add),
             reads=["my2_%d" % b, "mgt%d" % b, "mx1_%d" % b], writes=["mx1_%d" % b])
        P.dma("sp", out_d.ap()[t * 128:(t + 1) * 128, :], x1[b][:], reads=["mx1_%d" % b], writes=["out:%d" % t])


CAP = 256
NSLOT = 32 * CAP


def stage_O1(P, nc, din, identb, ya_s, ybT_s, sg_s, mgT_s):
    proj_a = din("proj_a", [1024, D])
    proj_b = din("proj_b", [1024, D])
    PA = P.sb("oPA", [128, 8, D], BF16)
    PB = P.sb("oPB", [128, 8, D], BF16)
    wst = P.sb("owst", [128, 8, 512], F32)
    for wi, (src, dst, nm) in enumerate(((proj_a, PA, "oPA"), (proj_b, PB, "oPB"))):
        for cb in range(4):
            P.dma("sp", wst[:], src.ap()[:, cb * 512:(cb + 1) * 512].rearrange("(k p) c -> p k c", p=128), writes=["owst"])
            eng = "dve" if cb % 2 else "pool"
            P.op(eng, lambda e, dst=dst, cb=cb: e.tensor_copy(dst[:, :, cb * 512:(cb + 1) * 512], wst[:]), reads=["owst"], writes=[nm + "_%d" % cb])
    yat = [P.sb("oya%d" % i, [128, 1024], F32) for i in range(2)]
    yab = P.sb("oyab", [128, 1024], BF16)
    yaT = P.sb("oyaT", [128, 8, 128], BF16)
    ybT = [P.sb("oybT%d" % i, [128, 8, 128], BF16) for i in range(2)]
    sg = [P.sb("osg%d" % i, [128, 4096], BF16) for i in range(2)]
    m1 = P.sb("om1", [128, 512], F32)
    m2 = P.sb("om2", [128, 512], F32)
    MG = P.sb("oMG", [128, D], BF16)
    mgT = [P.sb("omgT%d" % i, [128, 16, 128], BF16) for i in range(2)]
    psTb = P.ps("opsTb", [128, 8, 128], BF16)
    psA = [P.ps("opsA%d" % i, [128, 512], F32) for i in range(2)]
    psB = [P.ps("opsB%d" % i, [128, 512], F32) for i in range(2)]
    for t in range(16):
        b = t % 2
        P.dma("sp", yat[b][:], ya_s.ap()[t * 128:(t + 1) * 128, :], reads=["ya_s:%d:%d" % (t, h) for h in range(8)], writes=["oya%d" % b])
        P.dma("sp", ybT[b][:], ybT_s.ap()[:, t * 128:(t + 1) * 128].rearrange("(k p) t -> p k t", p=128),
              reads=["ybT:%d:%d" % (t, h) for h in range(8)], writes=["oybT%d" % b])
        P.dma("sp", sg[b][:], sg_s.ap()[t * 128:(t + 1) * 128, :], reads=["sg_s:%d:%d" % (t, i) for i in range(8)], writes=["osg%d" % b])
        P.op("act", lambda e, b=b: e.copy(yab[:], yat[b][:]), reads=["oya%d" % b], writes=["oyab"])
        for k in range(8):
            P.op("pe", lambda e, k=k: e.transpose(psTb[:, k, :], yab[:, k * 128:(k + 1) * 128], identb[:]), reads=["oyab", "identb"], writes=["opsTb"], signal=(k == 7))
        P.op("dve", lambda e: e.tensor_copy(yaT[:], psTb[:]), reads=["opsTb"], writes=["oyaT"])
        for cb in range(4):
            cs_ = slice(cb * 512, (cb + 1) * 512)
            pa, pbb = psA[cb % 2], psB[cb % 2]
            ka, kb_ = "opsA%d" % (cb % 2), "opsB%d" % (cb % 2)
            for k in range(8):
                P.op("pe", lambda e, pa=pa, k=k, cs_=cs_: e.matmul(pa[:], yaT[:, k, :], PA[:, k, cs_], start=(k == 0), stop=(k == 7)),
                     reads=["oyaT", "oPA_%d" % cb], writes=[ka], signal=(k == 7))
            for k in range(8):
                P.op("pe", lambda e, pbb=pbb, k=k, cs_=cs_, b=b: e.matmul(pbb[:], ybT[b][:, k, :], PB[:, k, cs_], start=(k == 0), stop=(k == 7)),
                     reads=["oybT%d" % b, "oPB_%d" % cb], writes=[kb_], signal=(k == 7))
            P.op("dve", lambda e, pa=pa, cs_=cs_, b=b: e.tensor_tensor(m1[:], pa[:], sg[b][:, cs_], ALU.mult), reads=[ka, "osg%d" % b], writes=["om1"])
            P.op("dve", lambda e, pbb=pbb, cb=cb, b=b: e.tensor_tensor(m2[:], pbb[:], sg[b][:, 2048 + cb * 512:2048 + (cb + 1) * 512], ALU.mult),
                 reads=[kb_, "osg%d" % b], writes=["om2"])
            P.op("pool", lambda e, cs_=cs_: e.tensor_tensor(MG[:, cs_], m1[:], m2[:], ALU.add), reads=["om1", "om2"], writes=["oMG_%d" % cb])
        for half in range(2):
            for k in range(8):
                kc = half * 8 + k
                P.op("pe", lambda e, k=k, kc=kc: e.transpose(psTb[:, k, :], MG[:, kc * 128:(kc + 1) * 128], identb[:]),
                     reads=["oMG_%d" % (kc // 4), "identb"], writes=["opsTb"], signal=(k == 7))
            P.op("act", lambda e, half=half, b=b: e.copy(mgT[b][:, half * 8:(half + 1) * 8, :], psTb[:]), reads=["opsTb"], writes=["omgT%d_%d" % (b, half)])
        P.dma("sp", mgT_s.ap()[t], mgT[b][:], reads=["omgT%d_0" % b, "omgT%d_1" % b], writes=["mgT_s:%d" % t])


def stage_O2(P, nc, din, ident, xw, mgT_s, x1_s, xs_d, slot_s, gate_s):
    w_out = [din("w_out_%d" % i, [1024, D]) for i in range(2)]
    norm2_g = din("norm2_g", [1, D])
    rw_d = din("router_w", [D, 36])
    rb_d = din("router_b", [1, 36])
    ebase_d = din("ebase_h", [1, 32])
    tsf_d = din("ts_h2", [128, 128])
    WO = P.sb("oWO", [128, 16, D], BF16)
    wst = P.sb("o2wst", [128, 8, 512], F32)
    for i in range(2):
        for cb in range(4):
            P.dma("sp", wst[:], w_out[i].ap()[:, cb * 512:(cb + 1) * 512].rearrange("(k p) c -> p k c", p=128), writes=["o2wst"])
            eng = "dve" if cb % 2 else "pool"
            P.op(eng, lambda e, i=i, cb=cb: e.tensor_copy(WO[:, i * 8:(i + 1) * 8, cb * 512:(cb + 1) * 512], wst[:]), reads=["o2wst"], writes=["oWO_%d_%d" % (i, cb)])
    g2b = P.sb("og2b", [128, D], F32)
    P.dma("sp", g2b[:], AP(norm2_g, 0, [[0, 128], [1, D]]), writes=["og2b"])
    RW = P.sb("oRW", [128, 16, 36], F32)
    P.dma("sp", RW[:], rw_d.ap().rearrange("(k p) c -> p k c", p=128), writes=["oRW"])
    rbb = P.sb("orbb", [128, 36], F32)
    P.dma("sp", rbb[:], AP(rb_d, 0, [[0, 128], [1, 36]]), writes=["orbb"])
    ebase = P.sb("oebase", [128, 32], F32)
    P.dma("sp", ebase[:], AP(ebase_d, 0, [[0, 128], [1, 32]]), writes=["oebase"])
    tsf = P.sb("otsf", [128, 128], F32)
    P.dma("sp", tsf[:], tsf_d.ap(), writes=["otsf"])
    onesf = P.sb("oones", [128, 128], F32)
    P.op("dve", lambda e: e.memset(onesf[:], 1.0), writes=["oones"])
    carry = P.sb("ocarry", [128, 32], F32)
    P.op("dve", lambda e: e.memset(carry[:], 0.0), writes=["ocarry"])
    zxs = P.sb("ozxs", [128, D], BF16)
    P.op("dve", lambda e: e.memset(zxs[:], 0.0), writes=["ozxs"])
    zk_ = []
    for i_ in range(NSLOT // 128):
        P.dma("sp", xs_d.ap()[i_ * 128:(i_ + 1) * 128, :], zxs[:], reads=["ozxs"], writes=["xsz:%d" % i_])
        zk_.append("xsz:%d" % i_)
    P.dma("sp", xs_d.ap()[NSLOT:NSLOT + 1, :], zxs[0:1, :], reads=["ozxs"], writes=["xsz:d"])
    zk_.append("xsz:d")
    P.wait_all("pool", zk_)
    mgT = [P.sb("o2mgT%d" % i, [128, 16, 128], BF16) for i in range(2)]
    xt = [P.sb("o2x%d" % i, [128, D], F32) for i in range(2)]
    X1 = P.sb("oX1", [128, D], F32)
    H2 = P.sb("oH2", [128, D], F32)
    H2b = [P.sb("oH2b%d" % i, [128, D], BF16) for i in range(2)]
    sq = P.sb("o2sq", [128, D], BF16)
    h2T = P.sb("oh2T", [128, 16, 128], F32)
    r = P.sb("or", [128, 256], F32)
    slot_i = [P.sb("oslot%d" % i, [128, 2], I32) for i in range(2)]
    gate2 = [P.sb("ogate%d" % i, [128, 2], F32) for i in range(2)]
    psX = [P.ps("opsX%d" % i, [128, 512], F32) for i in range(2)]
    psT = P.ps("o2psT", [128, 4, 128], F32)
    psR = P.ps("opsR", [128, 512], F32)
    psP = P.ps("opsP", [128, 512], F32)

    def R(a, b_):
        return r[:, a:b_]

    for t in range(16):
        b = t % 2
        P.dma("sp", mgT[b][:], mgT_s.ap()[t], reads=["mgT_s:%d" % t], writes=["o2mgT%d" % b])
        P.dma("sp", xt[b][:], xw.ap()[NTOK + t * 128:NTOK + (t + 1) * 128, :], writes=["o2x%d" % b])
        for cb in range(4):
            cs_ = slice(cb * 512, (cb + 1) * 512)
            px, kx = psX[cb % 2], "opsX%d" % (cb % 2)
            for k in range(16):
                P.op("pe", lambda e, px=px, k=k, cs_=cs_, b=b: e.matmul(px[:], mgT[b][:, k, :], WO[:, k, cs_], start=(k == 0), stop=(k == 15)),
                     reads=["o2mgT%d" % b, "oWO_%d_%d" % (k // 8, cb)], writes=[kx], signal=(k == 15))
            P.op("dve", lambda e, px=px, cs_=cs_, b=b: e.tensor_tensor(X1[:, cs_], px[:], xt[b][:, cs_], ALU.add), reads=[kx, "o2x%d" % b], writes=["oX1_%d" % cb])
        x1k = ["oX1_%d" % cb for cb in range(4)]
        P.dma("sp", x1_s.ap()[t * 128:(t + 1) * 128, :], X1[:], reads=x1k, writes=["x1_s:%d" % t])
        P.op("act", lambda e: e.activation(sq[:], X1[:], AF.Square, accum_out=R(0, 1)), reads=x1k, writes=["o2sq", "or"])
        P.op("act", lambda e: e.activation(R(1, 2), R(0, 1), AF.Sqrt, bias=EPSB[0][:, 0:1], scale=1.0 / D), reads=["or", "epsb"], writes=["or"])
        P.op("dve", lambda e: e.reciprocal(R(2, 3), R(1, 2)), reads=["or"], writes=["or"])
        P.op("dve", lambda e: e.scalar_tensor_tensor(out=H2[:], in0=X1[:], scalar=R(2, 3), in1=g2b[:], op0=ALU.mult, op1=ALU.mult),
             reads=x1k + ["or", "og2b"], writes=["oH2"])
        P.op("act", lambda e, b=b: e.copy(H2b[b][:], H2[:]), reads=["oH2"], writes=["oH2b%d" % b])
        for c4 in range(4):
            for j in range(4):
                kc = c4 * 4 + j
                P.op("pe", lambda e, j=j, kc=kc: e.transpose(psT[:, j, :], H2[:, kc * 128:(kc + 1) * 128], ident[:]), reads=["oH2", "ident"], writes=["o2psT"], signal=(j == 3))
            eng = "act" if c4 % 2 else "dve"
            if eng == "act":
                P.op("act", lambda e, c4=c4: e.copy(h2T[:, c4 * 4:(c4 + 1) * 4, :], psT[:]), reads=["o2psT"], writes=["oh2T_%d" % c4])
            else:
                P.op("dve", lambda e, c4=c4: e.tensor_copy(h2T[:, c4 * 4:(c4 + 1) * 4, :], psT[:]), reads=["o2psT"], writes=["oh2T_%d" % c4])
        for k in range(16):
            P.op("pe", lambda e, k=k: e.matmul(psR[:, 0:36], h2T[:, k, :], RW[:, k, :], start=(k == 0), stop=(k == 15)),
                 reads=["oh2T_%d" % (k // 4), "oRW"], writes=["opsR"], signal=(k == 15))
        P.op("dve", lambda e: e.tensor_tensor(R(8, 12), psR[:, 0:4], rbb[:, 0:4], ALU.add), reads=["opsR", "orbb"], writes=["or"])
        P.op("dve", lambda e: e.tensor_tensor(R(16, 48), psR[:, 4:36], rbb[:, 4:36], ALU.add), reads=["opsR", "orbb"], writes=["or"])
        P.op("dve", lambda e: e.tensor_reduce(R(48, 49), R(8, 12), AX.X, ALU.max), reads=["or"], writes=["or"])
        P.op("dve", lambda e: e.tensor_scalar(R(52, 56), R(8, 12), R(48, 49), None, ALU.is_equal), reads=["or"], writes=["or"])
        P.op("dve", lambda e: e.tensor_scalar(R(56, 60), R(8, 12), R(48, 49), None, ALU.subtract), reads=["or"], writes=["or"])
        P.op("act", lambda e: e.activation(R(56, 60), R(56, 60), AF.Exp, accum_out=R(60, 61)), reads=["or"], writes=["or"])
        P.op("dve", lambda e: e.reciprocal(R(61, 62), R(60, 61)), reads=["or"], writes=["or"])
        P.op("dve", lambda e: e.tensor_scalar(R(64, 72), R(16, 24), R(52, 53), None, ALU.mult), reads=["or"], writes=["or"])
        for g in range(1, 4):
            P.op("dve", lambda e, g=g: e.scalar_tensor_tensor(out=R(64, 72), in0=R(16 + 8 * g, 24 + 8 * g), scalar=R(52 + g, 53 + g), in1=R(64, 72),
                                                              op0=ALU.mult, op1=ALU.add), reads=["or"], writes=["or"])
        P.op("dve", lambda e: e.max(R(72, 80), R(64, 72)), reads=["or"], writes=["or"])
        P.op("dve", lambda e: e.tensor_scalar(R(80, 88), R(64, 72), R(72, 73), None, ALU.is_equal), reads=["or"], writes=["or"])
        P.op("dve", lambda e: e.tensor_scalar(R(88, 96), R(64, 72), R(73, 74), None, ALU.is_equal), reads=["or"], writes=["or"])
        P.op("dve", lambda e: e.tensor_tensor(R(96, 97), R(73, 74), R(72, 73), ALU.subtract), reads=["or"], writes=["or"])
        P.op("act", lambda e: e.activation(R(97, 98), R(96, 97), AF.Exp), reads=["or"], writes=["or"])
        P.op("dve", lambda e: e.tensor_scalar(R(98, 99), R(97, 98), 1.0, None, ALU.add), reads=["or"], writes=["or"])
        P.op("dve", lambda e: e.reciprocal(R(98, 99), R(98, 99)), reads=["or"], writes=["or"])
        P.op("dve", lambda e: e.tensor_tensor(R(99, 100), R(61, 62), R(98, 99), ALU.mult), reads=["or"], writes=["or"])
        P.op("dve", lambda e: e.tensor_tensor(R(100, 101), R(61, 62), R(99, 100), ALU.subtract), reads=["or"], writes=["or"])
        for g in range(4):
            P.op("dve", lambda e, g=g: e.tensor_scalar(R(104 + 8 * g, 112 + 8 * g), R(80, 88), R(52 + g, 53 + g), None, ALU.mult), reads=["or"], writes=["or"])
            P.op("dve", lambda e, g=g: e.tensor_scalar(R(136 + 8 * g, 144 + 8 * g), R(88, 96), R(52 + g, 53 + g), None, ALU.mult), reads=["or"], writes=["or"])
        P.op("dve", lambda e: e.tensor_tensor(R(200, 232), R(104, 136), R(136, 168), ALU.add), reads=["or"], writes=["or"])
        P.op("pe", lambda e: e.matmul(psP[:, 0:32], tsf[:], R(200, 232), start=True, stop=True, skip_group_check=True), reads=["otsf", "or"], writes=["opsP"])
        P.op("pe", lambda e: e.matmul(psP[:, 32:64], onesf[:], R(200, 232), start=True, stop=True, skip_group_check=True), reads=["oones", "or"], writes=["opsP"])
        P.op("dve", lambda e: e.tensor_tensor(R(168, 200), psP[:, 0:32], carry[:], ALU.add), reads=["opsP", "ocarry"], writes=["or"])
        P.op("dve", lambda e: e.tensor_tensor(carry[:], carry[:], psP[:, 32:64], ALU.add), reads=["opsP", "ocarry"], writes=["ocarry"])
        S, Gt = slot_i[b], gate2[b]
        for k, (oh0, gcol) in enumerate(((104, 99), (136, 100))):
            P.op("dve", lambda e, oh0=oh0: e.tensor_tensor(R(200, 232), R(oh0, oh0 + 32), R(168, 200), ALU.mult), reads=["or"], writes=["or"])
            P.op("dve", lambda e: e.tensor_reduce(R(232, 233), R(200, 232), AX.X, ALU.add), reads=["or"], writes=["or"])
            P.op("dve", lambda e, oh0=oh0: e.tensor_tensor(R(200, 232), R(oh0, oh0 + 32), ebase[:], ALU.mult), reads=["or", "oebase"], writes=["or"])
            P.op("dve", lambda e: e.tensor_reduce(R(233, 234), R(200, 232), AX.X, ALU.add), reads=["or"], writes=["or"])
            P.op("dve", lambda e: e.tensor_scalar(R(234, 235), R(232, 233), float(CAP), None, ALU.is_lt), reads=["or"], writes=["or"])
            P.op("dve", lambda e: e.tensor_tensor(R(235, 236), R(232, 233), R(233, 234), ALU.add), reads=["or"], writes=["or"])
            P.op("dve", lambda e: e.tensor_scalar(R(235, 236), R(235, 236), float(NSLOT), None, ALU.subtract), reads=["or"], writes=["or"])
            P.op("dve", lambda e: e.tensor_tensor(R(236, 237), R(235, 236), R(234, 235), ALU.mult), reads=["or"], writes=["or"])
            P.op("dve", lambda e: e.tensor_scalar(R(236, 237), R(236, 237), float(NSLOT), None, ALU.add), reads=["or"], writes=["or"])
            P.op("dve", lambda e, S=S, k=k: e.tensor_copy(S[:, k:k + 1], R(236, 237)), reads=["or"], writes=["oslot%d" % b])
            P.op("dve", lambda e, Gt=Gt, k=k, gcol=gcol: e.tensor_tensor(Gt[:, k:k + 1], R(gcol, gcol + 1), R(234, 235), ALU.mult), reads=["or"], writes=["ogate%d" % b])
        for k in range(2):
            P.idma_scatter(xs_d, S[:, k:k + 1], H2b[b][:], reads=["oslot%d" % b, "oH2b%d" % b], writes=["xs:%d:%d" % (t, k)])
        P.dma("sp", slot_s.ap()[t], S[:], reads=["oslot%d" % b], writes=["slot_s:%d" % t])
        P.dma("sp", gate_s.ap()[t], Gt[:], reads=["ogate%d" % b], writes=["gate_s:%d" % t])
```

```python
import numpy as np
from contextlib import ExitStack
import concourse.bass as bass
import concourse.mybir as mybir
from concourse.bass_utils import run_bass_kernel_spmd

F32 = mybir.dt.float32
F32R = mybir.dt.float32r
BF16 = mybir.dt.bfloat16
I32 = mybir.dt.int32
U32 = mybir.dt.uint32
AF = mybir.ActivationFunctionType
ALU = mybir.AluOpType
AX = mybir.AxisListType

D = 2048
NTOK = 2048
WTOK = 4096
INW = 10688
RWW = 3520
NCORES = 8
import os
RSTOP = int(os.environ.get('RSTOP', '9'))


class Prog:
    ENG = ("pe", "act", "dve", "pool", "sp")

    def __init__(self, nc, es):
        self.nc = nc
        self.es = es
        self.q = {e: [] for e in self.ENG}
        self.sem = {e: es.enter_context(nc.semaphore("c_" + e)) for e in ("pe", "act", "dve", "pool")}
        self.cnt = {e: 0 for e in self.ENG}
        self.NR = 24
        self.ring = {e: [es.enter_context(nc.semaphore("r_%s%d" % (e, i))) for i in range(self.NR)]
                     for e in ("sp", "pool", "act")}
        self.ring_n = {e: 0 for e in ("sp", "pool", "act")}
        self.ring_val = {e: [0] * self.NR for e in ("sp", "pool", "act")}
        self.seen = {e: {} for e in self.ENG}
        self.lastw = {}
        self.reads = {}
        self.cur = es
        self.stack = []

    def sb(self, name, shape, dt):
        return self.cur.enter_context(self.nc.sbuf_tensor(name, list(shape), dt))

    def ps(self, name, shape, dt=F32):
        return self.cur.enter_context(self.nc.psum_tensor(name, list(shape), dt))

    def push(self):
        self.stack.append(self.cur)
        self.cur = ExitStack()

    def pop(self):
        self.barrier()
        self.cur.close()
        self.cur = self.stack.pop()

    def barrier(self):
        for eng in self.ENG:
            for e2 in ("pe", "act", "dve", "pool"):
                if self.cnt[e2]:
                    self._wait(eng, (self.sem[e2], self.cnt[e2], "bar"))
            for qn in self.ring:
                for i in range(self.NR):
                    if self.ring_val[qn][i]:
                        self._wait(eng, (self.ring[qn][i], self.ring_val[qn][i], "dma"))

    def dram(self, name, shape, dt, kind="Internal"):
        return self.nc.dram_tensor(name, list(shape), dt, kind=kind)

    def _wait(self, eng, ev):
        sem, val, src = ev
        if eng == "pe" and src == "pe":
            return
        key = id(sem)
        if self.seen[eng].get(key, 0) >= val:
            return
        self.seen[eng][key] = val
        self.q[eng].append(lambda e, sem=sem, val=val: e.wait_ge(sem, val))

    def _deps(self, eng, reads, writes):
        for k in reads:
            ev = self.lastw.get(k)
            if ev is not None:
                self._wait(eng, ev)
        for k in writes:
            ev = self.lastw.get(k)
            if ev is not None:
                self._wait(eng, ev)
            for ev in self.reads.get(k, ()):
                self._wait(eng, ev)

    def _commit(self, ev, reads, writes):
        for k in writes:
            self.lastw[k] = ev
            self.reads[k] = []
        for k in reads:
            self.reads.setdefault(k, []).append(ev)
            if len(self.reads[k]) > 64:
                self.reads[k] = self.reads[k][-64:]

    def op(self, eng, fn, reads=(), writes=(), signal=True):
        self._deps(eng, reads, writes)
        sem = self.sem[eng]
        if eng == "pe" and not signal:
            self.q[eng].append(lambda e, fn=fn: fn(e))
            self._commit((sem, self.cnt[eng] + 1, eng), reads, writes)
            return
        self.cnt[eng] += 1
        n = self.cnt[eng]
        self.q[eng].append(lambda e, fn=fn, sem=sem: fn(e).then_inc(sem, 1))
        self._commit((sem, n, eng), reads, writes)

    def dma(self, eng, out, in_, reads=(), writes=(), **kw):
        self._deps(eng, reads, writes)
        i = self.ring_n[eng] % self.NR
        self.ring_n[eng] += 1
        sem = self.ring[eng][i]
        prev = self.ring_val[eng][i]
        if prev:
            self._wait(eng, (sem, prev, "dma"))
        val = prev + 16
        self.ring_val[eng][i] = val
        self.q[eng].append(lambda e, out=out, in_=in_, sem=sem, kw=kw: e.dma_start(out=out, in_=in_, **kw).then_inc(sem, 16))
        ev = (sem, val, "dma")
        self._commit(ev, reads, writes)
        return ev

    def _idma(self, mk, reads, writes):
        eng = "pool"
        self._deps(eng, reads, writes)
        i = self.ring_n[eng] % self.NR
        self.ring_n[eng] += 1
        sem = self.ring[eng][i]
        prev = self.ring_val[eng][i]
        if prev:
            self._wait(eng, (sem, prev, "dma"))
        val = prev + 16
        self.ring_val[eng][i] = val
        self.q[eng].append(lambda e, mk=mk, sem=sem: mk(e).then_inc(sem, 16))
        self._commit((sem, val, "dma"), reads, writes)

    def idma_scatter(self, dram, idx, src, reads=(), writes=()):
        nrow = dram.shape[0]
        self._idma(lambda e: e.indirect_dma_start(out=dram[:, :], out_offset=bass.IndirectOffsetOnAxis(ap=idx, axis=0), in_=src,
                                                  in_offset=None), reads, writes)

    def idma_gather(self, dst, dram, idx, reads=(), writes=()):
        nrow = dram.shape[0]
        self._idma(lambda e: e.indirect_dma_start(out=dst, out_offset=None, in_=dram[:, :],
                                                  in_offset=bass.IndirectOffsetOnAxis(ap=idx, axis=0)), reads, writes)

    def wait_all(self, eng, keys):
        for k in keys:
            ev = self.lastw.get(k)
            if ev is not None:
                self._wait(eng, ev)

    def emit(self):
        nc = self.nc
        with nc.Block() as blk:
            @blk.tensor
            def _(e):
                for f in self.q["pe"]:
                    f(e)

            @blk.scalar
            def _(e):
                for f in self.q["act"]:
                    f(e)

            @blk.vector
            def _(e):
                for f in self.q["dve"]:
                    f(e)

            @blk.gpsimd
            def _(e):
                for f in self.q["pool"]:
                    f(e)

            @blk.sync
            def _(e):
                for f in self.q["sp"]:
                    f(e)


def AP(t, off, dims):
    return bass.AP(t, off, [list(d) for d in dims])


def host_consts():
    c = {}
    c["ident_h"] = np.eye(128, dtype=np.float32)
    c["tri_h"] = np.triu(np.ones((128, 128), np.float32))
    c.update(rwkv_consts())
    return c


def build(stages=("A",), debug=False):
    nc = bass.Bass("TRN2", target_bir_lowering=False)
    es = ExitStack()
    P = Prog(nc, es)
    dr = {}

    def din(name, shape, dt=F32):
        dr[name] = nc.dram_tensor(name, list(shape), dt, kind="ExternalInput")
        return dr[name]

    xw = din("xw", [WTOK, D])
    cs = din("cs", [WTOK, 16])
    norm1_g = din("norm1_g", [1, D])
    w_in = [din("win_%d" % i, [D, cw]) for i, (c0, cw, kind, bi) in enumerate(col_blocks())]
    q_norm_g = din("q_norm_g", [1, 64])
    k_norm_g = din("k_norm_g", [1, 64])
    ident_d = din("ident_h", [128, 128])
    qT_s = P.dram("qT_s", [8, 128, NTOK], BF16)
    kT_s = P.dram("kT_s", [8, 128, WTOK], BF16)
    v_s = P.dram("v_s", [WTOK, 1024], BF16)
    prw = P.dram("prw", [WTOK + 1, RWW], F32)
    sg_s = P.dram("sg_s", [NTOK, 4096], BF16)
    outs = {}
    ya_s = P.dram("ya_s", [NTOK, 1024], F32)
    ybT_s = P.dram("ybT_s", [1024, NTOK], BF16)
    if debug:
        dbg = debug if isinstance(debug, (list, tuple, set)) else ("qT", "kT", "v", "prw", "sg")
        shp = {"qT": ([8, 128, NTOK], BF16), "kT": ([8, 128, WTOK], BF16), "v": ([WTOK, 1024], BF16),
               "prw": ([WTOK + 1, RWW], F32), "sg": ([NTOK, 4096], BF16), "ya": ([NTOK, 1024], F32), "ybT": ([1024, NTOK], BF16),
               "x1": ([NTOK, D], F32), "slot": ([16, 128, 2], I32), "gate": ([16, 128, 2], F32)}
        for k in dbg:
            outs["dbg_" + k] = nc.dram_tensor("dbg_" + k, shp[k][0], shp[k][1], kind="ExternalOutput")
        qT_s = outs.get("dbg_qT", qT_s); kT_s = outs.get("dbg_kT", kT_s); v_s = outs.get("dbg_v", v_s)
        prw = outs.get("dbg_prw", prw); sg_s = outs.get("dbg_sg", sg_s); ya_s = outs.get("dbg_ya", ya_s)
        ybT_s = outs.get("dbg_ybT", ybT_s)
    if not outs and "M" not in stages:
        outs["dummy_out"] = nc.dram_tensor("dummy_out", [1, 64], F32, kind="ExternalOutput")
    validT = din("validT", [128, 32])
    tri_d = din("tri_h", [128, 128])
    lamv = [din(n, [1, 64]) for n in ("lam_q1", "lam_k1", "lam_q2", "lam_k2")]
    subln_g = din("subln_g", [1, 128])

    ident = P.sb("ident", [128, 128], F32)
    identb = P.sb("identb", [128, 128], BF16)
    epsb = P.sb("epsb", [128, 4], F32)
    EPSB[0] = epsb
    P.op("dve", lambda e: e.memset(epsb[:, 0:1], 1e-6), writes=["epsb"])
    P.op("dve", lambda e: e.memset(epsb[:, 1:2], 1e-5), writes=["epsb"])
    P.op("dve", lambda e: e.memset(epsb[:, 2:3], 64e-5), writes=["epsb"])
    P.op("dve", lambda e: e.memset(epsb[:, 3:4], 1.0), writes=["epsb"])
    P.dma("sp", ident[:], ident_d.ap(), writes=["ident"])
    P.op("dve", lambda e: e.tensor_copy(identb[:], ident[:]), reads=["ident"], writes=["identb"])
    P.push()
    zrow = P.sb("zrow", [1, RWW], F32)
    P.op("dve", lambda e: e.memset(zrow[:], 0.0), writes=["zrow"])
    P.dma("sp", prw.ap()[0:1, :], zrow[:], reads=["zrow"], writes=["prw"])
    P.pop()

    if "A" in stages:
        P.push()
        g1b = P.sb("g1b", [128, D], F32)
        qgb = P.sb("qgb", [128, 512], F32)
        kgb = P.sb("kgb", [128, 512], F32)
        P.dma("sp", g1b[:], AP(norm1_g, 0, [[0, 128], [1, D]]), writes=["g1b"])
        for j in range(8):
            P.dma("sp", qgb[:, j * 64:(j + 1) * 64], AP(q_norm_g, 0, [[0, 128], [1, 64]]), writes=["qgb"])
            P.dma("sp", kgb[:, j * 64:(j + 1) * 64], AP(k_norm_g, 0, [[0, 128], [1, 64]]), writes=["kgb"])
        P.op("dve", lambda e: e.tensor_scalar(qgb[:], qgb[:], 0.125, None, ALU.mult), reads=["qgb"], writes=["qgb"])
        stage_A(P, nc, xw, cs, w_in, ident, identb, g1b, qgb, kgb, qT_s, kT_s, v_s, prw, sg_s)
        P.pop()
    if "B" in stages:
        P.push()
        stage_B(P, nc, qT_s, kT_s, v_s, validT, tri_d, lamv, subln_g, ya_s)
        P.pop()

    if "R" in stages:
        P.push()
        stage_R(P, nc, din, prw, ident, identb, ybT_s)
        P.pop()

    if "O" in stages:
        mgT_s = P.dram("mgT_s", [16, 128, 16, 128], BF16)
        x1_s = outs.get("dbg_x1") or P.dram("x1_s", [NTOK, D], F32)
        xs_d = P.dram("xs_d", [NSLOT + 1, D], BF16)
        slot_s = outs.get("dbg_slot") or P.dram("slot_s", [16, 128, 2], I32)
        gate_s = outs.get("dbg_gate") or P.dram("gate_s", [16, 128, 2], F32)
        P.push()
        stage_O1(P, nc, din, identb, ya_s, ybT_s, sg_s, mgT_s)
        P.pop()
        P.push()
        stage_O2(P, nc, din, ident, xw, mgT_s, x1_s, xs_d, slot_s, gate_s)
        P.pop()
    if "M" in stages:
        y_d = P.dram("y_d", [NSLOT + 1, D], F32)
        out_d = nc.dram_tensor("out", [NTOK, D], F32, kind="ExternalOutput")
        outs["out"] = out_d
        P.push()
        stage_M(P, nc, din, identb, xs_d, y_d, x1_s, slot_s, gate_s, out_d)
        P.pop()
        P.push()
        stage_M2(P, nc, y_d, x1_s, slot_s, gate_s, out_d)
        P.pop()

    P.wait_all("sp", list(P.lastw.keys()))
    P.emit()
    es.close()
    LASTP[0] = P
    return nc, list(outs.keys())


def col_blocks():
    blks = []
    for i in range(2):
        blks.append((i * 512, 512, "q", i))
    for i in range(2):
        blks.append((1024 + i * 512, 512, "k", i))
    for i in range(2):
        blks.append((2048 + i * 512, 512, "v", i))
    for i in range(7):
        w = 512 if i < 6 else RWW - 6 * 512
        blks.append((3072 + i * 512, w, "rw", i))
    for i in range(8):
        blks.append((6592 + i * 512, 512, "g", i))
    return blks


def stage_A(P, nc, xw, cs, w_in, ident, identb, g1b, qgb, kgb, qT_s, kT_s, v_s, prw, sg_s):
    hT = P.sb("hT", [128, 16, 16, 128], BF16)
    xt = [P.sb("xt%d" % i, [128, D], F32) for i in range(2)]
    ht = [P.sb("ht%d" % i, [128, D], F32) for i in range(2)]
    sq = P.sb("sqjunk", [128, D], BF16)
    st = [P.sb("st%d" % i, [128, 4], F32) for i in range(2)]
    wst = P.sb("wst", [128, 16, 512], F32)
    wbf = [P.sb("wbf%d" % i, [128, 16, 512], BF16) for i in range(2)]
    pT = [P.ps("pT%d" % i, [128, 4, 128], F32) for i in range(2)]
    pacc = [P.ps("pacc%d" % i, [128, 512], F32) for i in range(2)]
    pTb = [P.ps("pTb%d" % i, [128, 4, 128], BF16) for i in range(2)]
    ev = [P.sb("ev%d" % i, [128, 512], F32) for i in range(2)]
    evb = [P.sb("evb%d" % i, [128, 512], BF16) for i in range(2)]
    qn = [P.sb("qn%d" % i, [128, 512], F32) for i in range(2)]
    qr = [P.sb("qr%d" % i, [128, 512], BF16) for i in range(2)]
    qs = [P.sb("qs%d" % i, [128, 16], F32) for i in range(2)]
    cst = [P.sb("cst%d" % i, [128, 16], F32) for i in range(2)]
    tmp8 = [P.sb("tmp8_%d" % i, [128, 8, 16], F32) for i in range(2)]
    qTb = [P.sb("qTb%d" % i, [128, 4, 128], BF16) for i in range(2)]
    blks = col_blocks()
    items = [(g_, wblk) + blk for g_ in range(2) for wblk, blk in enumerate(blks) if not (g_ == 0 and blk[2] in ("q", "g"))]

    def issue_w(idx):
        g_, wblk, c0, cw, kind, bi = items[idx]
        wb = wbf[idx % 2]
        wk = "wbf%d" % (idx % 2)
        P.dma("sp", wst[:, :, 0:cw], w_in[wblk].ap().rearrange("(k p) c -> p k c", p=128), writes=["wst"])
        for hh in range(2):
            eng = "pool" if hh == 0 else "dve"
            P.op(eng, lambda e, wb=wb, hh=hh, cw=cw: e.tensor_copy(wb[:, hh * 8:(hh + 1) * 8, 0:cw], wst[:, hh * 8:(hh + 1) * 8, 0:cw]),
                 reads=["wst"], writes=[wk + "_%d" % hh])
    for grp in range(2):
        for t in range(16):
            gt = grp * 16 + t
            b = t % 2
            X, H, S = xt[b], ht[b], st[b]
            P.dma("sp", X[:], xw.ap()[gt * 128:(gt + 1) * 128, :], writes=["xt%d" % b])
            P.op("act", lambda e, X=X, S=S: e.activation(sq[:], X[:], AF.Square, accum_out=S[:, 0:1]),
                 reads=["xt%d" % b], writes=["sq", "st%d" % b])
            P.op("act", lambda e, S=S: e.activation(S[:, 1:2], S[:, 0:1], AF.Sqrt, bias=EPSB[0][:, 0:1], scale=1.0 / D),
                 reads=["st%d" % b, "epsb"], writes=["st%d" % b])
            P.op("dve", lambda e, S=S: e.reciprocal(S[:, 2:3], S[:, 1:2]), reads=["st%d" % b], writes=["st%d" % b])
            P.op("dve", lambda e, X=X, H=H, S=S: e.scalar_tensor_tensor(out=H[:], in0=X[:], scalar=S[:, 2:3], in1=g1b[:],
                                                                      op0=ALU.mult, op1=ALU.mult),
                 reads=["xt%d" % b, "st%d" % b, "g1b"], writes=["ht%d" % b])
            for c4 in range(4):
                pb = c4 % 2
                for j in range(4):
                    kc = c4 * 4 + j
                    P.op("pe", lambda e, H=H, kc=kc, pb=pb, j=j: e.transpose(pT[pb][:, j, :], H[:, kc * 128:(kc + 1) * 128], ident[:]),
                         reads=["ht%d" % b, "ident"], writes=["pT%d" % pb], signal=(j == 3))
                eng = "act" if c4 % 2 == 0 else "dve"
                if eng == "act":
                    P.op("act", lambda e, t=t, c4=c4, pb=pb: e.copy(hT[:, t, c4 * 4:(c4 + 1) * 4, :], pT[pb][:]),
                         reads=["pT%d" % pb], writes=["hT%d_%d" % (t, c4)])
                else:
                    P.op("dve", lambda e, t=t, c4=c4, pb=pb: e.tensor_copy(hT[:, t, c4 * 4:(c4 + 1) * 4, :], pT[pb][:]),
                         reads=["pT%d" % pb], writes=["hT%d_%d" % (t, c4)])
        for wblk, (c0, cw, kind, bi) in enumerate(blks):
            if grp == 0 and kind in ("q", "g"):
                continue
            idx = [i_ for i_, it in enumerate(items) if it[0] == grp and it[1] == wblk][0]
            if idx == 0:
                issue_w(0)
            if idx + 1 < len(items):
                issue_w(idx + 1)
            wb = wbf[idx % 2]
            wk = "wbf%d" % (idx % 2)
            for t in range(16):
                gt = grp * 16 + t
                tok0 = gt * 128
                pa = pacc[t % 2]
                pk = "pacc%d" % (t % 2)
                for kc in range(16):
                    P.op("pe", lambda e, pa=pa, t=t, kc=kc, wb=wb, cw=cw: e.matmul(pa[:, 0:cw], hT[:, t, kc, :], wb[:, kc, 0:cw],
                                                                              start=(kc == 0), stop=(kc == 15)),
                         reads=["hT%d_%d" % (t, kc // 4), wk + "_%d" % (kc // 8)], writes=[pk], signal=(kc == 15))
                b = t % 2
                if kind == "v":
                    P.op("act", lambda e, pa=pa, b=b: e.copy(evb[b][:], pa[:]), reads=[pk], writes=["evb%d" % b])
                    P.dma("act", v_s.ap()[tok0:tok0 + 128, bi * 512:(bi + 1) * 512], evb[b][:], reads=["evb%d" % b], writes=["v_s:%d:%d" % (gt, bi)])
                elif kind == "rw":
                    P.op("act", lambda e, pa=pa, b=b, cw=cw: e.copy(ev[b][:, 0:cw], pa[:, 0:cw]), reads=[pk], writes=["ev%d" % b])
                    P.dma("act", prw.ap()[1 + tok0:1 + tok0 + 128, bi * 512:bi * 512 + cw], ev[b][:, 0:cw], reads=["ev%d" % b], writes=["prw:%d:%d" % (gt, bi)])
                elif kind == "g":
                    P.op("act", lambda e, pa=pa, b=b: e.activation(evb[b][:], pa[:], AF.Sigmoid), reads=[pk], writes=["evb%d" % b])
                    P.dma("act", sg_s.ap()[t * 128:(t + 1) * 128, bi * 512:(bi + 1) * 512], evb[b][:], reads=["evb%d" % b], writes=["sg_s:%d:%d" % (t, bi)])
                else:
                    gb_ = qgb if kind == "q" else kgb
                    QN, QS, QR, CS, T8 = qn[b], qs[b], qr[b], cst[b], tmp8[b]
                    P.dma("pool", CS[:], cs.ap()[tok0:tok0 + 128, :], writes=["cst%d" % b])
                    P.op("act", lambda e, pa=pa, QN=QN: e.activation(QN[:], pa[:], AF.Square), reads=[pk], writes=["qn%d" % b])
                    P.op("dve", lambda e, QN=QN, QS=QS: e.tensor_reduce(QS[:, 0:8], QN[:].rearrange("p (g d) -> p g d", d=64), AX.X, ALU.add),
                         reads=["qn%d" % b], writes=["qs%d" % b])
                    P.op("act", lambda e, QS=QS: e.activation(QS[:, 0:8], QS[:, 0:8], AF.Sqrt, bias=EPSB[0][:, 0:1], scale=1.0 / 64),
                         reads=["qs%d" % b, "epsb"], writes=["qs%d" % b])
                    P.op("dve", lambda e, QS=QS: e.reciprocal(QS[:, 8:16], QS[:, 0:8]), reads=["qs%d" % b], writes=["qs%d" % b])
                    P.op("dve", lambda e, pa=pa, QN=QN, QS=QS: e.tensor_tensor(
                        QN[:].rearrange("p (g d) -> p g d", d=64), pa[:].rearrange("p (g d) -> p g d", d=64),
                        AP(QS, 8, [[16, 128], [1, 8], [0, 64]]), ALU.mult),
                        reads=[pk, "qs%d" % b], writes=["qn%d" % b])
                    P.op("dve", lambda e, QN=QN, gb_=gb_: e.tensor_tensor(QN[:], QN[:], gb_[:], ALU.mult),
                         reads=["qn%d" % b, "qgb", "kgb"], writes=["qn%d" % b])
                    x1 = AP(QN, 0, [[512, 128], [64, 8], [1, 8]])
                    x2 = AP(QN, 8, [[512, 128], [64, 8], [1, 8]])
                    cosb = AP(CS, 0, [[16, 128], [0, 8], [1, 8]])
                    sinb = AP(CS, 8, [[16, 128], [0, 8], [1, 8]])
                    t_a = AP(T8, 0, [[128, 128], [16, 8], [1, 8]])
                    t_b = AP(T8, 8, [[128, 128], [16, 8], [1, 8]])
                    P.op("dve", lambda e, t_a=t_a, x2=x2, sinb=sinb: e.tensor_tensor(t_a, x2, sinb, ALU.mult), reads=["qn%d" % b, "cst%d" % b], writes=["tmp8_%d" % b])
                    P.op("dve", lambda e, t_b=t_b, x1=x1, sinb=sinb: e.tensor_tensor(t_b, x1, sinb, ALU.mult), reads=["qn%d" % b, "cst%d" % b], writes=["tmp8_%d" % b])
                    P.op("dve", lambda e, x1=x1, cosb=cosb: e.tensor_tensor(x1, x1, cosb, ALU.mult), reads=["qn%d" % b, "cst%d" % b], writes=["qn%d" % b])
                    P.op("dve", lambda e, x2=x2, cosb=cosb: e.tensor_tensor(x2, x2, cosb, ALU.mult), reads=["qn%d" % b, "cst%d" % b], writes=["qn%d" % b])
                    P.op("dve", lambda e, x1=x1, t_a=t_a: e.tensor_tensor(x1, x1, t_a, ALU.subtract), reads=["qn%d" % b, "tmp8_%d" % b], writes=["qn%d" % b])
                    P.op("dve", lambda e, x2=x2, t_b=t_b: e.tensor_tensor(x2, x2, t_b, ALU.add), reads=["qn%d" % b, "tmp8_%d" % b], writes=["qn%d" % b])
                    P.op("act", lambda e, QN=QN, QR=QR: e.copy(QR[:], QN[:]), reads=["qn%d" % b], writes=["qr%d" % b])
                    for j in range(4):
                        P.op("pe", lambda e, QR=QR, j=j, b=b: e.transpose(pTb[b][:, j, :], QR[:, j * 128:(j + 1) * 128], identb[:]),
                             reads=["qr%d" % b, "identb"], writes=["pTb%d" % b], signal=(j == 3))
                    P.op("act", lambda e, b=b: e.copy(qTb[b][:], pTb[b][:]), reads=["pTb%d" % b], writes=["qTb%d" % b])
                    if kind == "q":
                        dst = qT_s.ap()[bi * 4:(bi + 1) * 4, :, t * 128:(t + 1) * 128].rearrange("h p t -> p h t")
                        P.dma("act", dst, qTb[b][:], reads=["qTb%d" % b], writes=["qT_s:%d:%d" % (t, bi)])
                    else:
                        dst = kT_s.ap()[bi * 4:(bi + 1) * 4, :, tok0:tok0 + 128].rearrange("h p t -> p h t")
                        P.dma("act", dst, qTb[b][:], reads=["qTb%d" % b], writes=["kT_s:%d:%d" % (gt, bi)])


def stage_B(P, nc, qT_s, kT_s, v_s, validT, tri_d, lamv, subln_g, ya_s):
    LAM_INIT = 0.2
    trif = P.sb("trif", [128, 128], F32)
    tri = P.sb("tri", [128, 128], BF16)
    P.dma("sp", trif[:], tri_d.ap(), writes=["trif"])
    P.op("dve", lambda e: e.tensor_copy(tri[:], trif[:]), reads=["trif"], writes=["tri"])
    validc = P.sb("validc", [128, 32], F32)
    P.dma("sp", validc[:], validT.ap(), writes=["validc"])
    sgb = P.sb("sgb", [128, 128], F32)
    P.dma("sp", sgb[:], AP(subln_g, 0, [[0, 128], [1, 128]]), writes=["sgb"])
    P.op("dve", lambda e: e.tensor_scalar(sgb[:], sgb[:], 1.0 - LAM_INIT, None, ALU.mult), reads=["sgb"], writes=["sgb"])
    lv = P.sb("lv", [1, 4, 64], F32)
    for i in range(4):
        P.dma("sp", lv[:, i, :], lamv[i].ap(), writes=["lv"])
    lp = P.sb("lp", [1, 2, 64], F32)
    ls = P.sb("ls", [1, 8], F32)
    ones1 = P.sb("ones1", [1, 128], F32)
    nlamb = P.sb("nlamb", [128, 1], F32)
    plam = P.ps("plam", [128, 2], F32)
    P.op("dve", lambda e: e.memset(ones1[:], 1.0), writes=["ones1"])
    P.op("dve", lambda e: e.memset(ls[:], 0.0), writes=["ls"])
    P.op("dve", lambda e: e.tensor_tensor(lp[:, 0, :], lv[:, 0, :], lv[:, 1, :], ALU.mult), reads=["lv"], writes=["lp"])
    P.op("dve", lambda e: e.tensor_tensor(lp[:, 1, :], lv[:, 2, :], lv[:, 3, :], ALU.mult), reads=["lv"], writes=["lp"])
    P.op("dve", lambda e: e.tensor_reduce(ls[:, 0:2], lp[:], AX.X, ALU.add), reads=["lp"], writes=["ls"])
    P.op("act", lambda e: e.activation(ls[:, 2:4], ls[:, 0:2], AF.Exp), reads=["ls"], writes=["ls"])
    P.op("dve", lambda e: e.tensor_tensor(ls[:, 4:5], ls[:, 3:4], ls[:, 2:3], ALU.subtract), reads=["ls"], writes=["ls"])
    P.op("dve", lambda e: e.tensor_scalar(ls[:, 6:8], ls[:, 4:6], -LAM_INIT, None, ALU.add), reads=["ls"], writes=["ls"])
    P.op("pe", lambda e: e.matmul(plam[:, 0:2], ones1[:], ls[:, 6:8], start=True, stop=True), reads=["ones1", "ls"], writes=["plam"])
    P.op("dve", lambda e: e.tensor_copy(nlamb[:], plam[:, 0:1]), reads=["plam"], writes=["nlamb"])

    qT = [P.sb("aqT%d" % i, [128, NTOK], BF16) for i in range(2)]
    kT = [P.sb("akT%d" % i, [128, WTOK], BF16) for i in range(2)]
    Vh = [P.sb("aVh%d" % i, [128, 32, 130], BF16) for i in range(2)]
    psS = [P.ps("psS%d" % i, [128, 512], F32) for i in range(2)]
    Oacc = [P.ps("Oacc%d" % i, [128, 512], F32) for i in range(4)]
    PT = [P.sb("aPT%d" % i, [128, 512], BF16) for i in range(3)]
    Oc = [P.sb("aOc%d" % i, [128, 4, 130], F32) for i in range(2)]
    rl = P.sb("arl", [128, 8], F32)
    ssq[0] = P.sb("assq", [128, 4], F32)
    otmp = P.sb("aotmp", [128, 128], F32)
    obuf = P.sb("aobuf", [128, 128], F32)
    osq = P.sb("aosq", [128, 128], BF16)
    yat = [P.sb("ayat%d" % i, [128, 128], F32) for i in range(2)]
    nexp = 0
    nya = 0
    for h in range(8):
        hb = h % 2
        Q, Kt, V = qT[hb], kT[hb], Vh[hb]
        P.dma("sp", Q[:], qT_s.ap()[h], reads=["qT_s:%d:%d" % (t, h // 4) for t in range(16)], writes=["aqT%d" % hb])
        P.dma("sp", Kt[:], kT_s.ap()[h], reads=["kT_s:%d:%d" % (t, h // 4) for t in range(32)], writes=["akT%d" % hb])
        P.dma("sp", V[:, :, 0:128], v_s.ap()[:, h * 128:(h + 1) * 128].rearrange("(kt p) d -> p kt d", p=128),
              reads=["v_s:%d:%d" % (t, h // 4) for t in range(32)], writes=["aVh%d" % hb])
        P.op("dve", lambda e, V=V: e.tensor_copy(V[:, :, 128:129], validc[:].rearrange("p (k o) -> p k o", o=1)),
             reads=["validc"], writes=["aVh%d" % hb])
        for G in range(4):
            for c in range(2):
                nkt = 16 + 4 * G + 4
                for kt in range(nkt):
                    sb_ = kt % 2
                    P.op("pe", lambda e, sb_=sb_, c=c, kt=kt, G=G, Q=Q, Kt=Kt: e.matmul(
                        psS[sb_][:], Kt[c * 64:(c + 1) * 64, kt * 128:(kt + 1) * 128], Q[c * 64:(c + 1) * 64, G * 512:(G + 1) * 512],
                        start=True, stop=True), reads=["aqT%d" % hb, "akT%d" % hb], writes=["psS%d" % sb_])
                    pb = nexp % 3
                    nexp += 1
                    pt = PT[pb]
                    P.op("act", lambda e, pt=pt, sb_=sb_: e.activation(pt[:], psS[sb_][:], AF.Exp), reads=["psS%d" % sb_], writes=["aPT%d" % pb])
                    rel = kt - (16 + 4 * G)
                    for j in range(4):
                        if rel > j:
                            continue
                        if rel == j:
                            P.op("dve", lambda e, pt=pt, j=j: e.tensor_tensor(pt[:, j * 128:(j + 1) * 128], pt[:, j * 128:(j + 1) * 128], tri[:], ALU.mult),
                                 reads=["aPT%d" % pb, "tri"], writes=["aPT%d" % pb])
                        last = (kt == 16 + 4 * G + j)
                        P.op("pe", lambda e, pt=pt, j=j, kt=kt, V=V, last=last: e.matmul(
                            Oacc[j][:, 0:129], pt[:, j * 128:(j + 1) * 128], V[:, kt, 0:129],
                            start=(kt == 0), stop=last),
                            reads=["aPT%d" % pb, "aVh%d" % hb], writes=["Oacc%d" % j], signal=last)
                for a in range(4):
                    P.op("act", lambda e, a=a, c=c: e.copy(Oc[c][:, a, 0:129], Oacc[a][:, 0:129]), reads=["Oacc%d" % a], writes=["aOc%d" % c])
            P.op("dve", lambda e: e.reciprocal(rl[:, 0:4], Oc[0][:, :, 128]), reads=["aOc0"], writes=["arl"])
            P.op("dve", lambda e: e.reciprocal(rl[:, 4:8], Oc[1][:, :, 128]), reads=["aOc1"], writes=["arl"])
            P.op("dve", lambda e: e.tensor_scalar(rl[:, 4:8], rl[:, 4:8], nlamb[:, 0:1], None, ALU.mult), reads=["arl", "nlamb"], writes=["arl"])
            for j in range(4):
                yb_ = nya % 2
                nya += 1
                Y = yat[yb_]
                P.op("dve", lambda e, j=j: e.tensor_scalar(otmp[:], Oc[0][:, j, 0:128], rl[:, j:j + 1], None, ALU.mult), reads=["aOc0", "arl"], writes=["aotmp"])
                P.op("dve", lambda e, j=j: e.scalar_tensor_tensor(out=obuf[:], in0=Oc[1][:, j, 0:128], scalar=rl[:, 4 + j:5 + j], in1=otmp[:],
                                                                  op0=ALU.mult, op1=ALU.add), reads=["aOc1", "arl", "aotmp"], writes=["aobuf"])
                P.op("act", lambda e: e.activation(osq[:], obuf[:], AF.Square, accum_out=ssq[0][:, 0:1]),
                     reads=["aobuf"], writes=["aosq", "assq"])
                P.op("act", lambda e: e.activation(ssq[0][:, 1:2], ssq[0][:, 0:1], AF.Sqrt, bias=EPSB[0][:, 1:2], scale=1.0 / 128), reads=["assq", "epsb"], writes=["assq"])
                P.op("dve", lambda e: e.reciprocal(ssq[0][:, 2:3], ssq[0][:, 1:2]), reads=["assq"], writes=["assq"])
                P.op("dve", lambda e, Y=Y: e.scalar_tensor_tensor(out=Y[:], in0=obuf[:], scalar=ssq[0][:, 2:3], in1=sgb[:], op0=ALU.mult, op1=ALU.mult),
                     reads=["aobuf", "assq", "sgb"], writes=["ayat%d" % yb_])
                qt = 4 * G + j
                P.dma("sp", ya_s.ap()[qt * 128:(qt + 1) * 128, h * 128:(h + 1) * 128], Y[:], reads=["ayat%d" % yb_], writes=["ya_s:%d:%d" % (qt, h)])


ssq = [None]


EPSB = [None]
LASTP = [None]


def rope_table():
    inv_freq = (500000.0 ** (-np.arange(0, 16, 2, dtype=np.float32) / 16)).astype(np.float32)
    ang = np.arange(4096, dtype=np.float32)[:, None] * inv_freq[None, :]
    return np.cos(ang).astype(np.float32), np.sin(ang).astype(np.float32)


def make_in_maps(I, ncores=NCORES, with_experts=True):
    cos, sin = rope_table()
    cst = host_consts()
    maps = []
    for c in range(ncores):
        b, sh = c // 2, c % 2
        x = I["x"][b]
        xwin = np.zeros((WTOK, D), np.float32)
        cswin = np.zeros((WTOK, 16), np.float32)
        if sh == 0:
            xwin[NTOK:] = x[:NTOK]
            cswin[NTOK:, 0:8] = cos[:NTOK]
            cswin[NTOK:, 8:16] = sin[:NTOK]
            cswin[:NTOK, 0:8] = 1.0
        else:
            xwin[:] = x
            cswin[:, 0:8] = cos
            cswin[:, 8:16] = sin
        valid = np.ones((WTOK,), np.float32)
        if sh == 0:
            valid[:NTOK] = 0.0
        m = {"xw": xwin, "cs": cswin, "validT": np.ascontiguousarray(valid.reshape(32, 128).T)}
        for k in ("norm1_g", "q_norm_g", "k_norm_g", "lam_q1", "lam_k1", "lam_q2", "lam_k2", "subln_g"):
            m[k] = np.ascontiguousarray(I[k]).reshape(1, -1)
        for k in ("shift_mu", "w0", "a0", "k_k", "k_a"):
            m[k] = np.ascontiguousarray(I[k]).reshape(1, -1)
        for k in ("r_k", "lnx_g", "lnx_b"):
            m[k + "_c"] = np.ascontiguousarray(I[k].reshape(8, 128).T)
        for k in ("w_up", "a_up", "g_up"):
            m[k] = np.ascontiguousarray(I[k][0])
        m["proj_a"] = np.ascontiguousarray(I["proj_a"][0])
        m["proj_b"] = np.ascontiguousarray(I["proj_b"][0])
        for i in range(2):
            m["w_out_%d" % i] = np.ascontiguousarray(I["w_out"][0][i * 1024:(i + 1) * 1024])
        m["norm2_g"] = np.ascontiguousarray(I["norm2_g"]).reshape(1, -1)
        m["router_w"] = np.ascontiguousarray(np.concatenate([I["router_g"][0], I["router_e"][0]], axis=1))
        m["router_b"] = np.ascontiguousarray(np.concatenate([I["router_g_b"][0], I["router_e_b"][0]]).reshape(1, 36))
        m["ebase_h"] = (np.arange(32, dtype=np.float32) * CAP).reshape(1, 32)
        m["ts_h2"] = cst["ts_h"]
        if with_experts:
            for e in range(32):
                m["wg_%d" % e] = np.ascontiguousarray(I["w_gate_e"][0][e])
                m["wu_%d" % e] = np.ascontiguousarray(I["w_up_e"][0][e])
                m["wd_%d" % e] = np.ascontiguousarray(I["w_down_e"][0][e])
        for i, (c0, cw, kind, bi) in enumerate(col_blocks()):
            m["win_%d" % i] = np.ascontiguousarray(I["w_in"][0][:, c0:c0 + cw])
        m.update(cst)
        maps.append(m)
    return maps


def kernel(**inputs):
    I = {k: np.asarray(v) for k, v in inputs.items()}
    nc, onames = build(stages=("A", "B", "R", "O", "M"), debug=False)
    in_maps = make_in_maps(I, NCORES)
    res = run_bass_kernel_spmd(nc, in_maps, core_ids=list(range(NCORES)))
    out = np.empty((4, 4096, D), np.float32)
    for c in range(NCORES):
        b, sh = c // 2, c % 2
        out[b, sh * NTOK:(sh + 1) * NTOK] = np.asarray(res.results[c]["out"])
    return out


def rwkv_consts():
    s = np.arange(128)[:, None]
    t = np.arange(128)[None, :]
    c = {}
    c["cmat_h"] = ((s <= t).astype(np.float32) - (s <= 63).astype(np.float32))
    m2 = np.zeros((128, 2), np.float32)
    m2[:64, 0] = 1.0
    m2[64:, 1] = 1.0
    c["msk2_h"] = m2
    c["ts_h"] = (s < t).astype(np.float32)
    c["ti_h"] = (s <= t).astype(np.float32)
    c["tsl_h"] = (s > t).astype(np.float32)
    bd = np.zeros((128, 128), np.float32)
    bd[:64, :64] = 1.0
    bd[64:, 64:] = 1.0
    c["bd1_h"] = bd
    return c


def stage_R(P, nc, din, prw, ident, identb, ybT_s):
    shift_mu = din("shift_mu", [1, RWW])
    pv = {n: din(n, [1, 1024]) for n in ("w0", "a0", "k_k", "k_a")}
    colp = {n: din(n + "_c", [128, 8]) for n in ("r_k", "lnx_g", "lnx_b")}
    w_up = din("w_up", [96, 1024])
    a_up = din("a_up", [96, 1024])
    g_up = din("g_up", [256, 1024])
    cd = {n: din(n, [128, 2] if n == "msk2_h" else [128, 128]) for n in ("cmat_h", "msk2_h", "ts_h", "ti_h", "tsl_h", "bd1_h")}

    def ld(name, shape, src, dt=F32):
        t = P.sb(name, shape, dt)
        P.dma("sp", t[:], src, writes=[name])
        return t

    mu_b = ld("mu_b", [128, RWW], AP(shift_mu, 0, [[0, 128], [1, RWW]]))
    w0_b = ld("w0_b", [128, 1024], AP(pv["w0"], 0, [[0, 128], [1, 1024]]))
    a0_b = ld("a0_b", [128, 1024], AP(pv["a0"], 0, [[0, 128], [1, 1024]]))
    kk_b = ld("kk_b", [128, 1024], AP(pv["k_k"], 0, [[0, 128], [1, 1024]]))
    ka_b = ld("ka_b", [128, 1024], AP(pv["k_a"], 0, [[0, 128], [1, 1024]]))
    rk_c = ld("rk_c", [128, 8], colp["r_k"].ap())
    lg_c = ld("lg_c", [128, 8], colp["lnx_g"].ap())
    lb_c = ld("lb_c", [128, 8], colp["lnx_b"].ap())
    wup = P.sb("wup", [128, 1024], F32)
    aup = P.sb("aup", [128, 1024], F32)
    for t_, src_, nm_ in ((wup, w_up, "wup"), (aup, a_up, "aup")):
        P.op("dve", lambda e, t_=t_: e.memset(t_[:], 0.0), writes=[nm_])
        P.dma("sp", t_[0:96, :], src_.ap(), writes=[nm_])
    gup = ld("gup", [128, 2, 1024], g_up.ap().rearrange("(k p) c -> p k c", p=128))
    cmat = ld("cmat", [128, 128], cd["cmat_h"].ap())
    msk2 = ld("msk2", [128, 2], cd["msk2_h"].ap())
    bdf = ld("bdf", [128, 128], cd["bd1_h"].ap())
    tsf = ld("tsf", [128, 128], cd["ts_h"].ap())
    tif = ld("tif", [128, 128], cd["ti_h"].ap())
    tslf = ld("tslf", [128, 128], cd["tsl_h"].ap())
    TS = P.sb("TSb", [128, 128], BF16)
    TI = P.sb("TIb", [128, 128], BF16)
    TSL = P.sb("TSLb", [128, 128], BF16)
    bd1 = P.sb("bd1b", [128, 128], BF16)
    bd64 = P.sb("bd64", [128, 128], F32)
    P.op("dve", lambda e: e.tensor_copy(TS[:], tsf[:]), reads=["tsf"], writes=["TSb"])
    P.op("dve", lambda e: e.tensor_copy(TI[:], tif[:]), reads=["tif"], writes=["TIb"])
    P.op("dve", lambda e: e.tensor_copy(TSL[:], tslf[:]), reads=["tslf"], writes=["TSLb"])
    P.op("dve", lambda e: e.tensor_copy(bd1[:], bdf[:]), reads=["bdf"], writes=["bd1b"])
    P.op("dve", lambda e: e.tensor_scalar(bd64[:], bdf[:], 1.0 / 64, None, ALU.mult), reads=["bdf"], writes=["bd64"])

    P0 = P.sb("rP0", [128, RWW], F32)
    P1 = P.sb("rP1", [128, RWW], F32)
    LI = P.sb("rLI", [128, 512], F32)
    P.op("dve", lambda e: e.memset(LI[:], 0.0), writes=["rLI"])
    LIT = [P.sb("rLIT%d" % i, [128, 4, 128], F32) for i in range(2)]
    U = P.sb("rU", [128, 1024], F32)
    AS = P.sb("rAS", [128, 1024], F32)
    E1, E2, E3 = P1[:, 0:1024], P1[:, 1024:2048], P1[:, 2048:3072]
    KK = P.sb("rKK", [128, 1024], F32)
    T1 = P.sb("rT1", [128, 1024], F32)
    KM = P.sb("rKM", [128, 1024], F32)
    ss = P.sb("rss", [128, 48], F32)
    TM = [[P.sb("rTM%d_%d" % (k, i), [128, 1024], BF16) for i in range(1)] * 2 for k in range(5)]
    FF = [P.sb("rFF%d" % i, [128, 5, 8, 128], BF16) for i in range(1)] * 2
    SC = [P.sb("rSC%d" % i, [128, 8, 2], F32) for i in range(2)]
    slots = [P.ps("rsl%d" % i, [128, 4, 128], F32) for i in range(8)]
    psT = slots[4]
    pb = [slots[5][:].rearrange("p a c -> p (a c)"), slots[6][:].rearrange("p a c -> p (a c)")]
    psTb = slots[7].bitcast(BF16)[:].rearrange("p a (b c) -> p (a b) c", c=128)
    St = P.sb("rSt", [128, 8, 64], F32)
    Stb = P.sb("rStb", [128, 8, 64], BF16)
    Y2 = P.sb("rY2", [128, 8, 128], F32)
    P.op("dve", lambda e: e.memset(St[:], 0.0), writes=["rSt%d" % h for h in range(16)])
    P.op("dve", lambda e: e.memset(Stb[:], 0.0), writes=["rStb%d" % h for h in range(16)])
    NL = 8
    lane = []
    for l in range(NL):
        d = {}
        for n in ("Nab", "NabT", "Ma", "MaT", "Mb", "MbT", "Pm", "Nka", "Mbr", "Mkr", "AX", "WU", "E", "Pp"):
            d[n] = P.sb("rl%d_%s" % (l, n), [128, 128], BF16)
        for n in ("Yl", "Qp", "AXf"):
            d[n] = P.sb("rl%d_%s" % (l, n), [128, 128], F32)
        d["n"] = 0
        lane.append(d)
    fin = {n: P.sb("rf_" + n, [128, 128], F32) for n in ("YC", "SQ", "SD", "YN", "BN")}
    finb = {n: P.sb("rf_" + n, [128, 128], BF16) for n in ("RK",)}
    YB = [P.sb("rf_YB%d" % i, [128, 128], BF16) for i in range(2)]
    evq = [0]

    def slot(l):
        d = lane[l]
        i = d["n"] % 4
        d["n"] += 1
        return slots[l][:, i, :], "rsl%d" % l

    def ev_eng():
        evq[0] += 1
        return "dve" if evq[0] % 3 else "act"

    def mm(out, okey, lhsT, rhs, reads, start=True, stop=True):
        P.op("pe", lambda e: e.matmul(out, lhsT, rhs, start=start, stop=stop, skip_group_check=True), reads=reads, writes=[okey])

    _lo, _hi = (int(v) for v in os.environ.get('RCHUNKS', '0,32').split(','))
    for c in range(_lo, _hi):
        own = c >= 16
        t0 = c * 128
        cb = c % 2
        F, S_, Lt = FF[cb], SC[cb], LIT[cb]
        tmA, tmR, tmB, tmK, tmV = (TM[k][cb] for k in range(5))
        kA, kR, kB, kK, kV = ("rTM%d_0" % k for k in range(5))
        kF, kS, kL = "rFF0", "rSC%d" % cb, "rLIT%d" % cb
        rd = ["prw:%d:%d" % (c, b) for b in range(7)] + (["prw:%d:%d" % (c - 1, b) for b in range(7)] if c else ["prw"])
        P.dma("sp", P1[:], prw.ap()[1 + t0:1 + t0 + 128, :], reads=rd, writes=["rP1", "rE1", "rE2", "rE3"])
        P.dma("sp", P0[:], prw.ap()[t0:t0 + 128, :], reads=rd, writes=["rP0"])
        P.op("dve", lambda e: e.tensor_tensor(P0[:], P0[:], P1[:], ALU.subtract), reads=["rP0", "rP1"], writes=["rP0"])
        P.op("pool", lambda e: e.tensor_tensor(P0[:], P0[:], mu_b[:], ALU.mult), reads=["rP0", "mu_b"], writes=["rP0"])
        P.op("dve", lambda e: e.tensor_tensor(P0[:], P0[:], P1[:], ALU.add), reads=["rP0", "rP1"], writes=["rP0", "rE1", "rE2", "rE3"])
        Z = P0
        zr, zk, zv = Z[:, 0:1024], Z[:, 1024:2048], Z[:, 2048:3072]
        P.op("act", lambda e: e.activation(LI[:, 0:96], Z[:, 3072:3168], AF.Tanh), reads=["rP0"], writes=["rLI"])
        P.op("act", lambda e: e.copy(LI[:, 128:224], Z[:, 3168:3264]), reads=["rP0"], writes=["rLI"])
        P.op("act", lambda e: e.activation(LI[:, 256:512], Z[:, 3264:3520], AF.Sigmoid), reads=["rP0"], writes=["rLI"])
        for j_ in range(4):
            P.op("pe", lambda e, j_=j_: e.transpose(psT[:, j_, :], LI[:, j_ * 128:(j_ + 1) * 128], ident[:]), reads=["rLI", "ident"], writes=["rsl4"])
        P.op("act", lambda e, Lt=Lt: e.copy(Lt[:], psT[:]), reads=["rsl4"], writes=[kL])
        for hf in range(2):
            mm(pb[hf][:], "rsl%d" % (5 + hf), Lt[:, 0, :], wup[:, hf * 512:(hf + 1) * 512], [kL, "wup"])
            P.op("dve", lambda e, hf=hf: e.tensor_tensor(U[:, hf * 512:(hf + 1) * 512], pb[hf][:], w0_b[:, hf * 512:(hf + 1) * 512], ALU.add),
                 reads=["rsl%d" % (5 + hf), "w0_b"], writes=["rU"])
        P.op("act", lambda e: e.activation(U[:], U[:], AF.Sigmoid), reads=["rU"], writes=["rU"])
        P.op("dve", lambda e: e.tensor_scalar(U[:], U[:], -0.6065306597126334, None, ALU.mult), reads=["rU"], writes=["rU"])
        for hf in range(2):
            mm(pb[hf][:], "rsl%d" % (5 + hf), Lt[:, 1, :], aup[:, hf * 512:(hf + 1) * 512], [kL, "aup"])
            P.op("dve", lambda e, hf=hf: e.tensor_tensor(AS[:, hf * 512:(hf + 1) * 512], pb[hf][:], a0_b[:, hf * 512:(hf + 1) * 512], ALU.add),
                 reads=["rsl%d" % (5 + hf), "a0_b"], writes=["rAS"])
        P.op("act", lambda e: e.activation(AS[:], AS[:], AF.Sigmoid), reads=["rAS"], writes=["rAS"])
        for hf in range(2):
            hs_ = slice(hf * 512, (hf + 1) * 512)
            mm(pb[hf][:], "rsl%d" % (5 + hf), cmat[:], U[:, hs_], ["cmat", "rU"])
            P.op("act", lambda e, hf=hf, hs_=hs_: e.activation(E1[:, hs_], pb[hf][:], AF.Exp), reads=["rsl%d" % (5 + hf)], writes=["rE1"])
            P.op("act", lambda e, hf=hf, hs_=hs_: e.activation(E2[:, hs_], pb[hf][:], AF.Exp, scale=-1.0), reads=["rsl%d" % (5 + hf)], writes=["rE2"])
            P.op("dve", lambda e, hf=hf, hs_=hs_: e.tensor_tensor(E3[:, hs_], pb[hf][:], U[:, hs_], ALU.subtract), reads=["rsl%d" % (5 + hf), "rU"], writes=["rE3"])
        P.op("act", lambda e: e.activation(E3, E3, AF.Exp), reads=["rE3"], writes=["rE3"])
        for hp in range(8):
            P.op("pe", lambda e, hp=hp: e.matmul(psT[:, 0, 2 * hp:2 * hp + 2], U[:, hp * 128:(hp + 1) * 128], msk2[:], start=True, stop=True, skip_group_check=True),
                 reads=["rU", "msk2", kL], writes=["rsl4"])
        P.op("act", lambda e, S_=S_: e.activation(S_[:].rearrange("p h t -> p (h t)"), psT[:, 0, 0:16], AF.Exp), reads=["rsl4"], writes=[kS])
        P.op("pool", lambda e: e.tensor_tensor(KK[:], zk, kk_b[:], ALU.mult), reads=["rP0", "kk_b"], writes=["rKK"])
        P.op("pool", lambda e: e.tensor_tensor(T1[:], KK[:], KK[:], ALU.mult), reads=["rKK"], writes=["rT1"])
        P.op("dve", lambda e: e.tensor_reduce(ss[:, 0:16], T1[:].rearrange("p (h d) -> p h d", d=64), AX.X, ALU.add), reads=["rT1"], writes=["rss"])
        P.op("act", lambda e: e.activation(ss[:, 0:16], ss[:, 0:16], AF.Sqrt), reads=["rss"], writes=["rss"])
        P.op("dve", lambda e: e.tensor_scalar(ss[:, 0:16], ss[:, 0:16], 1e-12, None, ALU.max), reads=["rss"], writes=["rss"])
        P.op("dve", lambda e: e.reciprocal(ss[:, 16:32], ss[:, 0:16]), reads=["rss"], writes=["rss"])
        P.op("dve", lambda e: e.tensor_tensor(KK[:].rearrange("p (h d) -> p h d", d=64), KK[:].rearrange("p (h d) -> p h d", d=64),
                                              AP(ss, 16, [[48, 128], [1, 16], [0, 64]]), ALU.mult), reads=["rKK", "rss"], writes=["rKK"])
        P.op("dve", lambda e: e.scalar_tensor_tensor(out=T1[:], in0=AS[:], scalar=-1.0, in1=ka_b[:], op0=ALU.add, op1=ALU.mult),
             reads=["rAS", "ka_b"], writes=["rT1"])
        P.op("dve", lambda e: e.scalar_tensor_tensor(out=KM[:], in0=T1[:], scalar=1.0, in1=zk, op0=ALU.add, op1=ALU.mult),
             reads=["rT1", "rP0"], writes=["rKM"])
        P.op("dve", lambda e, o=tmA: e.scalar_tensor_tensor(out=o[:], in0=KK[:], scalar=-1.0, in1=E3, op0=ALU.mult, op1=ALU.mult),
             reads=["rKK", "rE3"], writes=[kA])
        P.op("pool", lambda e: e.tensor_tensor(T1[:], KK[:], AS[:], ALU.mult), reads=["rKK", "rAS"], writes=["rT1"])
        P.op("dve", lambda e, o=tmB: e.tensor_tensor(o[:], T1[:], E2, ALU.mult), reads=["rT1", "rE2"], writes=[kB])
        P.op("pool", lambda e, o=tmR: e.tensor_tensor(o[:], zr, E1, ALU.mult), reads=["rP0", "rE1"], writes=[kR])
        P.op("dve", lambda e, o=tmK: e.tensor_tensor(o[:], KM[:], E2, ALU.mult), reads=["rKM", "rE2"], writes=[kK])
        P.op("act", lambda e, o=tmV: e.copy(o[:], zv), reads=["rP0"], writes=[kV])
        for kind, (tm, kk_) in enumerate(((tmA, kA), (tmR, kR), (tmB, kB), (tmK, kK), (tmV, kV))):
            if kind == 1 and not own:
                continue
            if kind == 4 and not own:
                continue
            for hp in range(8):
                P.op("pe", lambda e, tm=tm, hp=hp: e.transpose(psTb[:, hp, :], tm[:, hp * 128:(hp + 1) * 128], identb[:]),
                     reads=[kk_, "identb"], writes=["rsl7"], signal=(hp == 7))
            eng = "act" if kind % 2 else "dve"
            if eng == "act":
                P.op("act", lambda e, F=F, kind=kind: e.copy(F[:, kind, :, :], psTb[:]), reads=["rsl7"], writes=[kF + "_%d" % kind])
            else:
                P.op("dve", lambda e, F=F, kind=kind: e.tensor_copy(F[:, kind, :, :], psTb[:]), reads=["rsl7"], writes=[kF + "_%d" % kind])

        def head_gen(h, l, S_=S_, own=own, kS=kS):
            d = lane[l]
            par, hp = h % 2, h // 2
            eo = par * 64
            hs = slice(h * 64, (h + 1) * 64)
            es = slice(eo, eo + 64)
            kn = lambda n: "rl%d_%s" % (l, n)
            Af, Rf, Bf, Kf = (F[es, k, hp, :] for k in range(4))
            fk = [kF + "_%d" % k for k in range(5)]

            def evac(dst, dkey, src, skey, mask=None, mkey=None):
                P.op("act", lambda e: e.copy(dst, src), reads=[skey], writes=[dkey])
                if mask is not None:
                    P.op("pool", lambda e: e.tensor_tensor(dst, dst, mask, ALU.mult), reads=[dkey, mkey], writes=[dkey])

            o, ok = slot(l)
            mm(o, ok, Bf, Af, [fk[2], fk[0]])
            evac(d["Nab"][:], kn("Nab"), o, ok, TS[:], "TSb")
            o, ok = slot(l)
            mm(o, ok, Af, Bf, [fk[0], fk[2]])
            evac(d["NabT"][:], kn("NabT"), o, ok, TSL[:], "TSLb")
            P.op("pool", lambda e: e.tensor_tensor(d["Pm"][:], d["Nab"][:], identb[:], ALU.add), reads=[kn("Nab"), "identb"], writes=[kn("Pm")])
            yield
            if RSTOP < 2:
                return
            o, ok = slot(l)
            mm(o, ok, Kf, Af, [fk[3], fk[0]])
            evac(d["Nka"][:], kn("Nka"), o, ok, TS[:], "TSb")
            if own:
                o, ok = slot(l)
                mm(o, ok, Bf, Rf, [fk[2], fk[1]])
                evac(d["Mbr"][:], kn("Mbr"), o, ok, TI[:], "TIb")
                o, ok = slot(l)
                mm(o, ok, Kf, Rf, [fk[3], fk[1]])
                evac(d["Mkr"][:], kn("Mkr"), o, ok, TI[:], "TIb")
            yield
            if RSTOP < 3:
                return
            M, MT, kM, kMT = d["Nab"], d["NabT"], kn("Nab"), kn("NabT")
            for lev in range(1, 7):
                nM, nMT = (d["Ma"], d["MaT"]) if lev % 2 else (d["Mb"], d["MbT"])
                knM, knMT = (kn("Ma"), kn("MaT")) if lev % 2 else (kn("Mb"), kn("MbT"))
                if lev < 6:
                    o, ok = slot(l)
                    mm(o, ok, MT[:], M[:], [kMT, kM])
                    evac(nM[:], knM, o, ok)
                o, ok = slot(l)
                mm(o, ok, M[:], MT[:], [kM, kMT])
                evac(nMT[:], knMT, o, ok)
                yield
                o, ok = slot(l)
                mm(o, ok, nMT[:], d["Pm"][:], [knMT, kn("Pm")])
                P.op("dve", lambda e, o=o: e.tensor_tensor(d["Pm"][:], o, d["Pm"][:], ALU.add), reads=[ok, kn("Pm")], writes=[kn("Pm")])
                M, MT, kM, kMT = nM, nMT, knM, knMT
                yield
            if RSTOP < 4:
                return
            o, ok = slot(l)
            mm(o[:, 0:64], ok, d["Nka"][:], tmV[:, hs], [kn("Nka"), kV])
            evac(d["AX"][:, 64:128], kn("AX"), o[:, 0:64], ok)
            P.op("pool", lambda e: e.tensor_copy(d["AX"][:, 0:64], tmA[:, hs]), reads=[kA], writes=[kn("AX")])
            yield
            o, ok = slot(l)
            mm(o, ok, d["Pm"][:], d["AX"][:], [kn("Pm"), kn("AX")])
            evac(d["WU"][:], kn("WU"), o, ok)
            yield
            WT, UT = d["WU"][:, 0:64], d["WU"][:, 64:128]
            if RSTOP < 5:
                return
            o, ok = slot(l)
            mm(o[es, 0:64], ok, WT, tmB[:, hs], [kn("WU"), kB])
            P.op("dve", lambda e, o=o: e.tensor_tensor(d["Qp"][es, 64:128], o[es, 0:64], ident[es, es], ALU.add), reads=[ok, "ident"], writes=[kn("Qp") + "t"])
            P.op("dve", lambda e: e.tensor_scalar(d["Pp"][es, 0:64], d["Qp"][es, 64:128], S_[es, hp, 0:1], None, ALU.mult),
                 reads=[kn("Qp") + "t", kS], writes=[kn("Pp")])
            o, ok = slot(l)
            mm(o[es, 0:64], ok, tmB[:, hs], UT, [kB, kn("WU")], start=True, stop=False)
            mm(o[es, 0:64], ok, tmK[:, hs], tmV[:, hs], [kK, kV], start=False, stop=True)
            P.op("dve", lambda e, o=o: e.tensor_scalar(d["Qp"][es, 0:64], o[es, 0:64], S_[es, hp, 1:2], None, ALU.mult),
                 reads=[ok, kS], writes=[kn("Qp")])
            if own:
                o, ok = slot(l)
                mm(o[es, :], ok, WT, d["Mbr"][:], [kn("WU"), kn("Mbr")])
                P.op("dve", lambda e, o=o: e.tensor_tensor(d["Yl"][es, :], o[es, :], Rf, ALU.add), reads=[ok, fk[1]], writes=[kn("Yl") + "e"])
                P.op("dve", lambda e: e.tensor_scalar(d["E"][es, :], d["Yl"][es, :], S_[es, hp, 0:1], None, ALU.mult),
                     reads=[kn("Yl") + "e", kS], writes=[kn("E")])
                o, ok = slot(l)
                mm(o[es, :], ok, UT, d["Mbr"][:], [kn("WU"), kn("Mbr")], start=True, stop=False)
                mm(o[es, :], ok, tmV[:, hs], d["Mkr"][:], [kV, kn("Mkr")], start=False, stop=True)
                P.op("act", lambda e, o=o: e.copy(d["AXf"][es, :], o[es, :]), reads=[ok], writes=[kn("AXf")])
            yield
            if RSTOP < 6:
                return
            if own:
                o, ok = slot(l)
                mm(o[es, :], ok, Stb[es, hp, :], d["E"][es, :], ["rStb%d" % h, kn("E")])
                P.op("dve", lambda e, o=o: e.tensor_tensor(Y2[es, hp, :], o[es, :], d["AXf"][es, :], ALU.add), reads=[ok, kn("AXf")], writes=["rY2_%d" % h])
            o, ok = slot(l)
            mm(o[es, 0:64], ok, d["Pp"][es, 0:64], Stb[es, hp, :], [kn("Pp"), "rStb%d" % h])
            P.op("dve", lambda e, o=o: e.scalar_tensor_tensor(out=St[es, hp, :], in0=o[es, 0:64], scalar=S_[es, hp, 1:2], in1=d["Qp"][es, 0:64],
                                                             op0=ALU.mult, op1=ALU.add), reads=[ok, kS, kn("Qp")], writes=["rSt%d" % h])
            P.op("act", lambda e: e.copy(Stb[es, hp, :], St[es, hp, :]), reads=["rSt%d" % h], writes=["rStb%d" % h])
            yield

        for grp in range(2 if RSTOP >= 1 else 0):
            gens = [head_gen(grp * NL + l, l) for l in range(NL)]
            alive = list(gens)
            while alive:
                nxt = []
                for g in alive:
                    try:
                        next(g)
                        nxt.append(g)
                    except StopIteration:
                        pass
                alive = nxt
        if own and RSTOP >= 7:
            for hp in range(8):
                l = hp % NL
                yk = ["rY2_%d" % (2 * hp), "rY2_%d" % (2 * hp + 1)]
                o, ok = slot(l)
                mm(o, ok, bd64[:], Y2[:, hp, :], ["bd64"] + yk)
                P.op("dve", lambda e, o=o, hp=hp: e.tensor_tensor(fin["YC"][:], Y2[:, hp, :], o, ALU.subtract), reads=yk + [ok], writes=["rfYC"])
                P.op("act", lambda e: e.activation(fin["SQ"][:], fin["YC"][:], AF.Square), reads=["rfYC"], writes=["rfSQ"])
                o, ok = slot(l)
                mm(o, ok, bd64[:], fin["SQ"][:], ["bd64", "rfSQ"])
                P.op("act", lambda e, o=o: e.activation(fin["SD"][:], o, AF.Sqrt, bias=EPSB[0][:, 2:3], scale=1.0), reads=[ok, "epsb"], writes=["rfSD"])
                P.op("dve", lambda e: e.reciprocal(fin["SD"][:], fin["SD"][:]), reads=["rfSD"], writes=["rfSD"])
                P.op("dve", lambda e: e.tensor_tensor(fin["YN"][:], fin["YC"][:], fin["SD"][:], ALU.mult), reads=["rfYC", "rfSD"], writes=["rfYN"])
                P.op("dve", lambda e, hp=hp: e.tensor_scalar(fin["YN"][:], fin["YN"][:], lg_c[:, hp:hp + 1], lb_c[:, hp:hp + 1], ALU.mult, ALU.add),
                     reads=["rfYN", "lg_c", "lb_c"], writes=["rfYN"])
                P.op("dve", lambda e, hp=hp: e.scalar_tensor_tensor(out=finb["RK"][:], in0=F[:, 1, hp, :], scalar=rk_c[:, hp:hp + 1], in1=F[:, 3, hp, :],
                                                                    op0=ALU.mult, op1=ALU.mult), reads=[kF + "_1", kF + "_3", "rk_c"], writes=["rfRK"])
                o, ok = slot(l)
                mm(o, ok, bd1[:], finb["RK"][:], ["bd1b", "rfRK"])
                P.op("dve", lambda e, o=o, hp=hp: e.tensor_tensor(fin["BN"][:], o, F[:, 4, hp, :], ALU.mult), reads=[ok, kF + "_4"], writes=["rfBN"])
                P.op("dve", lambda e: e.tensor_tensor(fin["YN"][:], fin["YN"][:], fin["BN"][:], ALU.add), reads=["rfYN", "rfBN"], writes=["rfYN"])
                o, ok = slot(l)
                mm(o, ok, gup[:, 0, hp * 128:(hp + 1) * 128], Lt[:, 2, :], ["gup", kL], start=True, stop=False)
                mm(o, ok, gup[:, 1, hp * 128:(hp + 1) * 128], Lt[:, 3, :], ["gup", kL], start=False, stop=True)
                yb = YB[hp % 2]
                P.op("dve", lambda e, o=o, yb=yb: e.tensor_tensor(yb[:], fin["YN"][:], o, ALU.mult), reads=["rfYN", ok], writes=["rfYB%d" % (hp % 2)])
                q0 = (c - 16) * 128
                P.dma("sp", ybT_s.ap()[hp * 128:(hp + 1) * 128, q0:q0 + 128], yb[:], reads=["rfYB%d" % (hp % 2)], writes=["ybT:%d:%d" % (c - 16, hp)])


def stage_M(P, nc, din, identb, xs_d, y_d, x1_s, slot_s, gate_s, out_d):
    wg = [din("wg_%d" % e, [D, 1024]) for e in range(32)]
    wu = [din("wu_%d" % e, [D, 1024]) for e in range(32)]
    wd = [din("wd_%d" % e, [1024, D]) for e in range(32)]
    NS = CAP // 128
    xs = [P.sb("mxs%d" % i, [128, D], BF16) for i in range(2 * NS)]
    xT = P.sb("mxT", [128, 16, CAP], BF16)
    NST = 4
    wst = [P.sb("mwst%d" % i, [128, 16, 256], F32) for i in range(NST)]
    wgb = [P.sb("mwgb%d" % i, [128, 16, 256], BF16) for i in range(2)]
    wub = [P.sb("mwub%d" % i, [128, 16, 256], BF16) for i in range(2)]
    wdb = [P.sb("mwdb%d" % i, [128, 8, 512], BF16) for i in range(2)]
    hT = P.sb("mhT", [128, 8, CAP], BF16)
    sl = P.sb("msl", [128, CAP], F32)
    yo = [P.sb("myo%d" % i, [128, 512], F32) for i in range(2)]
    psTb = P.ps("mpsTb", [128, 8, 128], BF16)
    psG = [P.ps("mpsG%d" % i, [128, 512], F32) for i in range(2)]
    psU = [P.ps("mpsU%d" % i, [128, 512], F32) for i in range(2)]
    psY = [P.ps("mpsY%d" % i, [128, 512], F32) for i in range(2)]
    zr = P.sb("mzr", [1, D], F32)
    P.op("dve", lambda e: e.memset(zr[:], 0.0), writes=["mzr"])
    P.dma("sp", y_d.ap()[NSLOT:NSLOT + 1, :], zr[:], reads=["mzr"], writes=["y_dump"])
    ncast = [0]
    nst = [0]

    def cast(dst, src, rk, wk):
        ncast[0] += 1
        eng = ("dve", "pool", "act")[ncast[0] % 3]
        if eng == "act":
            P.op("act", lambda e: e.copy(dst, src), reads=[rk], writes=[wk])
        else:
            P.op(eng, lambda e: e.tensor_copy(dst, src), reads=[rk], writes=[wk])

    items = []
    for ex in range(32):
        items += [("gu", ex, cb) for cb in range(4)] + [("dn", ex, cb) for cb in range(4)]

    def issue_w(i):
        kind, ex, cb = items[i]
        bb = i % 2
        if kind == "gu":
            for (wsrc, wdst, nm) in ((wg[ex], wgb[bb], "mwgb%d" % bb), (wu[ex], wub[bb], "mwub%d" % bb)):
                st_i = nst[0] % NST
                nst[0] += 1
                W = wst[st_i]
                P.dma("sp", W[:], wsrc.ap()[:, cb * 256:(cb + 1) * 256].rearrange("(k p) c -> p k c", p=128), writes=["mwst%d" % st_i])
                cast(wdst[:], W[:], "mwst%d" % st_i, nm)
        else:
            st_i = nst[0] % NST
            nst[0] += 1
            Wv = wst[st_i][:].rearrange("p (a k) c -> p a (k c)", a=8)
            P.dma("sp", Wv, wd[ex].ap()[:, cb * 512:(cb + 1) * 512].rearrange("(k p) c -> p k c", p=128), writes=["mwst%d" % st_i])
            cast(wdb[bb][:], Wv, "mwst%d" % st_i, "mwdb%d" % bb)

    def load_xs(ex):
        for s_ in range(NS):
            xi = (ex * NS + s_) % (2 * NS)
            r0 = ex * CAP + s_ * 128
            P.dma("sp", xs[xi][:], xs_d.ap()[r0:r0 + 128, :], reads=["xs_all"], writes=["mxs%d" % xi])

    load_xs(0)
    issue_w(0)
    xk = ["mxT_%d_%d" % (s_, half) for s_ in range(NS) for half in range(2)]
    hk = ["mhT_%d" % i for i in range(8)]
    for i, (kind, ex, cb) in enumerate(items):
        bb = i % 2
        if i + 1 < len(items):
            issue_w(i + 1)
        if kind == "gu" and cb == 0:
            for s_ in range(NS):
                xi = (ex * NS + s_) % (2 * NS)
                X = xs[xi]
                for half in range(2):
                    for k in range(8):
                        kc = half * 8 + k
                        P.op("pe", lambda e, X=X, k=k, kc=kc: e.transpose(psTb[:, k, :], X[:, kc * 128:(kc + 1) * 128], identb[:]),
                             reads=["mxs%d" % xi, "identb"], writes=["mpsTb"], signal=(k == 7))
                    P.op("dve", lambda e, half=half, s_=s_: e.tensor_copy(xT[:, half * 8:(half + 1) * 8, s_ * 128:(s_ + 1) * 128], psTb[:]),
                         reads=["mpsTb"], writes=["mxT_%d_%d" % (s_, half)])
            if ex + 1 < 32:
                load_xs(ex + 1)
        if kind == "gu":
            for hc in range(2):
                pg, pu = psG[hc], psU[hc]
                for k in range(16):
                    P.op("pe", lambda e, pg=pg, k=k, hc=hc, bb=bb: e.matmul(pg[:, 0:CAP], wgb[bb][:, k, hc * 128:(hc + 1) * 128], xT[:, k, :], start=(k == 0), stop=(k == 15)),
                         reads=["mwgb%d" % bb] + xk, writes=["mpsG%d" % hc], signal=(k == 15))
                for k in range(16):
                    P.op("pe", lambda e, pu=pu, k=k, hc=hc, bb=bb: e.matmul(pu[:, 0:CAP], wub[bb][:, k, hc * 128:(hc + 1) * 128], xT[:, k, :], start=(k == 0), stop=(k == 15)),
                         reads=["mwub%d" % bb] + xk, writes=["mpsU%d" % hc], signal=(k == 15))
                hi = cb * 2 + hc
                P.op("act", lambda e, pg=pg: e.activation(sl[:], pg[:, 0:CAP], AF.Silu), reads=["mpsG%d" % hc], writes=["msl"])
                P.op("dve", lambda e, pu=pu, hi=hi: e.tensor_tensor(hT[:, hi, :], sl[:], pu[:, 0:CAP], ALU.mult), reads=["msl", "mpsU%d" % hc], writes=["mhT_%d" % hi])
        else:
            for s_ in range(NS):
                py = psY[s_ % 2]
                for k in range(8):
                    P.op("pe", lambda e, py=py, k=k, s_=s_, bb=bb: e.matmul(py[:], hT[:, k, s_ * 128:(s_ + 1) * 128], wdb[bb][:, k, :], start=(k == 0), stop=(k == 7)),
                         reads=hk + ["mwdb%d" % bb], writes=["mpsY%d" % (s_ % 2)], signal=(k == 7))
                Y = yo[s_ % 2]
                P.op("act", lambda e, py=py, Y=Y: e.copy(Y[:], py[:]), reads=["mpsY%d" % (s_ % 2)], writes=["myo%d" % (s_ % 2)])
                r0 = ex * CAP + s_ * 128
                P.dma("act", y_d.ap()[r0:r0 + 128, cb * 512:(cb + 1) * 512], Y[:], reads=["myo%d" % (s_ % 2)], writes=["y_all"])


def stage_M2(P, nc, y_d, x1_s, slot_s, gate_s, out_d):
    x1 = [P.sb("mx1_%d" % i, [128, D], F32) for i in range(2)]
    y1 = [P.sb("my1_%d" % i, [128, D], F32) for i in range(2)]
    y2 = [P.sb("my2_%d" % i, [128, D], F32) for i in range(2)]
    si = [P.sb("msi%d" % i, [128, 2], I32) for i in range(2)]
    gt = [P.sb("mgt%d" % i, [128, 2], F32) for i in range(2)]
    for t in range(16):
        b = t % 2
        P.dma("sp", si[b][:], slot_s.ap()[t], writes=["msi%d" % b])
        P.dma("sp", gt[b][:], gate_s.ap()[t], writes=["mgt%d" % b])
        P.dma("sp", x1[b][:], x1_s.ap()[t * 128:(t + 1) * 128, :], writes=["mx1_%d" % b])
        P.idma_gather(y1[b][:], y_d, si[b][:, 0:1], reads=["msi%d" % b], writes=["my1_%d" % b])
        P.idma_gather(y2[b][:], y_d, si[b][:, 1:2], reads=["msi%d" % b], writes=["my2_%d" % b])
        P.op("dve", lambda e, b=b: e.scalar_tensor_tensor(out=x1[b][:], in0=y1[b][:], scalar=gt[b][:, 0:1], in1=x1[b][:], op0=ALU.mult, op1=ALU.add),
             reads=["my1_%d" % b, "mgt%d" % b, "mx1_%d" % b], writes=["mx1_%d" % b])
        P.op("dve", lambda e, b=b: e.scalar_tensor_tensor(out=x1[b][:], in0=y2[b][:], scalar=gt[b][:, 1:2], in1=x1[b][:], op0=ALU.mult, op1=ALU.add),
             reads=["my2_%d" % b, "mgt%d" % b, "mx1_%d" % b], writes=["mx1_%d" % b])
        P.dma("sp", out_d.ap()[t * 128:(t + 1) * 128, :], x1[b][:], reads=["mx1_%d" % b], writes=["out:%d" % t])


CAP = 256
NSLOT = 32 * CAP


def stage_O1(P, nc, din, identb, ya_s, ybT_s, sg_s, mgT_s):
    proj_a = din("proj_a", [1024, D])
    proj_b = din("proj_b", [1024, D])
    PA = P.sb("oPA", [128, 8, D], BF16)
    PB = P.sb("oPB", [128, 8, D], BF16)
    wst = P.sb("owst", [128, 8, 512], F32)
    for wi, (src, dst, nm) in enumerate(((proj_a, PA, "oPA"), (proj_b, PB, "oPB"))):
        for cb in range(4):
            P.dma("sp", wst[:], src.ap()[:, cb * 512:(cb + 1) * 512].rearrange("(k p) c -> p k c", p=128), writes=["owst"])
            eng = "dve" if cb % 2 else "pool"
            P.op(eng, lambda e, dst=dst, cb=cb: e.tensor_copy(dst[:, :, cb * 512:(cb + 1) * 512], wst[:]), reads=["owst"], writes=[nm + "_%d" % cb])
    yat = [P.sb("oya%d" % i, [128, 1024], F32) for i in range(2)]
    yab = P.sb("oyab", [128, 1024], BF16)
    yaT = P.sb("oyaT", [128, 8, 128], BF16)
    ybT = [P.sb("oybT%d" % i, [128, 8, 128], BF16) for i in range(2)]
    sg = [P.sb("osg%d" % i, [128, 4096], BF16) for i in range(2)]
    m1 = P.sb("om1", [128, 512], F32)
    m2 = P.sb("om2", [128, 512], F32)
    MG = P.sb("oMG", [128, D], BF16)
    mgT = [P.sb("omgT%d" % i, [128, 16, 128], BF16) for i in range(2)]
    psTb = P.ps("opsTb", [128, 8, 128], BF16)
    psA = [P.ps("opsA%d" % i, [128, 512], F32) for i in range(2)]
    psB = [P.ps("opsB%d" % i, [128, 512], F32) for i in range(2)]
    for t in range(16):
        b = t % 2
        P.dma("sp", yat[b][:], ya_s.ap()[t * 128:(t + 1) * 128, :], reads=["ya_s:%d:%d" % (t, h) for h in range(8)], writes=["oya%d" % b])
        P.dma("sp", ybT[b][:], ybT_s.ap()[:, t * 128:(t + 1) * 128].rearrange("(k p) t -> p k t", p=128),
              reads=["ybT:%d:%d" % (t, h) for h in range(8)], writes=["oybT%d" % b])
        P.dma("sp", sg[b][:], sg_s.ap()[t * 128:(t + 1) * 128, :], reads=["sg_s:%d:%d" % (t, i) for i in range(8)], writes=["osg%d" % b])
        P.op("act", lambda e, b=b: e.copy(yab[:], yat[b][:]), reads=["oya%d" % b], writes=["oyab"])
        for k in range(8):
            P.op("pe", lambda e, k=k: e.transpose(psTb[:, k, :], yab[:, k * 128:(k + 1) * 128], identb[:]), reads=["oyab", "identb"], writes=["opsTb"], signal=(k == 7))
        P.op("dve", lambda e: e.tensor_copy(yaT[:], psTb[:]), reads=["opsTb"], writes=["oyaT"])
        for cb in range(4):
            cs_ = slice(cb * 512, (cb + 1) * 512)
            pa, pbb = psA[cb % 2], psB[cb % 2]
            ka, kb_ = "opsA%d" % (cb % 2), "opsB%d" % (cb % 2)
            for k in range(8):
                P.op("pe", lambda e, pa=pa, k=k, cs_=cs_: e.matmul(pa[:], yaT[:, k, :], PA[:, k, cs_], start=(k == 0), stop=(k == 7)),
                     reads=["oyaT", "oPA_%d" % cb], writes=[ka], signal=(k == 7))
            for k in range(8):
                P.op("pe", lambda e, pbb=pbb, k=k, cs_=cs_, b=b: e.matmul(pbb[:], ybT[b][:, k, :], PB[:, k, cs_], start=(k == 0), stop=(k == 7)),
                     reads=["oybT%d" % b, "oPB_%d" % cb], writes=[kb_], signal=(k == 7))
            P.op("dve", lambda e, pa=pa, cs_=cs_, b=b: e.tensor_tensor(m1[:], pa[:], sg[b][:, cs_], ALU.mult), reads=[ka, "osg%d" % b], writes=["om1"])
            P.op("dve", lambda e, pbb=pbb, cb=cb, b=b: e.tensor_tensor(m2[:], pbb[:], sg[b][:, 2048 + cb * 512:2048 + (cb + 1) * 512], ALU.mult),
                 reads=[kb_, "osg%d" % b], writes=["om2"])
            P.op("pool", lambda e, cs_=cs_: e.tensor_tensor(MG[:, cs_], m1[:], m2[:], ALU.add), reads=["om1", "om2"], writes=["oMG_%d" % cb])
        for half in range(2):
            for k in range(8):
                kc = half * 8 + k
                P.op("pe", lambda e, k=k, kc=kc: e.transpose(psTb[:, k, :], MG[:, kc * 128:(kc + 1) * 128], identb[:]),
                     reads=["oMG_%d" % (kc // 4), "identb"], writes=["opsTb"], signal=(k == 7))
            P.op("act", lambda e, half=half, b=b: e.copy(mgT[b][:, half * 8:(half + 1) * 8, :], psTb[:]), reads=["opsTb"], writes=["omgT%d_%d" % (b, half)])
        P.dma("sp", mgT_s.ap()[t], mgT[b][:], reads=["omgT%d_0" % b, "omgT%d_1" % b], writes=["mgT_s:%d" % t])


def stage_O2(P, nc, din, ident, xw, mgT_s, x1_s, xs_d, slot_s, gate_s):
    w_out = [din("w_out_%d" % i, [1024, D]) for i in range(2)]
    norm2_g = din("norm2_g", [1, D])
    rw_d = din("router_w", [D, 36])
    rb_d = din("router_b", [1, 36])
    ebase_d = din("ebase_h", [1, 32])
    tsf_d = din("ts_h2", [128, 128])
    WO = P.sb("oWO", [128, 16, D], BF16)
    wst = P.sb("o2wst", [128, 8, 512], F32)
    for i in range(2):
        for cb in range(4):
            P.dma("sp", wst[:], w_out[i].ap()[:, cb * 512:(cb + 1) * 512].rearrange("(k p) c -> p k c", p=128), writes=["o2wst"])
            eng = "dve" if cb % 2 else "pool"
            P.op(eng, lambda e, i=i, cb=cb: e.tensor_copy(WO[:, i * 8:(i + 1) * 8, cb * 512:(cb + 1) * 512], wst[:]), reads=["o2wst"], writes=["oWO_%d_%d" % (i, cb)])
    g2b = P.sb("og2b", [128, D], F32)
    P.dma("sp", g2b[:], AP(norm2_g, 0, [[0, 128], [1, D]]), writes=["og2b"])
    RW = P.sb("oRW", [128, 16, 36], F32)
    P.dma("sp", RW[:], rw_d.ap().rearrange("(k p) c -> p k c", p=128), writes=["oRW"])
    rbb = P.sb("orbb", [128, 36], F32)
    P.dma("sp", rbb[:], AP(rb_d, 0, [[0, 128], [1, 36]]), writes=["orbb"])
    ebase = P.sb("oebase", [128, 32], F32)
    P.dma("sp", ebase[:], AP(ebase_d, 0, [[0, 128], [1, 32]]), writes=["oebase"])
    tsf = P.sb("otsf", [128, 128], F32)
    P.dma("sp", tsf[:], tsf_d.ap(), writes=["otsf"])
    onesf = P.sb("oones", [128, 128], F32)
    P.op("dve", lambda e: e.memset(onesf[:], 1.0), writes=["oones"])
    carry = P.sb("ocarry", [128, 32], F32)
    P.op("dve", lambda e: e.memset(carry[:], 0.0), writes=["ocarry"])
    zxs = P.sb("ozxs", [128, D], BF16)
    P.op("dve", lambda e: e.memset(zxs[:], 0.0), writes=["ozxs"])
    zk_ = []
    for i_ in range(NSLOT // 128):
        P.dma("sp", xs_d.ap()[i_ * 128:(i_ + 1) * 128, :], zxs[:], reads=["ozxs"], writes=["xsz:%d" % i_])
        zk_.append("xsz:%d" % i_)
    P.dma("sp", xs_d.ap()[NSLOT:NSLOT + 1, :], zxs[0:1, :], reads=["ozxs"], writes=["xsz:d"])
    zk_.append("xsz:d")
    P.wait_all("pool", zk_)
    mgT = [P.sb("o2mgT%d" % i, [128, 16, 128], BF16) for i in range(2)]
    xt = [P.sb("o2x%d" % i, [128, D], F32) for i in range(2)]
    X1 = P.sb("oX1", [128, D], F32)
    H2 = P.sb("oH2", [128, D], F32)
    H2b = [P.sb("oH2b%d" % i, [128, D], BF16) for i in range(2)]
    sq = P.sb("o2sq", [128, D], BF16)
    h2T = P.sb("oh2T", [128, 16, 128], F32)
    r = P.sb("or", [128, 256], F32)
    slot_i = [P.sb("oslot%d" % i, [128, 2], I32) for i in range(2)]
    gate2 = [P.sb("ogate%d" % i, [128, 2], F32) for i in range(2)]
    psX = [P.ps("opsX%d" % i, [128, 512], F32) for i in range(2)]
    psT = P.ps("o2psT", [128, 4, 128], F32)
    psR = P.ps("opsR", [128, 512], F32)
    psP = P.ps("opsP", [128, 512], F32)

    def R(a, b_):
        return r[:, a:b_]

    for t in range(16):
        b = t % 2
        P.dma("sp", mgT[b][:], mgT_s.ap()[t], reads=["mgT_s:%d" % t], writes=["o2mgT%d" % b])
        P.dma("sp", xt[b][:], xw.ap()[NTOK + t * 128:NTOK + (t + 1) * 128, :], writes=["o2x%d" % b])
        for cb in range(4):
            cs_ = slice(cb * 512, (cb + 1) * 512)
            px, kx = psX[cb % 2], "opsX%d" % (cb % 2)
            for k in range(16):
                P.op("pe", lambda e, px=px, k=k, cs_=cs_, b=b: e.matmul(px[:], mgT[b][:, k, :], WO[:, k, cs_], start=(k == 0), stop=(k == 15)),
                     reads=["o2mgT%d" % b, "oWO_%d_%d" % (k // 8, cb)], writes=[kx], signal=(k == 15))
            P.op("dve", lambda e, px=px, cs_=cs_, b=b: e.tensor_tensor(X1[:, cs_], px[:], xt[b][:, cs_], ALU.add), reads=[kx, "o2x%d" % b], writes=["oX1_%d" % cb])
        x1k = ["oX1_%d" % cb for cb in range(4)]
        P.dma("sp", x1_s.ap()[t * 128:(t + 1) * 128, :], X1[:], reads=x1k, writes=["x1_s:%d" % t])
        P.op("act", lambda e: e.activation(sq[:], X1[:], AF.Square, accum_out=R(0, 1)), reads=x1k, writes=["o2sq", "or"])
        P.op("act", lambda e: e.activation(R(1, 2), R(0, 1), AF.Sqrt, bias=EPSB[0][:, 0:1], scale=1.0 / D), reads=["or", "epsb"], writes=["or"])
        P.op("dve", lambda e: e.reciprocal(R(2, 3), R(1, 2)), reads=["or"], writes=["or"])
        P.op("dve", lambda e: e.scalar_tensor_tensor(out=H2[:], in0=X1[:], scalar=R(2, 3), in1=g2b[:], op0=ALU.mult, op1=ALU.mult),
             reads=x1k + ["or", "og2b"], writes=["oH2"])
        P.op("act", lambda e, b=b: e.copy(H2b[b][:], H2[:]), reads=["oH2"], writes=["oH2b%d" % b])
        for c4 in range(4):
            for j in range(4):
                kc = c4 * 4 + j
                P.op("pe", lambda e, j=j, kc=kc: e.transpose(psT[:, j, :], H2[:, kc * 128:(kc + 1) * 128], ident[:]), reads=["oH2", "ident"], writes=["o2psT"], signal=(j == 3))
            eng = "act" if c4 % 2 else "dve"
            if eng == "act":
                P.op("act", lambda e, c4=c4: e.copy(h2T[:, c4 * 4:(c4 + 1) * 4, :], psT[:]), reads=["o2psT"], writes=["oh2T_%d" % c4])
            else:
                P.op("dve", lambda e, c4=c4: e.tensor_copy(h2T[:, c4 * 4:(c4 + 1) * 4, :], psT[:]), reads=["o2psT"], writes=["oh2T_%d" % c4])
        for k in range(16):
            P.op("pe", lambda e, k=k: e.matmul(psR[:, 0:36], h2T[:, k, :], RW[:, k, :], start=(k == 0), stop=(k == 15)),
                 reads=["oh2T_%d" % (k // 4), "oRW"], writes=["opsR"], signal=(k == 15))
        P.op("dve", lambda e: e.tensor_tensor(R(8, 12), psR[:, 0:4], rbb[:, 0:4], ALU.add), reads=["opsR", "orbb"], writes=["or"])
        P.op("dve", lambda e: e.tensor_tensor(R(16, 48), psR[:, 4:36], rbb[:, 4:36], ALU.add), reads=["opsR", "orbb"], writes=["or"])
        P.op("dve", lambda e: e.tensor_reduce(R(48, 49), R(8, 12), AX.X, ALU.max), reads=["or"], writes=["or"])
        P.op("dve", lambda e: e.tensor_scalar(R(52, 56), R(8, 12), R(48, 49), None, ALU.is_equal), reads=["or"], writes=["or"])
        P.op("dve", lambda e: e.tensor_scalar(R(56, 60), R(8, 12), R(48, 49), None, ALU.subtract), reads=["or"], writes=["or"])
        P.op("act", lambda e: e.activation(R(56, 60), R(56, 60), AF.Exp, accum_out=R(60, 61)), reads=["or"], writes=["or"])
        P.op("dve", lambda e: e.reciprocal(R(61, 62), R(60, 61)), reads=["or"], writes=["or"])
        P.op("dve", lambda e: e.tensor_scalar(R(64, 72), R(16, 24), R(52, 53), None, ALU.mult), reads=["or"], writes=["or"])
        for g in range(1, 4):
            P.op("dve", lambda e, g=g: e.scalar_tensor_tensor(out=R(64, 72), in0=R(16 + 8 * g, 24 + 8 * g), scalar=R(52 + g, 53 + g), in1=R(64, 72),
                                                              op0=ALU.mult, op1=ALU.add), reads=["or"], writes=["or"])
        P.op("dve", lambda e: e.max(R(72, 80), R(64, 72)), reads=["or"], writes=["or"])
        P.op("dve", lambda e: e.tensor_scalar(R(80, 88), R(64, 72), R(72, 73), None, ALU.is_equal), reads=["or"], writes=["or"])
        P.op("dve", lambda e: e.tensor_scalar(R(88, 96), R(64, 72), R(73, 74), None, ALU.is_equal), reads=["or"], writes=["or"])
        P.op("dve", lambda e: e.tensor_tensor(R(96, 97), R(73, 74), R(72, 73), ALU.subtract), reads=["or"], writes=["or"])
        P.op("act", lambda e: e.activation(R(97, 98), R(96, 97), AF.Exp), reads=["or"], writes=["or"])
        P.op("dve", lambda e: e.tensor_scalar(R(98, 99), R(97, 98), 1.0, None, ALU.add), reads=["or"], writes=["or"])
        P.op("dve", lambda e: e.reciprocal(R(98, 99), R(98, 99)), reads=["or"], writes=["or"])
        P.op("dve", lambda e: e.tensor_tensor(R(99, 100), R(61, 62), R(98, 99), ALU.mult), reads=["or"], writes=["or"])
        P.op("dve", lambda e: e.tensor_tensor(R(100, 101), R(61, 62), R(99, 100), ALU.subtract), reads=["or"], writes=["or"])
        for g in range(4):
            P.op("dve", lambda e, g=g: e.tensor_scalar(R(104 + 8 * g, 112 + 8 * g), R(80, 88), R(52 + g, 53 + g), None, ALU.mult), reads=["or"], writes=["or"])
            P.op("dve", lambda e, g=g: e.tensor_scalar(R(136 + 8 * g, 144 + 8 * g), R(88, 96), R(52 + g, 53 + g), None, ALU.mult), reads=["or"], writes=["or"])
        P.op("dve", lambda e: e.tensor_tensor(R(200, 232), R(104, 136), R(136, 168), ALU.add), reads=["or"], writes=["or"])
        P.op("pe", lambda e: e.matmul(psP[:, 0:32], tsf[:], R(200, 232), start=True, stop=True, skip_group_check=True), reads=["otsf", "or"], writes=["opsP"])
        P.op("pe", lambda e: e.matmul(psP[:, 32:64], onesf[:], R(200, 232), start=True, stop=True, skip_group_check=True), reads=["oones", "or"], writes=["opsP"])
        P.op("dve", lambda e: e.tensor_tensor(R(168, 200), psP[:, 0:32], carry[:], ALU.add), reads=["opsP", "ocarry"], writes=["or"])
        P.op("dve", lambda e: e.tensor_tensor(carry[:], carry[:], psP[:, 32:64], ALU.add), reads=["opsP", "ocarry"], writes=["ocarry"])
        S, Gt = slot_i[b], gate2[b]
        for k, (oh0, gcol) in enumerate(((104, 99), (136, 100))):
            P.op("dve", lambda e, oh0=oh0: e.tensor_tensor(R(200, 232), R(oh0, oh0 + 32), R(168, 200), ALU.mult), reads=["or"], writes=["or"])
            P.op("dve", lambda e: e.tensor_reduce(R(232, 233), R(200, 232), AX.X, ALU.add), reads=["or"], writes=["or"])
            P.op("dve", lambda e, oh0=oh0: e.tensor_tensor(R(200, 232), R(oh0, oh0 + 32), ebase[:], ALU.mult), reads=["or", "oebase"], writes=["or"])
            P.op("dve", lambda e: e.tensor_reduce(R(233, 234), R(200, 232), AX.X, ALU.add), reads=["or"], writes=["or"])
            P.op("dve", lambda e: e.tensor_scalar(R(234, 235), R(232, 233), float(CAP), None, ALU.is_lt), reads=["or"], writes=["or"])
            P.op("dve", lambda e: e.tensor_tensor(R(235, 236), R(232, 233), R(233, 234), ALU.add), reads=["or"], writes=["or"])
            P.op("dve", lambda e: e.tensor_scalar(R(235, 236), R(235, 236), float(NSLOT), None, ALU.subtract), reads=["or"], writes=["or"])
            P.op("dve", lambda e: e.tensor_tensor(R(236, 237), R(235, 236), R(234, 235), ALU.mult), reads=["or"], writes=["or"])
            P.op("dve", lambda e: e.tensor_scalar(R(236, 237), R(236, 237), float(NSLOT), None, ALU.add), reads=["or"], writes=["or"])
            P.op("dve", lambda e, S=S, k=k: e.tensor_copy(S[:, k:k + 1], R(236, 237)), reads=["or"], writes=["oslot%d" % b])
            P.op("dve", lambda e, Gt=Gt, k=k, gcol=gcol: e.tensor_tensor(Gt[:, k:k + 1], R(gcol, gcol + 1), R(234, 235), ALU.mult), reads=["or"], writes=["ogate%d" % b])
        for k in range(2):
            P.idma_scatter(xs_d, S[:, k:k + 1], H2b[b][:], reads=["oslot%d" % b, "oH2b%d" % b], writes=["xs:%d:%d" % (t, k)])
        P.dma("sp", slot_s.ap()[t], S[:], reads=["oslot%d" % b], writes=["slot_s:%d" % t])
        P.dma("sp", gate_s.ap()[t], Gt[:], reads=["ogate%d" % b], writes=["gate_s:%d" % t])
```
